# Optimizing a Trainium2 kernel written in Bass

```python
import math
import jax, jax.numpy as jnp
from jax import lax
import numpy as np

D_MODEL = 2048
BATCH = 2
SEQ = 8192
DEPTH = 1


GRID_W = 64
N_Q_HEADS = 8
N_KV_HEADS = 2
HEAD_DIM = 128
ATTN_WIDTH = N_Q_HEADS * HEAD_DIM
KV_WIDTH = N_KV_HEADS * HEAD_DIM
ROPE_THETA = 10000.0
Q_BLOCK = 128
QK_EPS = 1e-6
HYENA_WIDTH = D_MODEL // 2
HYENA_GROUPS = 8
SHORT_CONV = 3
FILTER_BANDS = 16
FILTER_EMB = 1 + 2 * FILTER_BANDS
FILTER_HIDDEN = 64
DECAY_TARGET = 1e-2
FAST_DECAY_PCT = 0.3
SLOW_DECAY_PCT = 1.5
FILTER_EPS = 1e-6
N_BRANCH = 2
IN_WIDTH = ATTN_WIDTH + 2 * KV_WIDTH + 3 * HYENA_WIDTH + N_BRANCH * D_MODEL
N_GROUPS = 4
EXPERTS_PER_GROUP = 8
N_EXPERTS = N_GROUPS * EXPERTS_PER_GROUP
TOP_K = 2
D_EXPERT = D_MODEL // 2
MOE_BLOCK = 128
DEEPNORM_ALPHA = (2 * DEPTH) ** 0.25
DEEPNORM_BETA = (8 * DEPTH) ** -0.25
LN_EPS = 1e-5

kernel_name = 'hybrid_gqa_hyena_hmoe_deepnorm_encoder'


def layer_norm(x, g, b):
    xf = x.astype(jnp.float32)
    mu = jnp.mean(xf, axis=-1, keepdims=True)
    var = jnp.mean(jnp.square(xf - mu), axis=-1, keepdims=True)
    return ((xf - mu) * lax.rsqrt(var + LN_EPS) * g.astype(jnp.float32) + b.astype(jnp.float32)).astype(x.dtype)


def rms_norm_heads(x, g):
    xf = x.astype(jnp.float32)
    xf = xf * lax.rsqrt(jnp.mean(jnp.square(xf), axis=-1, keepdims=True) + QK_EPS)
    return (xf * g.astype(jnp.float32)).astype(x.dtype)


def axial_rope_tables(seq_len):
    rows = seq_len // GRID_W
    row_idx = jnp.repeat(jnp.arange(rows, dtype=jnp.int32), GRID_W).astype(jnp.float32)
    col_idx = jnp.tile(jnp.arange(GRID_W, dtype=jnp.int32), rows).astype(jnp.float32)
    half = HEAD_DIM // 2
    inv_freq = ROPE_THETA ** (-jnp.arange(0, half, 2, dtype=jnp.float32) / half)
    ang_r = row_idx[:, None] * inv_freq[None, :]
    ang_c = col_idx[:, None] * inv_freq[None, :]
    return jnp.cos(ang_r), jnp.sin(ang_r), jnp.cos(ang_c), jnp.sin(ang_c)


def rotate_half(x, cos, sin):
    d2 = x.shape[-1] // 2
    x1, x2 = x[..., :d2], x[..., d2:]
    c = cos[None, :, None, :]
    s = sin[None, :, None, :]
    return jnp.concatenate([x1 * c - x2 * s, x2 * c + x1 * s], axis=-1)


def apply_axial_rope(x, tables):
    cr, sr, cc, sc = tables
    xf = x.astype(jnp.float32)
    half = HEAD_DIM // 2
    out = jnp.concatenate([rotate_half(xf[..., :half], cr, sr),
                           rotate_half(xf[..., half:], cc, sc)], axis=-1)
    return out.astype(x.dtype)


def block_attention(q, k, v):
    b, s, _, _ = q.shape
    n_blk = s // Q_BLOCK
    grp = N_Q_HEADS // N_KV_HEADS
    qb = q.reshape(b, n_blk, Q_BLOCK, N_KV_HEADS, grp, HEAD_DIM).transpose(1, 0, 2, 3, 4, 5)
    scale = HEAD_DIM ** -0.5

    def one_block(q_blk):
        sc = jnp.einsum('bqkgd,bskd->bkgqs', q_blk, k).astype(jnp.float32) * scale
        p = jax.nn.softmax(sc, axis=-1).astype(v.dtype)
        return jnp.einsum('bkgqs,bskd->bqkgd', p, v)

    o = lax.map(one_block, qb)
    return o.transpose(1, 0, 2, 3, 4, 5).reshape(b, s, ATTN_WIDTH)


def short_conv(x, w, bias):
    s = x.shape[1]
    pad = SHORT_CONV // 2
    xp = jnp.pad(x, ((0, 0), (pad, pad), (0, 0)))
    out = bias
    for i in range(SHORT_CONV):
        out = out + xp[:, i:i + s] * w[i]
    return out


def hyena_filters(seq_len, w1, b1, f1, w2, b2, f2, w3):
    f32 = jnp.float32
    t = jnp.linspace(0.0, 1.0, seq_len, dtype=f32)[:, None]
    w = 2.0 * math.pi * jnp.arange(seq_len, dtype=f32)[:, None] / seq_len
    bands = jnp.linspace(1e-4, FILTER_BANDS - 1, FILTER_BANDS, dtype=f32)[None, :]
    feats = jnp.concatenate([t, jnp.cos(bands * w), -jnp.sin(bands * w)], axis=-1)
    h = jnp.sin(f1.astype(f32) * (feats @ w1.astype(f32) + b1.astype(f32)))
    h = jnp.sin(f2.astype(f32) * (h @ w2.astype(f32) + b2.astype(f32)))
    filt = h @ w3.astype(f32)
    min_decay = math.log(DECAY_TARGET) / SLOW_DECAY_PCT
    max_decay = math.log(DECAY_TARGET) / FAST_DECAY_PCT
    deltas = jnp.linspace(min_decay, max_decay, HYENA_WIDTH, dtype=f32)
    decay = jnp.exp(-t * jnp.abs(deltas)[None, :])
    h_fwd = filt[:, :HYENA_WIDTH] * decay
    h_bwd = filt[:, HYENA_WIDTH:] * decay
    two_sided = jnp.concatenate([h_fwd, jnp.zeros((1, HYENA_WIDTH), f32), h_bwd[:0:-1]], axis=0)
    two_sided = two_sided * lax.rsqrt(jnp.sum(jnp.square(two_sided), axis=0, keepdims=True) + FILTER_EPS)
    return jnp.fft.rfft(two_sided, axis=0)


def long_conv(z, filt_hat, d_bias):
    s = z.shape[1]
    zf = z.astype(jnp.float32)
    z_hat = jnp.fft.rfft(zf, n=2 * s, axis=1)
    y = jnp.fft.irfft(z_hat * filt_hat[None], n=2 * s, axis=1)[:, :s]
    return (y + zf * d_bias.astype(jnp.float32)).astype(z.dtype)


def hier_moe(x, w_rg, b_rg, w_re, b_re, w_gate, w_up, w_down):
    b, s, d = x.shape
    n_tok = b * s
    xt = x.reshape(n_tok, d)
    coarse = (xt @ w_rg + b_rg).astype(jnp.float32)
    p_grp, grp = lax.top_k(jax.nn.softmax(coarse, axis=-1), 1)
    fine = (xt @ w_re + b_re).astype(jnp.float32).reshape(n_tok, N_GROUPS, EXPERTS_PER_GROUP)
    fine_sel = fine[jnp.arange(n_tok), grp[:, 0]]
    top_p, top_i = lax.top_k(jax.nn.softmax(fine_sel, axis=-1), TOP_K)
    gate = p_grp * top_p / jnp.sum(top_p, axis=-1, keepdims=True)
    expert = grp * EXPERTS_PER_GROUP + top_i

    n_assign = n_tok * TOP_K
    e_flat = expert.reshape(-1).astype(jnp.int32)
    tok_flat = jnp.repeat(jnp.arange(n_tok, dtype=jnp.int32), TOP_K)
    w_flat = gate.reshape(-1)
    order = jnp.argsort(e_flat)
    se, st, sw = e_flat[order], tok_flat[order], w_flat[order]
    counts = jnp.bincount(e_flat, length=N_EXPERTS)
    padded = (counts + MOE_BLOCK - 1) // MOE_BLOCK * MOE_BLOCK
    pad_end = jnp.cumsum(padded)
    pad_start = pad_end - padded
    start = jnp.cumsum(counts) - counts
    dest = pad_start[se] + jnp.arange(n_assign, dtype=jnp.int32) - start[se]
    n_rows = n_assign + N_EXPERTS * MOE_BLOCK
    n_blk = n_rows // MOE_BLOCK
    row_tok = jnp.zeros((n_rows,), jnp.int32).at[dest].set(st)
    row_w = jnp.zeros((n_rows,), jnp.float32).at[dest].set(sw)
    blk_start = jnp.arange(n_blk, dtype=jnp.int32) * MOE_BLOCK
    blk_expert = jnp.minimum(jnp.searchsorted(pad_end, blk_start, side='right'), N_EXPERTS - 1)
    xs = xt[row_tok].reshape(n_blk, MOE_BLOCK, d)

    def expert_block(args):
        xb, e = args
        hdn = jax.nn.silu(xb @ w_gate[e]) * (xb @ w_up[e])
        return hdn @ w_down[e]

    ys = lax.map(expert_block, (xs, blk_expert)).reshape(n_rows, d)
    out = jnp.zeros((n_tok, d), jnp.float32).at[row_tok].add(ys.astype(jnp.float32) * row_w[:, None])
    return out.astype(x.dtype).reshape(b, s, d)


def setup_inputs(seed: int = 0) -> dict:
    key = jax.random.key(seed)
    ks = iter(jax.random.split(key, 40))
    f32 = jnp.float32

    def nrm(shape, scale):
        return jax.random.normal(next(ks), shape, f32) * scale

    def gain(shape):
        return jnp.ones(shape, f32) + nrm(shape, 0.02)

    L_, D_, C_ = DEPTH, D_MODEL, HYENA_WIDTH
    return {
        'x': nrm((BATCH, SEQ, D_), 1.0),
        'ln_in_g': gain((D_,)),
        'ln_in_b': nrm((D_,), 0.02),
        'w_in': nrm((L_, D_, IN_WIDTH), D_ ** -0.5),
        'b_gate': nrm((L_, N_BRANCH * D_), 0.02),
        'q_norm_g': gain((L_, HEAD_DIM)),
        'k_norm_g': gain((L_, HEAD_DIM)),
        'hy_conv_w': nrm((L_, SHORT_CONV, 3 * C_), SHORT_CONV ** -0.5),
        'hy_conv_b': nrm((L_, 3 * C_), 0.02),
        'filt_w1': nrm((L_, FILTER_EMB, FILTER_HIDDEN), FILTER_EMB ** -0.5),
        'filt_b1': nrm((L_, FILTER_HIDDEN), 0.02),
        'filt_f1': gain((L_, FILTER_HIDDEN)),
        'filt_w2': nrm((L_, FILTER_HIDDEN, FILTER_HIDDEN), FILTER_HIDDEN ** -0.5),
        'filt_b2': nrm((L_, FILTER_HIDDEN), 0.02),
        'filt_f2': gain((L_, FILTER_HIDDEN)),
        'filt_w3': nrm((L_, FILTER_HIDDEN, 2 * C_), FILTER_HIDDEN ** -0.5),
        'hy_bias_d': nrm((L_, C_), 0.5),
        'w_attn_o': nrm((L_, ATTN_WIDTH, D_), ATTN_WIDTH ** -0.5),
        'w_hy_o': nrm((L_, C_, D_), C_ ** -0.5),
        'w_out': nrm((L_, D_, D_), D_ ** -0.5 * DEEPNORM_BETA),
        'ln1_g': gain((L_, D_)),
        'ln1_b': nrm((L_, D_), 0.02),
        'w_route_grp': nrm((L_, D_, N_GROUPS), D_ ** -0.5),
        'b_route_grp': nrm((L_, N_GROUPS), 0.01),
        'w_route_exp': nrm((L_, D_, N_EXPERTS), D_ ** -0.5),
        'b_route_exp': nrm((L_, N_EXPERTS), 0.01),
        'w_exp_gate': nrm((L_, N_EXPERTS, D_, D_EXPERT), D_ ** -0.5),
        'w_exp_up': nrm((L_, N_EXPERTS, D_, D_EXPERT), D_ ** -0.5),
        'w_exp_down': nrm((L_, N_EXPERTS, D_EXPERT, D_), D_EXPERT ** -0.5 * DEEPNORM_BETA),
        'ln2_g': gain((L_, D_)),
        'ln2_b': nrm((L_, D_), 0.02),
    }


def reference(x, ln_in_g, ln_in_b, w_in, b_gate, q_norm_g, k_norm_g, hy_conv_w, hy_conv_b,
              filt_w1, filt_b1, filt_f1, filt_w2, filt_b2, filt_f2, filt_w3, hy_bias_d,
              w_attn_o, w_hy_o, w_out, ln1_g, ln1_b, w_route_grp, b_route_grp, w_route_exp,
              b_route_exp, w_exp_gate, w_exp_up, w_exp_down, ln2_g, ln2_b):
    b, s, _ = x.shape
    rope = axial_rope_tables(s)
    split_at = [ATTN_WIDTH, ATTN_WIDTH + KV_WIDTH, ATTN_WIDTH + 2 * KV_WIDTH,
                ATTN_WIDTH + 2 * KV_WIDTH + 3 * HYENA_WIDTH]
    h = layer_norm(x, ln_in_g, ln_in_b)
    for l in range(DEPTH):
        proj = h @ w_in[l]
        q, k, v, hy, gates = jnp.split(proj, split_at, axis=-1)
        q = apply_axial_rope(rms_norm_heads(q.reshape(b, s, N_Q_HEADS, HEAD_DIM), q_norm_g[l]), rope)
        k = apply_axial_rope(rms_norm_heads(k.reshape(b, s, N_KV_HEADS, HEAD_DIM), k_norm_g[l]), rope)
        v = v.reshape(b, s, N_KV_HEADS, HEAD_DIM)
        y_attn = block_attention(q, k, v) @ w_attn_o[l]

        hy = short_conv(hy, hy_conv_w[l], hy_conv_b[l])
        x0, x1, hv = jnp.split(hy, 3, axis=-1)
        filt_hat = hyena_filters(s, filt_w1[l], filt_b1[l], filt_f1[l], filt_w2[l], filt_b2[l],
                                 filt_f2[l], filt_w3[l])
        y_hy = (x0 * long_conv(hv * x1, filt_hat, hy_bias_d[l])) @ w_hy_o[l]

        g_attn, g_hy = jnp.split(jax.nn.sigmoid(gates + b_gate[l]), N_BRANCH, axis=-1)
        mixed = (g_attn * y_attn + g_hy * y_hy) @ w_out[l]
        h = layer_norm(DEEPNORM_ALPHA * h + mixed, ln1_g[l], ln1_b[l])

        moe = hier_moe(h, w_route_grp[l], b_route_grp[l], w_route_exp[l], b_route_exp[l],
                       w_exp_gate[l], w_exp_up[l], w_exp_down[l])
        h = layer_norm(DEEPNORM_ALPHA * h + moe, ln2_g[l], ln2_b[l])
    return h
```

```python
import math
from contextlib import ExitStack
import numpy as np
import concourse.bass as bass
import concourse.mybir as mybir
from concourse.bass_utils import run_bass_kernel_spmd

F32 = mybir.dt.float32
BF16 = mybir.dt.bfloat16
ALU = mybir.AluOpType
AF = mybir.ActivationFunctionType

D = 2048
S = 8192
T = 2048
NOWN = 17 * 128
C = 1024
INW = 8704
ALPHA = 2.0 ** 0.25
LN_EPS = 1e-5
QK_EPS = 1e-6
FILTER_EPS = 1e-6
NFFT = 16384
PI = math.pi


class Buf:
    __slots__ = ("w", "r")

    def __init__(self):
        self.w = None
        self.r = []


class FW:
    ENGS = ("tensor", "vector", "scalar", "gpsimd", "sync")

    def __init__(self, nc, stack, ndma=20):
        self.nc = nc
        self.cnt = {e: 0 for e in self.ENGS}
        self.seen = {e: {} for e in self.ENGS}
        self.sems = {}
        self.ndma = ndma
        self.dma_next = {e: 0 for e in self.ENGS}
        self.dma_tot = {}
        for e in self.ENGS:
            self.sems[e] = stack.enter_context(nc.semaphore("s_" + e))
        for q in ("sync", "gpsimd", "scalar"):
            for i in range(ndma):
                k = ("d", q, i)
                self.sems[k] = stack.enter_context(nc.semaphore("d_%s_%d" % (q, i)))
                self.dma_tot[k] = 0

    def _waits(self, eng, reads, writes):
        deps = {}

        def add(t):
            if t is not None and deps.get(t[0], 0) < t[1]:
                deps[t[0]] = t[1]
        for b in reads:
            add(b.w)
        for b in writes:
            add(b.w)
            for t in b.r:
                add(t)
        out = []
        seen = self.seen[eng]
        for key, val in deps.items():
            if key == eng and eng == "tensor":
                continue
            if seen.get(key, 0) >= val:
                continue
            seen[key] = val
            out.append((key, val))
        return out

    def _mark(self, tag, reads, writes):
        for b in writes:
            b.w = tag
            b.r = []
        for b in reads:
            b.r = [t for t in b.r if t[0] != tag[0]] + [tag]

    def op(self, eng, fn, reads=(), writes=()):
        waits = self._waits(eng, reads, writes)
        self.cnt[eng] += 1
        tag = (eng, self.cnt[eng])
        h = getattr(self.nc, eng)
        for key, val in waits:
            h.wait_ge(self.sems[key], val)
        fn(h).then_inc(self.sems[eng], 1)
        self._mark(tag, reads, writes)

    def idma(self, out, in_, idx, gather, bound, reads=(), writes=()):
        if not hasattr(self, "bregs"):
            self.bregs = {}
        if bound not in self.bregs:
            r = self.nc.gpsimd.alloc_register("bnd%d" % bound)
            self.nc.gpsimd.reg_mov(r, bound)
            self.bregs[bound] = r
        bound = self.bregs[bound]
        if gather:
            fn = lambda h: h.indirect_dma_start(out=out, out_offset=None, in_=in_, in_offset=bass.IndirectOffsetOnAxis(ap=idx, axis=0),
                                                bounds_check=bound, oob_is_err=False)
        else:
            fn = lambda h: h.indirect_dma_start(out=out, out_offset=bass.IndirectOffsetOnAxis(ap=idx, axis=0), in_=in_, in_offset=None,
                                                bounds_check=bound, oob_is_err=False)
        self.dma("gpsimd", None, None, reads=reads, writes=writes, fn=fn)

    def dma(self, q, out, in_, reads=(), writes=(), slow=False, fn=None):
        i = self.dma_next[q]
        self.dma_next[q] = (i + 1) % self.ndma
        key = ("d", q, i)
        waits = self._waits(q, reads, writes)
        prev = self.dma_tot[key]
        if prev > 0 and self.seen[q].get(key, 0) < prev:
            self.seen[q][key] = prev
            waits.append((key, prev))
        self.dma_tot[key] = prev + 16
        tag = (key, prev + 16)
        h = getattr(self.nc, q)
        for k2, val in waits:
            h.wait_ge(self.sems[k2], val)
        if fn is not None:
            inst = fn(h)
        elif slow:
            inst = h.dma_start(out=out, in_=in_, allow_slow_non_contiguous=True)
        else:
            inst = h.dma_start(out=out, in_=in_)
        inst.then_inc(self.sems[key], 16)
        self._mark(tag, reads, writes)

    def barrier(self):
        for e in self.ENGS:
            h = getattr(self.nc, e)
            seen = self.seen[e]
            for o in self.ENGS:
                if o == e or self.cnt[o] == 0:
                    continue
                if seen.get(o, 0) < self.cnt[o]:
                    seen[o] = self.cnt[o]
                    h.wait_ge(self.sems[o], self.cnt[o])
            for k, tot in self.dma_tot.items():
                if tot > 0 and seen.get(k, 0) < tot:
                    seen[k] = tot
                    h.wait_ge(self.sems[k], tot)


def build_nc(dbg=None, stop=None):
    nc = bass.Bass("TRN2", target_bir_lowering=False)
    ins = {}

    def IN(name, shape, dt=F32):
        ins[name] = nc.dram_tensor(name, list(shape), dt, kind="ExternalInput").ap()
        return ins[name]

    x_seq = IN("x_seq", [S, D]); x_own = IN("x_own", [NOWN, D]); m_own = IN("m_own", [128, 17])
    ln_in_g = IN("ln_in_g", [1, D]); ln_in_b = IN("ln_in_b", [1, D])
    w_in = IN("w_in", [D, INW]); b_gate = IN("b_gate", [1, 4096])
    q_norm_g = IN("q_norm_g", [1, 128]); k_norm_g = IN("k_norm_g", [1, 128])
    hy_conv_w = IN("hy_conv_w", [3, 3072]); hy_conv_b = IN("hy_conv_b", [1, 3072])
    filt_w1 = IN("filt_w1", [33, 64]); filt_b1 = IN("filt_b1", [64, 1]); filt_f1 = IN("filt_f1", [64, 1])
    filt_w2 = IN("filt_w2", [64, 64]); filt_b2 = IN("filt_b2", [64, 1]); filt_f2 = IN("filt_f2", [64, 1])
    filt_w3 = IN("filt_w3", [64, 2048]); hy_bias_d = IN("hy_bias_d", [1, C])
    w_attn_o = IN("w_attn_o", [1024, D]); w_hy_o = IN("w_hy_o", [C, D]); w_out = IN("w_out", [D, D])
    ln1_g = IN("ln1_g", [1, D]); ln1_b = IN("ln1_b", [1, D])
    w_route = IN("w_route", [D, 36]); b_route = IN("b_route", [1, 36])
    w_eg = IN("w_eg", [32, D, 1024]); w_eu = IN("w_eu", [32, D, 1024]); w_ed = IN("w_ed", [32, 1024, D])
    ln2_g = IN("ln2_g", [1, D]); ln2_b = IN("ln2_b", [1, D])
    ropeC_k = IN("ropeC_k", [S, 128]); ropeS_k = IN("ropeS_k", [S, 128])
    ropeC_q = IN("ropeC_q", [T, 128]); ropeS_q = IN("ropeS_q", [T, 128])
    cFc = IN("cFc", [128, 128]); cFs = IN("cFs", [128, 128]); cFsn = IN("cFsn", [128, 128]); cFcn = IN("cFcn", [128, 128])
    cC16 = IN("cC16", [128, 128]); cS16 = IN("cS16", [128, 128]); cS16n = IN("cS16n", [128, 128])
    ci2re = IN("ci2re", [128, 16]); ci2im = IN("ci2im", [128, 16])
    pcol = IN("pcol", [128, 1]); featsT = IN("featsT", [33, S]); negt = IN("negt", [128, 64]); absdelta = IN("absdelta", [1, C]); m0 = IN("m0", [128, 1])

    out = nc.dram_tensor("out", [T, D], F32, kind="ExternalOutput").ap()

    def DR(name, shape, dt):
        kind = "ExternalOutput" if (dbg and name in dbg) else "Internal"
        return nc.dram_tensor(name, list(shape), dt, kind=kind).ap()

    hT_d = DR("hT_d", [D, S + 2], BF16); hTo_d = DR("hTo_d", [D, NOWN], BF16); ho_d = DR("ho_d", [NOWN, D], F32)
    kT_d = DR("kT_d", [128, 2, S], BF16); v_d = DR("v_d", [S, 256], BF16); qT_d = DR("qT_d", [128, 8, T], BF16)
    z_d = DR("z_d", [S, C], BF16); x0_d = DR("x0_d", [T, C], BF16); g_d = DR("g_d", [T, 4096], BF16)
    at_d = DR("at_d", [128, 8, T], BF16)
    hf_d = DR("hf_d", [S, C], BF16); hg_d = DR("hg_d", [S, C], BF16)
    a_d = [[DR("a_d%d%d" % (i, r), [128, 128, C], BF16) for r in range(2)] for i in range(3)]
    hh_d = [DR("hh_d%d" % r, [128, 128, C], BF16) for r in range(2)]
    b_d = [DR("b_d%d" % r, [128, 128, C], BF16) for r in range(2)]
    y_d = DR("y_d", [T, C], F32)
    xs_d = DR("xs_d", [8192, D], BF16); ys_d = DR("ys_d", [8192, D], F32); preT_d = DR("preT_d", [D, T], BF16); h1_d = DR("h1_d", [T, D], F32); h1T_d = DR("h1T_d", [D, T], BF16)

    with ExitStack() as top:
        fw = FW(nc, top)
        Dm = {}

        def dbuf(ap_name):
            return Buf()

        uid = [0]

        def SB(st, name, shape, dt):
            uid[0] += 1
            return st.enter_context(nc.sbuf_tensor("%s_u%d" % (name, uid[0]), list(shape), dt))

        PS = [top.enter_context(nc.psum_tensor("ps%d" % i, [128, 512], F32)) for i in range(6)]
        PSB = [Buf() for _ in range(6)]
        PT = [top.enter_context(nc.psum_tensor("pt%d" % i, [128, 1024], BF16)) for i in range(2)]
        PTB = [Buf() for _ in range(2)]

        ident = SB(top, "ident", [128, 128], BF16); identf = SB(top, "identf", [128, 128], F32)
        ones16 = SB(top, "ones16", [128, 128], BF16); onesf = SB(top, "onesf", [128, 128], F32)
        Bc = Buf()
        fw.op("gpsimd", lambda h: h.memset(identf[:], 1.0), writes=[Bc])
        fw.op("gpsimd", lambda h: h.affine_select(out=identf[:], in_=identf[:], pattern=[[-1, 128]],
                                                   compare_op=ALU.is_equal, fill=0.0, base=0, channel_multiplier=1),
              reads=[Bc], writes=[Bc])
        fw.op("vector", lambda h: h.tensor_copy(out=ident[:], in_=identf[:]), reads=[Bc], writes=[Bc])
        fw.op("vector", lambda h: h.memset(ones16[:], 1.0), writes=[Bc])
        fw.op("vector", lambda h: h.memset(onesf[:], 1.0), writes=[Bc])

        def rep_load(st, name, src_row, n, dt=F32, q="sync"):
            t = SB(st, name, [128, n], dt)
            b = Buf()
            fw.dma(q, t[:], src_row.to_broadcast([128, n]), writes=[b])
            return t, b

        def layernorm(xt, xb, grep, brep, gb, st8, sb8, mv, mvb, rstd, eps=LN_EPS):
            for i in range(4):
                fw.op("vector", lambda h, i=i: h.bn_stats(out=st8[:, i * 6:(i + 1) * 6], in_=xt[:, i * 512:(i + 1) * 512]),
                      reads=[xb], writes=[sb8])
            fw.op("vector", lambda h: h.bn_aggr(out=mv[:], in_=st8[:]), reads=[sb8], writes=[mvb])
            fw.op("scalar", lambda h: h.activation(out=rstd[:], in_=mv[:, 1:2], func=AF.Sqrt, bias=eps, scale=1.0),
                  reads=[mvb], writes=[sb8])
            fw.op("vector", lambda h: h.reciprocal(out=rstd[:], in_=rstd[:]), reads=[sb8], writes=[sb8])
            fw.op("vector", lambda h: h.tensor_scalar(out=xt[:], in0=xt[:], scalar1=mv[:, 0:1], scalar2=rstd[:, 0:1],
                                                       op0=ALU.subtract, op1=ALU.mult), reads=[xb, mvb, sb8], writes=[xb])
            fw.op("gpsimd", lambda h: h.tensor_tensor(out=xt[:], in0=xt[:], in1=grep[:], op=ALU.mult), reads=[xb, gb], writes=[xb])
            fw.op("vector", lambda h: h.tensor_tensor(out=xt[:], in0=xt[:], in1=brep[:], op=ALU.add), reads=[xb, gb], writes=[xb])

        def transpose_to(src16, srcb, dst, dstb, nchunk, col0=0, ncols=128):
            for g4 in range(0, nchunk, 8):
                n = min(8, nchunk - g4)
                pi = (g4 // 8) % 2
                for c in range(n):
                    fw.op("tensor", lambda h, c=c: h.transpose(PT[pi][:, c * 128:c * 128 + ncols],
                                                               src16[0:ncols, (g4 + c) * 128:(g4 + c + 1) * 128], ident[0:ncols, 0:ncols]),
                          reads=[srcb, Bc], writes=[PTB[pi]])
                eng = "vector" if pi == 0 else "scalar"
                if eng == "vector":
                    fw.op("vector", lambda h: h.tensor_copy(
                        out=dst[:, g4:g4 + n, col0:col0 + ncols],
                        in_=PT[pi][:, 0:n * 128].rearrange("p (c t) -> p c t", t=128)[:, :, 0:ncols]),
                        reads=[PTB[pi]], writes=[dstb])
                else:
                    fw.op("scalar", lambda h: h.copy(
                        out=dst[:, g4:g4 + n, col0:col0 + ncols],
                        in_=PT[pi][:, 0:n * 128].rearrange("p (c t) -> p c t", t=128)[:, :, 0:ncols]),
                        reads=[PTB[pi]], writes=[dstb])

        with ExitStack() as st:
            grep, gb = rep_load(st, "lng", ln_in_g, D)
            brep, bb_ = rep_load(st, "lnb", ln_in_b, D)
            gbb = Buf()
            fw.op("vector", lambda h: h.tensor_copy(out=grep[:, 0:1], in_=grep[:, 0:1]), reads=[gb, bb_], writes=[gbb])
            xts = [SB(st, "xt%d" % i, [128, D], F32) for i in range(2)]; xbs = [Buf(), Buf()]
            h16 = [SB(st, "h16_%d" % i, [128, D], BF16) for i in range(2)]; h16b = [Buf(), Buf()]
            hTs = [SB(st, "hTs%d" % i, [128, 16, 512], BF16) for i in range(2)]; hTb = [Buf(), Buf()]
            st8 = SB(st, "st8", [128, 24], F32); sb8 = Buf(); mv = SB(st, "mv", [128, 2], F32); mvb = Buf()
            rstd = SB(st, "rstd", [128, 1], F32)
            mk = SB(st, "mk", [128, 17], F32); mkb = Buf()
            zt = SB(st, "zt", [128, 16, 1], BF16); ztb = Buf()
            fw.op("vector", lambda h: h.memset(zt[:], 0.0), writes=[ztb])
            hTv = hT_d.rearrange("(c p) s -> p c s", p=128)
            fw.dma("gpsimd", hTv[:, :, 0:1], zt[:], reads=[ztb], writes=[dbuf("hT_d")], slow=True)
            fw.dma("gpsimd", hTv[:, :, S + 1:S + 2], zt[:], reads=[ztb], writes=[dbuf("hT_d")], slow=True)
            fw.dma("sync", mk[:], m_own, writes=[mkb])
            hTov = hTo_d.rearrange("(c p) s -> p c s", p=128)
            it = 0
            for grp in range(16 + 5):
                own = grp >= 16
                ntile = 4 if not own else (4 if grp < 20 else 1)
                hb = grp % 2
                for ti in range(ntile):
                    tile = (grp * 4 + ti) if not own else ((grp - 16) * 4 + ti)
                    k = it % 2; it += 1
                    src = x_seq if not own else x_own
                    fw.dma("sync", xts[k][:], src[tile * 128:(tile + 1) * 128, :], writes=[xbs[k]])
                    layernorm(xts[k], xbs[k], grep, brep, gbb, st8, sb8, mv, mvb, rstd)
                    if own:
                        fw.op("scalar", lambda h, k=k, tile=tile: h.activation(out=xts[k][:], in_=xts[k][:], func=AF.Identity,
                                                                                scale=mk[:, tile:tile + 1]),
                              reads=[xbs[k], mkb], writes=[xbs[k]])
                        fw.dma("gpsimd", ho_d[tile * 128:(tile + 1) * 128, :], xts[k][:], reads=[xbs[k]], writes=[dbuf("ho_d")])
                    fw.op("scalar", lambda h, k=k: h.copy(out=h16[k][:], in_=xts[k][:]), reads=[xbs[k]], writes=[h16b[k]])
                    transpose_to(h16[k], h16b[k], hTs[hb], hTb[hb], 16, col0=ti * 128)
                if not own:
                    fw.dma("gpsimd", hTv[:, :, 1 + grp * 512:1 + grp * 512 + 512], hTs[hb][:], reads=[hTb[hb]], writes=[dbuf("hT_d")])
                else:
                    g0 = (grp - 16) * 512
                    fw.dma("gpsimd", hTov[:, :, g0:g0 + ntile * 128], hTs[hb][:, :, 0:ntile * 128], reads=[hTb[hb]], writes=[dbuf("hTo_d")])
        fw.barrier()

        w_in_v = w_in.rearrange("(c p) n -> p c n", p=128)

        def proj_pass(st, src_v, srcname, ntok_tiles, blocks, epilogue):
            nb = len(blocks)
            wts = []
            wb = Buf()
            for bi, (col0, cidx, bias) in enumerate(blocks):
                nsh = 3 if cidx is not None else 1
                for sh in range(nsh):
                    wt = SB(st, "w_%d_%d" % (bi, sh), [128, 16, 512], BF16)
                    fw.dma("gpsimd", wt[:], w_in_v[:, :, col0:col0 + 512], writes=[wb])
                    if cidx is not None:
                        cw, cwb = rep_load(st, "cw_%d_%d" % (bi, sh), hy_conv_w[sh:sh + 1, cidx:cidx + 512], 512)
                        for c in range(16):
                            fw.op("gpsimd", lambda h, c=c, wt=wt, cw=cw: h.tensor_tensor(out=wt[:, c, :], in0=wt[:, c, :], in1=cw[:], op=ALU.mult),
                                  reads=[wb, cwb], writes=[wb])
                    wts.append((bi, sh if cidx is not None else 1, wt))
                if bias is not None:
                    b16 = SB(st, "b16_%d" % bi, [1, 512], BF16)
                    fw.dma("gpsimd", b16[:], bias, writes=[wb])
                    wts.append((bi, -1, b16))
            hw = [SB(st, "hw%d" % i, [128, 16, 514], BF16) for i in range(2)]; hwb = [Buf(), Buf()]
            ngrp = (ntok_tiles + 3) // 4
            for g in range(ngrp):
                k = g % 2
                nt = min(4, ntok_tiles - g * 4)
                wdt = nt * 128 + 2
                fw.dma("sync", hw[k][:, :, 0:wdt], src_v[:, :, g * 512:g * 512 + wdt], reads=[dbuf(srcname)], writes=[hwb[k]])
                for m in range(nt):
                    tile = g * 4 + m
                    pidx = [(tile * nb + bi) % 6 for bi in range(nb)]
                    for bi in range(nb):
                        mine = [(sh, wt) for (b2, sh, wt) in wts if b2 == bi]
                        nsteps = sum(16 if sh >= 0 else 1 for sh, _ in mine)
                        step = 0
                        for sh, wt in mine:
                            if sh < 0:
                                fw.op("tensor", lambda h, wt=wt, p=pidx[bi], s0=(step == 0), s1=(step == nsteps - 1):
                                      h.matmul(PS[p][:], ones16[0:1, :], wt[:], start=s0, stop=s1), reads=[wb, Bc], writes=[PSB[pidx[bi]]])
                                step += 1
                                continue
                            for c in range(16):
                                fw.op("tensor", lambda h, wt=wt, c=c, p=pidx[bi], off=m * 128 + sh, s0=(step == 0), s1=(step == nsteps - 1), k=k:
                                      h.matmul(PS[p][:], hw[k][:, c, off:off + 128], wt[:, c, :], start=s0, stop=s1),
                                      reads=[wb, hwb[k]], writes=[PSB[pidx[bi]]])
                                step += 1
                    epilogue(tile, pidx)

        def qk_epilogue_factory(st, gsrc, ropeC, ropeS, nheads_list, dstT, dstname, pref):
            grep_, gb_ = rep_load(st, pref + "g", gsrc, 128)
            ss = SB(st, pref + "ss", [128, 4], F32); ssb = Buf()
            junk = SB(st, pref + "junk", [128, 128], F32)
            xn = SB(st, pref + "xn", [128, 128], F32); xnb = Buf()
            t1 = SB(st, pref + "t1", [128, 128], F32); t2 = SB(st, pref + "t2", [128, 128], F32); tb_ = Buf()
            x16 = [SB(st, pref + "x16%d" % i, [128, 128], BF16) for i in range(2)]; x16b = [Buf(), Buf()]
            rc = [SB(st, pref + "rc%d" % i, [128, 128], F32) for i in range(2)]
            rs = [SB(st, pref + "rs%d" % i, [128, 128], F32) for i in range(2)]; rb = [Buf(), Buf()]
            stg = SB(st, pref + "stg", [128, 8, 128], BF16); stgb = Buf()
            cnt = [0]

            def fn(tile, p, heads):
                k = tile % 2
                fw.dma("sync", rc[k][:], ropeC[tile * 128:(tile + 1) * 128, :], writes=[rb[k]])
                fw.dma("sync", rs[k][:], ropeS[tile * 128:(tile + 1) * 128, :], writes=[rb[k]])
                for hi, (co, dh) in enumerate(heads):
                    fw.op("scalar", lambda h, hi=hi, co=co: h.activation(out=junk[:], in_=PS[p][:, co:co + 128], func=AF.Square,
                                                                         accum_out=ss[:, hi:hi + 1]), reads=[PSB[p]], writes=[ssb])
                nh = len(heads)
                fw.op("scalar", lambda h: h.activation(out=ss[:, 0:nh], in_=ss[:, 0:nh], func=AF.Sqrt, bias=QK_EPS, scale=1.0 / 128),
                      reads=[ssb], writes=[ssb])
                fw.op("vector", lambda h: h.reciprocal(out=ss[:, 0:nh], in_=ss[:, 0:nh]), reads=[ssb], writes=[ssb])
                for hi, (co, dh) in enumerate(heads):
                    j = cnt[0] % 2; cnt[0] += 1
                    fw.op("vector", lambda h, hi=hi, co=co: h.scalar_tensor_tensor(out=xn[:], in0=PS[p][:, co:co + 128], scalar=ss[:, hi:hi + 1],
                                                                                  in1=grep_[:], op0=ALU.mult, op1=ALU.mult),
                          reads=[PSB[p], ssb, gb_], writes=[xnb])
                    fw.op("vector", lambda h: h.tensor_tensor(out=t1[:], in0=xn[:], in1=rc[k][:], op=ALU.mult), reads=[xnb, rb[k]], writes=[tb_])
                    xv = xn[:].rearrange("p (a h d) -> p a h d", a=2, h=2)
                    sv = rs[k][:].rearrange("p (a h d) -> p a h d", a=2, h=2)
                    tv = t2[:].rearrange("p (a h d) -> p a h d", a=2, h=2)
                    fw.op("gpsimd", lambda h: h.tensor_tensor(out=tv[:, :, 0, :], in0=xv[:, :, 1, :], in1=sv[:, :, 0, :], op=ALU.mult),
                          reads=[xnb, rb[k]], writes=[tb_])
                    fw.op("gpsimd", lambda h: h.tensor_tensor(out=tv[:, :, 1, :], in0=xv[:, :, 0, :], in1=sv[:, :, 1, :], op=ALU.mult),
                          reads=[xnb, rb[k]], writes=[tb_])
                    fw.op("vector", lambda h, j=j: h.tensor_tensor(out=x16[j][:], in0=t1[:], in1=t2[:], op=ALU.add), reads=[tb_], writes=[x16b[j]])
                    fw.op("tensor", lambda h, j=j, hi=hi: h.transpose(PT[0][:, hi * 128:(hi + 1) * 128], x16[j][:], ident[:]),
                          reads=[x16b[j], Bc], writes=[PTB[0]])
                fw.op("scalar", lambda h: h.copy(out=stg[:, 0:nh, :], in_=PT[0][:, 0:nh * 128].rearrange("p (c t) -> p c t", t=128)),
                      reads=[PTB[0]], writes=[stgb])
                for hi, (co, dh) in enumerate(heads):
                    fw.dma("gpsimd", dstT[:, dh, tile * 128:(tile + 1) * 128], stg[:, hi, :], reads=[stgb], writes=[dbuf(dstname)])
            return fn

        hTv = hT_d.rearrange("(c p) s -> p c s", p=128)
        hTov = hTo_d.rearrange("(c p) s -> p c s", p=128)

        if stop == "S0":
            return nc
        with ExitStack() as st:
            kfn = qk_epilogue_factory(st, k_norm_g, ropeC_k, ropeS_k, 2, kT_d, "kT_d", "k")
            v16 = [SB(st, "v16_%d" % i, [128, 256], BF16) for i in range(2)]; v16b = [Buf(), Buf()]

            def kv_ep(tile, pidx):
                p = pidx[0]
                kfn(tile, p, [(0, 0), (128, 1)])
                k = tile % 2
                fw.op("scalar", lambda h: h.copy(out=v16[k][:], in_=PS[p][:, 256:512]), reads=[PSB[p]], writes=[v16b[k]])
                fw.dma("gpsimd", v_d[tile * 128:(tile + 1) * 128, :], v16[k][:], reads=[v16b[k]], writes=[dbuf("v_d")])
            proj_pass(st, hTv, "hT_d", 64, [(1024, None, None)], kv_ep)
        fw.barrier()

        if stop == "KV":
            return nc
        for qb in range(2):
            with ExitStack() as st:
                qfn = qk_epilogue_factory(st, q_norm_g, ropeC_q, ropeS_q, 4, qT_d, "qT_d", "q")

                def q_ep(tile, pidx, qb=qb):
                    qfn(tile, pidx[0], [(i * 128, qb * 4 + i) for i in range(4)])
                proj_pass(st, hTov, "hTo_d", 16, [(qb * 512, None, None)], q_ep)
            fw.barrier()

        if stop == "Q":
            return nc
        for cb in range(2):
            with ExitStack() as st:
                x1s = [SB(st, "x1s%d" % i, [128, 512], F32) for i in range(2)]; x1b = [Buf(), Buf()]
                z16 = [SB(st, "z16_%d" % i, [128, 512], BF16) for i in range(2)]; z16b = [Buf(), Buf()]

                def z_ep(tile, pidx, cb=cb):
                    k = tile % 2
                    fw.op("scalar", lambda h: h.copy(out=x1s[k][:], in_=PS[pidx[0]][:]), reads=[PSB[pidx[0]]], writes=[x1b[k]])
                    fw.op("vector", lambda h: h.tensor_tensor(out=z16[k][:], in0=PS[pidx[1]][:], in1=x1s[k][:], op=ALU.mult),
                          reads=[PSB[pidx[1]], x1b[k]], writes=[z16b[k]])
                    fw.dma("gpsimd", z_d[tile * 128:(tile + 1) * 128, cb * 512:(cb + 1) * 512], z16[k][:], reads=[z16b[k]], writes=[dbuf("z_d")])
                c1 = 1024 + cb * 512; c2 = 2048 + cb * 512
                proj_pass(st, hTv, "hT_d", 64,
                          [(1536 + c1, c1, hy_conv_b[0:1, c1:c1 + 512]), (1536 + c2, c2, hy_conv_b[0:1, c2:c2 + 512])], z_ep)
            fw.barrier()

        if stop == "Z":
            return nc
        for cb in range(2):
            with ExitStack() as st:
                o16 = [SB(st, "o16_%d" % i, [128, 512], BF16) for i in range(2)]; o16b = [Buf(), Buf()]

                def x0_ep(tile, pidx, cb=cb):
                    k = tile % 2
                    fw.op("scalar", lambda h: h.copy(out=o16[k][:], in_=PS[pidx[0]][:]), reads=[PSB[pidx[0]]], writes=[o16b[k]])
                    fw.dma("gpsimd", x0_d[tile * 128:(tile + 1) * 128, cb * 512:(cb + 1) * 512], o16[k][:], reads=[o16b[k]], writes=[dbuf("x0_d")])
                c0 = cb * 512
                proj_pass(st, hTov, "hTo_d", 16, [(1536 + c0, c0, hy_conv_b[0:1, c0:c0 + 512])], x0_ep)
            fw.barrier()

        for cb in range(4):
            with ExitStack() as st:
                o16 = [SB(st, "g16_%d" % i, [128, 1024], BF16) for i in range(2)]; o16b = [Buf(), Buf()]

                def g_ep(tile, pidx, cb=cb):
                    k = tile % 2
                    for bi in range(2):
                        fw.op("scalar", lambda h, bi=bi: h.activation(out=o16[k][:, bi * 512:(bi + 1) * 512], in_=PS[pidx[bi]][:], func=AF.Sigmoid),
                              reads=[PSB[pidx[bi]]], writes=[o16b[k]])
                    fw.dma("gpsimd", g_d[tile * 128:(tile + 1) * 128, cb * 1024:(cb + 1) * 1024], o16[k][:], reads=[o16b[k]], writes=[dbuf("g_d")])
                c0 = cb * 1024
                proj_pass(st, hTov, "hTo_d", 16,
                          [(4608 + c0, None, b_gate[0:1, c0:c0 + 512]), (4608 + c0 + 512, None, b_gate[0:1, c0 + 512:c0 + 1024])], g_ep)
            fw.barrier()

        if stop == "S1":
            return nc
        with ExitStack() as st:
            kT = SB(st, "kT", [128, 2, S], BF16); kTb = Buf()
            vs = SB(st, "vs", [128, 64, 256], BF16); vsb = Buf()
            qT = SB(st, "qT", [128, 8, T], BF16); qTb = Buf()
            aT = SB(st, "aT", [128, 8, T], BF16); aTb = Buf()
            pT = [SB(st, "pT%d" % i, [128, 512], BF16) for i in range(3)]; pTb = [Buf() for _ in range(3)]
            rl = SB(st, "rl", [128, 512], F32); rlb = Buf()
            for hh in range(2):
                fw.dma("sync", kT[:, hh, :], kT_d[:, hh, :], reads=[dbuf("kT_d")], writes=[kTb])
            for g in range(4):
                fw.dma("sync", vs[:, g * 16:(g + 1) * 16, :], v_d[g * 2048:(g + 1) * 2048, :].rearrange("(t p) c -> p t c", p=128),
                       reads=[dbuf("v_d")], writes=[vsb])
            for g in range(4):
                fw.dma("sync", qT[:, g * 2:(g + 1) * 2, :], qT_d[:, g * 2:(g + 1) * 2, :], reads=[dbuf("qT_d")], writes=[qTb])
            sc = 1.0 / math.sqrt(128.0)
            it = 0
            for hd in range(8):
                kvh = hd // 4
                for qb in range(4):
                    o = 3 + (it % 2) * 0
                    po, pl = (3, 4)
                    for kc in range(64):
                        si = it % 3; it += 1
                        fw.op("tensor", lambda h, si=si, kc=kc: h.matmul(PS[si][:], kT[:, kvh, kc * 128:(kc + 1) * 128], qT[:, hd, qb * 512:(qb + 1) * 512],
                                                                        start=True, stop=True), reads=[kTb, qTb], writes=[PSB[si]])
                        fw.op("scalar", lambda h, si=si: h.activation(out=pT[si][:], in_=PS[si][:], func=AF.Exp, scale=sc),
                              reads=[PSB[si]], writes=[pTb[si]])
                        fw.op("tensor", lambda h, si=si, kc=kc: h.matmul(PS[po][:], vs[:, kc, kvh * 128:(kvh + 1) * 128], pT[si][:],
                                                                        start=(kc == 0), stop=(kc == 63)), reads=[vsb, pTb[si]], writes=[PSB[po]])
                        fw.op("tensor", lambda h, si=si, kc=kc: h.matmul(PS[pl][:], ones16[:], pT[si][:],
                                                                        start=(kc == 0), stop=(kc == 63)), reads=[Bc, pTb[si]], writes=[PSB[pl]])
                    fw.op("vector", lambda h: h.reciprocal(out=rl[:], in_=PS[pl][:]), reads=[PSB[pl]], writes=[rlb])
                    fw.op("vector", lambda h: h.tensor_tensor(out=aT[:, hd, qb * 512:(qb + 1) * 512], in0=PS[po][:], in1=rl[:], op=ALU.mult),
                          reads=[PSB[po], rlb], writes=[aTb])
            for g in range(4):
                fw.dma("gpsimd", at_d[:, g * 2:(g + 1) * 2, :], aT[:, g * 2:(g + 1) * 2, :], reads=[aTb], writes=[dbuf("at_d")])
        fw.barrier()

        if stop == "S2":
            return nc
        persist = ExitStack()
        top.enter_context(persist)
        scl = SB(persist, "scl", [128, C], F32); sclb = Buf()
        drep, drepb = rep_load(persist, "drep", hy_bias_d, C)
        with ExitStack() as st:
            w1 = SB(st, "fw1", [33, 64], F32); w2 = SB(st, "fw2", [64, 64], F32); w3 = SB(st, "fw3", [64, 2048], F32)
            fb = SB(st, "fb", [64, 4], F32); fbb = Buf(); wl = Buf()
            fw.dma("sync", w1[:], filt_w1, writes=[wl]); fw.dma("sync", w2[:], filt_w2, writes=[wl]); fw.dma("sync", w3[:], filt_w3, writes=[wl])
            fw.dma("sync", fb[:, 0:1], filt_f1, writes=[fbb]); fw.dma("sync", fb[:, 1:2], filt_b1, writes=[fbb])
            fw.dma("sync", fb[:, 2:3], filt_f2, writes=[fbb]); fw.dma("sync", fb[:, 3:4], filt_b2, writes=[fbb])
            fbp = SB(st, "fbp", [64, 2], F32)
            fw.op("vector", lambda h: h.tensor_tensor(out=fbp[:, 0:1], in0=fb[:, 0:1], in1=fb[:, 1:2], op=ALU.mult), reads=[fbb], writes=[fbb])
            fw.op("vector", lambda h: h.tensor_tensor(out=fbp[:, 1:2], in0=fb[:, 2:3], in1=fb[:, 3:4], op=ALU.mult), reads=[fbb], writes=[fbb])
            fT = SB(st, "fT", [33, S], F32); fTb = Buf()
            fw.dma("sync", fT[:], featsT, writes=[fTb])
            h1T = SB(st, "fh1T", [64, 512], F32); h1b = Buf()
            h2T = SB(st, "fh2T", [64, S], F32); h2b = Buf()
            ar = SB(st, "far", [64, 512], F32); m1 = SB(st, "fm1", [64, 512], F32); m2 = SB(st, "fm2", [64, 512], F32); arb = Buf()

            def sin_layer(pidx, fcol, bcol, dst_ap, dstb):
                fw.op("scalar", lambda h: h.activation(out=ar[:], in_=PS[pidx][0:64, :], func=AF.Identity, scale=fb[:, fcol:fcol + 1], bias=fbp[:, bcol:bcol + 1]),
                      reads=[PSB[pidx], fbb], writes=[arb])
                fw.op("vector", lambda h: h.tensor_scalar(out=m1[:], in0=ar[:], scalar1=PI, scalar2=-2 * PI, op0=ALU.is_gt, op1=ALU.mult), reads=[arb], writes=[arb])
                fw.op("vector", lambda h: h.tensor_scalar(out=m2[:], in0=ar[:], scalar1=-PI, scalar2=2 * PI, op0=ALU.is_lt, op1=ALU.mult), reads=[arb], writes=[arb])
                fw.op("vector", lambda h: h.tensor_tensor(out=ar[:], in0=ar[:], in1=m1[:], op=ALU.add), reads=[arb], writes=[arb])
                fw.op("vector", lambda h: h.tensor_tensor(out=ar[:], in0=ar[:], in1=m2[:], op=ALU.add), reads=[arb], writes=[arb])
                fw.op("scalar", lambda h: h.activation(out=dst_ap, in_=ar[:], func=AF.Sin), reads=[arb], writes=[dstb])

            for pb in range(16):
                fw.op("tensor", lambda h, pb=pb: h.matmul(PS[0][0:64, :], w1[:], fT[:, pb * 512:(pb + 1) * 512], start=True, stop=True),
                      reads=[wl, fTb], writes=[PSB[0]])
                sin_layer(0, 0, 0, h1T[:], h1b)
                fw.op("tensor", lambda h: h.matmul(PS[1][0:64, :], w2[:], h1T[:], start=True, stop=True), reads=[wl, h1b], writes=[PSB[1]])
                sin_layer(1, 2, 1, h2T[:, pb * 512:(pb + 1) * 512], h2b)
            adl, adlb = rep_load(st, "adl", absdelta, C)
            ngt = SB(st, "ngt", [128, 64], F32); m0s = SB(st, "m0s", [128, 1], F32); ngb = Buf()
            fw.dma("sync", ngt[:], negt, writes=[ngb]); fw.dma("sync", m0s[:], m0, writes=[ngb])
            dec = [SB(st, "dec%d" % i, [128, C], F32) for i in range(2)]; decb = [Buf(), Buf()]
            fo = [SB(st, "fo%d" % i, [128, 2048], F32) for i in range(2)]; fob = [Buf(), Buf()]
            fo16 = [SB(st, "fo16_%d" % i, [128, 2048], BF16) for i in range(2)]; fo16b = [Buf(), Buf()]
            sq = [SB(st, "fsq%d" % i, [128, 2048], F32) for i in range(2)]; sqb = [Buf(), Buf()]
            for pt in range(64):
                k = pt % 2
                fw.op("scalar", lambda h: h.activation(out=dec[k][:], in_=adl[:], func=AF.Exp, scale=ngt[:, pt:pt + 1]), reads=[adlb, ngb], writes=[decb[k]])
                for cb in range(4):
                    fw.op("tensor", lambda h, cb=cb: h.matmul(PS[cb][:], h2T[:, pt * 128:(pt + 1) * 128], w3[:, cb * 512:(cb + 1) * 512], start=True, stop=True),
                          reads=[wl, h2b], writes=[PSB[cb]])
                    fw.op("vector", lambda h, cb=cb: h.tensor_tensor(out=fo[k][:, cb * 512:(cb + 1) * 512], in0=PS[cb][:],
                                                                    in1=dec[k][:, (cb % 2) * 512:(cb % 2) * 512 + 512], op=ALU.mult),
                          reads=[PSB[cb], decb[k]], writes=[fob[k]])
                if pt == 0:
                    fw.op("vector", lambda h: h.tensor_scalar(out=fo[k][:, 1024:2048], in0=fo[k][:, 1024:2048], scalar1=m0s[:, 0:1], scalar2=None, op0=ALU.mult),
                          reads=[fob[k], ngb], writes=[fob[k]])
                fw.op("scalar", lambda h: h.copy(out=fo16[k][:], in_=fo[k][:]), reads=[fob[k]], writes=[fo16b[k]])
                fw.op("gpsimd", lambda h: h.tensor_tensor(out=sq[k][:], in0=fo[k][:], in1=fo[k][:], op=ALU.mult), reads=[fob[k]], writes=[sqb[k]])
                for cb in range(4):
                    fw.op("tensor", lambda h, cb=cb: h.matmul(PS[4 + cb % 2][:], onesf[:], sq[k][:, cb * 512:(cb + 1) * 512],
                                                              start=(pt == 0 and cb < 2), stop=(pt == 63 and cb >= 2)), reads=[Bc, sqb[k]], writes=[PSB[4 + cb % 2]])
                fw.dma("gpsimd", hf_d[pt * 128:(pt + 1) * 128, :], fo16[k][:, 0:1024], reads=[fo16b[k]], writes=[dbuf("hf_d")])
                fw.dma("gpsimd", hg_d[pt * 128:(pt + 1) * 128, :], fo16[k][:, 1024:2048], reads=[fo16b[k]], writes=[dbuf("hg_d")])
            for cb in range(2):
                fw.op("scalar", lambda h, cb=cb: h.activation(out=scl[:, cb * 512:(cb + 1) * 512], in_=PS[4 + cb][:], func=AF.Sqrt, bias=FILTER_EPS, scale=1.0),
                      reads=[PSB[4 + cb]], writes=[sclb])
            fw.op("vector", lambda h: h.reciprocal(out=scl[:], in_=scl[:]), reads=[sclb], writes=[sclb])
        fw.barrier()

        if stop == "S3a":
            return nc
        with ExitStack() as st:
            def cload(name, src, shape, dt=BF16):
                t = SB(st, name, shape, dt); b = Buf()
                fw.dma("gpsimd" if dt == BF16 else "sync", t[:], src, writes=[b])
                return t, b
            Fc, Fb1 = cload("Fc", cFc, [128, 128]); Fs, Fb2 = cload("Fs", cFs, [128, 128])
            Fsn, Fb3 = cload("Fsn", cFsn, [128, 128]); Fcn, Fb4 = cload("Fcn", cFcn, [128, 128])
            C16, Fb5 = cload("C16", cC16, [128, 128], F32); S16, Fb6 = cload("S16", cS16, [128, 128], F32); S16n, Fb7 = cload("S16n", cS16n, [128, 128], F32)
            i2re, Fb8 = cload("i2re", ci2re, [128, 16]); i2im, Fb9 = cload("i2im", ci2im, [128, 16])
            FB = Buf()
            fw.op("vector", lambda h: h.tensor_copy(out=C16[:, 0:1], in_=C16[:, 0:1]), reads=[Fb1, Fb2, Fb3, Fb4, Fb5, Fb6, Fb7, Fb8, Fb9], writes=[FB])

            def twiddle(pre, pim, trc, tic, ticn, o_re, o_im, ob, tmp, tmpb, rd):
                fw.op("scalar", lambda h: h.activation(out=tmp[:, 0:512], in_=PS[pim][:], func=AF.Identity, scale=ticn), reads=[PSB[pim]] + rd, writes=[tmpb])
                fw.op("scalar", lambda h: h.activation(out=tmp[:, 512:1024], in_=PS[pim][:], func=AF.Identity, scale=trc), reads=[PSB[pim]] + rd, writes=[tmpb])
                fw.op("vector", lambda h: h.scalar_tensor_tensor(out=o_re, in0=PS[pre][:], scalar=trc, in1=tmp[:, 0:512], op0=ALU.mult, op1=ALU.add),
                      reads=[PSB[pre], tmpb] + rd, writes=[ob])
                fw.op("vector", lambda h: h.scalar_tensor_tensor(out=o_im, in0=PS[pre][:], scalar=tic, in1=tmp[:, 512:1024], op0=ALU.mult, op1=ALU.add),
                      reads=[PSB[pre], tmpb] + rd, writes=[ob])

            zt_ = [SB(st, "f1z%d" % i, [64, 32, 512], BF16) for i in range(2)]; ztb_ = [Buf(), Buf()]
            ao = [SB(st, "f1o%d" % i, [128, 1024], BF16) for i in range(2)]; aob = [Buf(), Buf()]
            tmp = SB(st, "ftmp", [128, 1024], F32); tmpb = Buf()

            def f1_pass(src, srcname, dst, dstname):
                sv = src.rearrange("(a b) c -> a b c", b=128)
                it = 0
                for cb in range(2):
                    for nb in range(4):
                        k = (cb * 4 + nb) % 2
                        fw.dma("sync", zt_[k][:], sv[:, nb * 32:(nb + 1) * 32, cb * 512:(cb + 1) * 512], reads=[dbuf(srcname)], writes=[ztb_[k]])
                        for nn in range(32):
                            n1 = nb * 32 + nn
                            pr = (it % 3) * 2; pi_ = pr + 1; j = it % 2; it += 1
                            fw.op("tensor", lambda h, nn=nn, pr=pr: h.matmul(PS[pr][:], Fc[0:64, :], zt_[k][:, nn, :], start=True, stop=True),
                                  reads=[FB, ztb_[k]], writes=[PSB[pr]])
                            fw.op("tensor", lambda h, nn=nn, pi_=pi_: h.matmul(PS[pi_][:], Fsn[0:64, :], zt_[k][:, nn, :], start=True, stop=True),
                                  reads=[FB, ztb_[k]], writes=[PSB[pi_]])
                            twiddle(pr, pi_, C16[:, n1:n1 + 1], S16n[:, n1:n1 + 1], S16[:, n1:n1 + 1], ao[j][:, 0:512], ao[j][:, 512:1024], aob[j], tmp, tmpb, [FB])
                            fw.dma("gpsimd", dst[0][n1, :, cb * 512:(cb + 1) * 512], ao[j][:, 0:512], reads=[aob[j]], writes=[dbuf(dstname)])
                            fw.dma("gpsimd", dst[1][n1, :, cb * 512:(cb + 1) * 512], ao[j][:, 512:1024], reads=[aob[j]], writes=[dbuf(dstname)])

            f1_pass(hf_d, "hf_d", a_d[1], "a_d1")
            f1_pass(hg_d, "hg_d", a_d[2], "a_d2")
            f1_pass(z_d, "z_d", a_d[0], "a_d0")
            fw.barrier()

            ain = [[SB(st, "ain%d_%d" % (i, r), [128, 512], BF16) for r in range(4)] for i in range(2)]; ainb = [[Buf() for r in range(4)] for i in range(2)]
            ho = [SB(st, "hho%d" % i, [128, 1024], BF16) for i in range(2)]; hob = [Buf(), Buf()]
            it = 0
            for cb in range(2):
                cs = slice(cb * 512, (cb + 1) * 512)
                for k2 in range(128):
                    k = it % 2; pr = (it % 3) * 2; pi_ = pr + 1; it += 1
                    srcs = [(a_d[1][0], "a_d1"), (a_d[1][1], "a_d1"), (a_d[2][0], "a_d2"), (a_d[2][1], "a_d2")]
                    for r, (sd, sn) in enumerate(srcs):
                        fw.dma("sync", ain[k][r][:], sd[:, k2, cs], writes=[ainb[k][r]])
                    for r, lt in enumerate([Fc, Fs, Fc, Fs]):
                        fw.op("tensor", lambda h, r=r, lt=lt, pr=pr: h.matmul(PS[pr][:], lt[:], ain[k][r][:], start=(r == 0), stop=(r == 3)),
                              reads=[FB, ainb[k][r]], writes=[PSB[pr]])
                    for r, lt in enumerate([Fsn, Fc, Fs, Fcn]):
                        fw.op("tensor", lambda h, r=r, lt=lt, pi_=pi_: h.matmul(PS[pi_][:], lt[:], ain[k][r][:], start=(r == 0), stop=(r == 3)),
                              reads=[FB, ainb[k][r]], writes=[PSB[pi_]])
                    fw.op("vector", lambda h, pr=pr: h.tensor_tensor(out=tmp[:, 0:512], in0=PS[pr][:], in1=scl[:, cs], op=ALU.mult), reads=[PSB[pr], sclb], writes=[tmpb])
                    fw.op("vector", lambda h: h.tensor_tensor(out=ho[k][:, 0:512], in0=tmp[:, 0:512], in1=drep[:, cs], op=ALU.add), reads=[tmpb, drepb], writes=[hob[k]])
                    fw.op("vector", lambda h, pi_=pi_: h.tensor_tensor(out=ho[k][:, 512:1024], in0=PS[pi_][:], in1=scl[:, cs], op=ALU.mult), reads=[PSB[pi_], sclb], writes=[hob[k]])
                    fw.dma("gpsimd", hh_d[0][k2, :, cs], ho[k][:, 0:512], reads=[hob[k]], writes=[dbuf("hh_d")])
                    fw.dma("gpsimd", hh_d[1][k2, :, cs], ho[k][:, 512:1024], reads=[hob[k]], writes=[dbuf("hh_d")])

            fw.barrier()
            hin = [[SB(st, "hin%d_%d" % (i, r), [128, 512], BF16) for r in range(2)] for i in range(2)]; hinb = [[Buf(), Buf()] for i in range(2)]
            xs = SB(st, "fxs", [128, 1024], F32); xsb = Buf()
            ys = SB(st, "fys", [128, 1024], BF16); ysb = Buf()
            t4 = SB(st, "ft4", [128, 2048], F32); t4b = Buf()
            bo = [SB(st, "fbo%d" % i, [128, 1024], BF16) for i in range(2)]; bob = [Buf(), Buf()]
            it = 0
            for cb in range(2):
                cs = slice(cb * 512, (cb + 1) * 512)
                for k2 in range(128):
                    k = it % 2; it += 1
                    fw.dma("sync", ain[k][0][:], a_d[0][0][:, k2, cs], writes=[ainb[k][0]])
                    fw.dma("sync", ain[k][1][:], a_d[0][1][:, k2, cs], writes=[ainb[k][1]])
                    fw.dma("sync", hin[k][0][:], hh_d[0][k2, :, cs], writes=[hinb[k][0]])
                    fw.dma("sync", hin[k][1][:], hh_d[1][k2, :, cs], writes=[hinb[k][1]])
                    fw.op("tensor", lambda h: h.matmul(PS[0][:], Fc[:], ain[k][0][:], start=True, stop=False), reads=[FB, ainb[k][0]], writes=[PSB[0]])
                    fw.op("tensor", lambda h: h.matmul(PS[0][:], Fs[:], ain[k][1][:], start=False, stop=True), reads=[FB, ainb[k][1]], writes=[PSB[0]])
                    fw.op("tensor", lambda h: h.matmul(PS[1][:], Fsn[:], ain[k][0][:], start=True, stop=False), reads=[FB, ainb[k][0]], writes=[PSB[1]])
                    fw.op("tensor", lambda h: h.matmul(PS[1][:], Fc[:], ain[k][1][:], start=False, stop=True), reads=[FB, ainb[k][1]], writes=[PSB[1]])
                    fw.op("scalar", lambda h: h.copy(out=xs[:, 0:512], in_=PS[0][:]), reads=[PSB[0]], writes=[xsb])
                    fw.op("scalar", lambda h: h.copy(out=xs[:, 512:1024], in_=PS[1][:]), reads=[PSB[1]], writes=[xsb])
                    fw.op("vector", lambda h: h.tensor_tensor(out=t4[:, 0:512], in0=xs[:, 0:512], in1=hin[k][0][:], op=ALU.mult), reads=[xsb, hinb[k][0]], writes=[t4b])
                    fw.op("gpsimd", lambda h: h.tensor_tensor(out=t4[:, 512:1024], in0=xs[:, 512:1024], in1=hin[k][1][:], op=ALU.mult), reads=[xsb, hinb[k][1]], writes=[t4b])
                    fw.op("vector", lambda h: h.tensor_tensor(out=t4[:, 1024:1536], in0=xs[:, 0:512], in1=hin[k][1][:], op=ALU.mult), reads=[xsb, hinb[k][1]], writes=[t4b])
                    fw.op("gpsimd", lambda h: h.tensor_tensor(out=t4[:, 1536:2048], in0=xs[:, 512:1024], in1=hin[k][0][:], op=ALU.mult), reads=[xsb, hinb[k][0]], writes=[t4b])
                    fw.op("vector", lambda h: h.tensor_tensor(out=ys[:, 0:512], in0=t4[:, 0:512], in1=t4[:, 512:1024], op=ALU.subtract), reads=[t4b], writes=[ysb])
                    fw.op("gpsimd", lambda h: h.tensor_tensor(out=ys[:, 512:1024], in0=t4[:, 1024:1536], in1=t4[:, 1536:2048], op=ALU.add), reads=[t4b], writes=[ysb])
                    fw.op("tensor", lambda h: h.matmul(PS[2][:], Fc[:], ys[:, 0:512], start=True, stop=False), reads=[FB, ysb], writes=[PSB[2]])
                    fw.op("tensor", lambda h: h.matmul(PS[2][:], Fsn[:], ys[:, 512:1024], start=False, stop=True), reads=[FB, ysb], writes=[PSB[2]])
                    fw.op("tensor", lambda h: h.matmul(PS[3][:], Fs[:], ys[:, 0:512], start=True, stop=False), reads=[FB, ysb], writes=[PSB[3]])
                    fw.op("tensor", lambda h: h.matmul(PS[3][:], Fc[:], ys[:, 512:1024], start=False, stop=True), reads=[FB, ysb], writes=[PSB[3]])
                    twiddle(2, 3, C16[:, k2:k2 + 1], S16[:, k2:k2 + 1], S16n[:, k2:k2 + 1], bo[k][:, 0:512], bo[k][:, 512:1024], bob[k], tmp, tmpb, [FB])
                    fw.dma("gpsimd", b_d[0][k2, :, cs], bo[k][:, 0:512], reads=[bob[k]], writes=[dbuf("b_d")])
                    fw.dma("gpsimd", b_d[1][k2, :, cs], bo[k][:, 512:1024], reads=[bob[k]], writes=[dbuf("b_d")])

            fw.barrier()
            bin_ = [[SB(st, "bin%d_%d" % (i, r), [128, 16, 512], BF16) for r in range(2)] for i in range(2)]; binb = [Buf(), Buf()]
            yo = [SB(st, "fyo%d" % i, [16, 512], F32) for i in range(2)]; yob = [Buf(), Buf()]
            yv = y_d.rearrange("(a b) c -> a b c", b=128)
            it = 0
            for cb in range(2):
                cs = slice(cb * 512, (cb + 1) * 512)
                for nb in range(8):
                    k = (cb * 8 + nb) % 2
                    for r in range(2):
                        fw.dma("sync", bin_[k][r][:], b_d[r][:, nb * 16:(nb + 1) * 16, cs], reads=[dbuf("b_d")], writes=[binb[k]])
                    for nn in range(16):
                        n1 = nb * 16 + nn
                        p = it % 6; j = it % 2; it += 1
                        fw.op("tensor", lambda h, nn=nn, p=p: h.matmul(PS[p][0:16, :], i2re[:], bin_[k][0][:, nn, :], start=True, stop=False), reads=[FB, binb[k]], writes=[PSB[p]])
                        fw.op("tensor", lambda h, nn=nn, p=p: h.matmul(PS[p][0:16, :], i2im[:], bin_[k][1][:, nn, :], start=False, stop=True), reads=[FB, binb[k]], writes=[PSB[p]])
                        fw.op("scalar", lambda h, p=p, j=j: h.copy(out=yo[j][:], in_=PS[p][0:16, :]), reads=[PSB[p]], writes=[yob[j]])
                        fw.dma("gpsimd", yv[:, n1, cs], yo[j][:], reads=[yob[j]], writes=[dbuf("y_d")])
        persist.close()
        fw.barrier()

        if stop == "S3b":
            return nc
        def wload(st, name, src, nchunk, ncol):
            t = SB(st, name, [128, nchunk, ncol], BF16); b = Buf()
            sv = src.rearrange("(c p) n -> p c n", p=128)
            for c0 in range(0, ncol, 512):
                fw.dma("gpsimd", t[:, :, c0:c0 + 512], sv[:, :, c0:c0 + 512], writes=[b])
            return t, b

        with ExitStack() as st:
            Wa, Wab = wload(st, "Wa", w_attn_o, 8, D)
            Wh, Whb = wload(st, "Wh", w_hy_o, 8, D)
            yt = [SB(st, "yt%d" % i, [128, C], F32) for i in range(2)]
            x0t = [SB(st, "x0t%d" % i, [128, C], BF16) for i in range(2)]
            gt = [SB(st, "gt%d" % i, [128, 4096], BF16) for i in range(2)]
            atT = [SB(st, "atT%d" % i, [128, 8, 128], BF16) for i in range(2)]; inb = [Buf(), Buf()]
            yh16 = SB(st, "yh16", [128, C], BF16); yhb = Buf()
            yhT = SB(st, "yhT", [128, 8, 128], BF16); yhTb = Buf()
            ya = SB(st, "ya", [128, 512], F32); yab = Buf()
            pre16 = SB(st, "pre16", [128, D], BF16); preb = Buf()
            preT = SB(st, "preTs", [128, 16, 128], BF16); preTb = Buf()
            preTv = preT_d.rearrange("(c p) t -> p c t", p=128)
            for t in range(16):
                k = t % 2
                rows = slice(t * 128, (t + 1) * 128)
                fw.dma("sync", yt[k][:], y_d[rows, :], reads=[dbuf("y_d")], writes=[inb[k]])
                fw.dma("sync", x0t[k][:], x0_d[rows, :], reads=[dbuf("x0_d")], writes=[inb[k]])
                fw.dma("sync", gt[k][:], g_d[rows, :], reads=[dbuf("g_d")], writes=[inb[k]])
                fw.dma("sync", atT[k][:], at_d[:, :, rows], reads=[dbuf("at_d")], writes=[inb[k]])
                fw.op("vector", lambda h: h.tensor_tensor(out=yh16[:], in0=yt[k][:], in1=x0t[k][:], op=ALU.mult), reads=[inb[k]], writes=[yhb])
                transpose_to(yh16, yhb, yhT, yhTb, 8)
                for cb in range(4):
                    cs = slice(cb * 512, (cb + 1) * 512)
                    pa = (cb % 3) * 2; ph = pa + 1
                    for c in range(8):
                        fw.op("tensor", lambda h, c=c, pa=pa: h.matmul(PS[pa][:], atT[k][:, c, :], Wa[:, c, cs], start=(c == 0), stop=(c == 7)),
                              reads=[inb[k], Wab], writes=[PSB[pa]])
                    for c in range(8):
                        fw.op("tensor", lambda h, c=c, ph=ph: h.matmul(PS[ph][:], yhT[:, c, :], Wh[:, c, cs], start=(c == 0), stop=(c == 7)),
                              reads=[yhTb, Whb], writes=[PSB[ph]])
                    fw.op("vector", lambda h, pa=pa: h.tensor_tensor(out=ya[:], in0=PS[pa][:], in1=gt[k][:, cs], op=ALU.mult), reads=[PSB[pa], inb[k]], writes=[yab])
                    fw.op("vector", lambda h, ph=ph: h.tensor_tensor(out=pre16[:, cs], in0=PS[ph][:], in1=gt[k][:, 2048 + cb * 512:2048 + (cb + 1) * 512], op=ALU.mult),
                          reads=[PSB[ph], inb[k]], writes=[preb])
                    fw.op("vector", lambda h: h.tensor_tensor(out=pre16[:, cs], in0=pre16[:, cs], in1=ya[:], op=ALU.add), reads=[preb, yab], writes=[preb])
                transpose_to(pre16, preb, preT, preTb, 16)
                fw.dma("gpsimd", preTv[:, :, rows], preT[:], reads=[preTb], writes=[dbuf("preT_d")])
        fw.barrier()

        if stop == "S4a":
            return nc
        with ExitStack() as st:
            Wo, Wob = wload(st, "Wo", w_out, 16, D)
            g1, g1b = rep_load(st, "g1", ln1_g, D); b1, b1b = rep_load(st, "b1", ln1_b, D)
            gbb = Buf()
            fw.op("vector", lambda h: h.tensor_copy(out=g1[:, 0:1], in_=g1[:, 0:1]), reads=[g1b, b1b], writes=[gbb])
            pT_ = [SB(st, "ppT%d" % i, [128, 16, 128], BF16) for i in range(2)]
            hres = [SB(st, "hres%d" % i, [128, D], F32) for i in range(2)]; inb = [Buf(), Buf()]
            h1t = [SB(st, "h1t%d" % i, [128, D], F32) for i in range(2)]; h1b_ = [Buf(), Buf()]
            h116 = SB(st, "h116", [128, D], BF16); h116b = Buf()
            h1Ts = SB(st, "h1Ts", [128, 16, 128], BF16); h1Tb = Buf()
            st8 = SB(st, "st8b", [128, 24], F32); sb8 = Buf(); mv = SB(st, "mvb", [128, 2], F32); mvb = Buf(); rstd = SB(st, "rstdb", [128, 1], F32)
            preTv = preT_d.rearrange("(c p) t -> p c t", p=128)
            h1Tv = h1T_d.rearrange("(c p) t -> p c t", p=128)
            for t in range(16):
                k = t % 2
                rows = slice(t * 128, (t + 1) * 128)
                fw.dma("sync", pT_[k][:], preTv[:, :, rows], reads=[dbuf("preT_d")], writes=[inb[k]])
                fw.dma("sync", hres[k][:], ho_d[128 * t + 1:128 * t + 129, :], reads=[dbuf("ho_d")], writes=[inb[k]])
                for cb in range(4):
                    cs = slice(cb * 512, (cb + 1) * 512)
                    p = (t * 4 + cb) % 6
                    for c in range(16):
                        fw.op("tensor", lambda h, c=c, p=p: h.matmul(PS[p][:], pT_[k][:, c, :], Wo[:, c, cs], start=(c == 0), stop=(c == 15)),
                              reads=[inb[k], Wob], writes=[PSB[p]])
                    fw.op("vector", lambda h, p=p: h.scalar_tensor_tensor(out=h1t[k][:, cs], in0=hres[k][:, cs], scalar=ALPHA, in1=PS[p][:], op0=ALU.mult, op1=ALU.add),
                          reads=[inb[k], PSB[p]], writes=[h1b_[k]])
                layernorm(h1t[k], h1b_[k], g1, b1, gbb, st8, sb8, mv, mvb, rstd)
                fw.dma("gpsimd", h1_d[rows, :], h1t[k][:], reads=[h1b_[k]], writes=[dbuf("h1_d")])
                fw.op("scalar", lambda h: h.copy(out=h116[:], in_=h1t[k][:]), reads=[h1b_[k]], writes=[h116b])
                transpose_to(h116, h116b, h1Ts, h1Tb, 16)
                fw.dma("gpsimd", h1Tv[:, :, rows], h1Ts[:], reads=[h1Tb], writes=[dbuf("h1T_d")])
        fw.barrier()

        if stop == "S4b":
            return nc
        persist2 = ExitStack()
        top.enter_context(persist2)
        I32 = mybir.dt.int32
        OH1 = SB(persist2, "OH1", [128, 16, 32], F32); OH2 = SB(persist2, "OH2", [128, 16, 32], F32)
        G12 = SB(persist2, "G12", [128, 16, 2], F32); Gb = Buf()
        SL1 = SB(persist2, "SL1", [128, 16], I32); SL2 = SB(persist2, "SL2", [128, 16], I32); SLb = Buf()
        IG = SB(persist2, "IG", [128, 64, 16], I32); ID = SB(persist2, "ID", [128, 64, 8], I32); IXb = Buf()
        zt16 = SB(persist2, "zt16", [128, D], BF16); ztb16 = Buf()
        fw.op("gpsimd", lambda h: h.memset(zt16[:], 0.0), writes=[ztb16])
        for i in range(64):
            fw.dma("sync", xs_d[i * 128:(i + 1) * 128, :], zt16[:], reads=[ztb16], writes=[Buf()])
        h1Tv = h1T_d.rearrange("(c p) t -> p c t", p=128)
        BIG = 1.0e30
        with ExitStack() as st:
            Wr = SB(st, "Wr", [128, 16, 36], BF16); Wrb = Buf()
            fw.dma("gpsimd", Wr[:], w_route.rearrange("(c p) n -> p c n", p=128), writes=[Wrb])
            br16 = SB(st, "br16", [1, 36], BF16)
            fw.dma("gpsimd", br16[:], b_route, writes=[Wrb])
            hT_ = [SB(st, "rhT%d" % i, [128, 16, 128], BF16) for i in range(2)]; inb = [Buf(), Buf()]
            lg = SB(st, "lg", [128, 36], F32); lgb = Buf()
            sm = SB(st, "rsm", [128, 16], F32); smb = Buf()
            oh = SB(st, "roh", [128, 96], F32); ohb = Buf()
            for t in range(16):
                k = t % 2
                fw.dma("sync", hT_[k][:], h1Tv[:, :, t * 128:(t + 1) * 128], reads=[dbuf("h1T_d")], writes=[inb[k]])
                p = t % 6
                for c in range(16):
                    fw.op("tensor", lambda h, c=c, p=p: h.matmul(PS[p][:, 0:36], hT_[k][:, c, :], Wr[:, c, :], start=(c == 0), stop=False), reads=[inb[k], Wrb], writes=[PSB[p]])
                fw.op("tensor", lambda h, p=p: h.matmul(PS[p][:, 0:36], ones16[0:1, :], br16[:], start=False, stop=True), reads=[Wrb, Bc], writes=[PSB[p]])
                fw.op("vector", lambda h, p=p: h.tensor_copy(out=lg[:], in_=PS[p][:, 0:36]), reads=[PSB[p]], writes=[lgb])
                V = lambda fn, r, w: fw.op("vector", fn, reads=r, writes=w)
                V(lambda h: h.tensor_reduce(out=sm[:, 0:1], in_=lg[:, 0:4], axis=mybir.AxisListType.X, op=ALU.max), [lgb], [smb])
                V(lambda h: h.tensor_scalar(out=oh[:, 0:4], in0=lg[:, 0:4], scalar1=sm[:, 0:1], scalar2=None, op0=ALU.is_equal), [lgb, smb], [ohb])
                V(lambda h: h.tensor_scalar(out=oh[:, 4:8], in0=lg[:, 0:4], scalar1=sm[:, 0:1], scalar2=None, op0=ALU.subtract), [lgb, smb], [ohb])
                fw.op("scalar", lambda h: h.activation(out=oh[:, 4:8], in_=oh[:, 4:8], func=AF.Exp, accum_out=sm[:, 1:2]), reads=[ohb, smb], writes=[ohb, smb])
                V(lambda h: h.reciprocal(out=sm[:, 2:3], in_=sm[:, 1:2]), [smb], [smb])
                V(lambda h: h.tensor_scalar(out=oh[:, 8:12], in0=oh[:, 0:4], scalar1=-1.0, scalar2=BIG, op0=ALU.add, op1=ALU.mult), [ohb], [ohb])
                for g_ in range(4):
                    V(lambda h, g_=g_: h.tensor_scalar(out=oh[:, 32 + 8 * g_:40 + 8 * g_], in0=lg[:, 4 + 8 * g_:12 + 8 * g_], scalar1=oh[:, 8 + g_:9 + g_], scalar2=None, op0=ALU.add),
                      [lgb, ohb], [ohb])
                V(lambda h: h.tensor_reduce(out=sm[:, 3:4], in_=oh[:, 32:64], axis=mybir.AxisListType.X, op=ALU.max), [ohb], [smb])
                V(lambda h: h.tensor_scalar(out=oh[:, 64:96], in0=oh[:, 32:64], scalar1=sm[:, 3:4], scalar2=None, op0=ALU.is_equal), [ohb, smb], [ohb])
                V(lambda h: h.scalar_tensor_tensor(out=oh[:, 32:64], in0=oh[:, 64:96], scalar=-BIG, in1=oh[:, 32:64], op0=ALU.mult, op1=ALU.add), [ohb], [ohb])
                V(lambda h: h.tensor_reduce(out=sm[:, 4:5], in_=oh[:, 32:64], axis=mybir.AxisListType.X, op=ALU.max), [ohb], [smb])
                V(lambda h: h.tensor_scalar(out=oh[:, 32:64], in0=oh[:, 32:64], scalar1=sm[:, 4:5], scalar2=None, op0=ALU.is_equal), [ohb, smb], [ohb])
                V(lambda h: h.tensor_tensor(out=sm[:, 5:6], in0=sm[:, 4:5], in1=sm[:, 3:4], op=ALU.subtract), [smb], [smb])
                fw.op("scalar", lambda h: h.activation(out=sm[:, 6:7], in_=sm[:, 5:6], func=AF.Exp), reads=[smb], writes=[smb])
                V(lambda h: h.tensor_scalar(out=sm[:, 7:8], in0=sm[:, 6:7], scalar1=1.0, scalar2=None, op0=ALU.add), [smb], [smb])
                V(lambda h: h.reciprocal(out=sm[:, 8:9], in_=sm[:, 7:8]), [smb], [smb])
                V(lambda h: h.tensor_tensor(out=sm[:, 9:10], in0=sm[:, 8:9], in1=sm[:, 2:3], op=ALU.mult), [smb], [smb])
                V(lambda h: h.tensor_tensor(out=sm[:, 10:11], in0=sm[:, 9:10], in1=sm[:, 6:7], op=ALU.mult), [smb], [smb])
                V(lambda h, t=t: h.tensor_copy(out=OH1[:, t, :], in_=oh[:, 64:96]), [ohb], [Gb])
                V(lambda h, t=t: h.tensor_copy(out=OH2[:, t, :], in_=oh[:, 32:64]), [ohb], [Gb])
                V(lambda h, t=t: h.tensor_copy(out=G12[:, t, :], in_=sm[:, 9:11]), [smb], [Gb])
            A = SB(st, "mA", [128, 16, 32], F32); Ab = Buf()
            R = SB(st, "mR", [128, 16, 32], F32); Rb = Buf()
            U = SB(st, "mU", [128, 128], F32); Ub = Buf()
            base = SB(st, "mbase", [128, 32], F32); baseb = Buf()
            ci = SB(st, "mci", [128, 32], I32); padf = SB(st, "mpadf", [128, 32], F32); pe = SB(st, "mpe", [128, 32], F32)
            pst = SB(st, "mpst", [128, 32], F32); one32 = SB(st, "mone32", [128, 32], F32); pb_ = Buf()
            sf = SB(st, "msf", [128, 32], F32); eb = SB(st, "meb", [128, 64], F32); junk32 = SB(st, "mj32", [128, 32], F32); ebb = Buf()
            igf = SB(st, "migf", [128, 64, 16], F32); bG = SB(st, "mbG", [128, 64], F32); bD = SB(st, "mbD", [128, 64], F32)
            pc = SB(st, "mpc", [128, 1], F32)
            fw.dma("sync", pc[:], pcol, writes=[ebb])
            V(lambda h: h.tensor_tensor(out=A[:], in0=OH1[:], in1=OH2[:], op=ALU.add), [Gb], [Ab])
            fw.op("gpsimd", lambda h: h.memset(U[:], 1.0), writes=[Ub])
            fw.op("gpsimd", lambda h: h.affine_select(out=U[:], in_=U[:], pattern=[[1, 128]], compare_op=ALU.is_gt, fill=0.0, base=0, channel_multiplier=-1),
                  reads=[Ub], writes=[Ub])
            V(lambda h: h.memset(base[:], 0.0), [], [baseb])
            V(lambda h: h.memset(one32[:], 1.0), [], [pb_])
            for i in range(16):
                pa = (2 * i) % 6; pb2 = (2 * i + 1) % 6
                fw.op("tensor", lambda h, i=i, pa=pa: h.matmul(PS[pa][:, 0:32], U[:], A[:, i, :], start=True, stop=True), reads=[Ub, Ab], writes=[PSB[pa]])
                fw.op("tensor", lambda h, i=i, pb2=pb2: h.matmul(PS[pb2][:, 0:32], onesf[:], A[:, i, :], start=True, stop=True), reads=[Bc, Ab], writes=[PSB[pb2]])
                V(lambda h, i=i, pa=pa: h.tensor_tensor(out=R[:, i, :], in0=PS[pa][:, 0:32], in1=base[:], op=ALU.add), [PSB[pa], baseb], [Rb])
                V(lambda h, pb2=pb2: h.tensor_tensor(out=base[:], in0=PS[pb2][:, 0:32], in1=base[:], op=ALU.add), [PSB[pb2], baseb], [baseb])
            V(lambda h: h.tensor_scalar(out=ci[:], in0=base[:], scalar1=127.0, scalar2=None, op0=ALU.add), [baseb], [pb_])
            V(lambda h: h.tensor_scalar(out=ci[:], in0=ci[:], scalar1=7, scalar2=7, op0=ALU.arith_shift_right, op1=ALU.logical_shift_left), [pb_], [pb_])
            V(lambda h: h.tensor_copy(out=padf[:], in_=ci[:]), [pb_], [pb_])
            V(lambda h: h.tensor_tensor_scan(out=pe[:], data0=one32[:], data1=padf[:], initial=0.0, op0=ALU.mult, op1=ALU.add), [pb_], [pb_])
            V(lambda h: h.tensor_tensor(out=pst[:], in0=pe[:], in1=padf[:], op=ALU.subtract), [pb_], [pb_])
            for i in range(16):
                V(lambda h, i=i: h.tensor_tensor(out=R[:, i, :], in0=R[:, i, :], in1=pst[:], op=ALU.add), [Rb, pb_], [Rb])
            for OH, SL, col in ((OH1, SL1, 0), (OH2, SL2, 1)):
                V(lambda h, OH=OH: h.tensor_tensor(out=A[:], in0=R[:], in1=OH[:], op=ALU.mult), [Rb, Gb, Ab], [Ab])
                V(lambda h: h.tensor_reduce(out=sf[:, 0:16], in_=A[:], axis=mybir.AxisListType.X, op=ALU.add), [Ab], [ebb])
                V(lambda h, SL=SL: h.tensor_copy(out=SL[:], in_=sf[:, 0:16]), [ebb], [SLb])
            for i in range(64):
                V(lambda h, i=i: h.tensor_scalar(out=junk32[:], in0=pe[:], scalar1=128.0 * i, scalar2=0.0, op0=ALU.is_le, op1=ALU.add, accum_out=eb[:, i:i + 1]),
                  [pb_], [ebb])
            V(lambda h: h.tensor_scalar(out=bG[:], in0=eb[:], scalar1=2048.0, scalar2=pc[:, 0:1], op0=ALU.mult, op1=ALU.add), [ebb], [ebb])
            V(lambda h: h.tensor_scalar(out=bD[:], in0=eb[:], scalar1=1024.0, scalar2=pc[:, 0:1], op0=ALU.mult, op1=ALU.add), [ebb], [ebb])
            for c in range(16):
                V(lambda h, c=c: h.tensor_scalar(out=igf[:, :, c], in0=bG[:], scalar1=128.0 * c, scalar2=None, op0=ALU.add), [ebb], [ebb])
            V(lambda h: h.tensor_copy(out=IG[:], in_=igf[:]), [ebb], [IXb])
            for c in range(8):
                V(lambda h, c=c: h.tensor_scalar(out=igf[:, :, c], in0=bD[:], scalar1=128.0 * c, scalar2=None, op0=ALU.add), [ebb, IXb], [ebb])
            V(lambda h: h.tensor_copy(out=ID[:], in_=igf[:, :, 0:8]), [ebb], [IXb])
        fw.barrier()

        with ExitStack() as st:
            hl = [SB(st, "s_hl%d" % i, [128, D], F32) for i in range(2)]; hlb = [Buf(), Buf()]
            h16_ = [SB(st, "s_h16%d" % i, [128, D], BF16) for i in range(2)]; h16b_ = [Buf(), Buf()]
            for t in range(16):
                k = t % 2
                fw.dma("sync", hl[k][:], h1_d[t * 128:(t + 1) * 128, :], writes=[hlb[k]])
                fw.op("scalar", lambda h: h.copy(out=h16_[k][:], in_=hl[k][:]), reads=[hlb[k]], writes=[h16b_[k]])
                fw.idma(xs_d[:, :], h16_[k][:, :], SL1[:, t:t + 1], False, 8191, reads=[h16b_[k], SLb], writes=[Buf()])
                fw.idma(xs_d[:, :], h16_[k][:, :], SL2[:, t:t + 1], False, 8191, reads=[h16b_[k], SLb], writes=[Buf()])
        fw.barrier()

        weg = w_eg.rearrange("e k n -> (e k) n"); weu = w_eu.rearrange("e k n -> (e k) n"); wed = w_ed.rearrange("e k n -> (e k) n")
        with ExitStack() as st:
            NW = 8
            xb = [SB(st, "b_xb%d" % i, [128, D], BF16) for i in range(2)]; xbb = [Buf(), Buf()]
            xT = [SB(st, "b_xT%d" % i, [128, 16, 128], BF16) for i in range(2)]; xTb = [Buf(), Buf()]
            wgt = [SB(st, "b_wg%d" % i, [128, 1024], BF16) for i in range(NW)]; wgtb = [Buf() for _ in range(NW)]
            wut = [SB(st, "b_wu%d" % i, [128, 1024], BF16) for i in range(NW)]; wutb = [Buf() for _ in range(NW)]
            wdt = [[SB(st, "b_wd%d_%d" % (i, c), [128, D], BF16) for c in range(8)] for i in range(2)]
            wdtb = [[Buf() for c in range(8)] for i in range(2)]
            sg = [SB(st, "b_sg%d" % i, [128, 512], F32) for i in range(2)]; sgb = [Buf(), Buf()]
            Hs = [SB(st, "b_H%d" % i, [128, 1024], BF16) for i in range(2)]; Hsb = [Buf(), Buf()]
            HT = [SB(st, "b_HT%d" % i, [128, 8, 128], BF16) for i in range(2)]; HTb = [Buf(), Buf()]
            Ys = [SB(st, "b_Y%d" % i, [128, D], F32) for i in range(2)]; Ysb = [Buf(), Buf()]
            iw = 0
            for i in range(64):
                k = i % 2
                fw.dma("sync", xb[k][:], xs_d[i * 128:(i + 1) * 128, :], writes=[xbb[k]])
                transpose_to(xb[k], xbb[k], xT[k], xTb[k], 16)
                for c in range(16):
                    j = iw % NW; iw += 1
                    fw.idma(wgt[j][:, :], weg[:, :], IG[:, i, c:c + 1], True, 32 * 2048 - 1, reads=[IXb], writes=[wgtb[j]])
                    fw.idma(wut[j][:, :], weu[:, :], IG[:, i, c:c + 1], True, 32 * 2048 - 1, reads=[IXb], writes=[wutb[j]])
                    for hf_ in range(2):
                        fw.op("tensor", lambda h, c=c, j=j, hf_=hf_: h.matmul(PS[hf_][:], xT[k][:, c, :], wgt[j][:, hf_ * 512:(hf_ + 1) * 512], start=(c == 0), stop=(c == 15)),
                              reads=[xTb[k], wgtb[j]], writes=[PSB[hf_]])
                    for hf_ in range(2):
                        fw.op("tensor", lambda h, c=c, j=j, hf_=hf_: h.matmul(PS[2 + hf_][:], xT[k][:, c, :], wut[j][:, hf_ * 512:(hf_ + 1) * 512], start=(c == 0), stop=(c == 15)),
                              reads=[xTb[k], wutb[j]], writes=[PSB[2 + hf_]])
                for c in range(8):
                    fw.idma(wdt[k][c][:, :], wed[:, :], ID[:, i, c:c + 1], True, 32 * 1024 - 1, reads=[IXb], writes=[wdtb[k][c]])
                for hf_ in range(2):
                    fw.op("scalar", lambda h, hf_=hf_: h.activation(out=sg[hf_][:], in_=PS[hf_][:], func=AF.Silu), reads=[PSB[hf_]], writes=[sgb[hf_]])
                    fw.op("vector", lambda h, hf_=hf_: h.tensor_tensor(out=Hs[k][:, hf_ * 512:(hf_ + 1) * 512], in0=PS[2 + hf_][:], in1=sg[hf_][:], op=ALU.mult),
                          reads=[PSB[2 + hf_], sgb[hf_]], writes=[Hsb[k]])
                transpose_to(Hs[k], Hsb[k], HT[k], HTb[k], 8)
                for hf_ in range(2):
                    for c in range(8):
                        for q in range(2):
                            fw.op("tensor", lambda h, c=c, q=q, hf_=hf_: h.matmul(PS[4 + q][:], HT[k][:, c, :], wdt[k][c][:, hf_ * 1024 + q * 512:hf_ * 1024 + (q + 1) * 512],
                                                                                  start=(c == 0), stop=(c == 7)), reads=[HTb[k], wdtb[k][c]], writes=[PSB[4 + q]])
                    fw.op("scalar", lambda h, hf_=hf_: h.copy(out=Ys[k][:, hf_ * 1024:hf_ * 1024 + 512], in_=PS[4][:]), reads=[PSB[4]], writes=[Ysb[k]])
                    fw.op("vector", lambda h, hf_=hf_: h.tensor_copy(out=Ys[k][:, hf_ * 1024 + 512:(hf_ + 1) * 1024], in_=PS[5][:]), reads=[PSB[5]], writes=[Ysb[k]])
                fw.dma("sync", ys_d[i * 128:(i + 1) * 128, :], Ys[k][:], reads=[Ysb[k]], writes=[Buf()])
        fw.barrier()

        with ExitStack() as st:
            g2, g2b = rep_load(st, "g2", ln2_g, D); b2, b2b = rep_load(st, "b2", ln2_b, D)
            gbb = Buf()
            fw.op("vector", lambda h: h.tensor_copy(out=g2[:, 0:1], in_=g2[:, 0:1]), reads=[g2b, b2b], writes=[gbb])
            ht = [SB(st, "f_h%d" % i, [128, D], F32) for i in range(2)]; htb = [Buf(), Buf()]
            y1 = [SB(st, "f_y1%d" % i, [128, D], F32) for i in range(2)]; y1b = [Buf(), Buf()]
            y2 = [SB(st, "f_y2%d" % i, [128, D], F32) for i in range(2)]; y2b = [Buf(), Buf()]
            st8 = SB(st, "st8c", [128, 24], F32); sb8 = Buf(); mv = SB(st, "mvc", [128, 2], F32); mvb = Buf(); rstd = SB(st, "rstdc", [128, 1], F32)
            for t in range(16):
                k = t % 2
                rows = slice(t * 128, (t + 1) * 128)
                fw.dma("sync", ht[k][:], h1_d[rows, :], writes=[htb[k]])
                fw.idma(y1[k][:, :], ys_d[:, :], SL1[:, t:t + 1], True, 8191, reads=[SLb], writes=[y1b[k]])
                fw.idma(y2[k][:, :], ys_d[:, :], SL2[:, t:t + 1], True, 8191, reads=[SLb], writes=[y2b[k]])
                fw.op("vector", lambda h: h.tensor_scalar(out=ht[k][:], in0=ht[k][:], scalar1=ALPHA, scalar2=None, op0=ALU.mult), reads=[htb[k]], writes=[htb[k]])
                fw.op("vector", lambda h, t=t: h.scalar_tensor_tensor(out=ht[k][:], in0=y1[k][:], scalar=G12[:, t, 0:1], in1=ht[k][:], op0=ALU.mult, op1=ALU.add),
                      reads=[y1b[k], htb[k], Gb], writes=[htb[k]])
                fw.op("vector", lambda h, t=t: h.scalar_tensor_tensor(out=ht[k][:], in0=y2[k][:], scalar=G12[:, t, 1:2], in1=ht[k][:], op0=ALU.mult, op1=ALU.add),
                      reads=[y2b[k], htb[k], Gb], writes=[htb[k]])
                layernorm(ht[k], htb[k], g2, b2, gbb, st8, sb8, mv, mvb, rstd)
                fw.dma("sync", out[rows, :], ht[k][:], reads=[htb[k]], writes=[Buf()])
        persist2.close()
        fw.barrier()
    return nc


_CONST = {}


def _consts():
    if _CONST:
        return _CONST
    f32 = np.float32
    half = 64
    inv_freq = (10000.0 ** (-np.arange(0, half, 2, dtype=np.float64) / half)).astype(f32)
    row_idx = (np.arange(S) // 64).astype(f32)
    col_idx = (np.arange(S) % 64).astype(f32)
    ang_r = (row_idx[:, None] * inv_freq[None, :]).astype(f32)
    ang_c = (col_idx[:, None] * inv_freq[None, :]).astype(f32)
    cr, sr, cc, sc = np.cos(ang_r), np.sin(ang_r), np.cos(ang_c), np.sin(ang_c)
    _CONST["ropeC"] = np.concatenate([cr, cr, cc, cc], axis=1).astype(f32)
    _CONST["ropeS"] = np.concatenate([-sr, sr, -sc, sc], axis=1).astype(f32)
    n = np.arange(128, dtype=np.float64)
    a128 = 2 * np.pi * np.outer(n, n) / 128.0
    _CONST["Fc"] = np.cos(a128).astype(f32); _CONST["Fs"] = np.sin(a128).astype(f32)
    a16 = 2 * np.pi * np.outer(n, n) / float(NFFT)
    _CONST["C16"] = np.cos(a16).astype(f32); _CONST["S16"] = np.sin(a16).astype(f32)
    t = np.linspace(0.0, 1.0, S, dtype=f32)
    w = (2.0 * math.pi * np.arange(S, dtype=f32) / S).astype(f32)[:, None]
    bands = np.linspace(1e-4, 15, 16, dtype=f32)[None, :]
    feats = np.concatenate([t[:, None], np.cos(bands * w), -np.sin(bands * w)], axis=-1).astype(f32)
    _CONST["featsT"] = np.ascontiguousarray(feats.T)
    _CONST["negt"] = np.ascontiguousarray((-t).reshape(64, 128).T)
    min_decay = math.log(1e-2) / 1.5
    max_decay = math.log(1e-2) / 0.3
    _CONST["absdelta"] = np.abs(np.linspace(min_decay, max_decay, C, dtype=f32)).reshape(1, C).astype(f32)
    m0 = np.ones((128, 1), f32); m0[0, 0] = 0.0
    _CONST["m0"] = m0
    return _CONST


def _in_maps(inp):
    cs = _consts()
    f32 = np.float32
    x = np.asarray(inp["x"], f32)
    common = {
        "ln_in_g": inp["ln_in_g"].reshape(1, D), "ln_in_b": inp["ln_in_b"].reshape(1, D),
        "w_in": inp["w_in"][0], "b_gate": inp["b_gate"][0].reshape(1, 4096),
        "q_norm_g": inp["q_norm_g"][0].reshape(1, 128), "k_norm_g": inp["k_norm_g"][0].reshape(1, 128),
        "hy_conv_w": inp["hy_conv_w"][0], "hy_conv_b": inp["hy_conv_b"][0].reshape(1, 3072),
        "filt_w1": inp["filt_w1"][0], "filt_b1": inp["filt_b1"][0].reshape(64, 1), "filt_f1": inp["filt_f1"][0].reshape(64, 1),
        "filt_w2": inp["filt_w2"][0], "filt_b2": inp["filt_b2"][0].reshape(64, 1), "filt_f2": inp["filt_f2"][0].reshape(64, 1),
        "filt_w3": inp["filt_w3"][0], "hy_bias_d": inp["hy_bias_d"][0].reshape(1, C),
        "w_attn_o": inp["w_attn_o"][0], "w_hy_o": inp["w_hy_o"][0], "w_out": inp["w_out"][0],
        "ln1_g": inp["ln1_g"][0].reshape(1, D), "ln1_b": inp["ln1_b"][0].reshape(1, D),
        "w_route": np.concatenate([inp["w_route_grp"][0], inp["w_route_exp"][0]], axis=1),
        "b_route": np.concatenate([inp["b_route_grp"][0], inp["b_route_exp"][0]], axis=0).reshape(1, 36),
        "w_eg": inp["w_exp_gate"][0], "w_eu": inp["w_exp_up"][0], "w_ed": inp["w_exp_down"][0],
        "ln2_g": inp["ln2_g"][0].reshape(1, D), "ln2_b": inp["ln2_b"][0].reshape(1, D),
        "ropeC_k": cs["ropeC"], "ropeS_k": cs["ropeS"],
        "cFc": cs["Fc"], "cFs": cs["Fs"], "cFsn": -cs["Fs"], "cFcn": -cs["Fc"],
        "cC16": cs["C16"], "cS16": cs["S16"], "cS16n": -cs["S16"],
        "pcol": np.arange(128, dtype=np.float32).reshape(128, 1), "featsT": cs["featsT"], "negt": cs["negt"], "absdelta": cs["absdelta"], "m0": cs["m0"],
    }
    common = {k: np.ascontiguousarray(np.asarray(v, f32)) for k, v in common.items()}
    maps = []
    for c in range(8):
        b, j = c // 4, c % 4
        q0 = j * T
        xo = np.zeros((NOWN, D), f32); mo = np.zeros((NOWN, 1), f32)
        lo = max(q0 - 1, 0); hi = min(q0 + T + 1, S)
        r0 = lo - (q0 - 1)
        xo[r0:r0 + (hi - lo)] = x[b, lo:hi]
        mo[r0:r0 + (hi - lo)] = 1.0
        m = dict(common)
        m["x_seq"] = np.ascontiguousarray(x[b]); m["x_own"] = xo; m["m_own"] = np.ascontiguousarray(mo.reshape(17, 128).T)
        m["ropeC_q"] = np.ascontiguousarray(cs["ropeC"][q0:q0 + T]); m["ropeS_q"] = np.ascontiguousarray(cs["ropeS"][q0:q0 + T])
        n2 = np.arange(16 * j, 16 * j + 16)
        m["ci2re"] = np.ascontiguousarray(cs["Fc"][:, n2] / float(NFFT)).astype(f32)
        m["ci2im"] = np.ascontiguousarray(-cs["Fs"][:, n2] / float(NFFT)).astype(f32)
        maps.append(m)
    return maps


def kernel(**inputs):
    inp = {k: np.asarray(v) for k, v in inputs.items()}
    nc = build_nc()
    maps = _in_maps(inp)
    res = run_bass_kernel_spmd(nc, maps, core_ids=list(range(8)))
    outp = np.zeros((2, S, D), np.float32)
    for c in range(8):
        b, j = c // 4, c % 4
        outp[b, j * T:(j + 1) * T] = res.results[c]["out"]
    return outp
```

```python
import math
from contextlib import ExitStack
import numpy as np
import concourse.bass as bass
import concourse.mybir as mybir
from concourse.bass_utils import run_bass_kernel_spmd

F32 = mybir.dt.float32
BF16 = mybir.dt.bfloat16
ALU = mybir.AluOpType
AF = mybir.ActivationFunctionType

D = 2048
S = 8192
T = 2048
NOWN = 17 * 128
C = 1024
INW = 8704
ALPHA = 2.0 ** 0.25
LN_EPS = 1e-5
QK_EPS = 1e-6
FILTER_EPS = 1e-6
NFFT = 16384
PI = math.pi


class Buf:
    __slots__ = ("w", "r")

    def __init__(self):
        self.w = None
        self.r = []


class FW:
    ENGS = ("tensor", "vector", "scalar", "gpsimd", "sync")

    def __init__(self, nc, stack, ndma=20):
        self.nc = nc
        self.cnt = {e: 0 for e in self.ENGS}
        self.seen = {e: {} for e in self.ENGS}
        self.sems = {}
        self.ndma = ndma
        self.dma_next = {e: 0 for e in self.ENGS}
        self.dma_tot = {}
        for e in self.ENGS:
            self.sems[e] = stack.enter_context(nc.semaphore("s_" + e))
        for q in ("sync", "gpsimd", "scalar"):
            for i in range(ndma):
                k = ("d", q, i)
                self.sems[k] = stack.enter_context(nc.semaphore("d_%s_%d" % (q, i)))
                self.dma_tot[k] = 0

    def _waits(self, eng, reads, writes):
        deps = {}

        def add(t):
            if t is not None and deps.get(t[0], 0) < t[1]:
                deps[t[0]] = t[1]
        for b in reads:
            add(b.w)
        for b in writes:
            add(b.w)
            for t in b.r:
                add(t)
        out = []
        seen = self.seen[eng]
        for key, val in deps.items():
            if key == eng and eng == "tensor":
                continue
            if seen.get(key, 0) >= val:
                continue
            seen[key] = val
            out.append((key, val))
        return out

    def _mark(self, tag, reads, writes):
        for b in writes:
            b.w = tag
            b.r = []
        for b in reads:
            b.r = [t for t in b.r if t[0] != tag[0]] + [tag]

    def op(self, eng, fn, reads=(), writes=()):
        waits = self._waits(eng, reads, writes)
        self.cnt[eng] += 1
        tag = (eng, self.cnt[eng])
        h = getattr(self.nc, eng)
        for key, val in waits:
            h.wait_ge(self.sems[key], val)
        fn(h).then_inc(self.sems[eng], 1)
        self._mark(tag, reads, writes)

    def idma(self, out, in_, idx, gather, bound, reads=(), writes=()):
        if not hasattr(self, "bregs"):
            self.bregs = {}
        if bound not in self.bregs:
            r = self.nc.gpsimd.alloc_register("bnd%d" % bound)
            self.nc.gpsimd.reg_mov(r, bound)
            self.bregs[bound] = r
        bound = self.bregs[bound]
        if gather:
            fn = lambda h: h.indirect_dma_start(out=out, out_offset=None, in_=in_, in_offset=bass.IndirectOffsetOnAxis(ap=idx, axis=0),
                                                bounds_check=bound, oob_is_err=False)
        else:
            fn = lambda h: h.indirect_dma_start(out=out, out_offset=bass.IndirectOffsetOnAxis(ap=idx, axis=0), in_=in_, in_offset=None,
                                                bounds_check=bound, oob_is_err=False)
        self.dma("gpsimd", None, None, reads=reads, writes=writes, fn=fn)

    def dma(self, q, out, in_, reads=(), writes=(), slow=False, fn=None):
        i = self.dma_next[q]
        self.dma_next[q] = (i + 1) % self.ndma
        key = ("d", q, i)
        waits = self._waits(q, reads, writes)
        prev = self.dma_tot[key]
        if prev > 0 and self.seen[q].get(key, 0) < prev:
            self.seen[q][key] = prev
            waits.append((key, prev))
        self.dma_tot[key] = prev + 16
        tag = (key, prev + 16)
        h = getattr(self.nc, q)
        for k2, val in waits:
            h.wait_ge(self.sems[k2], val)
        if fn is not None:
            inst = fn(h)
        elif slow:
            inst = h.dma_start(out=out, in_=in_, allow_slow_non_contiguous=True)
        else:
            inst = h.dma_start(out=out, in_=in_)
        inst.then_inc(self.sems[key], 16)
        self._mark(tag, reads, writes)

    def barrier(self):
        for e in self.ENGS:
            h = getattr(self.nc, e)
            seen = self.seen[e]
            for o in self.ENGS:
                if o == e or self.cnt[o] == 0:
                    continue
                if seen.get(o, 0) < self.cnt[o]:
                    seen[o] = self.cnt[o]
                    h.wait_ge(self.sems[o], self.cnt[o])
            for k, tot in self.dma_tot.items():
                if tot > 0 and seen.get(k, 0) < tot:
                    seen[k] = tot
                    h.wait_ge(self.sems[k], tot)


def build_nc(dbg=None, stop=None):
    nc = bass.Bass("TRN2", target_bir_lowering=False)
    ins = {}

    def IN(name, shape, dt=F32):
        ins[name] = nc.dram_tensor(name, list(shape), dt, kind="ExternalInput").ap()
        return ins[name]

    x_seq = IN("x_seq", [S, D]); x_own = IN("x_own", [NOWN, D]); m_own = IN("m_own", [128, 17])
    ln_in_g = IN("ln_in_g", [1, D]); ln_in_b = IN("ln_in_b", [1, D])
    w_in = IN("w_in", [D, INW]); b_gate = IN("b_gate", [1, 4096])
    q_norm_g = IN("q_norm_g", [1, 128]); k_norm_g = IN("k_norm_g", [1, 128])
    hy_conv_w = IN("hy_conv_w", [3, 3072]); hy_conv_b = IN("hy_conv_b", [1, 3072])
    filt_w1 = IN("filt_w1", [33, 64]); filt_b1 = IN("filt_b1", [64, 1]); filt_f1 = IN("filt_f1", [64, 1])
    filt_w2 = IN("filt_w2", [64, 64]); filt_b2 = IN("filt_b2", [64, 1]); filt_f2 = IN("filt_f2", [64, 1])
    filt_w3 = IN("filt_w3", [64, 2048]); hy_bias_d = IN("hy_bias_d", [1, C])
    w_attn_o = IN("w_attn_o", [1024, D]); w_hy_o = IN("w_hy_o", [C, D]); w_out = IN("w_out", [D, D])
    ln1_g = IN("ln1_g", [1, D]); ln1_b = IN("ln1_b", [1, D])
    w_route = IN("w_route", [D, 36]); b_route = IN("b_route", [1, 36])
    w_eg = IN("w_eg", [32, D, 1024]); w_eu = IN("w_eu", [32, D, 1024]); w_ed = IN("w_ed", [32, 1024, D])
    ln2_g = IN("ln2_g", [1, D]); ln2_b = IN("ln2_b", [1, D])
    ropeC_k = IN("ropeC_k", [S, 128]); ropeS_k = IN("ropeS_k", [S, 128])
    ropeC_q = IN("ropeC_q", [T, 128]); ropeS_q = IN("ropeS_q", [T, 128])
    cFc = IN("cFc", [128, 128]); cFs = IN("cFs", [128, 128]); cFsn = IN("cFsn", [128, 128]); cFcn = IN("cFcn", [128, 128])
    cC16 = IN("cC16", [128, 128]); cS16 = IN("cS16", [128, 128]); cS16n = IN("cS16n", [128, 128])
    cE = IN("cE", [128, 128, 7, 128], BF16); ci2re = IN("ci2re", [128, 16]); ci2im = IN("ci2im", [128, 16])
    pcol = IN("pcol", [128, 1]); featsT = IN("featsT", [33, S]); negt = IN("negt", [128, 64]); absdelta = IN("absdelta", [1, C]); m0 = IN("m0", [128, 1])

    out = nc.dram_tensor("out", [T, D], F32, kind="ExternalOutput").ap()

    def DR(name, shape, dt):
        kind = "ExternalOutput" if (dbg and name in dbg) else "Internal"
        return nc.dram_tensor(name, list(shape), dt, kind=kind).ap()

    hT_d = DR("hT_d", [D, S + 2], BF16); hTo_d = DR("hTo_d", [D, NOWN], BF16); ho_d = DR("ho_d", [NOWN, D], F32)
    kT_d = DR("kT_d", [128, 2, S], BF16); v_d = DR("v_d", [S, 256], BF16); qT_d = DR("qT_d", [128, 8, T], BF16)
    z_d = DR("z_d", [S, C], BF16); x0_d = DR("x0_d", [T, C], BF16); g_d = DR("g_d", [T, 4096], BF16)
    at_d = DR("at_d", [128, 8, T], BF16)
    hf_d = DR("hf_d", [S, C], BF16); hg_d = DR("hg_d", [S, C], BF16)
    a_d = [[DR("a_d%d%d" % (i, r), [128, 128, C], BF16) for r in range(2)] for i in range(3)]
    hh_d = [DR("hh_d%d" % r, [128, 128, C], BF16) for r in range(2)]
    b_d = [DR("b_d%d" % r, [128, 128, C], BF16) for r in range(2)]
    y_d = DR("y_d", [T, C], F32)
    xs_d = DR("xs_d", [8192, D], BF16); ys_d = DR("ys_d", [8192, D], F32); preT_d = DR("preT_d", [D, T], BF16); h1_d = DR("h1_d", [T, D], F32); h1T_d = DR("h1T_d", [D, T], BF16)

    with ExitStack() as top:
        fw = FW(nc, top)
        Dm = {}

        def dbuf(ap_name):
            return Buf()

        uid = [0]

        def SB(st, name, shape, dt):
            uid[0] += 1
            return st.enter_context(nc.sbuf_tensor("%s_u%d" % (name, uid[0]), list(shape), dt))

        PS = [top.enter_context(nc.psum_tensor("ps%d" % i, [128, 512], F32)) for i in range(6)]
        PSB = [Buf() for _ in range(6)]
        PT = [top.enter_context(nc.psum_tensor("pt%d" % i, [128, 1024], BF16)) for i in range(2)]
        PTB = [Buf() for _ in range(2)]

        ident = SB(top, "ident", [128, 128], BF16); identf = SB(top, "identf", [128, 128], F32)
        ones16 = SB(top, "ones16", [128, 128], BF16); onesf = SB(top, "onesf", [128, 128], F32)
        Bc = Buf()
        fw.op("gpsimd", lambda h: h.memset(identf[:], 1.0), writes=[Bc])
        fw.op("gpsimd", lambda h: h.affine_select(out=identf[:], in_=identf[:], pattern=[[-1, 128]],
                                                   compare_op=ALU.is_equal, fill=0.0, base=0, channel_multiplier=1),
              reads=[Bc], writes=[Bc])
        fw.op("vector", lambda h: h.tensor_copy(out=ident[:], in_=identf[:]), reads=[Bc], writes=[Bc])
        fw.op("vector", lambda h: h.memset(ones16[:], 1.0), writes=[Bc])
        fw.op("vector", lambda h: h.memset(onesf[:], 1.0), writes=[Bc])

        def rep_load(st, name, src_row, n, dt=F32, q="sync"):
            t = SB(st, name, [128, n], dt)
            b = Buf()
            fw.dma(q, t[:], src_row.to_broadcast([128, n]), writes=[b])
            return t, b

        def layernorm(xt, xb, grep, brep, gb, st8, sb8, mv, mvb, rstd, eps=LN_EPS):
            for i in range(4):
                fw.op("vector", lambda h, i=i: h.bn_stats(out=st8[:, i * 6:(i + 1) * 6], in_=xt[:, i * 512:(i + 1) * 512]),
                      reads=[xb], writes=[sb8])
            fw.op("vector", lambda h: h.bn_aggr(out=mv[:], in_=st8[:]), reads=[sb8], writes=[mvb])
            fw.op("scalar", lambda h: h.activation(out=rstd[:], in_=mv[:, 1:2], func=AF.Sqrt, bias=eps, scale=1.0),
                  reads=[mvb], writes=[sb8])
            fw.op("vector", lambda h: h.reciprocal(out=rstd[:], in_=rstd[:]), reads=[sb8], writes=[sb8])
            fw.op("vector", lambda h: h.tensor_scalar(out=xt[:], in0=xt[:], scalar1=mv[:, 0:1], scalar2=rstd[:, 0:1],
                                                       op0=ALU.subtract, op1=ALU.mult), reads=[xb, mvb, sb8], writes=[xb])
            fw.op("gpsimd", lambda h: h.tensor_tensor(out=xt[:], in0=xt[:], in1=grep[:], op=ALU.mult), reads=[xb, gb], writes=[xb])
            fw.op("vector", lambda h: h.tensor_tensor(out=xt[:], in0=xt[:], in1=brep[:], op=ALU.add), reads=[xb, gb], writes=[xb])

        def transpose_to(src16, srcb, dst, dstb, nchunk, col0=0, ncols=128):
            for g4 in range(0, nchunk, 8):
                n = min(8, nchunk - g4)
                pi = (g4 // 8) % 2
                for c in range(n):
                    fw.op("tensor", lambda h, c=c: h.transpose(PT[pi][:, c * 128:c * 128 + ncols],
                                                               src16[0:ncols, (g4 + c) * 128:(g4 + c + 1) * 128], ident[0:ncols, 0:ncols]),
                          reads=[srcb, Bc], writes=[PTB[pi]])
                eng = "vector" if pi == 0 else "scalar"
                if eng == "vector":
                    fw.op("vector", lambda h: h.tensor_copy(
                        out=dst[:, g4:g4 + n, col0:col0 + ncols],
                        in_=PT[pi][:, 0:n * 128].rearrange("p (c t) -> p c t", t=128)[:, :, 0:ncols]),
                        reads=[PTB[pi]], writes=[dstb])
                else:
                    fw.op("scalar", lambda h: h.copy(
                        out=dst[:, g4:g4 + n, col0:col0 + ncols],
                        in_=PT[pi][:, 0:n * 128].rearrange("p (c t) -> p c t", t=128)[:, :, 0:ncols]),
                        reads=[PTB[pi]], writes=[dstb])

        with ExitStack() as st:
            grep, gb = rep_load(st, "lng", ln_in_g, D)
            brep, bb_ = rep_load(st, "lnb", ln_in_b, D)
            gbb = Buf()
            fw.op("vector", lambda h: h.tensor_copy(out=grep[:, 0:1], in_=grep[:, 0:1]), reads=[gb, bb_], writes=[gbb])
            xts = [SB(st, "xt%d" % i, [128, D], F32) for i in range(2)]; xbs = [Buf(), Buf()]
            h16 = [SB(st, "h16_%d" % i, [128, D], BF16) for i in range(2)]; h16b = [Buf(), Buf()]
            hTs = [SB(st, "hTs%d" % i, [128, 16, 512], BF16) for i in range(2)]; hTb = [Buf(), Buf()]
            st8 = SB(st, "st8", [128, 24], F32); sb8 = Buf(); mv = SB(st, "mv", [128, 2], F32); mvb = Buf()
            rstd = SB(st, "rstd", [128, 1], F32)
            mk = SB(st, "mk", [128, 17], F32); mkb = Buf()
            zt = SB(st, "zt", [128, 16, 1], BF16); ztb = Buf()
            fw.op("vector", lambda h: h.memset(zt[:], 0.0), writes=[ztb])
            hTv = hT_d.rearrange("(c p) s -> p c s", p=128)
            fw.dma("gpsimd", hTv[:, :, 0:1], zt[:], reads=[ztb], writes=[dbuf("hT_d")], slow=True)
            fw.dma("gpsimd", hTv[:, :, S + 1:S + 2], zt[:], reads=[ztb], writes=[dbuf("hT_d")], slow=True)
            fw.dma("sync", mk[:], m_own, writes=[mkb])
            hTov = hTo_d.rearrange("(c p) s -> p c s", p=128)
            it = 0
            for grp in range(16 + 5):
                own = grp >= 16
                ntile = 4 if not own else (4 if grp < 20 else 1)
                hb = grp % 2
                for ti in range(ntile):
                    tile = (grp * 4 + ti) if not own else ((grp - 16) * 4 + ti)
                    k = it % 2; it += 1
                    src = x_seq if not own else x_own
                    fw.dma("sync", xts[k][:], src[tile * 128:(tile + 1) * 128, :], writes=[xbs[k]])
                    layernorm(xts[k], xbs[k], grep, brep, gbb, st8, sb8, mv, mvb, rstd)
                    if own:
                        fw.op("scalar", lambda h, k=k, tile=tile: h.activation(out=xts[k][:], in_=xts[k][:], func=AF.Identity,
                                                                                scale=mk[:, tile:tile + 1]),
                              reads=[xbs[k], mkb], writes=[xbs[k]])
                        fw.dma("gpsimd", ho_d[tile * 128:(tile + 1) * 128, :], xts[k][:], reads=[xbs[k]], writes=[dbuf("ho_d")])
                    fw.op("scalar", lambda h, k=k: h.copy(out=h16[k][:], in_=xts[k][:]), reads=[xbs[k]], writes=[h16b[k]])
                    transpose_to(h16[k], h16b[k], hTs[hb], hTb[hb], 16, col0=ti * 128)
                if not own:
                    fw.dma("gpsimd", hTv[:, :, 1 + grp * 512:1 + grp * 512 + 512], hTs[hb][:], reads=[hTb[hb]], writes=[dbuf("hT_d")])
                else:
                    g0 = (grp - 16) * 512
                    fw.dma("gpsimd", hTov[:, :, g0:g0 + ntile * 128], hTs[hb][:, :, 0:ntile * 128], reads=[hTb[hb]], writes=[dbuf("hTo_d")])
        fw.barrier()

        w_in_v = w_in.rearrange("(c p) n -> p c n", p=128)

        def proj_pass(st, src_v, srcname, ntok_tiles, blocks, epilogue):
            nb = len(blocks)
            wts = []
            wb = Buf()
            for bi, (col0, cidx, bias) in enumerate(blocks):
                nsh = 3 if cidx is not None else 1
                for sh in range(nsh):
                    wt = SB(st, "w_%d_%d" % (bi, sh), [128, 16, 512], BF16)
                    fw.dma("gpsimd", wt[:], w_in_v[:, :, col0:col0 + 512], writes=[wb])
                    if cidx is not None:
                        cw, cwb = rep_load(st, "cw_%d_%d" % (bi, sh), hy_conv_w[sh:sh + 1, cidx:cidx + 512], 512)
                        for c in range(16):
                            fw.op("gpsimd", lambda h, c=c, wt=wt, cw=cw: h.tensor_tensor(out=wt[:, c, :], in0=wt[:, c, :], in1=cw[:], op=ALU.mult),
                                  reads=[wb, cwb], writes=[wb])
                    wts.append((bi, sh if cidx is not None else 1, wt))
                if bias is not None:
                    b16 = SB(st, "b16_%d" % bi, [1, 512], BF16)
                    fw.dma("gpsimd", b16[:], bias, writes=[wb])
                    wts.append((bi, -1, b16))
            hw = [SB(st, "hw%d" % i, [128, 16, 514], BF16) for i in range(2)]; hwb = [Buf(), Buf()]
            ngrp = (ntok_tiles + 3) // 4
            for g in range(ngrp):
                k = g % 2
                nt = min(4, ntok_tiles - g * 4)
                wdt = nt * 128 + 2
                fw.dma("sync", hw[k][:, :, 0:wdt], src_v[:, :, g * 512:g * 512 + wdt], reads=[dbuf(srcname)], writes=[hwb[k]])
                for m in range(nt):
                    tile = g * 4 + m
                    pidx = [(tile * nb + bi) % 6 for bi in range(nb)]
                    for bi in range(nb):
                        mine = [(sh, wt) for (b2, sh, wt) in wts if b2 == bi]
                        nsteps = sum(16 if sh >= 0 else 1 for sh, _ in mine)
                        step = 0
                        for sh, wt in mine:
                            if sh < 0:
                                fw.op("tensor", lambda h, wt=wt, p=pidx[bi], s0=(step == 0), s1=(step == nsteps - 1):
                                      h.matmul(PS[p][:], ones16[0:1, :], wt[:], start=s0, stop=s1), reads=[wb, Bc], writes=[PSB[pidx[bi]]])
                                step += 1
                                continue
                            for c in range(16):
                                fw.op("tensor", lambda h, wt=wt, c=c, p=pidx[bi], off=m * 128 + sh, s0=(step == 0), s1=(step == nsteps - 1), k=k:
                                      h.matmul(PS[p][:], hw[k][:, c, off:off + 128], wt[:, c, :], start=s0, stop=s1),
                                      reads=[wb, hwb[k]], writes=[PSB[pidx[bi]]])
                                step += 1
                    epilogue(tile, pidx)

        def qk_epilogue_factory(st, gsrc, ropeC, ropeS, nheads_list, dstT, dstname, pref):
            grep_, gb_ = rep_load(st, pref + "g", gsrc, 128)
            ss = SB(st, pref + "ss", [128, 4], F32); ssb = Buf()
            junk = SB(st, pref + "junk", [128, 128], F32)
            xn = SB(st, pref + "xn", [128, 128], F32); xnb = Buf()
            t1 = SB(st, pref + "t1", [128, 128], F32); t2 = SB(st, pref + "t2", [128, 128], F32); tb_ = Buf()
            x16 = [SB(st, pref + "x16%d" % i, [128, 128], BF16) for i in range(2)]; x16b = [Buf(), Buf()]
            rc = [SB(st, pref + "rc%d" % i, [128, 128], F32) for i in range(2)]
            rs = [SB(st, pref + "rs%d" % i, [128, 128], F32) for i in range(2)]; rb = [Buf(), Buf()]
            stg = SB(st, pref + "stg", [128, 8, 128], BF16); stgb = Buf()
            cnt = [0]

            def fn(tile, p, heads):
                k = tile % 2
                fw.dma("sync", rc[k][:], ropeC[tile * 128:(tile + 1) * 128, :], writes=[rb[k]])
                fw.dma("sync", rs[k][:], ropeS[tile * 128:(tile + 1) * 128, :], writes=[rb[k]])
                for hi, (co, dh) in enumerate(heads):
                    fw.op("scalar", lambda h, hi=hi, co=co: h.activation(out=junk[:], in_=PS[p][:, co:co + 128], func=AF.Square,
                                                                         accum_out=ss[:, hi:hi + 1]), reads=[PSB[p]], writes=[ssb])
                nh = len(heads)
                fw.op("scalar", lambda h: h.activation(out=ss[:, 0:nh], in_=ss[:, 0:nh], func=AF.Sqrt, bias=QK_EPS, scale=1.0 / 128),
                      reads=[ssb], writes=[ssb])
                fw.op("vector", lambda h: h.reciprocal(out=ss[:, 0:nh], in_=ss[:, 0:nh]), reads=[ssb], writes=[ssb])
                for hi, (co, dh) in enumerate(heads):
                    j = cnt[0] % 2; cnt[0] += 1
                    fw.op("vector", lambda h, hi=hi, co=co: h.scalar_tensor_tensor(out=xn[:], in0=PS[p][:, co:co + 128], scalar=ss[:, hi:hi + 1],
                                                                                  in1=grep_[:], op0=ALU.mult, op1=ALU.mult),
                          reads=[PSB[p], ssb, gb_], writes=[xnb])
                    fw.op("vector", lambda h: h.tensor_tensor(out=t1[:], in0=xn[:], in1=rc[k][:], op=ALU.mult), reads=[xnb, rb[k]], writes=[tb_])
                    xv = xn[:].rearrange("p (a h d) -> p a h d", a=2, h=2)
                    sv = rs[k][:].rearrange("p (a h d) -> p a h d", a=2, h=2)
                    tv = t2[:].rearrange("p (a h d) -> p a h d", a=2, h=2)
                    fw.op("gpsimd", lambda h: h.tensor_tensor(out=tv[:, :, 0, :], in0=xv[:, :, 1, :], in1=sv[:, :, 0, :], op=ALU.mult),
                          reads=[xnb, rb[k]], writes=[tb_])
                    fw.op("gpsimd", lambda h: h.tensor_tensor(out=tv[:, :, 1, :], in0=xv[:, :, 0, :], in1=sv[:, :, 1, :], op=ALU.mult),
                          reads=[xnb, rb[k]], writes=[tb_])
                    fw.op("vector", lambda h, j=j: h.tensor_tensor(out=x16[j][:], in0=t1[:], in1=t2[:], op=ALU.add), reads=[tb_], writes=[x16b[j]])
                    fw.op("tensor", lambda h, j=j, hi=hi: h.transpose(PT[0][:, hi * 128:(hi + 1) * 128], x16[j][:], ident[:]),
                          reads=[x16b[j], Bc], writes=[PTB[0]])
                fw.op("scalar", lambda h: h.copy(out=stg[:, 0:nh, :], in_=PT[0][:, 0:nh * 128].rearrange("p (c t) -> p c t", t=128)),
                      reads=[PTB[0]], writes=[stgb])
                for hi, (co, dh) in enumerate(heads):
                    fw.dma("gpsimd", dstT[:, dh, tile * 128:(tile + 1) * 128], stg[:, hi, :], reads=[stgb], writes=[dbuf(dstname)])
            return fn

        hTv = hT_d.rearrange("(c p) s -> p c s", p=128)
        hTov = hTo_d.rearrange("(c p) s -> p c s", p=128)

        if stop == "S0":
            return nc
        with ExitStack() as st:
            kfn = qk_epilogue_factory(st, k_norm_g, ropeC_k, ropeS_k, 2, kT_d, "kT_d", "k")
            v16 = [SB(st, "v16_%d" % i, [128, 256], BF16) for i in range(2)]; v16b = [Buf(), Buf()]

            def kv_ep(tile, pidx):
                p = pidx[0]
                kfn(tile, p, [(0, 0), (128, 1)])
                k = tile % 2
                fw.op("scalar", lambda h: h.copy(out=v16[k][:], in_=PS[p][:, 256:512]), reads=[PSB[p]], writes=[v16b[k]])
                fw.dma("gpsimd", v_d[tile * 128:(tile + 1) * 128, :], v16[k][:], reads=[v16b[k]], writes=[dbuf("v_d")])
            proj_pass(st, hTv, "hT_d", 64, [(1024, None, None)], kv_ep)
        fw.barrier()

        if stop == "KV":
            return nc
        for qb in range(2):
            with ExitStack() as st:
                qfn = qk_epilogue_factory(st, q_norm_g, ropeC_q, ropeS_q, 4, qT_d, "qT_d", "q")

                def q_ep(tile, pidx, qb=qb):
                    qfn(tile, pidx[0], [(i * 128, qb * 4 + i) for i in range(4)])
                proj_pass(st, hTov, "hTo_d", 16, [(qb * 512, None, None)], q_ep)
            fw.barrier()

        if stop == "Q":
            return nc
        for cb in range(2):
            with ExitStack() as st:
                x1s = [SB(st, "x1s%d" % i, [128, 512], F32) for i in range(2)]; x1b = [Buf(), Buf()]
                z16 = [SB(st, "z16_%d" % i, [128, 512], BF16) for i in range(2)]; z16b = [Buf(), Buf()]

                def z_ep(tile, pidx, cb=cb):
                    k = tile % 2
                    fw.op("scalar", lambda h: h.copy(out=x1s[k][:], in_=PS[pidx[0]][:]), reads=[PSB[pidx[0]]], writes=[x1b[k]])
                    fw.op("vector", lambda h: h.tensor_tensor(out=z16[k][:], in0=PS[pidx[1]][:], in1=x1s[k][:], op=ALU.mult),
                          reads=[PSB[pidx[1]], x1b[k]], writes=[z16b[k]])
                    fw.dma("gpsimd", z_d[tile * 128:(tile + 1) * 128, cb * 512:(cb + 1) * 512], z16[k][:], reads=[z16b[k]], writes=[dbuf("z_d")])
                c1 = 1024 + cb * 512; c2 = 2048 + cb * 512
                proj_pass(st, hTv, "hT_d", 64,
                          [(1536 + c1, c1, hy_conv_b[0:1, c1:c1 + 512]), (1536 + c2, c2, hy_conv_b[0:1, c2:c2 + 512])], z_ep)
            fw.barrier()

        if stop == "Z":
            return nc
        for cb in range(2):
            with ExitStack() as st:
                o16 = [SB(st, "o16_%d" % i, [128, 512], BF16) for i in range(2)]; o16b = [Buf(), Buf()]

                def x0_ep(tile, pidx, cb=cb):
                    k = tile % 2
                    fw.op("scalar", lambda h: h.copy(out=o16[k][:], in_=PS[pidx[0]][:]), reads=[PSB[pidx[0]]], writes=[o16b[k]])
                    fw.dma("gpsimd", x0_d[tile * 128:(tile + 1) * 128, cb * 512:(cb + 1) * 512], o16[k][:], reads=[o16b[k]], writes=[dbuf("x0_d")])
                c0 = cb * 512
                proj_pass(st, hTov, "hTo_d", 16, [(1536 + c0, c0, hy_conv_b[0:1, c0:c0 + 512])], x0_ep)
            fw.barrier()

        for cb in range(4):
            with ExitStack() as st:
                o16 = [SB(st, "g16_%d" % i, [128, 1024], BF16) for i in range(2)]; o16b = [Buf(), Buf()]

                def g_ep(tile, pidx, cb=cb):
                    k = tile % 2
                    for bi in range(2):
                        fw.op("scalar", lambda h, bi=bi: h.activation(out=o16[k][:, bi * 512:(bi + 1) * 512], in_=PS[pidx[bi]][:], func=AF.Sigmoid),
                              reads=[PSB[pidx[bi]]], writes=[o16b[k]])
                    fw.dma("gpsimd", g_d[tile * 128:(tile + 1) * 128, cb * 1024:(cb + 1) * 1024], o16[k][:], reads=[o16b[k]], writes=[dbuf("g_d")])
                c0 = cb * 1024
                proj_pass(st, hTov, "hTo_d", 16,
                          [(4608 + c0, None, b_gate[0:1, c0:c0 + 512]), (4608 + c0 + 512, None, b_gate[0:1, c0 + 512:c0 + 1024])], g_ep)
            fw.barrier()

        if stop == "S1":
            return nc
        with ExitStack() as st:
            kT = SB(st, "kT", [128, 2, S], BF16); kTb = Buf()
            vs = SB(st, "vs", [128, 64, 256], BF16); vsb = Buf()
            qT = SB(st, "qT", [128, 8, T], BF16); qTb = Buf()
            aT = SB(st, "aT", [128, 8, T], BF16); aTb = Buf()
            pT = [SB(st, "pT%d" % i, [128, 512], BF16) for i in range(3)]; pTb = [Buf() for _ in range(3)]
            rl = SB(st, "rl", [128, 512], F32); rlb = Buf()
            for hh in range(2):
                fw.dma("sync", kT[:, hh, :], kT_d[:, hh, :], reads=[dbuf("kT_d")], writes=[kTb])
            for g in range(4):
                fw.dma("sync", vs[:, g * 16:(g + 1) * 16, :], v_d[g * 2048:(g + 1) * 2048, :].rearrange("(t p) c -> p t c", p=128),
                       reads=[dbuf("v_d")], writes=[vsb])
            for g in range(4):
                fw.dma("sync", qT[:, g * 2:(g + 1) * 2, :], qT_d[:, g * 2:(g + 1) * 2, :], reads=[dbuf("qT_d")], writes=[qTb])
            sc = 1.0 / math.sqrt(128.0)
            pT4 = pT + [SB(st, "pT3", [128, 512], BF16)]; pTb4 = pTb + [Buf()]
            acc = [SB(st, "aacc%d" % i, [128, 512], F32) for i in range(2)]; accb = [Buf(), Buf()]
            iters = [(hd, qb, kc) for hd in range(8) for qb in range(4) for kc in range(64)]
            NIT = len(iters)

            def emit_qk(n):
                hd, qb, kc = iters[n]; kvh = hd // 4; si = n % 3; pj = n % 4
                fw.op("tensor", lambda h: h.matmul(PS[si][:], kT[:, kvh, kc * 128:(kc + 1) * 128], qT[:, hd, qb * 512:(qb + 1) * 512],
                                                   start=True, stop=True), reads=[kTb, qTb], writes=[PSB[si]])
                fw.op("scalar", lambda h: h.activation(out=pT4[pj][:], in_=PS[si][:], func=AF.Exp, scale=sc), reads=[PSB[si]], writes=[pTb4[pj]])

            def emit_pv(n):
                hd, qb, kc = iters[n]; kvh = hd // 4; pj = n % 4; g = n // 64; po = 3 + g % 2; a = g % 2
                fw.op("tensor", lambda h: h.matmul(PS[po][:], vs[:, kc, kvh * 128:(kvh + 1) * 128], pT4[pj][:], start=(kc == 0), stop=(kc == 63)),
                      reads=[vsb, pTb4[pj]], writes=[PSB[po]])
                if kc == 0:
                    fw.op("vector", lambda h: h.tensor_copy(out=acc[a][:], in_=pT4[pj][:]), reads=[pTb4[pj]], writes=[accb[a]])
                else:
                    fw.op("vector", lambda h: h.tensor_tensor(out=acc[a][:], in0=pT4[pj][:], in1=acc[a][:], op=ALU.add), reads=[pTb4[pj], accb[a]], writes=[accb[a]])
                if kc == 63:
                    fw.op("tensor", lambda h: h.matmul(PS[5][:], onesf[:], acc[a][:], start=True, stop=True), reads=[Bc, accb[a]], writes=[PSB[5]])
                    fw.op("vector", lambda h: h.reciprocal(out=rl[:], in_=PS[5][:]), reads=[PSB[5]], writes=[rlb])
                    fw.op("vector", lambda h: h.tensor_tensor(out=aT[:, hd, qb * 512:(qb + 1) * 512], in0=PS[po][:], in1=rl[:], op=ALU.mult),
                          reads=[PSB[po], rlb], writes=[aTb])

            emit_qk(0); emit_qk(1)
            for n in range(NIT):
                if n + 2 < NIT:
                    emit_qk(n + 2)
                emit_pv(n)
            for g in range(4):
                fw.dma("gpsimd", at_d[:, g * 2:(g + 1) * 2, :], aT[:, g * 2:(g + 1) * 2, :], reads=[aTb], writes=[dbuf("at_d")])
        fw.barrier()

        if stop == "S2":
            return nc
        persist = ExitStack()
        top.enter_context(persist)
        scl = SB(persist, "scl", [128, C], F32); sclb = Buf()
        drep, drepb = rep_load(persist, "drep", hy_bias_d, C)
        with ExitStack() as st:
            w1 = SB(st, "fw1", [33, 64], F32); w2 = SB(st, "fw2", [64, 64], F32); w3 = SB(st, "fw3", [64, 2048], F32)
            fb = SB(st, "fb", [64, 4], F32); fbb = Buf(); wl = Buf()
            fw.dma("sync", w1[:], filt_w1, writes=[wl]); fw.dma("sync", w2[:], filt_w2, writes=[wl]); fw.dma("sync", w3[:], filt_w3, writes=[wl])
            fw.dma("sync", fb[:, 0:1], filt_f1, writes=[fbb]); fw.dma("sync", fb[:, 1:2], filt_b1, writes=[fbb])
            fw.dma("sync", fb[:, 2:3], filt_f2, writes=[fbb]); fw.dma("sync", fb[:, 3:4], filt_b2, writes=[fbb])
            fbp = SB(st, "fbp", [64, 2], F32)
            fw.op("vector", lambda h: h.tensor_tensor(out=fbp[:, 0:1], in0=fb[:, 0:1], in1=fb[:, 1:2], op=ALU.mult), reads=[fbb], writes=[fbb])
            fw.op("vector", lambda h: h.tensor_tensor(out=fbp[:, 1:2], in0=fb[:, 2:3], in1=fb[:, 3:4], op=ALU.mult), reads=[fbb], writes=[fbb])
            fT = SB(st, "fT", [33, S], F32); fTb = Buf()
            fw.dma("sync", fT[:], featsT, writes=[fTb])
            h1T = SB(st, "fh1T", [64, 512], F32); h1b = Buf()
            h2T = SB(st, "fh2T", [64, S], F32); h2b = Buf()
            ar = SB(st, "far", [64, 512], F32); m1 = SB(st, "fm1", [64, 512], F32); m2 = SB(st, "fm2", [64, 512], F32); arb = Buf()

            def sin_layer(pidx, fcol, bcol, dst_ap, dstb):
                fw.op("scalar", lambda h: h.activation(out=ar[:], in_=PS[pidx][0:64, :], func=AF.Identity, scale=fb[:, fcol:fcol + 1], bias=fbp[:, bcol:bcol + 1]),
                      reads=[PSB[pidx], fbb], writes=[arb])
                fw.op("vector", lambda h: h.tensor_scalar(out=m1[:], in0=ar[:], scalar1=PI, scalar2=-2 * PI, op0=ALU.is_gt, op1=ALU.mult), reads=[arb], writes=[arb])
                fw.op("vector", lambda h: h.tensor_scalar(out=m2[:], in0=ar[:], scalar1=-PI, scalar2=2 * PI, op0=ALU.is_lt, op1=ALU.mult), reads=[arb], writes=[arb])
                fw.op("vector", lambda h: h.tensor_tensor(out=ar[:], in0=ar[:], in1=m1[:], op=ALU.add), reads=[arb], writes=[arb])
                fw.op("vector", lambda h: h.tensor_tensor(out=ar[:], in0=ar[:], in1=m2[:], op=ALU.add), reads=[arb], writes=[arb])
                fw.op("scalar", lambda h: h.activation(out=dst_ap, in_=ar[:], func=AF.Sin), reads=[arb], writes=[dstb])

            for pb in range(16):
                fw.op("tensor", lambda h, pb=pb: h.matmul(PS[0][0:64, :], w1[:], fT[:, pb * 512:(pb + 1) * 512], start=True, stop=True),
                      reads=[wl, fTb], writes=[PSB[0]])
                sin_layer(0, 0, 0, h1T[:], h1b)
                fw.op("tensor", lambda h: h.matmul(PS[1][0:64, :], w2[:], h1T[:], start=True, stop=True), reads=[wl, h1b], writes=[PSB[1]])
                sin_layer(1, 2, 1, h2T[:, pb * 512:(pb + 1) * 512], h2b)
            adl, adlb = rep_load(st, "adl", absdelta, C)
            ngt = SB(st, "ngt", [128, 64], F32); m0s = SB(st, "m0s", [128, 1], F32); ngb = Buf()
            fw.dma("sync", ngt[:], negt, writes=[ngb]); fw.dma("sync", m0s[:], m0, writes=[ngb])
            dec = [SB(st, "dec%d" % i, [128, C], F32) for i in range(2)]; decb = [Buf(), Buf()]
            fo = [SB(st, "fo%d" % i, [128, 2048], F32) for i in range(2)]; fob = [Buf(), Buf()]
            fo16 = [SB(st, "fo16_%d" % i, [128, 2048], BF16) for i in range(2)]; fo16b = [Buf(), Buf()]
            sq = [SB(st, "fsq%d" % i, [128, 2048], F32) for i in range(2)]; sqb = [Buf(), Buf()]
            for pt in range(64):
                k = pt % 2
                fw.op("scalar", lambda h: h.activation(out=dec[k][:], in_=adl[:], func=AF.Exp, scale=ngt[:, pt:pt + 1]), reads=[adlb, ngb], writes=[decb[k]])
                for cb in range(4):
                    fw.op("tensor", lambda h, cb=cb: h.matmul(PS[cb][:], h2T[:, pt * 128:(pt + 1) * 128], w3[:, cb * 512:(cb + 1) * 512], start=True, stop=True),
                          reads=[wl, h2b], writes=[PSB[cb]])
                    fw.op("vector", lambda h, cb=cb: h.tensor_tensor(out=fo[k][:, cb * 512:(cb + 1) * 512], in0=PS[cb][:],
                                                                    in1=dec[k][:, (cb % 2) * 512:(cb % 2) * 512 + 512], op=ALU.mult),
                          reads=[PSB[cb], decb[k]], writes=[fob[k]])
                if pt == 0:
                    fw.op("vector", lambda h: h.tensor_scalar(out=fo[k][:, 1024:2048], in0=fo[k][:, 1024:2048], scalar1=m0s[:, 0:1], scalar2=None, op0=ALU.mult),
                          reads=[fob[k], ngb], writes=[fob[k]])
                fw.op("scalar", lambda h: h.copy(out=fo16[k][:], in_=fo[k][:]), reads=[fob[k]], writes=[fo16b[k]])
                fw.op("gpsimd", lambda h: h.tensor_tensor(out=sq[k][:], in0=fo[k][:], in1=fo[k][:], op=ALU.mult), reads=[fob[k]], writes=[sqb[k]])
                for cb in range(4):
                    fw.op("tensor", lambda h, cb=cb: h.matmul(PS[4 + cb % 2][:], onesf[:], sq[k][:, cb * 512:(cb + 1) * 512],
                                                              start=(pt == 0 and cb < 2), stop=(pt == 63 and cb >= 2)), reads=[Bc, sqb[k]], writes=[PSB[4 + cb % 2]])
                fw.dma("gpsimd", hf_d[pt * 128:(pt + 1) * 128, :], fo16[k][:, 0:1024], reads=[fo16b[k]], writes=[dbuf("hf_d")])
                fw.dma("gpsimd", hg_d[pt * 128:(pt + 1) * 128, :], fo16[k][:, 1024:2048], reads=[fo16b[k]], writes=[dbuf("hg_d")])
            for cb in range(2):
                fw.op("scalar", lambda h, cb=cb: h.activation(out=scl[:, cb * 512:(cb + 1) * 512], in_=PS[4 + cb][:], func=AF.Sqrt, bias=FILTER_EPS, scale=1.0),
                      reads=[PSB[4 + cb]], writes=[sclb])
            fw.op("vector", lambda h: h.reciprocal(out=scl[:], in_=scl[:]), reads=[sclb], writes=[sclb])
        fw.barrier()

        if stop == "S3a":
            return nc
        with ExitStack() as st:
            def cload(name, src, shape, dt=BF16):
                t = SB(st, name, shape, dt); b = Buf()
                fw.dma("gpsimd" if dt == BF16 else "sync", t[:], src, writes=[b])
                return t, b
            Fc, Fb1 = cload("Fc", cFc, [128, 128]); Fsn, Fb3 = cload("Fsn", cFsn, [128, 128])
            i2re, Fb8 = cload("i2re", ci2re, [128, 16]); i2im, Fb9 = cload("i2im", ci2im, [128, 16])
            FB = Buf()
            fw.op("vector", lambda h: h.tensor_copy(out=Fc[:, 0:1], in_=Fc[:, 0:1]), reads=[Fb1, Fb3, Fb8, Fb9], writes=[FB])

            st1 = ExitStack()
            zt_ = [SB(st1, "f1z%d" % i, [64, 32, 512], BF16) for i in range(2)]; ztb_ = [Buf(), Buf()]
            ao = [SB(st1, "f1o%d" % i, [128, 2, 4, 512], BF16) for i in range(2)]; aob = [Buf(), Buf()]

            def f1_pass(src, dst):
                sv = src.rearrange("(a b) c -> a b c", b=128)
                it = 0
                for cb in range(2):
                    cs = slice(cb * 512, (cb + 1) * 512)
                    for nb in range(4):
                        k = (cb * 4 + nb) % 2
                        fw.dma("sync", zt_[k][:], sv[:, nb * 32:(nb + 1) * 32, cs], writes=[ztb_[k]])
                        for nn in range(32):
                            n1 = nb * 32 + nn
                            pr = (it % 3) * 2; pi_ = pr + 1; j = (it // 4) % 2; q = it % 4; it += 1
                            fw.op("tensor", lambda h, nn=nn, pr=pr: h.matmul(PS[pr][:], Fc[0:64, :], zt_[k][:, nn, :], start=True, stop=True),
                                  reads=[FB, ztb_[k]], writes=[PSB[pr]])
                            fw.op("tensor", lambda h, nn=nn, pi_=pi_: h.matmul(PS[pi_][:], Fsn[0:64, :], zt_[k][:, nn, :], start=True, stop=True),
                                  reads=[FB, ztb_[k]], writes=[PSB[pi_]])
                            fw.op("scalar", lambda h, pr=pr, j=j, q=q: h.copy(out=ao[j][:, 0, q, :], in_=PS[pr][:]), reads=[PSB[pr]], writes=[aob[j]])
                            fw.op("vector", lambda h, pi_=pi_, j=j, q=q: h.tensor_copy(out=ao[j][:, 1, q, :], in_=PS[pi_][:]), reads=[PSB[pi_]], writes=[aob[j]])
                            if q == 3:
                                for r in range(2):
                                    fw.dma("sync", dst[r][n1 - 3:n1 + 1, :, cs].rearrange("n k c -> k n c"), ao[j][:, r, :, :], reads=[aob[j]], writes=[Buf()])

            f1_pass(hf_d, a_d[1])
            f1_pass(hg_d, a_d[2])
            f1_pass(z_d, a_d[0])
            fw.barrier()
            st1.close()
            if stop == "S3b1":
                return nc

            Et = [SB(st, "Et%d" % i, [128, 7, 128], BF16) for i in range(2)]; Etb = [Buf(), Buf()]
            ain = [[SB(st, "ain%d_%d" % (i, r), [128, 512], BF16) for r in range(4)] for i in range(2)]; ainb = [[Buf() for r in range(4)] for i in range(2)]
            ho = [SB(st, "hho%d" % i, [128, 1024], BF16) for i in range(2)]; hob = [Buf(), Buf()]
            tmps = [SB(st, "ftmp%d" % i, [128, 512], F32) for i in range(2)]; tmpbs = [Buf(), Buf()]

            def f2_loads(u):
                k2, cb = u // 2, u % 2
                k = u % 2
                cs = slice(cb * 512, (cb + 1) * 512)
                if cb == 0:
                    fw.dma("sync", Et[k2 % 2][:], cE[k2], writes=[Etb[k2 % 2]])
                for r, sd in enumerate([a_d[1][0], a_d[1][1], a_d[2][0], a_d[2][1]]):
                    fw.dma("sync", ain[k][r][:], sd[:, k2, cs], writes=[ainb[k][r]])
            f2_loads(0)
            for u in range(256):
                k2, cb = u // 2, u % 2
                k = u % 2; e = k2 % 2
                cs = slice(cb * 512, (cb + 1) * 512)
                pr = (u % 3) * 2; pi_ = pr + 1
                if u + 1 < 256:
                    f2_loads(u + 1)
                for r, m in enumerate([0, 1, 0, 1]):
                    fw.op("tensor", lambda h, r=r, m=m: h.matmul(PS[pr][:], Et[e][:, m, :], ain[k][r][:], start=(r == 0), stop=(r == 3)),
                          reads=[Etb[e], ainb[k][r]], writes=[PSB[pr]])
                for r, m in enumerate([2, 0, 1, 3]):
                    fw.op("tensor", lambda h, r=r, m=m: h.matmul(PS[pi_][:], Et[e][:, m, :], ain[k][r][:], start=(r == 0), stop=(r == 3)),
                          reads=[Etb[e], ainb[k][r]], writes=[PSB[pi_]])
                fw.op("vector", lambda h: h.tensor_tensor(out=tmps[k][:], in0=PS[pr][:], in1=scl[:, cs], op=ALU.mult), reads=[PSB[pr], sclb], writes=[tmpbs[k]])
                fw.op("gpsimd", lambda h: h.tensor_tensor(out=ho[k][:, 0:512], in0=tmps[k][:], in1=drep[:, cs], op=ALU.add), reads=[tmpbs[k], drepb], writes=[hob[k]])
                fw.op("vector", lambda h: h.tensor_tensor(out=ho[k][:, 512:1024], in0=PS[pi_][:], in1=scl[:, cs], op=ALU.mult), reads=[PSB[pi_], sclb], writes=[hob[k]])
                fw.dma("sync", hh_d[0][k2, :, cs], ho[k][:, 0:512], reads=[hob[k]], writes=[Buf()])
                fw.dma("sync", hh_d[1][k2, :, cs], ho[k][:, 512:1024], reads=[hob[k]], writes=[Buf()])
            fw.barrier()
            if stop == "S3b2":
                return nc

            hin = [[SB(st, "hin%d_%d" % (i, r), [128, 512], BF16) for r in range(2)] for i in range(2)]; hinb = [[Buf(), Buf()] for i in range(2)]
            xs = [SB(st, "fxs%d" % i, [128, 1024], BF16) for i in range(2)]; xsb = [Buf(), Buf()]
            ys = [SB(st, "fys%d" % i, [128, 1024], BF16) for i in range(2)]; ysb = [Buf(), Buf()]
            t4 = [SB(st, "ft4%d" % i, [128, 2048], F32) for i in range(2)]; t4b = [[Buf() for _ in range(4)] for i in range(2)]
            bo = [SB(st, "fbo%d" % i, [128, 1024], BF16) for i in range(2)]; bob = [Buf(), Buf()]

            def fu_loads(u):
                k2, cb = u // 2, u % 2
                k = u % 2
                cs = slice(cb * 512, (cb + 1) * 512)
                if cb == 0:
                    fw.dma("sync", Et[k2 % 2][:], cE[k2], writes=[Etb[k2 % 2]])
                fw.dma("sync", ain[k][0][:], a_d[0][0][:, k2, cs], writes=[ainb[k][0]])
                fw.dma("sync", ain[k][1][:], a_d[0][1][:, k2, cs], writes=[ainb[k][1]])
                fw.dma("sync", hin[k][0][:], hh_d[0][k2, :, cs], writes=[hinb[k][0]])
                fw.dma("sync", hin[k][1][:], hh_d[1][k2, :, cs], writes=[hinb[k][1]])
            fu_loads(0)
            for u in range(256):
                k2, cb = u // 2, u % 2
                k = u % 2; e = k2 % 2
                cs = slice(cb * 512, (cb + 1) * 512)
                px = 0 if k == 0 else 4
                if u + 1 < 256:
                    fu_loads(u + 1)
                fw.op("tensor", lambda h: h.matmul(PS[px][:], Et[e][:, 0, :], ain[k][0][:], start=True, stop=False), reads=[Etb[e], ainb[k][0]], writes=[PSB[px]])
                fw.op("tensor", lambda h: h.matmul(PS[px][:], Et[e][:, 1, :], ain[k][1][:], start=False, stop=True), reads=[Etb[e], ainb[k][1]], writes=[PSB[px]])
                fw.op("tensor", lambda h: h.matmul(PS[px + 1][:], Et[e][:, 2, :], ain[k][0][:], start=True, stop=False), reads=[Etb[e], ainb[k][0]], writes=[PSB[px + 1]])
                fw.op("tensor", lambda h: h.matmul(PS[px + 1][:], Et[e][:, 0, :], ain[k][1][:], start=False, stop=True), reads=[Etb[e], ainb[k][1]], writes=[PSB[px + 1]])
                fw.op("scalar", lambda h: h.copy(out=xs[k][:, 0:512], in_=PS[px][:]), reads=[PSB[px]], writes=[xsb[k]])
                fw.op("scalar", lambda h: h.copy(out=xs[k][:, 512:1024], in_=PS[px + 1][:]), reads=[PSB[px + 1]], writes=[xsb[k]])
                fw.op("vector", lambda h: h.tensor_tensor(out=t4[k][:, 0:512], in0=xs[k][:, 0:512], in1=hin[k][0][:], op=ALU.mult), reads=[xsb[k], hinb[k][0]], writes=[t4b[k][0]])
                fw.op("gpsimd", lambda h: h.tensor_tensor(out=t4[k][:, 512:1024], in0=xs[k][:, 512:1024], in1=hin[k][1][:], op=ALU.mult), reads=[xsb[k], hinb[k][1]], writes=[t4b[k][1]])
                fw.op("vector", lambda h: h.tensor_tensor(out=t4[k][:, 1024:1536], in0=xs[k][:, 0:512], in1=hin[k][1][:], op=ALU.mult), reads=[xsb[k], hinb[k][1]], writes=[t4b[k][2]])
                fw.op("gpsimd", lambda h: h.tensor_tensor(out=t4[k][:, 1536:2048], in0=xs[k][:, 512:1024], in1=hin[k][0][:], op=ALU.mult), reads=[xsb[k], hinb[k][0]], writes=[t4b[k][3]])
                fw.op("vector", lambda h: h.tensor_tensor(out=ys[k][:, 0:512], in0=t4[k][:, 0:512], in1=t4[k][:, 512:1024], op=ALU.subtract), reads=[t4b[k][0], t4b[k][1]], writes=[ysb[k]])
                fw.op("gpsimd", lambda h: h.tensor_tensor(out=ys[k][:, 512:1024], in0=t4[k][:, 1024:1536], in1=t4[k][:, 1536:2048], op=ALU.add), reads=[t4b[k][2], t4b[k][3]], writes=[ysb[k]])
                fw.op("tensor", lambda h: h.matmul(PS[2][:], Et[e][:, 4, :], ys[k][:, 0:512], start=True, stop=False), reads=[Etb[e], ysb[k]], writes=[PSB[2]])
                fw.op("tensor", lambda h: h.matmul(PS[2][:], Et[e][:, 6, :], ys[k][:, 512:1024], start=False, stop=True), reads=[Etb[e], ysb[k]], writes=[PSB[2]])
                fw.op("tensor", lambda h: h.matmul(PS[3][:], Et[e][:, 5, :], ys[k][:, 0:512], start=True, stop=False), reads=[Etb[e], ysb[k]], writes=[PSB[3]])
                fw.op("tensor", lambda h: h.matmul(PS[3][:], Et[e][:, 4, :], ys[k][:, 512:1024], start=False, stop=True), reads=[Etb[e], ysb[k]], writes=[PSB[3]])
                fw.op("scalar", lambda h: h.copy(out=bo[k][:, 0:512], in_=PS[2][:]), reads=[PSB[2]], writes=[bob[k]])
                fw.op("vector", lambda h: h.tensor_copy(out=bo[k][:, 512:1024], in_=PS[3][:]), reads=[PSB[3]], writes=[bob[k]])
                fw.dma("sync", b_d[0][k2, :, cs], bo[k][:, 0:512], reads=[bob[k]], writes=[Buf()])
                fw.dma("sync", b_d[1][k2, :, cs], bo[k][:, 512:1024], reads=[bob[k]], writes=[Buf()])
            fw.barrier()
            if stop == "S3b3":
                return nc

            bin_ = [[SB(st, "bin%d_%d" % (i, r), [128, 16, 512], BF16) for r in range(2)] for i in range(2)]; binb = [Buf(), Buf()]
            yo = [SB(st, "fyo%d" % i, [16, 512], F32) for i in range(2)]; yob = [Buf(), Buf()]
            yv = y_d.rearrange("(a b) c -> a b c", b=128)
            it = 0
            for cb in range(2):
                cs = slice(cb * 512, (cb + 1) * 512)
                for nb in range(8):
                    k = (cb * 8 + nb) % 2
                    for r in range(2):
                        fw.dma("sync", bin_[k][r][:], b_d[r][:, nb * 16:(nb + 1) * 16, cs], reads=[dbuf("b_d")], writes=[binb[k]])
                    for nn in range(16):
                        n1 = nb * 16 + nn
                        p = it % 6; j = it % 2; it += 1
                        fw.op("tensor", lambda h, nn=nn, p=p: h.matmul(PS[p][0:16, :], i2re[:], bin_[k][0][:, nn, :], start=True, stop=False), reads=[FB, binb[k]], writes=[PSB[p]])
                        fw.op("tensor", lambda h, nn=nn, p=p: h.matmul(PS[p][0:16, :], i2im[:], bin_[k][1][:, nn, :], start=False, stop=True), reads=[FB, binb[k]], writes=[PSB[p]])
                        fw.op("scalar", lambda h, p=p, j=j: h.copy(out=yo[j][:], in_=PS[p][0:16, :]), reads=[PSB[p]], writes=[yob[j]])
                        fw.dma("sync", yv[:, n1, cs], yo[j][:], reads=[yob[j]], writes=[dbuf("y_d")])
        persist.close()
        fw.barrier()

        if stop == "S3b":
            return nc
        def wload(st, name, src, nchunk, ncol):
            t = SB(st, name, [128, nchunk, ncol], BF16); b = Buf()
            sv = src.rearrange("(c p) n -> p c n", p=128)
            for c0 in range(0, ncol, 512):
                fw.dma("gpsimd", t[:, :, c0:c0 + 512], sv[:, :, c0:c0 + 512], writes=[b])
            return t, b

        with ExitStack() as st:
            Wa, Wab = wload(st, "Wa", w_attn_o, 8, D)
            Wh, Whb = wload(st, "Wh", w_hy_o, 8, D)
            yt = [SB(st, "yt%d" % i, [128, C], F32) for i in range(2)]
            x0t = [SB(st, "x0t%d" % i, [128, C], BF16) for i in range(2)]
            gt = [SB(st, "gt%d" % i, [128, 4096], BF16) for i in range(2)]
            atT = [SB(st, "atT%d" % i, [128, 8, 128], BF16) for i in range(2)]; inb = [Buf(), Buf()]
            yh16 = SB(st, "yh16", [128, C], BF16); yhb = Buf()
            yhT = SB(st, "yhT", [128, 8, 128], BF16); yhTb = Buf()
            ya = SB(st, "ya", [128, 512], F32); yab = Buf()
            pre16 = SB(st, "pre16", [128, D], BF16); preb = Buf()
            preT = SB(st, "preTs", [128, 16, 128], BF16); preTb = Buf()
            preTv = preT_d.rearrange("(c p) t -> p c t", p=128)
            for t in range(16):
                k = t % 2
                rows = slice(t * 128, (t + 1) * 128)
                fw.dma("sync", yt[k][:], y_d[rows, :], reads=[dbuf("y_d")], writes=[inb[k]])
                fw.dma("sync", x0t[k][:], x0_d[rows, :], reads=[dbuf("x0_d")], writes=[inb[k]])
                fw.dma("sync", gt[k][:], g_d[rows, :], reads=[dbuf("g_d")], writes=[inb[k]])
                fw.dma("sync", atT[k][:], at_d[:, :, rows], reads=[dbuf("at_d")], writes=[inb[k]])
                fw.op("vector", lambda h: h.tensor_tensor(out=yh16[:], in0=yt[k][:], in1=x0t[k][:], op=ALU.mult), reads=[inb[k]], writes=[yhb])
                transpose_to(yh16, yhb, yhT, yhTb, 8)
                for cb in range(4):
                    cs = slice(cb * 512, (cb + 1) * 512)
                    pa = (cb % 3) * 2; ph = pa + 1
                    for c in range(8):
                        fw.op("tensor", lambda h, c=c, pa=pa: h.matmul(PS[pa][:], atT[k][:, c, :], Wa[:, c, cs], start=(c == 0), stop=(c == 7)),
                              reads=[inb[k], Wab], writes=[PSB[pa]])
                    for c in range(8):
                        fw.op("tensor", lambda h, c=c, ph=ph: h.matmul(PS[ph][:], yhT[:, c, :], Wh[:, c, cs], start=(c == 0), stop=(c == 7)),
                              reads=[yhTb, Whb], writes=[PSB[ph]])
                    fw.op("vector", lambda h, pa=pa: h.tensor_tensor(out=ya[:], in0=PS[pa][:], in1=gt[k][:, cs], op=ALU.mult), reads=[PSB[pa], inb[k]], writes=[yab])
                    fw.op("vector", lambda h, ph=ph: h.tensor_tensor(out=pre16[:, cs], in0=PS[ph][:], in1=gt[k][:, 2048 + cb * 512:2048 + (cb + 1) * 512], op=ALU.mult),
                          reads=[PSB[ph], inb[k]], writes=[preb])
                    fw.op("vector", lambda h: h.tensor_tensor(out=pre16[:, cs], in0=pre16[:, cs], in1=ya[:], op=ALU.add), reads=[preb, yab], writes=[preb])
                transpose_to(pre16, preb, preT, preTb, 16)
                fw.dma("gpsimd", preTv[:, :, rows], preT[:], reads=[preTb], writes=[dbuf("preT_d")])
        fw.barrier()

        if stop == "S4a":
            return nc
        with ExitStack() as st:
            Wo, Wob = wload(st, "Wo", w_out, 16, D)
            g1, g1b = rep_load(st, "g1", ln1_g, D); b1, b1b = rep_load(st, "b1", ln1_b, D)
            gbb = Buf()
            fw.op("vector", lambda h: h.tensor_copy(out=g1[:, 0:1], in_=g1[:, 0:1]), reads=[g1b, b1b], writes=[gbb])
            pT_ = [SB(st, "ppT%d" % i, [128, 16, 128], BF16) for i in range(2)]
            hres = [SB(st, "hres%d" % i, [128, D], F32) for i in range(2)]; inb = [Buf(), Buf()]
            h1t = [SB(st, "h1t%d" % i, [128, D], F32) for i in range(2)]; h1b_ = [Buf(), Buf()]
            h116 = SB(st, "h116", [128, D], BF16); h116b = Buf()
            h1Ts = SB(st, "h1Ts", [128, 16, 128], BF16); h1Tb = Buf()
            st8 = SB(st, "st8b", [128, 24], F32); sb8 = Buf(); mv = SB(st, "mvb", [128, 2], F32); mvb = Buf(); rstd = SB(st, "rstdb", [128, 1], F32)
            preTv = preT_d.rearrange("(c p) t -> p c t", p=128)
            h1Tv = h1T_d.rearrange("(c p) t -> p c t", p=128)
            for t in range(16):
                k = t % 2
                rows = slice(t * 128, (t + 1) * 128)
                fw.dma("sync", pT_[k][:], preTv[:, :, rows], reads=[dbuf("preT_d")], writes=[inb[k]])
                fw.dma("sync", hres[k][:], ho_d[128 * t + 1:128 * t + 129, :], reads=[dbuf("ho_d")], writes=[inb[k]])
                for cb in range(4):
                    cs = slice(cb * 512, (cb + 1) * 512)
                    p = (t * 4 + cb) % 6
                    for c in range(16):
                        fw.op("tensor", lambda h, c=c, p=p: h.matmul(PS[p][:], pT_[k][:, c, :], Wo[:, c, cs], start=(c == 0), stop=(c == 15)),
                              reads=[inb[k], Wob], writes=[PSB[p]])
                    fw.op("vector", lambda h, p=p: h.scalar_tensor_tensor(out=h1t[k][:, cs], in0=hres[k][:, cs], scalar=ALPHA, in1=PS[p][:], op0=ALU.mult, op1=ALU.add),
                          reads=[inb[k], PSB[p]], writes=[h1b_[k]])
                layernorm(h1t[k], h1b_[k], g1, b1, gbb, st8, sb8, mv, mvb, rstd)
                fw.dma("gpsimd", h1_d[rows, :], h1t[k][:], reads=[h1b_[k]], writes=[dbuf("h1_d")])
                fw.op("scalar", lambda h: h.copy(out=h116[:], in_=h1t[k][:]), reads=[h1b_[k]], writes=[h116b])
                transpose_to(h116, h116b, h1Ts, h1Tb, 16)
                fw.dma("gpsimd", h1Tv[:, :, rows], h1Ts[:], reads=[h1Tb], writes=[dbuf("h1T_d")])
        fw.barrier()

        if stop == "S4b":
            return nc
        persist2 = ExitStack()
        top.enter_context(persist2)
        I32 = mybir.dt.int32
        OH1 = SB(persist2, "OH1", [128, 16, 32], F32); OH2 = SB(persist2, "OH2", [128, 16, 32], F32)
        G12 = SB(persist2, "G12", [128, 16, 2], F32); Gb = Buf()
        SL1 = SB(persist2, "SL1", [128, 16], I32); SL2 = SB(persist2, "SL2", [128, 16], I32); SLb = Buf()
        IG = SB(persist2, "IG", [128, 64, 16], I32); ID = SB(persist2, "ID", [128, 64, 8], I32); IXb = Buf()
        zt16 = SB(persist2, "zt16", [128, D], BF16); ztb16 = Buf()
        fw.op("gpsimd", lambda h: h.memset(zt16[:], 0.0), writes=[ztb16])
        for i in range(64):
            fw.dma("sync", xs_d[i * 128:(i + 1) * 128, :], zt16[:], reads=[ztb16], writes=[Buf()])
        h1Tv = h1T_d.rearrange("(c p) t -> p c t", p=128)
        BIG = 1.0e30
        with ExitStack() as st:
            Wr = SB(st, "Wr", [128, 16, 36], BF16); Wrb = Buf()
            fw.dma("gpsimd", Wr[:], w_route.rearrange("(c p) n -> p c n", p=128), writes=[Wrb])
            br16 = SB(st, "br16", [1, 36], BF16)
            fw.dma("gpsimd", br16[:], b_route, writes=[Wrb])
            hT_ = [SB(st, "rhT%d" % i, [128, 16, 128], BF16) for i in range(2)]; inb = [Buf(), Buf()]
            lg = SB(st, "lg", [128, 36], F32); lgb = Buf()
            sm = SB(st, "rsm", [128, 16], F32); smb = Buf()
            oh = SB(st, "roh", [128, 96], F32); ohb = Buf()
            for t in range(16):
                k = t % 2
                fw.dma("sync", hT_[k][:], h1Tv[:, :, t * 128:(t + 1) * 128], reads=[dbuf("h1T_d")], writes=[inb[k]])
                p = t % 6
                for c in range(16):
                    fw.op("tensor", lambda h, c=c, p=p: h.matmul(PS[p][:, 0:36], hT_[k][:, c, :], Wr[:, c, :], start=(c == 0), stop=False), reads=[inb[k], Wrb], writes=[PSB[p]])
                fw.op("tensor", lambda h, p=p: h.matmul(PS[p][:, 0:36], ones16[0:1, :], br16[:], start=False, stop=True), reads=[Wrb, Bc], writes=[PSB[p]])
                fw.op("vector", lambda h, p=p: h.tensor_copy(out=lg[:], in_=PS[p][:, 0:36]), reads=[PSB[p]], writes=[lgb])
                V = lambda fn, r, w: fw.op("vector", fn, reads=r, writes=w)
                V(lambda h: h.tensor_reduce(out=sm[:, 0:1], in_=lg[:, 0:4], axis=mybir.AxisListType.X, op=ALU.max), [lgb], [smb])
                V(lambda h: h.tensor_scalar(out=oh[:, 0:4], in0=lg[:, 0:4], scalar1=sm[:, 0:1], scalar2=None, op0=ALU.is_equal), [lgb, smb], [ohb])
                V(lambda h: h.tensor_scalar(out=oh[:, 4:8], in0=lg[:, 0:4], scalar1=sm[:, 0:1], scalar2=None, op0=ALU.subtract), [lgb, smb], [ohb])
                fw.op("scalar", lambda h: h.activation(out=oh[:, 4:8], in_=oh[:, 4:8], func=AF.Exp, accum_out=sm[:, 1:2]), reads=[ohb, smb], writes=[ohb, smb])
                V(lambda h: h.reciprocal(out=sm[:, 2:3], in_=sm[:, 1:2]), [smb], [smb])
                V(lambda h: h.tensor_scalar(out=oh[:, 8:12], in0=oh[:, 0:4], scalar1=-1.0, scalar2=BIG, op0=ALU.add, op1=ALU.mult), [ohb], [ohb])
                for g_ in range(4):
                    V(lambda h, g_=g_: h.tensor_scalar(out=oh[:, 32 + 8 * g_:40 + 8 * g_], in0=lg[:, 4 + 8 * g_:12 + 8 * g_], scalar1=oh[:, 8 + g_:9 + g_], scalar2=None, op0=ALU.add),
                      [lgb, ohb], [ohb])
                V(lambda h: h.tensor_reduce(out=sm[:, 3:4], in_=oh[:, 32:64], axis=mybir.AxisListType.X, op=ALU.max), [ohb], [smb])
                V(lambda h: h.tensor_scalar(out=oh[:, 64:96], in0=oh[:, 32:64], scalar1=sm[:, 3:4], scalar2=None, op0=ALU.is_equal), [ohb, smb], [ohb])
                V(lambda h: h.scalar_tensor_tensor(out=oh[:, 32:64], in0=oh[:, 64:96], scalar=-BIG, in1=oh[:, 32:64], op0=ALU.mult, op1=ALU.add), [ohb], [ohb])
                V(lambda h: h.tensor_reduce(out=sm[:, 4:5], in_=oh[:, 32:64], axis=mybir.AxisListType.X, op=ALU.max), [ohb], [smb])
                V(lambda h: h.tensor_scalar(out=oh[:, 32:64], in0=oh[:, 32:64], scalar1=sm[:, 4:5], scalar2=None, op0=ALU.is_equal), [ohb, smb], [ohb])
                V(lambda h: h.tensor_tensor(out=sm[:, 5:6], in0=sm[:, 4:5], in1=sm[:, 3:4], op=ALU.subtract), [smb], [smb])
                fw.op("scalar", lambda h: h.activation(out=sm[:, 6:7], in_=sm[:, 5:6], func=AF.Exp), reads=[smb], writes=[smb])
                V(lambda h: h.tensor_scalar(out=sm[:, 7:8], in0=sm[:, 6:7], scalar1=1.0, scalar2=None, op0=ALU.add), [smb], [smb])
                V(lambda h: h.reciprocal(out=sm[:, 8:9], in_=sm[:, 7:8]), [smb], [smb])
                V(lambda h: h.tensor_tensor(out=sm[:, 9:10], in0=sm[:, 8:9], in1=sm[:, 2:3], op=ALU.mult), [smb], [smb])
                V(lambda h: h.tensor_tensor(out=sm[:, 10:11], in0=sm[:, 9:10], in1=sm[:, 6:7], op=ALU.mult), [smb], [smb])
                V(lambda h, t=t: h.tensor_copy(out=OH1[:, t, :], in_=oh[:, 64:96]), [ohb], [Gb])
                V(lambda h, t=t: h.tensor_copy(out=OH2[:, t, :], in_=oh[:, 32:64]), [ohb], [Gb])
                V(lambda h, t=t: h.tensor_copy(out=G12[:, t, :], in_=sm[:, 9:11]), [smb], [Gb])
            A = SB(st, "mA", [128, 16, 32], F32); Ab = Buf()
            R = SB(st, "mR", [128, 16, 32], F32); Rb = Buf()
            U = SB(st, "mU", [128, 128], F32); Ub = Buf()
            base = SB(st, "mbase", [128, 32], F32); baseb = Buf()
            ci = SB(st, "mci", [128, 32], I32); padf = SB(st, "mpadf", [128, 32], F32); pe = SB(st, "mpe", [128, 32], F32)
            pst = SB(st, "mpst", [128, 32], F32); one32 = SB(st, "mone32", [128, 32], F32); pb_ = Buf()
            sf = SB(st, "msf", [128, 32], F32); eb = SB(st, "meb", [128, 64], F32); junk32 = SB(st, "mj32", [128, 32], F32); ebb = Buf()
            igf = SB(st, "migf", [128, 64, 16], F32); bG = SB(st, "mbG", [128, 64], F32); bD = SB(st, "mbD", [128, 64], F32)
            pc = SB(st, "mpc", [128, 1], F32)
            fw.dma("sync", pc[:], pcol, writes=[ebb])
            V(lambda h: h.tensor_tensor(out=A[:], in0=OH1[:], in1=OH2[:], op=ALU.add), [Gb], [Ab])
            fw.op("gpsimd", lambda h: h.memset(U[:], 1.0), writes=[Ub])
            fw.op("gpsimd", lambda h: h.affine_select(out=U[:], in_=U[:], pattern=[[1, 128]], compare_op=ALU.is_gt, fill=0.0, base=0, channel_multiplier=-1),
                  reads=[Ub], writes=[Ub])
            V(lambda h: h.memset(base[:], 0.0), [], [baseb])
            V(lambda h: h.memset(one32[:], 1.0), [], [pb_])
            for i in range(16):
                pa = (2 * i) % 6; pb2 = (2 * i + 1) % 6
                fw.op("tensor", lambda h, i=i, pa=pa: h.matmul(PS[pa][:, 0:32], U[:], A[:, i, :], start=True, stop=True), reads=[Ub, Ab], writes=[PSB[pa]])
                fw.op("tensor", lambda h, i=i, pb2=pb2: h.matmul(PS[pb2][:, 0:32], onesf[:], A[:, i, :], start=True, stop=True), reads=[Bc, Ab], writes=[PSB[pb2]])
                V(lambda h, i=i, pa=pa: h.tensor_tensor(out=R[:, i, :], in0=PS[pa][:, 0:32], in1=base[:], op=ALU.add), [PSB[pa], baseb], [Rb])
                V(lambda h, pb2=pb2: h.tensor_tensor(out=base[:], in0=PS[pb2][:, 0:32], in1=base[:], op=ALU.add), [PSB[pb2], baseb], [baseb])
            V(lambda h: h.tensor_scalar(out=ci[:], in0=base[:], scalar1=127.0, scalar2=None, op0=ALU.add), [baseb], [pb_])
            V(lambda h: h.tensor_scalar(out=ci[:], in0=ci[:], scalar1=7, scalar2=7, op0=ALU.arith_shift_right, op1=ALU.logical_shift_left), [pb_], [pb_])
            V(lambda h: h.tensor_copy(out=padf[:], in_=ci[:]), [pb_], [pb_])
            V(lambda h: h.tensor_tensor_scan(out=pe[:], data0=one32[:], data1=padf[:], initial=0.0, op0=ALU.mult, op1=ALU.add), [pb_], [pb_])
            V(lambda h: h.tensor_tensor(out=pst[:], in0=pe[:], in1=padf[:], op=ALU.subtract), [pb_], [pb_])
            for i in range(16):
                V(lambda h, i=i: h.tensor_tensor(out=R[:, i, :], in0=R[:, i, :], in1=pst[:], op=ALU.add), [Rb, pb_], [Rb])
            for OH, SL, col in ((OH1, SL1, 0), (OH2, SL2, 1)):
                V(lambda h, OH=OH: h.tensor_tensor(out=A[:], in0=R[:], in1=OH[:], op=ALU.mult), [Rb, Gb, Ab], [Ab])
                V(lambda h: h.tensor_reduce(out=sf[:, 0:16], in_=A[:], axis=mybir.AxisListType.X, op=ALU.add), [Ab], [ebb])
                V(lambda h, SL=SL: h.tensor_copy(out=SL[:], in_=sf[:, 0:16]), [ebb], [SLb])
            for i in range(64):
                V(lambda h, i=i: h.tensor_scalar(out=junk32[:], in0=pe[:], scalar1=128.0 * i, scalar2=0.0, op0=ALU.is_le, op1=ALU.add, accum_out=eb[:, i:i + 1]),
                  [pb_], [ebb])
            V(lambda h: h.tensor_scalar(out=bG[:], in0=eb[:], scalar1=2048.0, scalar2=pc[:, 0:1], op0=ALU.mult, op1=ALU.add), [ebb], [ebb])
            V(lambda h: h.tensor_scalar(out=bD[:], in0=eb[:], scalar1=1024.0, scalar2=pc[:, 0:1], op0=ALU.mult, op1=ALU.add), [ebb], [ebb])
            for c in range(16):
                V(lambda h, c=c: h.tensor_scalar(out=igf[:, :, c], in0=bG[:], scalar1=128.0 * c, scalar2=None, op0=ALU.add), [ebb], [ebb])
            V(lambda h: h.tensor_copy(out=IG[:], in_=igf[:]), [ebb], [IXb])
            for c in range(8):
                V(lambda h, c=c: h.tensor_scalar(out=igf[:, :, c], in0=bD[:], scalar1=128.0 * c, scalar2=None, op0=ALU.add), [ebb, IXb], [ebb])
            V(lambda h: h.tensor_copy(out=ID[:], in_=igf[:, :, 0:8]), [ebb], [IXb])
        fw.barrier()

        if stop == "S5":
            return nc
        with ExitStack() as st:
            hl = [SB(st, "s_hl%d" % i, [128, D], F32) for i in range(2)]; hlb = [Buf(), Buf()]
            h16_ = [SB(st, "s_h16%d" % i, [128, D], BF16) for i in range(2)]; h16b_ = [Buf(), Buf()]
            for t in range(16):
                k = t % 2
                fw.dma("sync", hl[k][:], h1_d[t * 128:(t + 1) * 128, :], writes=[hlb[k]])
                fw.op("scalar", lambda h: h.copy(out=h16_[k][:], in_=hl[k][:]), reads=[hlb[k]], writes=[h16b_[k]])
                fw.idma(xs_d[:, :], h16_[k][:, :], SL1[:, t:t + 1], False, 8191, reads=[h16b_[k], SLb], writes=[Buf()])
                fw.idma(xs_d[:, :], h16_[k][:, :], SL2[:, t:t + 1], False, 8191, reads=[h16b_[k], SLb], writes=[Buf()])
        fw.barrier()

        if stop == "S6a":
            return nc
        weg = w_eg.rearrange("e k n -> (e k) n"); weu = w_eu.rearrange("e k n -> (e k) n"); wed = w_ed.rearrange("e k n -> (e k) n")
        with ExitStack() as st:
            NW = 8
            xb = [SB(st, "b_xb%d" % i, [128, D], BF16) for i in range(2)]; xbb = [Buf(), Buf()]
            xT = [SB(st, "b_xT%d" % i, [128, 16, 128], BF16) for i in range(2)]; xTb = [Buf(), Buf()]
            wgt = [SB(st, "b_wg%d" % i, [128, 1024], BF16) for i in range(NW)]; wgtb = [Buf() for _ in range(NW)]
            wut = [SB(st, "b_wu%d" % i, [128, 1024], BF16) for i in range(NW)]; wutb = [Buf() for _ in range(NW)]
            wdt = [[SB(st, "b_wd%d_%d" % (i, c), [128, D], BF16) for c in range(8)] for i in range(2)]
            wdtb = [[Buf() for c in range(8)] for i in range(2)]
            sg = [SB(st, "b_sg%d" % i, [128, 512], F32) for i in range(2)]; sgb = [Buf(), Buf()]
            Hs = [SB(st, "b_H%d" % i, [128, 1024], BF16) for i in range(2)]; Hsb = [Buf(), Buf()]
            HT = [SB(st, "b_HT%d" % i, [128, 8, 128], BF16) for i in range(2)]; HTb = [Buf(), Buf()]
            Ys = [SB(st, "b_Y%d" % i, [128, D], F32) for i in range(2)]; Ysb = [Buf(), Buf()]
            iw = 0
            for i in range(64):
                k = i % 2
                fw.dma("sync", xb[k][:], xs_d[i * 128:(i + 1) * 128, :], writes=[xbb[k]])
                transpose_to(xb[k], xbb[k], xT[k], xTb[k], 16)
                for c in range(16):
                    j = iw % NW; iw += 1
                    fw.idma(wgt[j][:, :], weg[:, :], IG[:, i, c:c + 1], True, 32 * 2048 - 1, reads=[IXb], writes=[wgtb[j]])
                    fw.idma(wut[j][:, :], weu[:, :], IG[:, i, c:c + 1], True, 32 * 2048 - 1, reads=[IXb], writes=[wutb[j]])
                    for hf_ in range(2):
                        fw.op("tensor", lambda h, c=c, j=j, hf_=hf_: h.matmul(PS[hf_][:], xT[k][:, c, :], wgt[j][:, hf_ * 512:(hf_ + 1) * 512], start=(c == 0), stop=(c == 15)),
                              reads=[xTb[k], wgtb[j]], writes=[PSB[hf_]])
                    for hf_ in range(2):
                        fw.op("tensor", lambda h, c=c, j=j, hf_=hf_: h.matmul(PS[2 + hf_][:], xT[k][:, c, :], wut[j][:, hf_ * 512:(hf_ + 1) * 512], start=(c == 0), stop=(c == 15)),
                              reads=[xTb[k], wutb[j]], writes=[PSB[2 + hf_]])
                for c in range(8):
                    fw.idma(wdt[k][c][:, :], wed[:, :], ID[:, i, c:c + 1], True, 32 * 1024 - 1, reads=[IXb], writes=[wdtb[k][c]])
                for hf_ in range(2):
                    fw.op("scalar", lambda h, hf_=hf_: h.activation(out=sg[hf_][:], in_=PS[hf_][:], func=AF.Silu), reads=[PSB[hf_]], writes=[sgb[hf_]])
                    fw.op("vector", lambda h, hf_=hf_: h.tensor_tensor(out=Hs[k][:, hf_ * 512:(hf_ + 1) * 512], in0=PS[2 + hf_][:], in1=sg[hf_][:], op=ALU.mult),
                          reads=[PSB[2 + hf_], sgb[hf_]], writes=[Hsb[k]])
                transpose_to(Hs[k], Hsb[k], HT[k], HTb[k], 8)
                for hf_ in range(2):
                    for c in range(8):
                        for q in range(2):
                            fw.op("tensor", lambda h, c=c, q=q, hf_=hf_: h.matmul(PS[4 + q][:], HT[k][:, c, :], wdt[k][c][:, hf_ * 1024 + q * 512:hf_ * 1024 + (q + 1) * 512],
                                                                                  start=(c == 0), stop=(c == 7)), reads=[HTb[k], wdtb[k][c]], writes=[PSB[4 + q]])
                    fw.op("scalar", lambda h, hf_=hf_: h.copy(out=Ys[k][:, hf_ * 1024:hf_ * 1024 + 512], in_=PS[4][:]), reads=[PSB[4]], writes=[Ysb[k]])
                    fw.op("vector", lambda h, hf_=hf_: h.tensor_copy(out=Ys[k][:, hf_ * 1024 + 512:(hf_ + 1) * 1024], in_=PS[5][:]), reads=[PSB[5]], writes=[Ysb[k]])
                fw.dma("sync", ys_d[i * 128:(i + 1) * 128, :], Ys[k][:], reads=[Ysb[k]], writes=[Buf()])
        fw.barrier()

        if stop == "S6b":
            return nc
        with ExitStack() as st:
            g2, g2b = rep_load(st, "g2", ln2_g, D); b2, b2b = rep_load(st, "b2", ln2_b, D)
            gbb = Buf()
            fw.op("vector", lambda h: h.tensor_copy(out=g2[:, 0:1], in_=g2[:, 0:1]), reads=[g2b, b2b], writes=[gbb])
            ht = [SB(st, "f_h%d" % i, [128, D], F32) for i in range(2)]; htb = [Buf(), Buf()]
            y1 = [SB(st, "f_y1%d" % i, [128, D], F32) for i in range(2)]; y1b = [Buf(), Buf()]
            y2 = [SB(st, "f_y2%d" % i, [128, D], F32) for i in range(2)]; y2b = [Buf(), Buf()]
            st8 = SB(st, "st8c", [128, 24], F32); sb8 = Buf(); mv = SB(st, "mvc", [128, 2], F32); mvb = Buf(); rstd = SB(st, "rstdc", [128, 1], F32)
            for t in range(16):
                k = t % 2
                rows = slice(t * 128, (t + 1) * 128)
                fw.dma("sync", ht[k][:], h1_d[rows, :], writes=[htb[k]])
                fw.idma(y1[k][:, :], ys_d[:, :], SL1[:, t:t + 1], True, 8191, reads=[SLb], writes=[y1b[k]])
                fw.idma(y2[k][:, :], ys_d[:, :], SL2[:, t:t + 1], True, 8191, reads=[SLb], writes=[y2b[k]])
                fw.op("vector", lambda h: h.tensor_scalar(out=ht[k][:], in0=ht[k][:], scalar1=ALPHA, scalar2=None, op0=ALU.mult), reads=[htb[k]], writes=[htb[k]])
                fw.op("vector", lambda h, t=t: h.scalar_tensor_tensor(out=ht[k][:], in0=y1[k][:], scalar=G12[:, t, 0:1], in1=ht[k][:], op0=ALU.mult, op1=ALU.add),
                      reads=[y1b[k], htb[k], Gb], writes=[htb[k]])
                fw.op("vector", lambda h, t=t: h.scalar_tensor_tensor(out=ht[k][:], in0=y2[k][:], scalar=G12[:, t, 1:2], in1=ht[k][:], op0=ALU.mult, op1=ALU.add),
                      reads=[y2b[k], htb[k], Gb], writes=[htb[k]])
                layernorm(ht[k], htb[k], g2, b2, gbb, st8, sb8, mv, mvb, rstd)
                fw.dma("sync", out[rows, :], ht[k][:], reads=[htb[k]], writes=[Buf()])
        persist2.close()
        fw.barrier()
    return nc


_CONST = {}


def _consts():
    if _CONST:
        return _CONST
    f32 = np.float32
    half = 64
    inv_freq = (10000.0 ** (-np.arange(0, half, 2, dtype=np.float64) / half)).astype(f32)
    row_idx = (np.arange(S) // 64).astype(f32)
    col_idx = (np.arange(S) % 64).astype(f32)
    ang_r = (row_idx[:, None] * inv_freq[None, :]).astype(f32)
    ang_c = (col_idx[:, None] * inv_freq[None, :]).astype(f32)
    cr, sr, cc, sc = np.cos(ang_r), np.sin(ang_r), np.cos(ang_c), np.sin(ang_c)
    _CONST["ropeC"] = np.concatenate([cr, cr, cc, cc], axis=1).astype(f32)
    _CONST["ropeS"] = np.concatenate([-sr, sr, -sc, sc], axis=1).astype(f32)
    n = np.arange(128, dtype=np.float64)
    a128 = 2 * np.pi * np.outer(n, n) / 128.0
    _CONST["Fc"] = np.cos(a128).astype(f32); _CONST["Fs"] = np.sin(a128).astype(f32)
    a16 = 2 * np.pi * np.outer(n, n) / float(NFFT)
    _CONST["C16"] = np.cos(a16).astype(f32); _CONST["S16"] = np.sin(a16).astype(f32)
    t = np.linspace(0.0, 1.0, S, dtype=f32)
    w = (2.0 * math.pi * np.arange(S, dtype=f32) / S).astype(f32)[:, None]
    bands = np.linspace(1e-4, 15, 16, dtype=f32)[None, :]
    feats = np.concatenate([t[:, None], np.cos(bands * w), -np.sin(bands * w)], axis=-1).astype(f32)
    _CONST["featsT"] = np.ascontiguousarray(feats.T)
    _CONST["negt"] = np.ascontiguousarray((-t).reshape(64, 128).T)
    min_decay = math.log(1e-2) / 1.5
    max_decay = math.log(1e-2) / 0.3
    _CONST["absdelta"] = np.abs(np.linspace(min_decay, max_decay, C, dtype=f32)).reshape(1, C).astype(f32)
    import ml_dtypes
    n1 = np.arange(128, dtype=np.float64)[None, :, None]
    k1 = np.arange(128, dtype=np.float64)[None, None, :]
    k2 = np.arange(128, dtype=np.float64)[:, None, None]
    th = 2 * np.pi * n1 * (128.0 * k1 + k2) / float(NFFT)
    ec, es = np.cos(th), np.sin(th)
    ect, est = ec.transpose(0, 2, 1), es.transpose(0, 2, 1)
    _CONST["cE"] = np.ascontiguousarray(np.stack([ec, es, -es, -ec, ect, est, -est], axis=2)).astype(ml_dtypes.bfloat16)
    m0 = np.ones((128, 1), f32); m0[0, 0] = 0.0
    _CONST["m0"] = m0
    return _CONST


def _in_maps(inp):
    cs = _consts()
    f32 = np.float32
    x = np.asarray(inp["x"], f32)
    common = {
        "ln_in_g": inp["ln_in_g"].reshape(1, D), "ln_in_b": inp["ln_in_b"].reshape(1, D),
        "w_in": inp["w_in"][0], "b_gate": inp["b_gate"][0].reshape(1, 4096),
        "q_norm_g": inp["q_norm_g"][0].reshape(1, 128), "k_norm_g": inp["k_norm_g"][0].reshape(1, 128),
        "hy_conv_w": inp["hy_conv_w"][0], "hy_conv_b": inp["hy_conv_b"][0].reshape(1, 3072),
        "filt_w1": inp["filt_w1"][0], "filt_b1": inp["filt_b1"][0].reshape(64, 1), "filt_f1": inp["filt_f1"][0].reshape(64, 1),
        "filt_w2": inp["filt_w2"][0], "filt_b2": inp["filt_b2"][0].reshape(64, 1), "filt_f2": inp["filt_f2"][0].reshape(64, 1),
        "filt_w3": inp["filt_w3"][0], "hy_bias_d": inp["hy_bias_d"][0].reshape(1, C),
        "w_attn_o": inp["w_attn_o"][0], "w_hy_o": inp["w_hy_o"][0], "w_out": inp["w_out"][0],
        "ln1_g": inp["ln1_g"][0].reshape(1, D), "ln1_b": inp["ln1_b"][0].reshape(1, D),
        "w_route": np.concatenate([inp["w_route_grp"][0], inp["w_route_exp"][0]], axis=1),
        "b_route": np.concatenate([inp["b_route_grp"][0], inp["b_route_exp"][0]], axis=0).reshape(1, 36),
        "w_eg": inp["w_exp_gate"][0], "w_eu": inp["w_exp_up"][0], "w_ed": inp["w_exp_down"][0],
        "ln2_g": inp["ln2_g"][0].reshape(1, D), "ln2_b": inp["ln2_b"][0].reshape(1, D),
        "ropeC_k": cs["ropeC"], "ropeS_k": cs["ropeS"],
        "cFc": cs["Fc"], "cFs": cs["Fs"], "cFsn": -cs["Fs"], "cFcn": -cs["Fc"],
        "cC16": cs["C16"], "cS16": cs["S16"], "cS16n": -cs["S16"],
        "pcol": np.arange(128, dtype=np.float32).reshape(128, 1), "featsT": cs["featsT"], "negt": cs["negt"], "absdelta": cs["absdelta"], "m0": cs["m0"],
    }
    common = {k: np.ascontiguousarray(np.asarray(v, f32)) for k, v in common.items()}
    common["cE"] = cs["cE"]
    maps = []
    for c in range(8):
        b, j = c // 4, c % 4
        q0 = j * T
        xo = np.zeros((NOWN, D), f32); mo = np.zeros((NOWN, 1), f32)
        lo = max(q0 - 1, 0); hi = min(q0 + T + 1, S)
        r0 = lo - (q0 - 1)
        xo[r0:r0 + (hi - lo)] = x[b, lo:hi]
        mo[r0:r0 + (hi - lo)] = 1.0
        m = dict(common)
        m["x_seq"] = np.ascontiguousarray(x[b]); m["x_own"] = xo; m["m_own"] = np.ascontiguousarray(mo.reshape(17, 128).T)
        m["ropeC_q"] = np.ascontiguousarray(cs["ropeC"][q0:q0 + T]); m["ropeS_q"] = np.ascontiguousarray(cs["ropeS"][q0:q0 + T])
        n2 = np.arange(16 * j, 16 * j + 16)
        m["ci2re"] = np.ascontiguousarray(cs["Fc"][:, n2] / float(NFFT)).astype(f32)
        m["ci2im"] = np.ascontiguousarray(-cs["Fs"][:, n2] / float(NFFT)).astype(f32)
        maps.append(m)
    return maps


def kernel(**inputs):
    inp = {k: np.asarray(v) for k, v in inputs.items()}
    nc = build_nc()
    maps = _in_maps(inp)
    res = run_bass_kernel_spmd(nc, maps, core_ids=list(range(8)))
    outp = np.zeros((2, S, D), np.float32)
    for c in range(8):
        b, j = c // 4, c % 4
        outp[b, j * T:(j + 1) * T] = res.results[c]["out"]
    return outp
```

```python
import math
from contextlib import ExitStack
import numpy as np
import concourse.bass as bass
import concourse.mybir as mybir
from concourse.bass_utils import run_bass_kernel_spmd

F32 = mybir.dt.float32
BF16 = mybir.dt.bfloat16
ALU = mybir.AluOpType
AF = mybir.ActivationFunctionType

D = 2048
S = 8192
T = 2048
NOWN = 17 * 128
C = 1024
INW = 8704
ALPHA = 2.0 ** 0.25
LN_EPS = 1e-5
QK_EPS = 1e-6
FILTER_EPS = 1e-6
NFFT = 16384
PI = math.pi


class Buf:
    __slots__ = ("w", "r")

    def __init__(self):
        self.w = None
        self.r = []


class FW:
    ENGS = ("tensor", "vector", "scalar", "gpsimd", "sync")

    def __init__(self, nc, stack, ndma=20):
        self.nc = nc
        self.cnt = {e: 0 for e in self.ENGS}
        self.seen = {e: {} for e in self.ENGS}
        self.sems = {}
        self.ndma = ndma
        self.dma_next = {e: 0 for e in self.ENGS}
        self.dma_tot = {}
        for e in self.ENGS:
            self.sems[e] = stack.enter_context(nc.semaphore("s_" + e))
        for q in ("sync", "gpsimd", "scalar"):
            for i in range(ndma):
                k = ("d", q, i)
                self.sems[k] = stack.enter_context(nc.semaphore("d_%s_%d" % (q, i)))
                self.dma_tot[k] = 0

    def _waits(self, eng, reads, writes):
        deps = {}

        def add(t):
            if t is not None and deps.get(t[0], 0) < t[1]:
                deps[t[0]] = t[1]
        for b in reads:
            add(b.w)
        for b in writes:
            add(b.w)
            for t in b.r:
                add(t)
        out = []
        seen = self.seen[eng]
        for key, val in deps.items():
            if key == eng and eng == "tensor":
                continue
            if seen.get(key, 0) >= val:
                continue
            seen[key] = val
            out.append((key, val))
        return out

    def _mark(self, tag, reads, writes):
        for b in writes:
            b.w = tag
            b.r = []
        for b in reads:
            b.r = [t for t in b.r if t[0] != tag[0]] + [tag]

    def op(self, eng, fn, reads=(), writes=()):
        waits = self._waits(eng, reads, writes)
        self.cnt[eng] += 1
        tag = (eng, self.cnt[eng])
        h = getattr(self.nc, eng)
        for key, val in waits:
            h.wait_ge(self.sems[key], val)
        fn(h).then_inc(self.sems[eng], 1)
        self._mark(tag, reads, writes)

    def idma(self, out, in_, idx, gather, bound, reads=(), writes=()):
        if not hasattr(self, "bregs"):
            self.bregs = {}
        if bound not in self.bregs:
            r = self.nc.gpsimd.alloc_register("bnd%d" % bound)
            self.nc.gpsimd.reg_mov(r, bound)
            self.bregs[bound] = r
        bound = self.bregs[bound]
        if gather:
            fn = lambda h: h.indirect_dma_start(out=out, out_offset=None, in_=in_, in_offset=bass.IndirectOffsetOnAxis(ap=idx, axis=0),
                                                bounds_check=bound, oob_is_err=False)
        else:
            fn = lambda h: h.indirect_dma_start(out=out, out_offset=bass.IndirectOffsetOnAxis(ap=idx, axis=0), in_=in_, in_offset=None,
                                                bounds_check=bound, oob_is_err=False)
        self.dma("gpsimd", None, None, reads=reads, writes=writes, fn=fn)

    def dma(self, q, out, in_, reads=(), writes=(), slow=False, fn=None):
        i = self.dma_next[q]
        self.dma_next[q] = (i + 1) % self.ndma
        key = ("d", q, i)
        waits = self._waits(q, reads, writes)
        prev = self.dma_tot[key]
        if prev > 0 and self.seen[q].get(key, 0) < prev:
            self.seen[q][key] = prev
            waits.append((key, prev))
        self.dma_tot[key] = prev + 16
        tag = (key, prev + 16)
        h = getattr(self.nc, q)
        for k2, val in waits:
            h.wait_ge(self.sems[k2], val)
        if fn is not None:
            inst = fn(h)
        elif slow:
            inst = h.dma_start(out=out, in_=in_, allow_slow_non_contiguous=True)
        else:
            inst = h.dma_start(out=out, in_=in_)
        inst.then_inc(self.sems[key], 16)
        self._mark(tag, reads, writes)

    def barrier(self):
        for e in self.ENGS:
            h = getattr(self.nc, e)
            seen = self.seen[e]
            for o in self.ENGS:
                if o == e or self.cnt[o] == 0:
                    continue
                if seen.get(o, 0) < self.cnt[o]:
                    seen[o] = self.cnt[o]
                    h.wait_ge(self.sems[o], self.cnt[o])
            for k, tot in self.dma_tot.items():
                if tot > 0 and seen.get(k, 0) < tot:
                    seen[k] = tot
                    h.wait_ge(self.sems[k], tot)


def build_nc(dbg=None, stop=None):
    nc = bass.Bass("TRN2", target_bir_lowering=False)
    ins = {}

    def IN(name, shape, dt=F32):
        ins[name] = nc.dram_tensor(name, list(shape), dt, kind="ExternalInput").ap()
        return ins[name]

    x_seq = IN("x_seq", [S, D]); x_own = IN("x_own", [NOWN, D]); m_own = IN("m_own", [128, 17])
    ln_in_g = IN("ln_in_g", [1, D]); ln_in_b = IN("ln_in_b", [1, D])
    w_in = IN("w_in", [D, INW]); b_gate = IN("b_gate", [1, 4096])
    q_norm_g = IN("q_norm_g", [1, 128]); k_norm_g = IN("k_norm_g", [1, 128])
    hy_conv_w = IN("hy_conv_w", [3, 3072]); hy_conv_b = IN("hy_conv_b", [1, 3072])
    filt_w1 = IN("filt_w1", [33, 64]); filt_b1 = IN("filt_b1", [64, 1]); filt_f1 = IN("filt_f1", [64, 1])
    filt_w2 = IN("filt_w2", [64, 64]); filt_b2 = IN("filt_b2", [64, 1]); filt_f2 = IN("filt_f2", [64, 1])
    filt_w3 = IN("filt_w3", [64, 2048]); hy_bias_d = IN("hy_bias_d", [1, C])
    w_attn_o = IN("w_attn_o", [1024, D]); w_hy_o = IN("w_hy_o", [C, D]); w_out = IN("w_out", [D, D])
    ln1_g = IN("ln1_g", [1, D]); ln1_b = IN("ln1_b", [1, D])
    w_route = IN("w_route", [D, 36]); b_route = IN("b_route", [1, 36])
    w_eg = IN("w_eg", [32, D, 1024]); w_eu = IN("w_eu", [32, D, 1024]); w_ed = IN("w_ed", [32, 1024, D])
    ln2_g = IN("ln2_g", [1, D]); ln2_b = IN("ln2_b", [1, D])
    ropeC_k = IN("ropeC_k", [S, 128]); ropeS_k = IN("ropeS_k", [S, 128])
    ropeC_q = IN("ropeC_q", [T, 128]); ropeS_q = IN("ropeS_q", [T, 128])
    cFc = IN("cFc", [128, 128]); cFs = IN("cFs", [128, 128]); cFsn = IN("cFsn", [128, 128]); cFcn = IN("cFcn", [128, 128])
    cC16 = IN("cC16", [128, 128]); cS16 = IN("cS16", [128, 128]); cS16n = IN("cS16n", [128, 128])
    cE = IN("cE", [128, 128, 7, 128], BF16); ci2re = IN("ci2re", [128, 16]); ci2im = IN("ci2im", [128, 16])
    pcol = IN("pcol", [128, 1]); featsT = IN("featsT", [33, S]); negt = IN("negt", [128, 64]); absdelta = IN("absdelta", [1, C]); m0 = IN("m0", [128, 1])

    out = nc.dram_tensor("out", [T, D], F32, kind="ExternalOutput").ap()

    def DR(name, shape, dt):
        kind = "ExternalOutput" if (dbg and name in dbg) else "Internal"
        return nc.dram_tensor(name, list(shape), dt, kind=kind).ap()

    hT_d = DR("hT_d", [D, S + 2], BF16); hTo_d = DR("hTo_d", [D, NOWN], BF16); ho_d = DR("ho_d", [NOWN, D], F32)
    kT_d = DR("kT_d", [128, 2, S], BF16); v_d = DR("v_d", [S, 256], BF16); qT_d = DR("qT_d", [128, 8, T], BF16)
    z_d = DR("z_d", [S, C], BF16); x0_d = DR("x0_d", [T, C], BF16); g_d = DR("g_d", [T, 4096], BF16)
    at_d = DR("at_d", [128, 8, T], BF16)
    hf_d = DR("hf_d", [S, C], BF16); hg_d = DR("hg_d", [S, C], BF16)
    a_d = [[DR("a_d%d%d" % (i, r), [128, 128, C], BF16) for r in range(2)] for i in range(3)]
    hh_d = [DR("hh_d%d" % r, [128, 128, C], BF16) for r in range(2)]
    b_d = [DR("b_d%d" % r, [128, 128, C], BF16) for r in range(2)]
    y_d = DR("y_d", [T, C], F32)
    xs_d = DR("xs_d", [8192, D], BF16); ys_d = DR("ys_d", [8192, D], F32); preT_d = DR("preT_d", [D, T], BF16); h1_d = DR("h1_d", [T, D], F32); h1T_d = DR("h1T_d", [D, T], BF16)

    with ExitStack() as top:
        fw = FW(nc, top)
        Dm = {}

        def dbuf(ap_name):
            return Buf()

        uid = [0]

        def SB(st, name, shape, dt):
            uid[0] += 1
            return st.enter_context(nc.sbuf_tensor("%s_u%d" % (name, uid[0]), list(shape), dt))

        PS = [top.enter_context(nc.psum_tensor("ps%d" % i, [128, 512], F32)) for i in range(6)]
        PSB = [Buf() for _ in range(6)]
        PT = [top.enter_context(nc.psum_tensor("pt%d" % i, [128, 1024], BF16)) for i in range(2)]
        PTB = [Buf() for _ in range(2)]

        ident = SB(top, "ident", [128, 128], BF16); identf = SB(top, "identf", [128, 128], F32)
        ones16 = SB(top, "ones16", [128, 128], BF16); onesf = SB(top, "onesf", [128, 128], F32)
        Bc = Buf()
        fw.op("gpsimd", lambda h: h.memset(identf[:], 1.0), writes=[Bc])
        fw.op("gpsimd", lambda h: h.affine_select(out=identf[:], in_=identf[:], pattern=[[-1, 128]],
                                                   compare_op=ALU.is_equal, fill=0.0, base=0, channel_multiplier=1),
              reads=[Bc], writes=[Bc])
        fw.op("vector", lambda h: h.tensor_copy(out=ident[:], in_=identf[:]), reads=[Bc], writes=[Bc])
        fw.op("vector", lambda h: h.memset(ones16[:], 1.0), writes=[Bc])
        fw.op("vector", lambda h: h.memset(onesf[:], 1.0), writes=[Bc])

        def rep_load(st, name, src_row, n, dt=F32, q="sync"):
            t = SB(st, name, [128, n], dt)
            b = Buf()
            fw.dma(q, t[:], src_row.to_broadcast([128, n]), writes=[b])
            return t, b

        def layernorm(xt, xb, grep, brep, gb, st8, sb8, mv, mvb, rstd, eps=LN_EPS):
            for i in range(4):
                fw.op("vector", lambda h, i=i: h.bn_stats(out=st8[:, i * 6:(i + 1) * 6], in_=xt[:, i * 512:(i + 1) * 512]),
                      reads=[xb], writes=[sb8])
            fw.op("vector", lambda h: h.bn_aggr(out=mv[:], in_=st8[:]), reads=[sb8], writes=[mvb])
            fw.op("scalar", lambda h: h.activation(out=rstd[:], in_=mv[:, 1:2], func=AF.Sqrt, bias=eps, scale=1.0),
                  reads=[mvb], writes=[sb8])
            fw.op("vector", lambda h: h.reciprocal(out=rstd[:], in_=rstd[:]), reads=[sb8], writes=[sb8])
            fw.op("vector", lambda h: h.tensor_scalar(out=xt[:], in0=xt[:], scalar1=mv[:, 0:1], scalar2=rstd[:, 0:1],
                                                       op0=ALU.subtract, op1=ALU.mult), reads=[xb, mvb, sb8], writes=[xb])
            fw.op("vector", lambda h: h.tensor_tensor(out=xt[:], in0=xt[:], in1=grep[:], op=ALU.mult), reads=[xb, gb], writes=[xb])
            fw.op("vector", lambda h: h.tensor_tensor(out=xt[:], in0=xt[:], in1=brep[:], op=ALU.add), reads=[xb, gb], writes=[xb])

        def transpose_to(src16, srcb, dst, dstb, nchunk, col0=0, ncols=128):
            for g4 in range(0, nchunk, 8):
                n = min(8, nchunk - g4)
                pi = (g4 // 8) % 2
                for c in range(n):
                    fw.op("tensor", lambda h, c=c: h.transpose(PT[pi][:, c * 128:c * 128 + ncols],
                                                               src16[0:ncols, (g4 + c) * 128:(g4 + c + 1) * 128], ident[0:ncols, 0:ncols]),
                          reads=[srcb, Bc], writes=[PTB[pi]])
                eng = "vector" if pi == 0 else "scalar"
                if eng == "vector":
                    fw.op("vector", lambda h: h.tensor_copy(
                        out=dst[:, g4:g4 + n, col0:col0 + ncols],
                        in_=PT[pi][:, 0:n * 128].rearrange("p (c t) -> p c t", t=128)[:, :, 0:ncols]),
                        reads=[PTB[pi]], writes=[dstb])
                else:
                    fw.op("scalar", lambda h: h.copy(
                        out=dst[:, g4:g4 + n, col0:col0 + ncols],
                        in_=PT[pi][:, 0:n * 128].rearrange("p (c t) -> p c t", t=128)[:, :, 0:ncols]),
                        reads=[PTB[pi]], writes=[dstb])

        with ExitStack() as st:
            grep, gb = rep_load(st, "lng", ln_in_g, D)
            brep, bb_ = rep_load(st, "lnb", ln_in_b, D)
            gbb = Buf()
            fw.op("vector", lambda h: h.tensor_copy(out=grep[:, 0:1], in_=grep[:, 0:1]), reads=[gb, bb_], writes=[gbb])
            xts = [SB(st, "xt%d" % i, [128, D], F32) for i in range(2)]; xbs = [Buf(), Buf()]
            h16 = [SB(st, "h16_%d" % i, [128, D], BF16) for i in range(2)]; h16b = [Buf(), Buf()]
            hTs = [SB(st, "hTs%d" % i, [128, 16, 512], BF16) for i in range(2)]; hTb = [Buf(), Buf()]
            st8 = SB(st, "st8", [128, 24], F32); sb8 = Buf(); mv = SB(st, "mv", [128, 2], F32); mvb = Buf()
            rstd = SB(st, "rstd", [128, 1], F32)
            mk = SB(st, "mk", [128, 17], F32); mkb = Buf()
            zt = SB(st, "zt", [128, 16, 1], BF16); ztb = Buf()
            fw.op("vector", lambda h: h.memset(zt[:], 0.0), writes=[ztb])
            hTv = hT_d.rearrange("(c p) s -> p c s", p=128)
            fw.dma("gpsimd", hTv[:, :, 0:1], zt[:], reads=[ztb], writes=[dbuf("hT_d")], slow=True)
            fw.dma("gpsimd", hTv[:, :, S + 1:S + 2], zt[:], reads=[ztb], writes=[dbuf("hT_d")], slow=True)
            fw.dma("sync", mk[:], m_own, writes=[mkb])
            hTov = hTo_d.rearrange("(c p) s -> p c s", p=128)
            it = 0
            for grp in range(16 + 5):
                own = grp >= 16
                ntile = 4 if not own else (4 if grp < 20 else 1)
                hb = grp % 2
                for ti in range(ntile):
                    tile = (grp * 4 + ti) if not own else ((grp - 16) * 4 + ti)
                    k = it % 2; it += 1
                    src = x_seq if not own else x_own
                    fw.dma("sync", xts[k][:], src[tile * 128:(tile + 1) * 128, :], writes=[xbs[k]])
                    layernorm(xts[k], xbs[k], grep, brep, gbb, st8, sb8, mv, mvb, rstd)
                    if own:
                        fw.op("scalar", lambda h, k=k, tile=tile: h.activation(out=xts[k][:], in_=xts[k][:], func=AF.Identity,
                                                                                scale=mk[:, tile:tile + 1]),
                              reads=[xbs[k], mkb], writes=[xbs[k]])
                        fw.dma("gpsimd", ho_d[tile * 128:(tile + 1) * 128, :], xts[k][:], reads=[xbs[k]], writes=[dbuf("ho_d")])
                    fw.op("scalar", lambda h, k=k: h.copy(out=h16[k][:], in_=xts[k][:]), reads=[xbs[k]], writes=[h16b[k]])
                    transpose_to(h16[k], h16b[k], hTs[hb], hTb[hb], 16, col0=ti * 128)
                if not own:
                    fw.dma("gpsimd", hTv[:, :, 1 + grp * 512:1 + grp * 512 + 512], hTs[hb][:], reads=[hTb[hb]], writes=[dbuf("hT_d")])
                else:
                    g0 = (grp - 16) * 512
                    fw.dma("gpsimd", hTov[:, :, g0:g0 + ntile * 128], hTs[hb][:, :, 0:ntile * 128], reads=[hTb[hb]], writes=[dbuf("hTo_d")])
        fw.barrier()

        w_in_v = w_in.rearrange("(c p) n -> p c n", p=128)

        def proj_pass(st, src_v, srcname, ntok_tiles, blocks, epilogue):
            nb = len(blocks)
            wts = []
            wb = Buf()
            for bi, (col0, cidx, bias) in enumerate(blocks):
                nsh = 3 if cidx is not None else 1
                for sh in range(nsh):
                    wt = SB(st, "w_%d_%d" % (bi, sh), [128, 16, 512], BF16)
                    fw.dma("gpsimd", wt[:], w_in_v[:, :, col0:col0 + 512], writes=[wb])
                    if cidx is not None:
                        cw, cwb = rep_load(st, "cw_%d_%d" % (bi, sh), hy_conv_w[sh:sh + 1, cidx:cidx + 512], 512)
                        for c in range(16):
                            fw.op("vector", lambda h, c=c, wt=wt, cw=cw: h.tensor_tensor(out=wt[:, c, :], in0=wt[:, c, :], in1=cw[:], op=ALU.mult),
                                  reads=[wb, cwb], writes=[wb])
                    wts.append((bi, sh if cidx is not None else 1, wt))
                if bias is not None:
                    b16 = SB(st, "b16_%d" % bi, [1, 512], BF16)
                    fw.dma("gpsimd", b16[:], bias, writes=[wb])
                    wts.append((bi, -1, b16))
            hw = [SB(st, "hw%d" % i, [128, 16, 514], BF16) for i in range(2)]; hwb = [Buf(), Buf()]
            ngrp = (ntok_tiles + 3) // 4
            for g in range(ngrp):
                k = g % 2
                nt = min(4, ntok_tiles - g * 4)
                wdt = nt * 128 + 2
                fw.dma("sync", hw[k][:, :, 0:wdt], src_v[:, :, g * 512:g * 512 + wdt], reads=[dbuf(srcname)], writes=[hwb[k]])
                for m in range(nt):
                    tile = g * 4 + m
                    pidx = [(tile * nb + bi) % 6 for bi in range(nb)]
                    for bi in range(nb):
                        mine = [(sh, wt) for (b2, sh, wt) in wts if b2 == bi]
                        nsteps = sum(16 if sh >= 0 else 1 for sh, _ in mine)
                        step = 0
                        for sh, wt in mine:
                            if sh < 0:
                                fw.op("tensor", lambda h, wt=wt, p=pidx[bi], s0=(step == 0), s1=(step == nsteps - 1):
                                      h.matmul(PS[p][:], ones16[0:1, :], wt[:], start=s0, stop=s1), reads=[wb, Bc], writes=[PSB[pidx[bi]]])
                                step += 1
                                continue
                            for c in range(16):
                                fw.op("tensor", lambda h, wt=wt, c=c, p=pidx[bi], off=m * 128 + sh, s0=(step == 0), s1=(step == nsteps - 1), k=k:
                                      h.matmul(PS[p][:], hw[k][:, c, off:off + 128], wt[:, c, :], start=s0, stop=s1),
                                      reads=[wb, hwb[k]], writes=[PSB[pidx[bi]]])
                                step += 1
                    epilogue(tile, pidx)

        def qk_epilogue_factory(st, gsrc, ropeC, ropeS, nheads_list, dstT, dstname, pref):
            grep_, gb_ = rep_load(st, pref + "g", gsrc, 128)
            ss = SB(st, pref + "ss", [128, 4], F32); ssb = Buf()
            junk = SB(st, pref + "junk", [128, 128], F32)
            xn = SB(st, pref + "xn", [128, 128], F32); xnb = Buf()
            t1 = SB(st, pref + "t1", [128, 128], F32); t2 = SB(st, pref + "t2", [128, 128], F32); tb_ = Buf()
            x16 = [SB(st, pref + "x16%d" % i, [128, 128], BF16) for i in range(2)]; x16b = [Buf(), Buf()]
            rc = [SB(st, pref + "rc%d" % i, [128, 128], F32) for i in range(2)]
            rs = [SB(st, pref + "rs%d" % i, [128, 128], F32) for i in range(2)]; rb = [Buf(), Buf()]
            stg = SB(st, pref + "stg", [128, 8, 128], BF16); stgb = Buf()
            cnt = [0]

            def fn(tile, p, heads):
                k = tile % 2
                fw.dma("sync", rc[k][:], ropeC[tile * 128:(tile + 1) * 128, :], writes=[rb[k]])
                fw.dma("sync", rs[k][:], ropeS[tile * 128:(tile + 1) * 128, :], writes=[rb[k]])
                for hi, (co, dh) in enumerate(heads):
                    fw.op("scalar", lambda h, hi=hi, co=co: h.activation(out=junk[:], in_=PS[p][:, co:co + 128], func=AF.Square,
                                                                         accum_out=ss[:, hi:hi + 1]), reads=[PSB[p]], writes=[ssb])
                nh = len(heads)
                fw.op("scalar", lambda h: h.activation(out=ss[:, 0:nh], in_=ss[:, 0:nh], func=AF.Sqrt, bias=QK_EPS, scale=1.0 / 128),
                      reads=[ssb], writes=[ssb])
                fw.op("vector", lambda h: h.reciprocal(out=ss[:, 0:nh], in_=ss[:, 0:nh]), reads=[ssb], writes=[ssb])
                for hi, (co, dh) in enumerate(heads):
                    j = cnt[0] % 2; cnt[0] += 1
                    fw.op("vector", lambda h, hi=hi, co=co: h.scalar_tensor_tensor(out=xn[:], in0=PS[p][:, co:co + 128], scalar=ss[:, hi:hi + 1],
                                                                                  in1=grep_[:], op0=ALU.mult, op1=ALU.mult),
                          reads=[PSB[p], ssb, gb_], writes=[xnb])
                    fw.op("vector", lambda h: h.tensor_tensor(out=t1[:], in0=xn[:], in1=rc[k][:], op=ALU.mult), reads=[xnb, rb[k]], writes=[tb_])
                    xv = xn[:].rearrange("p (a h d) -> p a h d", a=2, h=2)
                    sv = rs[k][:].rearrange("p (a h d) -> p a h d", a=2, h=2)
                    tv = t2[:].rearrange("p (a h d) -> p a h d", a=2, h=2)
                    fw.op("vector", lambda h: h.tensor_tensor(out=tv[:, :, 0, :], in0=xv[:, :, 1, :], in1=sv[:, :, 0, :], op=ALU.mult),
                          reads=[xnb, rb[k]], writes=[tb_])
                    fw.op("vector", lambda h: h.tensor_tensor(out=tv[:, :, 1, :], in0=xv[:, :, 0, :], in1=sv[:, :, 1, :], op=ALU.mult),
                          reads=[xnb, rb[k]], writes=[tb_])
                    fw.op("vector", lambda h, j=j: h.tensor_tensor(out=x16[j][:], in0=t1[:], in1=t2[:], op=ALU.add), reads=[tb_], writes=[x16b[j]])
                    fw.op("tensor", lambda h, j=j, hi=hi: h.transpose(PT[0][:, hi * 128:(hi + 1) * 128], x16[j][:], ident[:]),
                          reads=[x16b[j], Bc], writes=[PTB[0]])
                fw.op("scalar", lambda h: h.copy(out=stg[:, 0:nh, :], in_=PT[0][:, 0:nh * 128].rearrange("p (c t) -> p c t", t=128)),
                      reads=[PTB[0]], writes=[stgb])
                for hi, (co, dh) in enumerate(heads):
                    fw.dma("gpsimd", dstT[:, dh, tile * 128:(tile + 1) * 128], stg[:, hi, :], reads=[stgb], writes=[dbuf(dstname)])
            return fn

        hTv = hT_d.rearrange("(c p) s -> p c s", p=128)
        hTov = hTo_d.rearrange("(c p) s -> p c s", p=128)

        if stop == "S0":
            return nc
        with ExitStack() as st:
            kfn = qk_epilogue_factory(st, k_norm_g, ropeC_k, ropeS_k, 2, kT_d, "kT_d", "k")
            v16 = [SB(st, "v16_%d" % i, [128, 256], BF16) for i in range(2)]; v16b = [Buf(), Buf()]

            def kv_ep(tile, pidx):
                p = pidx[0]
                kfn(tile, p, [(0, 0), (128, 1)])
                k = tile % 2
                fw.op("scalar", lambda h: h.copy(out=v16[k][:], in_=PS[p][:, 256:512]), reads=[PSB[p]], writes=[v16b[k]])
                fw.dma("gpsimd", v_d[tile * 128:(tile + 1) * 128, :], v16[k][:], reads=[v16b[k]], writes=[dbuf("v_d")])
            proj_pass(st, hTv, "hT_d", 64, [(1024, None, None)], kv_ep)
        fw.barrier()

        if stop == "KV":
            return nc
        for qb in range(2):
            with ExitStack() as st:
                qfn = qk_epilogue_factory(st, q_norm_g, ropeC_q, ropeS_q, 4, qT_d, "qT_d", "q")

                def q_ep(tile, pidx, qb=qb):
                    qfn(tile, pidx[0], [(i * 128, qb * 4 + i) for i in range(4)])
                proj_pass(st, hTov, "hTo_d", 16, [(qb * 512, None, None)], q_ep)
            fw.barrier()

        if stop == "Q":
            return nc
        for cb in range(2):
            with ExitStack() as st:
                x1s = [SB(st, "x1s%d" % i, [128, 512], F32) for i in range(2)]; x1b = [Buf(), Buf()]
                z16 = [SB(st, "z16_%d" % i, [128, 512], BF16) for i in range(2)]; z16b = [Buf(), Buf()]

                def z_ep(tile, pidx, cb=cb):
                    k = tile % 2
                    fw.op("scalar", lambda h: h.copy(out=x1s[k][:], in_=PS[pidx[0]][:]), reads=[PSB[pidx[0]]], writes=[x1b[k]])
                    fw.op("vector", lambda h: h.tensor_tensor(out=z16[k][:], in0=PS[pidx[1]][:], in1=x1s[k][:], op=ALU.mult),
                          reads=[PSB[pidx[1]], x1b[k]], writes=[z16b[k]])
                    fw.dma("gpsimd", z_d[tile * 128:(tile + 1) * 128, cb * 512:(cb + 1) * 512], z16[k][:], reads=[z16b[k]], writes=[dbuf("z_d")])
                c1 = 1024 + cb * 512; c2 = 2048 + cb * 512
                proj_pass(st, hTv, "hT_d", 64,
                          [(1536 + c1, c1, hy_conv_b[0:1, c1:c1 + 512]), (1536 + c2, c2, hy_conv_b[0:1, c2:c2 + 512])], z_ep)
            fw.barrier()

        if stop == "Z":
            return nc
        for cb in range(2):
            with ExitStack() as st:
                o16 = [SB(st, "o16_%d" % i, [128, 512], BF16) for i in range(2)]; o16b = [Buf(), Buf()]

                def x0_ep(tile, pidx, cb=cb):
                    k = tile % 2
                    fw.op("scalar", lambda h: h.copy(out=o16[k][:], in_=PS[pidx[0]][:]), reads=[PSB[pidx[0]]], writes=[o16b[k]])
                    fw.dma("gpsimd", x0_d[tile * 128:(tile + 1) * 128, cb * 512:(cb + 1) * 512], o16[k][:], reads=[o16b[k]], writes=[dbuf("x0_d")])
                c0 = cb * 512
                proj_pass(st, hTov, "hTo_d", 16, [(1536 + c0, c0, hy_conv_b[0:1, c0:c0 + 512])], x0_ep)
            fw.barrier()

        for cb in range(4):
            with ExitStack() as st:
                o16 = [SB(st, "g16_%d" % i, [128, 1024], BF16) for i in range(2)]; o16b = [Buf(), Buf()]

                def g_ep(tile, pidx, cb=cb):
                    k = tile % 2
                    for bi in range(2):
                        fw.op("scalar", lambda h, bi=bi: h.activation(out=o16[k][:, bi * 512:(bi + 1) * 512], in_=PS[pidx[bi]][:], func=AF.Sigmoid),
                              reads=[PSB[pidx[bi]]], writes=[o16b[k]])
                    fw.dma("gpsimd", g_d[tile * 128:(tile + 1) * 128, cb * 1024:(cb + 1) * 1024], o16[k][:], reads=[o16b[k]], writes=[dbuf("g_d")])
                c0 = cb * 1024
                proj_pass(st, hTov, "hTo_d", 16,
                          [(4608 + c0, None, b_gate[0:1, c0:c0 + 512]), (4608 + c0 + 512, None, b_gate[0:1, c0 + 512:c0 + 1024])], g_ep)
            fw.barrier()

        if stop == "S1":
            return nc
        with ExitStack() as st:
            kT = SB(st, "kT", [128, 2, S], BF16); kTb = Buf()
            vs = SB(st, "vs", [128, 64, 256], BF16); vsb = Buf()
            qT = SB(st, "qT", [128, 8, T], BF16); qTb = Buf()
            aT = SB(st, "aT", [128, 8, T], BF16); aTb = Buf()
            pT = [SB(st, "pT%d" % i, [128, 512], BF16) for i in range(3)]; pTb = [Buf() for _ in range(3)]
            rl = SB(st, "rl", [128, 512], F32); rlb = Buf()
            for hh in range(2):
                fw.dma("sync", kT[:, hh, :], kT_d[:, hh, :], reads=[dbuf("kT_d")], writes=[kTb])
            for g in range(4):
                fw.dma("sync", vs[:, g * 16:(g + 1) * 16, :], v_d[g * 2048:(g + 1) * 2048, :].rearrange("(t p) c -> p t c", p=128),
                       reads=[dbuf("v_d")], writes=[vsb])
            for g in range(4):
                fw.dma("sync", qT[:, g * 2:(g + 1) * 2, :], qT_d[:, g * 2:(g + 1) * 2, :], reads=[dbuf("qT_d")], writes=[qTb])
            sc = 1.0 / math.sqrt(128.0)
            pT4 = pT + [SB(st, "pT3", [128, 512], BF16)]; pTb4 = pTb + [Buf()]
            acc = [SB(st, "aacc%d" % i, [128, 512], F32) for i in range(2)]; accb = [Buf(), Buf()]
            iters = [(hd, qb, kc) for hd in range(8) for qb in range(4) for kc in range(64)]
            NIT = len(iters)

            def emit_qk(n):
                hd, qb, kc = iters[n]; kvh = hd // 4; si = n % 3; pj = n % 4
                fw.op("tensor", lambda h: h.matmul(PS[si][:], kT[:, kvh, kc * 128:(kc + 1) * 128], qT[:, hd, qb * 512:(qb + 1) * 512],
                                                   start=True, stop=True), reads=[kTb, qTb], writes=[PSB[si]])
                fw.op("scalar", lambda h: h.activation(out=pT4[pj][:], in_=PS[si][:], func=AF.Exp, scale=sc), reads=[PSB[si]], writes=[pTb4[pj]])

            def emit_pv(n):
                hd, qb, kc = iters[n]; kvh = hd // 4; pj = n % 4; g = n // 64; po = 3 + g % 2; a = g % 2
                fw.op("tensor", lambda h: h.matmul(PS[po][:], vs[:, kc, kvh * 128:(kvh + 1) * 128], pT4[pj][:], start=(kc == 0), stop=(kc == 63)),
                      reads=[vsb, pTb4[pj]], writes=[PSB[po]])
                if kc == 0:
                    fw.op("vector", lambda h: h.tensor_copy(out=acc[a][:], in_=pT4[pj][:]), reads=[pTb4[pj]], writes=[accb[a]])
                else:
                    fw.op("vector", lambda h: h.tensor_tensor(out=acc[a][:], in0=pT4[pj][:], in1=acc[a][:], op=ALU.add), reads=[pTb4[pj], accb[a]], writes=[accb[a]])
                if kc == 63:
                    fw.op("tensor", lambda h: h.matmul(PS[5][:], onesf[:], acc[a][:], start=True, stop=True), reads=[Bc, accb[a]], writes=[PSB[5]])
                    fw.op("vector", lambda h: h.reciprocal(out=rl[:], in_=PS[5][:]), reads=[PSB[5]], writes=[rlb])
                    fw.op("vector", lambda h: h.tensor_tensor(out=aT[:, hd, qb * 512:(qb + 1) * 512], in0=PS[po][:], in1=rl[:], op=ALU.mult),
                          reads=[PSB[po], rlb], writes=[aTb])

            emit_qk(0); emit_qk(1)
            for n in range(NIT):
                if n + 2 < NIT:
                    emit_qk(n + 2)
                emit_pv(n)
            for g in range(4):
                fw.dma("gpsimd", at_d[:, g * 2:(g + 1) * 2, :], aT[:, g * 2:(g + 1) * 2, :], reads=[aTb], writes=[dbuf("at_d")])
        fw.barrier()

        if stop == "S2":
            return nc
        persist = ExitStack()
        top.enter_context(persist)
        scl = SB(persist, "scl", [128, C], F32); sclb = Buf()
        drep, drepb = rep_load(persist, "drep", hy_bias_d, C)
        with ExitStack() as st:
            w1 = SB(st, "fw1", [33, 64], F32); w2 = SB(st, "fw2", [64, 64], F32); w3 = SB(st, "fw3", [64, 2048], F32)
            fb = SB(st, "fb", [64, 4], F32); fbb = Buf(); wl = Buf()
            fw.dma("sync", w1[:], filt_w1, writes=[wl]); fw.dma("sync", w2[:], filt_w2, writes=[wl]); fw.dma("sync", w3[:], filt_w3, writes=[wl])
            fw.dma("sync", fb[:, 0:1], filt_f1, writes=[fbb]); fw.dma("sync", fb[:, 1:2], filt_b1, writes=[fbb])
            fw.dma("sync", fb[:, 2:3], filt_f2, writes=[fbb]); fw.dma("sync", fb[:, 3:4], filt_b2, writes=[fbb])
            fbp = SB(st, "fbp", [64, 2], F32)
            fw.op("vector", lambda h: h.tensor_tensor(out=fbp[:, 0:1], in0=fb[:, 0:1], in1=fb[:, 1:2], op=ALU.mult), reads=[fbb], writes=[fbb])
            fw.op("vector", lambda h: h.tensor_tensor(out=fbp[:, 1:2], in0=fb[:, 2:3], in1=fb[:, 3:4], op=ALU.mult), reads=[fbb], writes=[fbb])
            fT = SB(st, "fT", [33, S], F32); fTb = Buf()
            fw.dma("sync", fT[:], featsT, writes=[fTb])
            h1T = SB(st, "fh1T", [64, 512], F32); h1b = Buf()
            h2T = SB(st, "fh2T", [64, S], F32); h2b = Buf()
            ar = SB(st, "far", [64, 512], F32); m1 = SB(st, "fm1", [64, 512], F32); m2 = SB(st, "fm2", [64, 512], F32); arb = Buf()

            def sin_layer(pidx, fcol, bcol, dst_ap, dstb):
                fw.op("scalar", lambda h: h.activation(out=ar[:], in_=PS[pidx][0:64, :], func=AF.Identity, scale=fb[:, fcol:fcol + 1], bias=fbp[:, bcol:bcol + 1]),
                      reads=[PSB[pidx], fbb], writes=[arb])
                fw.op("vector", lambda h: h.tensor_scalar(out=m1[:], in0=ar[:], scalar1=PI, scalar2=-2 * PI, op0=ALU.is_gt, op1=ALU.mult), reads=[arb], writes=[arb])
                fw.op("vector", lambda h: h.tensor_scalar(out=m2[:], in0=ar[:], scalar1=-PI, scalar2=2 * PI, op0=ALU.is_lt, op1=ALU.mult), reads=[arb], writes=[arb])
                fw.op("vector", lambda h: h.tensor_tensor(out=ar[:], in0=ar[:], in1=m1[:], op=ALU.add), reads=[arb], writes=[arb])
                fw.op("vector", lambda h: h.tensor_tensor(out=ar[:], in0=ar[:], in1=m2[:], op=ALU.add), reads=[arb], writes=[arb])
                fw.op("scalar", lambda h: h.activation(out=dst_ap, in_=ar[:], func=AF.Sin), reads=[arb], writes=[dstb])

            for pb in range(16):
                fw.op("tensor", lambda h, pb=pb: h.matmul(PS[0][0:64, :], w1[:], fT[:, pb * 512:(pb + 1) * 512], start=True, stop=True),
                      reads=[wl, fTb], writes=[PSB[0]])
                sin_layer(0, 0, 0, h1T[:], h1b)
                fw.op("tensor", lambda h: h.matmul(PS[1][0:64, :], w2[:], h1T[:], start=True, stop=True), reads=[wl, h1b], writes=[PSB[1]])
                sin_layer(1, 2, 1, h2T[:, pb * 512:(pb + 1) * 512], h2b)
            adl, adlb = rep_load(st, "adl", absdelta, C)
            ngt = SB(st, "ngt", [128, 64], F32); m0s = SB(st, "m0s", [128, 1], F32); ngb = Buf()
            fw.dma("sync", ngt[:], negt, writes=[ngb]); fw.dma("sync", m0s[:], m0, writes=[ngb])
            dec = [SB(st, "dec%d" % i, [128, C], F32) for i in range(2)]; decb = [Buf(), Buf()]
            fo = [SB(st, "fo%d" % i, [128, 2048], F32) for i in range(2)]; fob = [Buf(), Buf()]
            fo16 = [SB(st, "fo16_%d" % i, [128, 2048], BF16) for i in range(2)]; fo16b = [Buf(), Buf()]
            sq = [SB(st, "fsq%d" % i, [128, 2048], F32) for i in range(2)]; sqb = [Buf(), Buf()]
            for pt in range(64):
                k = pt % 2
                fw.op("scalar", lambda h: h.activation(out=dec[k][:], in_=adl[:], func=AF.Exp, scale=ngt[:, pt:pt + 1]), reads=[adlb, ngb], writes=[decb[k]])
                for cb in range(4):
                    fw.op("tensor", lambda h, cb=cb: h.matmul(PS[cb][:], h2T[:, pt * 128:(pt + 1) * 128], w3[:, cb * 512:(cb + 1) * 512], start=True, stop=True),
                          reads=[wl, h2b], writes=[PSB[cb]])
                    fw.op("vector", lambda h, cb=cb: h.tensor_tensor(out=fo[k][:, cb * 512:(cb + 1) * 512], in0=PS[cb][:],
                                                                    in1=dec[k][:, (cb % 2) * 512:(cb % 2) * 512 + 512], op=ALU.mult),
                          reads=[PSB[cb], decb[k]], writes=[fob[k]])
                if pt == 0:
                    fw.op("vector", lambda h: h.tensor_scalar(out=fo[k][:, 1024:2048], in0=fo[k][:, 1024:2048], scalar1=m0s[:, 0:1], scalar2=None, op0=ALU.mult),
                          reads=[fob[k], ngb], writes=[fob[k]])
                fw.op("scalar", lambda h: h.copy(out=fo16[k][:], in_=fo[k][:]), reads=[fob[k]], writes=[fo16b[k]])
                fw.op("scalar", lambda h: h.activation(out=sq[k][:], in_=fo[k][:], func=AF.Square), reads=[fob[k]], writes=[sqb[k]])
                for cb in range(4):
                    fw.op("tensor", lambda h, cb=cb: h.matmul(PS[4 + cb % 2][:], onesf[:], sq[k][:, cb * 512:(cb + 1) * 512],
                                                              start=(pt == 0 and cb < 2), stop=(pt == 63 and cb >= 2)), reads=[Bc, sqb[k]], writes=[PSB[4 + cb % 2]])
                fw.dma("gpsimd", hf_d[pt * 128:(pt + 1) * 128, :], fo16[k][:, 0:1024], reads=[fo16b[k]], writes=[dbuf("hf_d")])
                fw.dma("gpsimd", hg_d[pt * 128:(pt + 1) * 128, :], fo16[k][:, 1024:2048], reads=[fo16b[k]], writes=[dbuf("hg_d")])
            for cb in range(2):
                fw.op("scalar", lambda h, cb=cb: h.activation(out=scl[:, cb * 512:(cb + 1) * 512], in_=PS[4 + cb][:], func=AF.Sqrt, bias=FILTER_EPS, scale=1.0),
                      reads=[PSB[4 + cb]], writes=[sclb])
            fw.op("vector", lambda h: h.reciprocal(out=scl[:], in_=scl[:]), reads=[sclb], writes=[sclb])
        fw.barrier()

        if stop == "S3a":
            return nc
        with ExitStack() as st:
            def cload(name, src, shape, dt=BF16):
                t = SB(st, name, shape, dt); b = Buf()
                fw.dma("gpsimd" if dt == BF16 else "sync", t[:], src, writes=[b])
                return t, b
            Fc, Fb1 = cload("Fc", cFc, [128, 128]); Fsn, Fb3 = cload("Fsn", cFsn, [128, 128])
            i2re, Fb8 = cload("i2re", ci2re, [128, 16]); i2im, Fb9 = cload("i2im", ci2im, [128, 16])
            FB = Buf()
            fw.op("vector", lambda h: h.tensor_copy(out=Fc[:, 0:1], in_=Fc[:, 0:1]), reads=[Fb1, Fb3, Fb8, Fb9], writes=[FB])

            st1 = ExitStack()
            zt_ = [SB(st1, "f1z%d" % i, [64, 32, 512], BF16) for i in range(2)]; ztb_ = [Buf(), Buf()]
            ao = [SB(st1, "f1o%d" % i, [128, 2, 4, 512], BF16) for i in range(2)]; aob = [Buf(), Buf()]

            def f1_pass(src, dst):
                sv = src.rearrange("(a b) c -> a b c", b=128)
                it = 0
                for cb in range(2):
                    cs = slice(cb * 512, (cb + 1) * 512)
                    for nb in range(4):
                        k = (cb * 4 + nb) % 2
                        fw.dma("sync", zt_[k][:], sv[:, nb * 32:(nb + 1) * 32, cs], writes=[ztb_[k]])
                        for nn in range(32):
                            n1 = nb * 32 + nn
                            pr = (it % 3) * 2; pi_ = pr + 1; j = (it // 4) % 2; q = it % 4; it += 1
                            fw.op("tensor", lambda h, nn=nn, pr=pr: h.matmul(PS[pr][:], Fc[0:64, :], zt_[k][:, nn, :], start=True, stop=True),
                                  reads=[FB, ztb_[k]], writes=[PSB[pr]])
                            fw.op("tensor", lambda h, nn=nn, pi_=pi_: h.matmul(PS[pi_][:], Fsn[0:64, :], zt_[k][:, nn, :], start=True, stop=True),
                                  reads=[FB, ztb_[k]], writes=[PSB[pi_]])
                            fw.op("scalar", lambda h, pr=pr, j=j, q=q: h.copy(out=ao[j][:, 0, q, :], in_=PS[pr][:]), reads=[PSB[pr]], writes=[aob[j]])
                            fw.op("vector", lambda h, pi_=pi_, j=j, q=q: h.tensor_copy(out=ao[j][:, 1, q, :], in_=PS[pi_][:]), reads=[PSB[pi_]], writes=[aob[j]])
                            if q == 3:
                                for r in range(2):
                                    fw.dma("sync", dst[r][n1 - 3:n1 + 1, :, cs].rearrange("n k c -> k n c"), ao[j][:, r, :, :], reads=[aob[j]], writes=[Buf()])

            f1_pass(hf_d, a_d[1])
            f1_pass(hg_d, a_d[2])
            f1_pass(z_d, a_d[0])
            fw.barrier()
            st1.close()
            if stop == "S3b1":
                return nc

            Et = [SB(st, "Et%d" % i, [128, 7, 128], BF16) for i in range(2)]; Etb = [Buf(), Buf()]
            ain = [[SB(st, "ain%d_%d" % (i, r), [128, 1024], BF16) for r in range(4)] for i in range(2)]; ainb = [[Buf() for r in range(4)] for i in range(2)]
            ho = [SB(st, "hho%d" % i, [128, 2, 1024], BF16) for i in range(2)]; hob = [Buf(), Buf()]
            tmps = [SB(st, "ftmp%d" % i, [128, 512], F32) for i in range(2)]; tmpbs = [Buf(), Buf()]

            def f2_loads(k2):
                e = k2 % 2
                fw.dma("sync", Et[e][:], cE[k2], writes=[Etb[e]])
                for r, sd in enumerate([a_d[1][0], a_d[1][1], a_d[2][0], a_d[2][1]]):
                    fw.dma("sync", ain[e][r][:], sd[:, k2, :], writes=[ainb[e][r]])
            f2_loads(0)
            for u in range(256):
                k2, cb = u // 2, u % 2
                k = u % 2; e = k2 % 2
                cs = slice(cb * 512, (cb + 1) * 512)
                pr = (u % 3) * 2; pi_ = pr + 1
                if cb == 0 and k2 + 1 < 128:
                    f2_loads(k2 + 1)
                for r, m in enumerate([0, 1, 0, 1]):
                    fw.op("tensor", lambda h, r=r, m=m: h.matmul(PS[pr][:], Et[e][:, m, :], ain[e][r][:, cs], start=(r == 0), stop=(r == 3)),
                          reads=[Etb[e], ainb[e][r]], writes=[PSB[pr]])
                for r, m in enumerate([2, 0, 1, 3]):
                    fw.op("tensor", lambda h, r=r, m=m: h.matmul(PS[pi_][:], Et[e][:, m, :], ain[e][r][:, cs], start=(r == 0), stop=(r == 3)),
                          reads=[Etb[e], ainb[e][r]], writes=[PSB[pi_]])
                fw.op("vector", lambda h: h.tensor_tensor(out=tmps[k][:], in0=PS[pr][:], in1=scl[:, cs], op=ALU.mult), reads=[PSB[pr], sclb], writes=[tmpbs[k]])
                fw.op("vector", lambda h: h.tensor_tensor(out=ho[e][:, 0, cs], in0=tmps[k][:], in1=drep[:, cs], op=ALU.add), reads=[tmpbs[k], drepb], writes=[hob[e]])
                fw.op("vector", lambda h: h.tensor_tensor(out=ho[e][:, 1, cs], in0=PS[pi_][:], in1=scl[:, cs], op=ALU.mult), reads=[PSB[pi_], sclb], writes=[hob[e]])
                if cb == 1:
                    fw.dma("sync", hh_d[0][k2, :, :], ho[e][:, 0, :], reads=[hob[e]], writes=[Buf()])
                    fw.dma("sync", hh_d[1][k2, :, :], ho[e][:, 1, :], reads=[hob[e]], writes=[Buf()])
            fw.barrier()
            if stop == "S3b2":
                return nc

            hin = [[SB(st, "hin%d_%d" % (i, r), [128, 1024], BF16) for r in range(2)] for i in range(2)]; hinb = [[Buf(), Buf()] for i in range(2)]
            xs = [SB(st, "fxs%d" % i, [128, 1024], BF16) for i in range(2)]; xsb = [Buf(), Buf()]
            ys = [SB(st, "fys%d" % i, [128, 1024], BF16) for i in range(2)]; ysb = [Buf(), Buf()]
            t4 = [SB(st, "ft4%d" % i, [128, 2048], BF16) for i in range(2)]; t4b = [[Buf() for _ in range(4)] for i in range(2)]
            bo = [SB(st, "fbo%d" % i, [128, 2, 1024], BF16) for i in range(2)]; bob = [Buf(), Buf()]

            def fu_loads(k2):
                e = k2 % 2
                fw.dma("sync", Et[e][:], cE[k2], writes=[Etb[e]])
                fw.dma("sync", ain[e][0][:], a_d[0][0][:, k2, :], writes=[ainb[e][0]])
                fw.dma("sync", ain[e][1][:], a_d[0][1][:, k2, :], writes=[ainb[e][1]])
                fw.dma("sync", hin[e][0][:], hh_d[0][k2, :, :], writes=[hinb[e][0]])
                fw.dma("sync", hin[e][1][:], hh_d[1][k2, :, :], writes=[hinb[e][1]])
            fu_loads(0)
            for u in range(256):
                k2, cb = u // 2, u % 2
                k = u % 2; e = k2 % 2
                cs = slice(cb * 512, (cb + 1) * 512)
                px = 0 if k == 0 else 4
                if cb == 0 and k2 + 1 < 128:
                    fu_loads(k2 + 1)
                fw.op("tensor", lambda h: h.matmul(PS[px][:], Et[e][:, 0, :], ain[e][0][:, cs], start=True, stop=False), reads=[Etb[e], ainb[e][0]], writes=[PSB[px]])
                fw.op("tensor", lambda h: h.matmul(PS[px][:], Et[e][:, 1, :], ain[e][1][:, cs], start=False, stop=True), reads=[Etb[e], ainb[e][1]], writes=[PSB[px]])
                fw.op("tensor", lambda h: h.matmul(PS[px + 1][:], Et[e][:, 2, :], ain[e][0][:, cs], start=True, stop=False), reads=[Etb[e], ainb[e][0]], writes=[PSB[px + 1]])
                fw.op("tensor", lambda h: h.matmul(PS[px + 1][:], Et[e][:, 0, :], ain[e][1][:, cs], start=False, stop=True), reads=[Etb[e], ainb[e][1]], writes=[PSB[px + 1]])
                fw.op("scalar", lambda h: h.copy(out=xs[k][:, 0:512], in_=PS[px][:]), reads=[PSB[px]], writes=[xsb[k]])
                fw.op("scalar", lambda h: h.copy(out=xs[k][:, 512:1024], in_=PS[px + 1][:]), reads=[PSB[px + 1]], writes=[xsb[k]])
                fw.op("vector", lambda h: h.tensor_tensor(out=t4[k][:, 0:512], in0=xs[k][:, 0:512], in1=hin[e][0][:, cs], op=ALU.mult), reads=[xsb[k], hinb[e][0]], writes=[t4b[k][0]])
                fw.op("vector", lambda h: h.tensor_tensor(out=t4[k][:, 512:1024], in0=xs[k][:, 512:1024], in1=hin[e][1][:, cs], op=ALU.mult), reads=[xsb[k], hinb[e][1]], writes=[t4b[k][1]])
                fw.op("vector", lambda h: h.tensor_tensor(out=t4[k][:, 1024:1536], in0=xs[k][:, 0:512], in1=hin[e][1][:, cs], op=ALU.mult), reads=[xsb[k], hinb[e][1]], writes=[t4b[k][2]])
                fw.op("vector", lambda h: h.tensor_tensor(out=t4[k][:, 1536:2048], in0=xs[k][:, 512:1024], in1=hin[e][0][:, cs], op=ALU.mult), reads=[xsb[k], hinb[e][0]], writes=[t4b[k][3]])
                fw.op("vector", lambda h: h.tensor_tensor(out=ys[k][:, 0:512], in0=t4[k][:, 0:512], in1=t4[k][:, 512:1024], op=ALU.subtract), reads=[t4b[k][0], t4b[k][1]], writes=[ysb[k]])
                fw.op("vector", lambda h: h.tensor_tensor(out=ys[k][:, 512:1024], in0=t4[k][:, 1024:1536], in1=t4[k][:, 1536:2048], op=ALU.add), reads=[t4b[k][2], t4b[k][3]], writes=[ysb[k]])
                fw.op("tensor", lambda h: h.matmul(PS[2][:], Et[e][:, 4, :], ys[k][:, 0:512], start=True, stop=False), reads=[Etb[e], ysb[k]], writes=[PSB[2]])
                fw.op("tensor", lambda h: h.matmul(PS[2][:], Et[e][:, 6, :], ys[k][:, 512:1024], start=False, stop=True), reads=[Etb[e], ysb[k]], writes=[PSB[2]])
                fw.op("tensor", lambda h: h.matmul(PS[3][:], Et[e][:, 5, :], ys[k][:, 0:512], start=True, stop=False), reads=[Etb[e], ysb[k]], writes=[PSB[3]])
                fw.op("tensor", lambda h: h.matmul(PS[3][:], Et[e][:, 4, :], ys[k][:, 512:1024], start=False, stop=True), reads=[Etb[e], ysb[k]], writes=[PSB[3]])
                fw.op("scalar", lambda h: h.copy(out=bo[e][:, 0, cs], in_=PS[2][:]), reads=[PSB[2]], writes=[bob[e]])
                fw.op("scalar", lambda h: h.copy(out=bo[e][:, 1, cs], in_=PS[3][:]), reads=[PSB[3]], writes=[bob[e]])
                if cb == 1:
                    fw.dma("sync", b_d[0][k2, :, :], bo[e][:, 0, :], reads=[bob[e]], writes=[Buf()])
                    fw.dma("sync", b_d[1][k2, :, :], bo[e][:, 1, :], reads=[bob[e]], writes=[Buf()])
            fw.barrier()
            if stop == "S3b3":
                return nc

            bin_ = [[SB(st, "bin%d_%d" % (i, r), [128, 16, 512], BF16) for r in range(2)] for i in range(2)]; binb = [Buf(), Buf()]
            yo = [SB(st, "fyo%d" % i, [16, 512], F32) for i in range(2)]; yob = [Buf(), Buf()]
            yv = y_d.rearrange("(a b) c -> a b c", b=128)
            it = 0
            for cb in range(2):
                cs = slice(cb * 512, (cb + 1) * 512)
                for nb in range(8):
                    k = (cb * 8 + nb) % 2
                    for r in range(2):
                        fw.dma("sync", bin_[k][r][:], b_d[r][:, nb * 16:(nb + 1) * 16, cs], reads=[dbuf("b_d")], writes=[binb[k]])
                    for nn in range(16):
                        n1 = nb * 16 + nn
                        p = it % 6; j = it % 2; it += 1
                        fw.op("tensor", lambda h, nn=nn, p=p: h.matmul(PS[p][0:16, :], i2re[:], bin_[k][0][:, nn, :], start=True, stop=False), reads=[FB, binb[k]], writes=[PSB[p]])
                        fw.op("tensor", lambda h, nn=nn, p=p: h.matmul(PS[p][0:16, :], i2im[:], bin_[k][1][:, nn, :], start=False, stop=True), reads=[FB, binb[k]], writes=[PSB[p]])
                        fw.op("scalar", lambda h, p=p, j=j: h.copy(out=yo[j][:], in_=PS[p][0:16, :]), reads=[PSB[p]], writes=[yob[j]])
                        fw.dma("sync", yv[:, n1, cs], yo[j][:], reads=[yob[j]], writes=[dbuf("y_d")])
        persist.close()
        fw.barrier()

        if stop == "S3b":
            return nc
        def wload(st, name, src, nchunk, ncol):
            t = SB(st, name, [128, nchunk, ncol], BF16); b = Buf()
            sv = src.rearrange("(c p) n -> p c n", p=128)
            for c0 in range(0, ncol, 512):
                fw.dma("gpsimd", t[:, :, c0:c0 + 512], sv[:, :, c0:c0 + 512], writes=[b])
            return t, b

        with ExitStack() as st:
            Wa, Wab = wload(st, "Wa", w_attn_o, 8, D)
            Wh, Whb = wload(st, "Wh", w_hy_o, 8, D)
            yt = [SB(st, "yt%d" % i, [128, C], F32) for i in range(2)]
            x0t = [SB(st, "x0t%d" % i, [128, C], BF16) for i in range(2)]
            gt = [SB(st, "gt%d" % i, [128, 4096], BF16) for i in range(2)]
            atT = [SB(st, "atT%d" % i, [128, 8, 128], BF16) for i in range(2)]; inb = [Buf(), Buf()]
            yh16 = SB(st, "yh16", [128, C], BF16); yhb = Buf()
            yhT = SB(st, "yhT", [128, 8, 128], BF16); yhTb = Buf()
            ya = SB(st, "ya", [128, 512], F32); yab = Buf()
            pre16 = SB(st, "pre16", [128, D], BF16); preb = Buf()
            preT = SB(st, "preTs", [128, 16, 128], BF16); preTb = Buf()
            preTv = preT_d.rearrange("(c p) t -> p c t", p=128)
            for t in range(16):
                k = t % 2
                rows = slice(t * 128, (t + 1) * 128)
                fw.dma("sync", yt[k][:], y_d[rows, :], reads=[dbuf("y_d")], writes=[inb[k]])
                fw.dma("sync", x0t[k][:], x0_d[rows, :], reads=[dbuf("x0_d")], writes=[inb[k]])
                fw.dma("sync", gt[k][:], g_d[rows, :], reads=[dbuf("g_d")], writes=[inb[k]])
                fw.dma("sync", atT[k][:], at_d[:, :, rows], reads=[dbuf("at_d")], writes=[inb[k]])
                fw.op("vector", lambda h: h.tensor_tensor(out=yh16[:], in0=yt[k][:], in1=x0t[k][:], op=ALU.mult), reads=[inb[k]], writes=[yhb])
                transpose_to(yh16, yhb, yhT, yhTb, 8)
                for cb in range(4):
                    cs = slice(cb * 512, (cb + 1) * 512)
                    pa = (cb % 3) * 2; ph = pa + 1
                    for c in range(8):
                        fw.op("tensor", lambda h, c=c, pa=pa: h.matmul(PS[pa][:], atT[k][:, c, :], Wa[:, c, cs], start=(c == 0), stop=(c == 7)),
                              reads=[inb[k], Wab], writes=[PSB[pa]])
                    for c in range(8):
                        fw.op("tensor", lambda h, c=c, ph=ph: h.matmul(PS[ph][:], yhT[:, c, :], Wh[:, c, cs], start=(c == 0), stop=(c == 7)),
                              reads=[yhTb, Whb], writes=[PSB[ph]])
                    fw.op("vector", lambda h, pa=pa: h.tensor_tensor(out=ya[:], in0=PS[pa][:], in1=gt[k][:, cs], op=ALU.mult), reads=[PSB[pa], inb[k]], writes=[yab])
                    fw.op("vector", lambda h, ph=ph: h.tensor_tensor(out=pre16[:, cs], in0=PS[ph][:], in1=gt[k][:, 2048 + cb * 512:2048 + (cb + 1) * 512], op=ALU.mult),
                          reads=[PSB[ph], inb[k]], writes=[preb])
                    fw.op("vector", lambda h: h.tensor_tensor(out=pre16[:, cs], in0=pre16[:, cs], in1=ya[:], op=ALU.add), reads=[preb, yab], writes=[preb])
                transpose_to(pre16, preb, preT, preTb, 16)
                fw.dma("gpsimd", preTv[:, :, rows], preT[:], reads=[preTb], writes=[dbuf("preT_d")])
        fw.barrier()

        if stop == "S4a":
            return nc
        with ExitStack() as st:
            Wo, Wob = wload(st, "Wo", w_out, 16, D)
            g1, g1b = rep_load(st, "g1", ln1_g, D); b1, b1b = rep_load(st, "b1", ln1_b, D)
            gbb = Buf()
            fw.op("vector", lambda h: h.tensor_copy(out=g1[:, 0:1], in_=g1[:, 0:1]), reads=[g1b, b1b], writes=[gbb])
            pT_ = [SB(st, "ppT%d" % i, [128, 16, 128], BF16) for i in range(2)]
            hres = [SB(st, "hres%d" % i, [128, D], F32) for i in range(2)]; inb = [Buf(), Buf()]
            h1t = [SB(st, "h1t%d" % i, [128, D], F32) for i in range(2)]; h1b_ = [Buf(), Buf()]
            h116 = SB(st, "h116", [128, D], BF16); h116b = Buf()
            h1Ts = SB(st, "h1Ts", [128, 16, 128], BF16); h1Tb = Buf()
            st8 = SB(st, "st8b", [128, 24], F32); sb8 = Buf(); mv = SB(st, "mvb", [128, 2], F32); mvb = Buf(); rstd = SB(st, "rstdb", [128, 1], F32)
            preTv = preT_d.rearrange("(c p) t -> p c t", p=128)
            h1Tv = h1T_d.rearrange("(c p) t -> p c t", p=128)
            for t in range(16):
                k = t % 2
                rows = slice(t * 128, (t + 1) * 128)
                fw.dma("sync", pT_[k][:], preTv[:, :, rows], reads=[dbuf("preT_d")], writes=[inb[k]])
                fw.dma("sync", hres[k][:], ho_d[128 * t + 1:128 * t + 129, :], reads=[dbuf("ho_d")], writes=[inb[k]])
                for cb in range(4):
                    cs = slice(cb * 512, (cb + 1) * 512)
                    p = (t * 4 + cb) % 6
                    for c in range(16):
                        fw.op("tensor", lambda h, c=c, p=p: h.matmul(PS[p][:], pT_[k][:, c, :], Wo[:, c, cs], start=(c == 0), stop=(c == 15)),
                              reads=[inb[k], Wob], writes=[PSB[p]])
                    fw.op("vector", lambda h, p=p: h.scalar_tensor_tensor(out=h1t[k][:, cs], in0=hres[k][:, cs], scalar=ALPHA, in1=PS[p][:], op0=ALU.mult, op1=ALU.add),
                          reads=[inb[k], PSB[p]], writes=[h1b_[k]])
                layernorm(h1t[k], h1b_[k], g1, b1, gbb, st8, sb8, mv, mvb, rstd)
                fw.dma("gpsimd", h1_d[rows, :], h1t[k][:], reads=[h1b_[k]], writes=[dbuf("h1_d")])
                fw.op("scalar", lambda h: h.copy(out=h116[:], in_=h1t[k][:]), reads=[h1b_[k]], writes=[h116b])
                transpose_to(h116, h116b, h1Ts, h1Tb, 16)
                fw.dma("gpsimd", h1Tv[:, :, rows], h1Ts[:], reads=[h1Tb], writes=[dbuf("h1T_d")])
        fw.barrier()

        if stop == "S4b":
            return nc
        persist2 = ExitStack()
        top.enter_context(persist2)
        I32 = mybir.dt.int32
        OH1 = SB(persist2, "OH1", [128, 16, 32], F32); OH2 = SB(persist2, "OH2", [128, 16, 32], F32)
        G12 = SB(persist2, "G12", [128, 16, 2], F32); Gb = Buf()
        SL1 = SB(persist2, "SL1", [128, 16], I32); SL2 = SB(persist2, "SL2", [128, 16], I32); SLb = Buf()
        IG = SB(persist2, "IG", [128, 64, 16], I32); ID = SB(persist2, "ID", [128, 64, 8], I32); IXb = Buf()
        zt16 = SB(persist2, "zt16", [128, D], BF16); ztb16 = Buf()
        fw.op("gpsimd", lambda h: h.memset(zt16[:], 0.0), writes=[ztb16])
        for i in range(64):
            fw.dma("sync", xs_d[i * 128:(i + 1) * 128, :], zt16[:], reads=[ztb16], writes=[Buf()])
        h1Tv = h1T_d.rearrange("(c p) t -> p c t", p=128)
        BIG = 1.0e30
        with ExitStack() as st:
            Wr = SB(st, "Wr", [128, 16, 36], BF16); Wrb = Buf()
            fw.dma("gpsimd", Wr[:], w_route.rearrange("(c p) n -> p c n", p=128), writes=[Wrb])
            br16 = SB(st, "br16", [1, 36], BF16)
            fw.dma("gpsimd", br16[:], b_route, writes=[Wrb])
            hT_ = [SB(st, "rhT%d" % i, [128, 16, 128], BF16) for i in range(2)]; inb = [Buf(), Buf()]
            lg = SB(st, "lg", [128, 36], F32); lgb = Buf()
            sm = SB(st, "rsm", [128, 16], F32); smb = Buf()
            oh = SB(st, "roh", [128, 96], F32); ohb = Buf()
            for t in range(16):
                k = t % 2
                fw.dma("sync", hT_[k][:], h1Tv[:, :, t * 128:(t + 1) * 128], reads=[dbuf("h1T_d")], writes=[inb[k]])
                p = t % 6
                for c in range(16):
                    fw.op("tensor", lambda h, c=c, p=p: h.matmul(PS[p][:, 0:36], hT_[k][:, c, :], Wr[:, c, :], start=(c == 0), stop=False), reads=[inb[k], Wrb], writes=[PSB[p]])
                fw.op("tensor", lambda h, p=p: h.matmul(PS[p][:, 0:36], ones16[0:1, :], br16[:], start=False, stop=True), reads=[Wrb, Bc], writes=[PSB[p]])
                fw.op("vector", lambda h, p=p: h.tensor_copy(out=lg[:], in_=PS[p][:, 0:36]), reads=[PSB[p]], writes=[lgb])
                V = lambda fn, r, w: fw.op("vector", fn, reads=r, writes=w)
                V(lambda h: h.tensor_reduce(out=sm[:, 0:1], in_=lg[:, 0:4], axis=mybir.AxisListType.X, op=ALU.max), [lgb], [smb])
                V(lambda h: h.tensor_scalar(out=oh[:, 0:4], in0=lg[:, 0:4], scalar1=sm[:, 0:1], scalar2=None, op0=ALU.is_equal), [lgb, smb], [ohb])
                V(lambda h: h.tensor_scalar(out=oh[:, 4:8], in0=lg[:, 0:4], scalar1=sm[:, 0:1], scalar2=None, op0=ALU.subtract), [lgb, smb], [ohb])
                fw.op("scalar", lambda h: h.activation(out=oh[:, 4:8], in_=oh[:, 4:8], func=AF.Exp, accum_out=sm[:, 1:2]), reads=[ohb, smb], writes=[ohb, smb])
                V(lambda h: h.reciprocal(out=sm[:, 2:3], in_=sm[:, 1:2]), [smb], [smb])
                V(lambda h: h.tensor_scalar(out=oh[:, 8:12], in0=oh[:, 0:4], scalar1=-1.0, scalar2=BIG, op0=ALU.add, op1=ALU.mult), [ohb], [ohb])
                for g_ in range(4):
                    V(lambda h, g_=g_: h.tensor_scalar(out=oh[:, 32 + 8 * g_:40 + 8 * g_], in0=lg[:, 4 + 8 * g_:12 + 8 * g_], scalar1=oh[:, 8 + g_:9 + g_], scalar2=None, op0=ALU.add),
                      [lgb, ohb], [ohb])
                V(lambda h: h.tensor_reduce(out=sm[:, 3:4], in_=oh[:, 32:64], axis=mybir.AxisListType.X, op=ALU.max), [ohb], [smb])
                V(lambda h: h.tensor_scalar(out=oh[:, 64:96], in0=oh[:, 32:64], scalar1=sm[:, 3:4], scalar2=None, op0=ALU.is_equal), [ohb, smb], [ohb])
                V(lambda h: h.scalar_tensor_tensor(out=oh[:, 32:64], in0=oh[:, 64:96], scalar=-BIG, in1=oh[:, 32:64], op0=ALU.mult, op1=ALU.add), [ohb], [ohb])
                V(lambda h: h.tensor_reduce(out=sm[:, 4:5], in_=oh[:, 32:64], axis=mybir.AxisListType.X, op=ALU.max), [ohb], [smb])
                V(lambda h: h.tensor_scalar(out=oh[:, 32:64], in0=oh[:, 32:64], scalar1=sm[:, 4:5], scalar2=None, op0=ALU.is_equal), [ohb, smb], [ohb])
                V(lambda h: h.tensor_tensor(out=sm[:, 5:6], in0=sm[:, 4:5], in1=sm[:, 3:4], op=ALU.subtract), [smb], [smb])
                fw.op("scalar", lambda h: h.activation(out=sm[:, 6:7], in_=sm[:, 5:6], func=AF.Exp), reads=[smb], writes=[smb])
                V(lambda h: h.tensor_scalar(out=sm[:, 7:8], in0=sm[:, 6:7], scalar1=1.0, scalar2=None, op0=ALU.add), [smb], [smb])
                V(lambda h: h.reciprocal(out=sm[:, 8:9], in_=sm[:, 7:8]), [smb], [smb])
                V(lambda h: h.tensor_tensor(out=sm[:, 9:10], in0=sm[:, 8:9], in1=sm[:, 2:3], op=ALU.mult), [smb], [smb])
                V(lambda h: h.tensor_tensor(out=sm[:, 10:11], in0=sm[:, 9:10], in1=sm[:, 6:7], op=ALU.mult), [smb], [smb])
                V(lambda h, t=t: h.tensor_copy(out=OH1[:, t, :], in_=oh[:, 64:96]), [ohb], [Gb])
                V(lambda h, t=t: h.tensor_copy(out=OH2[:, t, :], in_=oh[:, 32:64]), [ohb], [Gb])
                V(lambda h, t=t: h.tensor_copy(out=G12[:, t, :], in_=sm[:, 9:11]), [smb], [Gb])
            A = SB(st, "mA", [128, 16, 32], F32); Ab = Buf()
            R = SB(st, "mR", [128, 16, 32], F32); Rb = Buf()
            U = SB(st, "mU", [128, 128], F32); Ub = Buf()
            base = SB(st, "mbase", [128, 32], F32); baseb = Buf()
            ci = SB(st, "mci", [128, 32], I32); padf = SB(st, "mpadf", [128, 32], F32); pe = SB(st, "mpe", [128, 32], F32)
            pst = SB(st, "mpst", [128, 32], F32); one32 = SB(st, "mone32", [128, 32], F32); pb_ = Buf()
            sf = SB(st, "msf", [128, 32], F32); eb = SB(st, "meb", [128, 64], F32); junk32 = SB(st, "mj32", [128, 32], F32); ebb = Buf()
            igf = SB(st, "migf", [128, 64, 16], F32); bG = SB(st, "mbG", [128, 64], F32); bD = SB(st, "mbD", [128, 64], F32)
            pc = SB(st, "mpc", [128, 1], F32)
            fw.dma("sync", pc[:], pcol, writes=[ebb])
            V(lambda h: h.tensor_tensor(out=A[:], in0=OH1[:], in1=OH2[:], op=ALU.add), [Gb], [Ab])
            fw.op("gpsimd", lambda h: h.memset(U[:], 1.0), writes=[Ub])
            fw.op("gpsimd", lambda h: h.affine_select(out=U[:], in_=U[:], pattern=[[1, 128]], compare_op=ALU.is_gt, fill=0.0, base=0, channel_multiplier=-1),
                  reads=[Ub], writes=[Ub])
            V(lambda h: h.memset(base[:], 0.0), [], [baseb])
            V(lambda h: h.memset(one32[:], 1.0), [], [pb_])
            for i in range(16):
                pa = (2 * i) % 6; pb2 = (2 * i + 1) % 6
                fw.op("tensor", lambda h, i=i, pa=pa: h.matmul(PS[pa][:, 0:32], U[:], A[:, i, :], start=True, stop=True), reads=[Ub, Ab], writes=[PSB[pa]])
                fw.op("tensor", lambda h, i=i, pb2=pb2: h.matmul(PS[pb2][:, 0:32], onesf[:], A[:, i, :], start=True, stop=True), reads=[Bc, Ab], writes=[PSB[pb2]])
                V(lambda h, i=i, pa=pa: h.tensor_tensor(out=R[:, i, :], in0=PS[pa][:, 0:32], in1=base[:], op=ALU.add), [PSB[pa], baseb], [Rb])
                V(lambda h, pb2=pb2: h.tensor_tensor(out=base[:], in0=PS[pb2][:, 0:32], in1=base[:], op=ALU.add), [PSB[pb2], baseb], [baseb])
            V(lambda h: h.tensor_scalar(out=ci[:], in0=base[:], scalar1=127.0, scalar2=None, op0=ALU.add), [baseb], [pb_])
            V(lambda h: h.tensor_scalar(out=ci[:], in0=ci[:], scalar1=7, scalar2=7, op0=ALU.arith_shift_right, op1=ALU.logical_shift_left), [pb_], [pb_])
            V(lambda h: h.tensor_copy(out=padf[:], in_=ci[:]), [pb_], [pb_])
            V(lambda h: h.tensor_tensor_scan(out=pe[:], data0=one32[:], data1=padf[:], initial=0.0, op0=ALU.mult, op1=ALU.add), [pb_], [pb_])
            V(lambda h: h.tensor_tensor(out=pst[:], in0=pe[:], in1=padf[:], op=ALU.subtract), [pb_], [pb_])
            for i in range(16):
                V(lambda h, i=i: h.tensor_tensor(out=R[:, i, :], in0=R[:, i, :], in1=pst[:], op=ALU.add), [Rb, pb_], [Rb])
            for OH, SL, col in ((OH1, SL1, 0), (OH2, SL2, 1)):
                V(lambda h, OH=OH: h.tensor_tensor(out=A[:], in0=R[:], in1=OH[:], op=ALU.mult), [Rb, Gb, Ab], [Ab])
                V(lambda h: h.tensor_reduce(out=sf[:, 0:16], in_=A[:], axis=mybir.AxisListType.X, op=ALU.add), [Ab], [ebb])
                V(lambda h, SL=SL: h.tensor_copy(out=SL[:], in_=sf[:, 0:16]), [ebb], [SLb])
            for i in range(64):
                V(lambda h, i=i: h.tensor_scalar(out=junk32[:], in0=pe[:], scalar1=128.0 * i, scalar2=0.0, op0=ALU.is_le, op1=ALU.add, accum_out=eb[:, i:i + 1]),
                  [pb_], [ebb])
            V(lambda h: h.tensor_scalar(out=bG[:], in0=eb[:], scalar1=2048.0, scalar2=pc[:, 0:1], op0=ALU.mult, op1=ALU.add), [ebb], [ebb])
            V(lambda h: h.tensor_scalar(out=bD[:], in0=eb[:], scalar1=1024.0, scalar2=pc[:, 0:1], op0=ALU.mult, op1=ALU.add), [ebb], [ebb])
            for c in range(16):
                V(lambda h, c=c: h.tensor_scalar(out=igf[:, :, c], in0=bG[:], scalar1=128.0 * c, scalar2=None, op0=ALU.add), [ebb], [ebb])
            V(lambda h: h.tensor_copy(out=IG[:], in_=igf[:]), [ebb], [IXb])
            for c in range(8):
                V(lambda h, c=c: h.tensor_scalar(out=igf[:, :, c], in0=bD[:], scalar1=128.0 * c, scalar2=None, op0=ALU.add), [ebb, IXb], [ebb])
            V(lambda h: h.tensor_copy(out=ID[:], in_=igf[:, :, 0:8]), [ebb], [IXb])
        fw.barrier()

        if stop == "S5":
            return nc
        with ExitStack() as st:
            hl = [SB(st, "s_hl%d" % i, [128, D], F32) for i in range(2)]; hlb = [Buf(), Buf()]
            h16_ = [SB(st, "s_h16%d" % i, [128, D], BF16) for i in range(2)]; h16b_ = [Buf(), Buf()]
            for t in range(16):
                k = t % 2
                fw.dma("sync", hl[k][:], h1_d[t * 128:(t + 1) * 128, :], writes=[hlb[k]])
                fw.op("scalar", lambda h: h.copy(out=h16_[k][:], in_=hl[k][:]), reads=[hlb[k]], writes=[h16b_[k]])
                fw.idma(xs_d[:, :], h16_[k][:, :], SL1[:, t:t + 1], False, 8191, reads=[h16b_[k], SLb], writes=[Buf()])
                fw.idma(xs_d[:, :], h16_[k][:, :], SL2[:, t:t + 1], False, 8191, reads=[h16b_[k], SLb], writes=[Buf()])
        fw.barrier()

        if stop == "S6a":
            return nc
        weg = w_eg.rearrange("e k n -> (e k) n"); weu = w_eu.rearrange("e k n -> (e k) n"); wed = w_ed.rearrange("e k n -> (e k) n")
        with ExitStack() as st:
            NW = 8
            xb = [SB(st, "b_xb%d" % i, [128, D], BF16) for i in range(2)]; xbb = [Buf(), Buf()]
            xT = [SB(st, "b_xT%d" % i, [128, 16, 128], BF16) for i in range(2)]; xTb = [Buf(), Buf()]
            wgt = [SB(st, "b_wg%d" % i, [128, 1024], BF16) for i in range(NW)]; wgtb = [Buf() for _ in range(NW)]
            wut = [SB(st, "b_wu%d" % i, [128, 1024], BF16) for i in range(NW)]; wutb = [Buf() for _ in range(NW)]
            wdt = [[SB(st, "b_wd%d_%d" % (i, c), [128, D], BF16) for c in range(8)] for i in range(2)]
            wdtb = [[Buf() for c in range(8)] for i in range(2)]
            sg = [SB(st, "b_sg%d" % i, [128, 512], F32) for i in range(2)]; sgb = [Buf(), Buf()]
            Hs = [SB(st, "b_H%d" % i, [128, 1024], BF16) for i in range(2)]; Hsb = [Buf(), Buf()]
            HT = [SB(st, "b_HT%d" % i, [128, 8, 128], BF16) for i in range(2)]; HTb = [Buf(), Buf()]
            Ys = [SB(st, "b_Y%d" % i, [128, D], F32) for i in range(2)]; Ysb = [Buf(), Buf()]
            iw = 0
            for i in range(64):
                k = i % 2
                fw.dma("sync", xb[k][:], xs_d[i * 128:(i + 1) * 128, :], writes=[xbb[k]])
                transpose_to(xb[k], xbb[k], xT[k], xTb[k], 16)
                for c in range(16):
                    j = iw % NW; iw += 1
                    fw.idma(wgt[j][:, :], weg[:, :], IG[:, i, c:c + 1], True, 32 * 2048 - 1, reads=[IXb], writes=[wgtb[j]])
                    fw.idma(wut[j][:, :], weu[:, :], IG[:, i, c:c + 1], True, 32 * 2048 - 1, reads=[IXb], writes=[wutb[j]])
                    for hf_ in range(2):
                        fw.op("tensor", lambda h, c=c, j=j, hf_=hf_: h.matmul(PS[hf_][:], xT[k][:, c, :], wgt[j][:, hf_ * 512:(hf_ + 1) * 512], start=(c == 0), stop=(c == 15)),
                              reads=[xTb[k], wgtb[j]], writes=[PSB[hf_]])
                    for hf_ in range(2):
                        fw.op("tensor", lambda h, c=c, j=j, hf_=hf_: h.matmul(PS[2 + hf_][:], xT[k][:, c, :], wut[j][:, hf_ * 512:(hf_ + 1) * 512], start=(c == 0), stop=(c == 15)),
                              reads=[xTb[k], wutb[j]], writes=[PSB[2 + hf_]])
                for c in range(8):
                    fw.idma(wdt[k][c][:, :], wed[:, :], ID[:, i, c:c + 1], True, 32 * 1024 - 1, reads=[IXb], writes=[wdtb[k][c]])
                for hf_ in range(2):
                    fw.op("scalar", lambda h, hf_=hf_: h.activation(out=sg[hf_][:], in_=PS[hf_][:], func=AF.Silu), reads=[PSB[hf_]], writes=[sgb[hf_]])
                    fw.op("vector", lambda h, hf_=hf_: h.tensor_tensor(out=Hs[k][:, hf_ * 512:(hf_ + 1) * 512], in0=PS[2 + hf_][:], in1=sg[hf_][:], op=ALU.mult),
                          reads=[PSB[2 + hf_], sgb[hf_]], writes=[Hsb[k]])
                transpose_to(Hs[k], Hsb[k], HT[k], HTb[k], 8)
                for hf_ in range(2):
                    for c in range(8):
                        for q in range(2):
                            fw.op("tensor", lambda h, c=c, q=q, hf_=hf_: h.matmul(PS[4 + q][:], HT[k][:, c, :], wdt[k][c][:, hf_ * 1024 + q * 512:hf_ * 1024 + (q + 1) * 512],
                                                                                  start=(c == 0), stop=(c == 7)), reads=[HTb[k], wdtb[k][c]], writes=[PSB[4 + q]])
                    fw.op("scalar", lambda h, hf_=hf_: h.copy(out=Ys[k][:, hf_ * 1024:hf_ * 1024 + 512], in_=PS[4][:]), reads=[PSB[4]], writes=[Ysb[k]])
                    fw.op("vector", lambda h, hf_=hf_: h.tensor_copy(out=Ys[k][:, hf_ * 1024 + 512:(hf_ + 1) * 1024], in_=PS[5][:]), reads=[PSB[5]], writes=[Ysb[k]])
                fw.dma("sync", ys_d[i * 128:(i + 1) * 128, :], Ys[k][:], reads=[Ysb[k]], writes=[Buf()])
        fw.barrier()

        if stop == "S6b":
            return nc
        with ExitStack() as st:
            g2, g2b = rep_load(st, "g2", ln2_g, D); b2, b2b = rep_load(st, "b2", ln2_b, D)
            gbb = Buf()
            fw.op("vector", lambda h: h.tensor_copy(out=g2[:, 0:1], in_=g2[:, 0:1]), reads=[g2b, b2b], writes=[gbb])
            ht = [SB(st, "f_h%d" % i, [128, D], F32) for i in range(2)]; htb = [Buf(), Buf()]
            y1 = [SB(st, "f_y1%d" % i, [128, D], F32) for i in range(2)]; y1b = [Buf(), Buf()]
            y2 = [SB(st, "f_y2%d" % i, [128, D], F32) for i in range(2)]; y2b = [Buf(), Buf()]
            st8 = SB(st, "st8c", [128, 24], F32); sb8 = Buf(); mv = SB(st, "mvc", [128, 2], F32); mvb = Buf(); rstd = SB(st, "rstdc", [128, 1], F32)
            for t in range(16):
                k = t % 2
                rows = slice(t * 128, (t + 1) * 128)
                fw.dma("sync", ht[k][:], h1_d[rows, :], writes=[htb[k]])
                fw.idma(y1[k][:, :], ys_d[:, :], SL1[:, t:t + 1], True, 8191, reads=[SLb], writes=[y1b[k]])
                fw.idma(y2[k][:, :], ys_d[:, :], SL2[:, t:t + 1], True, 8191, reads=[SLb], writes=[y2b[k]])
                fw.op("vector", lambda h: h.tensor_scalar(out=ht[k][:], in0=ht[k][:], scalar1=ALPHA, scalar2=None, op0=ALU.mult), reads=[htb[k]], writes=[htb[k]])
                fw.op("vector", lambda h, t=t: h.scalar_tensor_tensor(out=ht[k][:], in0=y1[k][:], scalar=G12[:, t, 0:1], in1=ht[k][:], op0=ALU.mult, op1=ALU.add),
                      reads=[y1b[k], htb[k], Gb], writes=[htb[k]])
                fw.op("vector", lambda h, t=t: h.scalar_tensor_tensor(out=ht[k][:], in0=y2[k][:], scalar=G12[:, t, 1:2], in1=ht[k][:], op0=ALU.mult, op1=ALU.add),
                      reads=[y2b[k], htb[k], Gb], writes=[htb[k]])
                layernorm(ht[k], htb[k], g2, b2, gbb, st8, sb8, mv, mvb, rstd)
                fw.dma("sync", out[rows, :], ht[k][:], reads=[htb[k]], writes=[Buf()])
        persist2.close()
        fw.barrier()
    return nc


_CONST = {}


def _consts():
    if _CONST:
        return _CONST
    f32 = np.float32
    half = 64
    inv_freq = (10000.0 ** (-np.arange(0, half, 2, dtype=np.float64) / half)).astype(f32)
    row_idx = (np.arange(S) // 64).astype(f32)
    col_idx = (np.arange(S) % 64).astype(f32)
    ang_r = (row_idx[:, None] * inv_freq[None, :]).astype(f32)
    ang_c = (col_idx[:, None] * inv_freq[None, :]).astype(f32)
    cr, sr, cc, sc = np.cos(ang_r), np.sin(ang_r), np.cos(ang_c), np.sin(ang_c)
    _CONST["ropeC"] = np.concatenate([cr, cr, cc, cc], axis=1).astype(f32)
    _CONST["ropeS"] = np.concatenate([-sr, sr, -sc, sc], axis=1).astype(f32)
    n = np.arange(128, dtype=np.float64)
    a128 = 2 * np.pi * np.outer(n, n) / 128.0
    _CONST["Fc"] = np.cos(a128).astype(f32); _CONST["Fs"] = np.sin(a128).astype(f32)
    a16 = 2 * np.pi * np.outer(n, n) / float(NFFT)
    _CONST["C16"] = np.cos(a16).astype(f32); _CONST["S16"] = np.sin(a16).astype(f32)
    t = np.linspace(0.0, 1.0, S, dtype=f32)
    w = (2.0 * math.pi * np.arange(S, dtype=f32) / S).astype(f32)[:, None]
    bands = np.linspace(1e-4, 15, 16, dtype=f32)[None, :]
    feats = np.concatenate([t[:, None], np.cos(bands * w), -np.sin(bands * w)], axis=-1).astype(f32)
    _CONST["featsT"] = np.ascontiguousarray(feats.T)
    _CONST["negt"] = np.ascontiguousarray((-t).reshape(64, 128).T)
    min_decay = math.log(1e-2) / 1.5
    max_decay = math.log(1e-2) / 0.3
    _CONST["absdelta"] = np.abs(np.linspace(min_decay, max_decay, C, dtype=f32)).reshape(1, C).astype(f32)
    import ml_dtypes
    n1 = np.arange(128, dtype=np.float64)[None, :, None]
    k1 = np.arange(128, dtype=np.float64)[None, None, :]
    k2 = np.arange(128, dtype=np.float64)[:, None, None]
    th = 2 * np.pi * n1 * (128.0 * k1 + k2) / float(NFFT)
    ec, es = np.cos(th), np.sin(th)
    ect, est = ec.transpose(0, 2, 1), es.transpose(0, 2, 1)
    _CONST["cE"] = np.ascontiguousarray(np.stack([ec, es, -es, -ec, ect, est, -est], axis=2)).astype(ml_dtypes.bfloat16)
    m0 = np.ones((128, 1), f32); m0[0, 0] = 0.0
    _CONST["m0"] = m0
    return _CONST


def _in_maps(inp):
    cs = _consts()
    f32 = np.float32
    x = np.asarray(inp["x"], f32)
    common = {
        "ln_in_g": inp["ln_in_g"].reshape(1, D), "ln_in_b": inp["ln_in_b"].reshape(1, D),
        "w_in": inp["w_in"][0], "b_gate": inp["b_gate"][0].reshape(1, 4096),
        "q_norm_g": inp["q_norm_g"][0].reshape(1, 128), "k_norm_g": inp["k_norm_g"][0].reshape(1, 128),
        "hy_conv_w": inp["hy_conv_w"][0], "hy_conv_b": inp["hy_conv_b"][0].reshape(1, 3072),
        "filt_w1": inp["filt_w1"][0], "filt_b1": inp["filt_b1"][0].reshape(64, 1), "filt_f1": inp["filt_f1"][0].reshape(64, 1),
        "filt_w2": inp["filt_w2"][0], "filt_b2": inp["filt_b2"][0].reshape(64, 1), "filt_f2": inp["filt_f2"][0].reshape(64, 1),
        "filt_w3": inp["filt_w3"][0], "hy_bias_d": inp["hy_bias_d"][0].reshape(1, C),
        "w_attn_o": inp["w_attn_o"][0], "w_hy_o": inp["w_hy_o"][0], "w_out": inp["w_out"][0],
        "ln1_g": inp["ln1_g"][0].reshape(1, D), "ln1_b": inp["ln1_b"][0].reshape(1, D),
        "w_route": np.concatenate([inp["w_route_grp"][0], inp["w_route_exp"][0]], axis=1),
        "b_route": np.concatenate([inp["b_route_grp"][0], inp["b_route_exp"][0]], axis=0).reshape(1, 36),
        "w_eg": inp["w_exp_gate"][0], "w_eu": inp["w_exp_up"][0], "w_ed": inp["w_exp_down"][0],
        "ln2_g": inp["ln2_g"][0].reshape(1, D), "ln2_b": inp["ln2_b"][0].reshape(1, D),
        "ropeC_k": cs["ropeC"], "ropeS_k": cs["ropeS"],
        "cFc": cs["Fc"], "cFs": cs["Fs"], "cFsn": -cs["Fs"], "cFcn": -cs["Fc"],
        "cC16": cs["C16"], "cS16": cs["S16"], "cS16n": -cs["S16"],
        "pcol": np.arange(128, dtype=np.float32).reshape(128, 1), "featsT": cs["featsT"], "negt": cs["negt"], "absdelta": cs["absdelta"], "m0": cs["m0"],
    }
    common = {k: np.ascontiguousarray(np.asarray(v, f32)) for k, v in common.items()}
    common["cE"] = cs["cE"]
    maps = []
    for c in range(8):
        b, j = c // 4, c % 4
        q0 = j * T
        xo = np.zeros((NOWN, D), f32); mo = np.zeros((NOWN, 1), f32)
        lo = max(q0 - 1, 0); hi = min(q0 + T + 1, S)
        r0 = lo - (q0 - 1)
        xo[r0:r0 + (hi - lo)] = x[b, lo:hi]
        mo[r0:r0 + (hi - lo)] = 1.0
        m = dict(common)
        m["x_seq"] = np.ascontiguousarray(x[b]); m["x_own"] = xo; m["m_own"] = np.ascontiguousarray(mo.reshape(17, 128).T)
        m["ropeC_q"] = np.ascontiguousarray(cs["ropeC"][q0:q0 + T]); m["ropeS_q"] = np.ascontiguousarray(cs["ropeS"][q0:q0 + T])
        n2 = np.arange(16 * j, 16 * j + 16)
        m["ci2re"] = np.ascontiguousarray(cs["Fc"][:, n2] / float(NFFT)).astype(f32)
        m["ci2im"] = np.ascontiguousarray(-cs["Fs"][:, n2] / float(NFFT)).astype(f32)
        maps.append(m)
    return maps


def kernel(**inputs):
    inp = {k: np.asarray(v) for k, v in inputs.items()}
    nc = build_nc()
    maps = _in_maps(inp)
    res = run_bass_kernel_spmd(nc, maps, core_ids=list(range(8)))
    outp = np.zeros((2, S, D), np.float32)
    for c in range(8):
        b, j = c // 4, c % 4
        outp[b, j * T:(j + 1) * T] = res.results[c]["out"]
    return outp
```

```python
import math
from contextlib import ExitStack
import numpy as np
import concourse.bass as bass
import concourse.mybir as mybir
from concourse.bass_utils import run_bass_kernel_spmd

F32 = mybir.dt.float32
BF16 = mybir.dt.bfloat16
ALU = mybir.AluOpType
AF = mybir.ActivationFunctionType

D = 2048
S = 8192
T = 2048
NOWN = 17 * 128
C = 1024
INW = 8704
ALPHA = 2.0 ** 0.25
LN_EPS = 1e-5
QK_EPS = 1e-6
FILTER_EPS = 1e-6
NFFT = 16384
PI = math.pi


class Buf:
    __slots__ = ("w", "r")

    def __init__(self):
        self.w = None
        self.r = []


class FW:
    ENGS = ("tensor", "vector", "scalar", "gpsimd", "sync")

    def __init__(self, nc, stack, ndma=20):
        self.nc = nc
        self.cnt = {e: 0 for e in self.ENGS}
        self.seen = {e: {} for e in self.ENGS}
        self.sems = {}
        self.ndma = ndma
        self.dma_next = {e: 0 for e in self.ENGS}
        self.dma_tot = {}
        for e in self.ENGS:
            self.sems[e] = stack.enter_context(nc.semaphore("s_" + e))
        for q in ("sync", "gpsimd", "scalar"):
            for i in range(ndma):
                k = ("d", q, i)
                self.sems[k] = stack.enter_context(nc.semaphore("d_%s_%d" % (q, i)))
                self.dma_tot[k] = 0

    def _waits(self, eng, reads, writes):
        deps = {}

        def add(t):
            if t is not None and deps.get(t[0], 0) < t[1]:
                deps[t[0]] = t[1]
        for b in reads:
            add(b.w)
        for b in writes:
            add(b.w)
            for t in b.r:
                add(t)
        out = []
        seen = self.seen[eng]
        for key, val in deps.items():
            if key == eng and eng == "tensor":
                continue
            if seen.get(key, 0) >= val:
                continue
            seen[key] = val
            out.append((key, val))
        return out

    def _mark(self, tag, reads, writes):
        for b in writes:
            b.w = tag
            b.r = []
        for b in reads:
            b.r = [t for t in b.r if t[0] != tag[0]] + [tag]

    def op(self, eng, fn, reads=(), writes=()):
        waits = self._waits(eng, reads, writes)
        self.cnt[eng] += 1
        tag = (eng, self.cnt[eng])
        h = getattr(self.nc, eng)
        for key, val in waits:
            h.wait_ge(self.sems[key], val)
        fn(h).then_inc(self.sems[eng], 1)
        self._mark(tag, reads, writes)

    def idma(self, out, in_, idx, gather, bound, reads=(), writes=()):
        if not hasattr(self, "bregs"):
            self.bregs = {}
        if bound not in self.bregs:
            r = self.nc.gpsimd.alloc_register("bnd%d" % bound)
            self.nc.gpsimd.reg_mov(r, bound)
            self.bregs[bound] = r
        bound = self.bregs[bound]
        if gather:
            fn = lambda h: h.indirect_dma_start(out=out, out_offset=None, in_=in_, in_offset=bass.IndirectOffsetOnAxis(ap=idx, axis=0),
                                                bounds_check=bound, oob_is_err=False)
        else:
            fn = lambda h: h.indirect_dma_start(out=out, out_offset=bass.IndirectOffsetOnAxis(ap=idx, axis=0), in_=in_, in_offset=None,
                                                bounds_check=bound, oob_is_err=False)
        self.dma("gpsimd", None, None, reads=reads, writes=writes, fn=fn)

    def dma(self, q, out, in_, reads=(), writes=(), slow=False, fn=None):
        i = self.dma_next[q]
        self.dma_next[q] = (i + 1) % self.ndma
        key = ("d", q, i)
        waits = self._waits(q, reads, writes)
        prev = self.dma_tot[key]
        if prev > 0 and self.seen[q].get(key, 0) < prev:
            self.seen[q][key] = prev
            waits.append((key, prev))
        self.dma_tot[key] = prev + 16
        tag = (key, prev + 16)
        h = getattr(self.nc, q)
        for k2, val in waits:
            h.wait_ge(self.sems[k2], val)
        if fn is not None:
            inst = fn(h)
        elif slow:
            inst = h.dma_start(out=out, in_=in_, allow_slow_non_contiguous=True)
        else:
            inst = h.dma_start(out=out, in_=in_)
        inst.then_inc(self.sems[key], 16)
        self._mark(tag, reads, writes)

    def barrier(self):
        for e in self.ENGS:
            h = getattr(self.nc, e)
            seen = self.seen[e]
            for o in self.ENGS:
                if o == e or self.cnt[o] == 0:
                    continue
                if seen.get(o, 0) < self.cnt[o]:
                    seen[o] = self.cnt[o]
                    h.wait_ge(self.sems[o], self.cnt[o])
            for k, tot in self.dma_tot.items():
                if tot > 0 and seen.get(k, 0) < tot:
                    seen[k] = tot
                    h.wait_ge(self.sems[k], tot)


def build_nc(dbg=None, stop=None):
    nc = bass.Bass("TRN2", target_bir_lowering=False)
    ins = {}

    def IN(name, shape, dt=F32):
        ins[name] = nc.dram_tensor(name, list(shape), dt, kind="ExternalInput").ap()
        return ins[name]

    x_seq = IN("x_seq", [S, D]); x_own = IN("x_own", [NOWN, D]); m_own = IN("m_own", [128, 17])
    ln_in_g = IN("ln_in_g", [1, D]); ln_in_b = IN("ln_in_b", [1, D])
    w_in = IN("w_in", [D, INW]); b_gate = IN("b_gate", [1, 4096])
    q_norm_g = IN("q_norm_g", [1, 128]); k_norm_g = IN("k_norm_g", [1, 128])
    hy_conv_w = IN("hy_conv_w", [3, 3072]); hy_conv_b = IN("hy_conv_b", [1, 3072])
    filt_w1 = IN("filt_w1", [33, 64]); filt_b1 = IN("filt_b1", [64, 1]); filt_f1 = IN("filt_f1", [64, 1])
    filt_w2 = IN("filt_w2", [64, 64]); filt_b2 = IN("filt_b2", [64, 1]); filt_f2 = IN("filt_f2", [64, 1])
    filt_w3 = IN("filt_w3", [64, 2048]); hy_bias_d = IN("hy_bias_d", [1, C])
    w_attn_o = IN("w_attn_o", [1024, D]); w_hy_o = IN("w_hy_o", [C, D]); w_out = IN("w_out", [D, D])
    ln1_g = IN("ln1_g", [1, D]); ln1_b = IN("ln1_b", [1, D])
    w_route = IN("w_route", [D, 36]); b_route = IN("b_route", [1, 36])
    w_eg = IN("w_eg", [32, D, 1024]); w_eu = IN("w_eu", [32, D, 1024]); w_ed = IN("w_ed", [32, 1024, D])
    ln2_g = IN("ln2_g", [1, D]); ln2_b = IN("ln2_b", [1, D])
    ropeC_k = IN("ropeC_k", [S, 128]); ropeS_k = IN("ropeS_k", [S, 128])
    ropeC_q = IN("ropeC_q", [T, 128]); ropeS_q = IN("ropeS_q", [T, 128])
    cFc = IN("cFc", [128, 128]); cFs = IN("cFs", [128, 128]); cFsn = IN("cFsn", [128, 128]); cFcn = IN("cFcn", [128, 128])
    cC16 = IN("cC16", [128, 128]); cS16 = IN("cS16", [128, 128]); cS16n = IN("cS16n", [128, 128])
    cE = IN("cE", [128, 128, 7, 128], BF16); ci2re = IN("ci2re", [128, 16]); ci2im = IN("ci2im", [128, 16])
    pcol = IN("pcol", [128, 1]); featsT = IN("featsT", [33, S]); negt = IN("negt", [128, 64]); absdelta = IN("absdelta", [1, C]); m0 = IN("m0", [128, 1])

    out = nc.dram_tensor("out", [T, D], F32, kind="ExternalOutput").ap()

    def DR(name, shape, dt):
        kind = "ExternalOutput" if (dbg and name in dbg) else "Internal"
        return nc.dram_tensor(name, list(shape), dt, kind=kind).ap()

    hT_d = DR("hT_d", [D, S + 2], BF16); hTo_d = DR("hTo_d", [D, NOWN], BF16); ho_d = DR("ho_d", [NOWN, D], F32)
    kT_d = DR("kT_d", [128, 2, S], BF16); v_d = DR("v_d", [S, 256], BF16); qT_d = DR("qT_d", [128, 8, T], BF16)
    p_d = DR("p_d", [S + 2, 2048], BF16); po_d = DR("po_d", [NOWN, 1024], BF16); z_d = DR("z_d", [S, C], BF16); x0_d = DR("x0_d", [T, C], BF16); g_d = DR("g_d", [T, 4096], BF16)
    at_d = DR("at_d", [128, 8, T], BF16)
    hf_d = DR("hf_d", [S, C], BF16); hg_d = DR("hg_d", [S, C], BF16)
    a_d = [[DR("a_d%d%d" % (i, r), [128, 128, C], BF16) for r in range(2)] for i in range(3)]
    hh_d = [DR("hh_d%d" % r, [128, 128, C], BF16) for r in range(2)]
    b_d = [DR("b_d%d" % r, [128, 128, C], BF16) for r in range(2)]
    y_d = DR("y_d", [T, C], F32)
    xs_d = DR("xs_d", [8192, D], BF16); ys_d = DR("ys_d", [8192, D], F32); preT_d = DR("preT_d", [D, T], BF16); h1_d = DR("h1_d", [T, D], F32); h1T_d = DR("h1T_d", [D, T], BF16)

    with ExitStack() as top:
        fw = FW(nc, top)
        Dm = {}

        def dbuf(ap_name):
            return Buf()

        uid = [0]

        def SB(st, name, shape, dt):
            uid[0] += 1
            return st.enter_context(nc.sbuf_tensor("%s_u%d" % (name, uid[0]), list(shape), dt))

        PS = [top.enter_context(nc.psum_tensor("ps%d" % i, [128, 512], F32)) for i in range(6)]
        PSB = [Buf() for _ in range(6)]
        PT = [top.enter_context(nc.psum_tensor("pt%d" % i, [128, 1024], BF16)) for i in range(2)]
        PTB = [Buf() for _ in range(2)]

        ident = SB(top, "ident", [128, 128], BF16); identf = SB(top, "identf", [128, 128], F32)
        ones16 = SB(top, "ones16", [128, 128], BF16); onesf = SB(top, "onesf", [128, 128], F32)
        Bc = Buf()
        fw.op("gpsimd", lambda h: h.memset(identf[:], 1.0), writes=[Bc])
        fw.op("gpsimd", lambda h: h.affine_select(out=identf[:], in_=identf[:], pattern=[[-1, 128]],
                                                   compare_op=ALU.is_equal, fill=0.0, base=0, channel_multiplier=1),
              reads=[Bc], writes=[Bc])
        fw.op("vector", lambda h: h.tensor_copy(out=ident[:], in_=identf[:]), reads=[Bc], writes=[Bc])
        fw.op("vector", lambda h: h.memset(ones16[:], 1.0), writes=[Bc])
        fw.op("vector", lambda h: h.memset(onesf[:], 1.0), writes=[Bc])

        def rep_load(st, name, src_row, n, dt=F32, q="sync"):
            t = SB(st, name, [128, n], dt)
            b = Buf()
            fw.dma(q, t[:], src_row.to_broadcast([128, n]), writes=[b])
            return t, b

        def layernorm(xt, xb, grep, brep, gb, st8, sb8, mv, mvb, rstd, eps=LN_EPS):
            for i in range(4):
                fw.op("vector", lambda h, i=i: h.bn_stats(out=st8[:, i * 6:(i + 1) * 6], in_=xt[:, i * 512:(i + 1) * 512]),
                      reads=[xb], writes=[sb8])
            fw.op("vector", lambda h: h.bn_aggr(out=mv[:], in_=st8[:]), reads=[sb8], writes=[mvb])
            fw.op("scalar", lambda h: h.activation(out=rstd[:], in_=mv[:, 1:2], func=AF.Sqrt, bias=eps, scale=1.0),
                  reads=[mvb], writes=[sb8])
            fw.op("vector", lambda h: h.reciprocal(out=rstd[:], in_=rstd[:]), reads=[sb8], writes=[sb8])
            fw.op("vector", lambda h: h.tensor_scalar(out=xt[:], in0=xt[:], scalar1=mv[:, 0:1], scalar2=rstd[:, 0:1],
                                                       op0=ALU.subtract, op1=ALU.mult), reads=[xb, mvb, sb8], writes=[xb])
            fw.op("vector", lambda h: h.tensor_tensor(out=xt[:], in0=xt[:], in1=grep[:], op=ALU.mult), reads=[xb, gb], writes=[xb])
            fw.op("vector", lambda h: h.tensor_tensor(out=xt[:], in0=xt[:], in1=brep[:], op=ALU.add), reads=[xb, gb], writes=[xb])

        def transpose_to(src16, srcb, dst, dstb, nchunk, col0=0, ncols=128):
            for g4 in range(0, nchunk, 8):
                n = min(8, nchunk - g4)
                pi = (g4 // 8) % 2
                for c in range(n):
                    fw.op("tensor", lambda h, c=c: h.transpose(PT[pi][:, c * 128:c * 128 + ncols],
                                                               src16[0:ncols, (g4 + c) * 128:(g4 + c + 1) * 128], ident[0:ncols, 0:ncols]),
                          reads=[srcb, Bc], writes=[PTB[pi]])
                eng = "vector" if pi == 0 else "scalar"
                if eng == "vector":
                    fw.op("vector", lambda h: h.tensor_copy(
                        out=dst[:, g4:g4 + n, col0:col0 + ncols],
                        in_=PT[pi][:, 0:n * 128].rearrange("p (c t) -> p c t", t=128)[:, :, 0:ncols]),
                        reads=[PTB[pi]], writes=[dstb])
                else:
                    fw.op("scalar", lambda h: h.copy(
                        out=dst[:, g4:g4 + n, col0:col0 + ncols],
                        in_=PT[pi][:, 0:n * 128].rearrange("p (c t) -> p c t", t=128)[:, :, 0:ncols]),
                        reads=[PTB[pi]], writes=[dstb])

        with ExitStack() as st:
            grep, gb = rep_load(st, "lng", ln_in_g, D)
            brep, bb_ = rep_load(st, "lnb", ln_in_b, D)
            gbb = Buf()
            fw.op("vector", lambda h: h.tensor_copy(out=grep[:, 0:1], in_=grep[:, 0:1]), reads=[gb, bb_], writes=[gbb])
            xts = [SB(st, "xt%d" % i, [128, D], F32) for i in range(2)]; xbs = [Buf(), Buf()]
            h16 = [SB(st, "h16_%d" % i, [128, D], BF16) for i in range(2)]; h16b = [Buf(), Buf()]
            hTs = [SB(st, "hTs%d" % i, [128, 16, 512], BF16) for i in range(2)]; hTb = [Buf(), Buf()]
            st8 = SB(st, "st8", [128, 24], F32); sb8 = Buf(); mv = SB(st, "mv", [128, 2], F32); mvb = Buf()
            rstd = SB(st, "rstd", [128, 1], F32)
            mk = SB(st, "mk", [128, 17], F32); mkb = Buf()
            zt = SB(st, "zt", [128, 16, 1], BF16); ztb = Buf()
            fw.op("vector", lambda h: h.memset(zt[:], 0.0), writes=[ztb])
            hTv = hT_d.rearrange("(c p) s -> p c s", p=128)
            fw.dma("gpsimd", hTv[:, :, 0:1], zt[:], reads=[ztb], writes=[dbuf("hT_d")], slow=True)
            fw.dma("gpsimd", hTv[:, :, S + 1:S + 2], zt[:], reads=[ztb], writes=[dbuf("hT_d")], slow=True)
            fw.dma("sync", mk[:], m_own, writes=[mkb])
            hTov = hTo_d.rearrange("(c p) s -> p c s", p=128)
            it = 0
            for grp in range(16 + 5):
                own = grp >= 16
                ntile = 4 if not own else (4 if grp < 20 else 1)
                hb = grp % 2
                for ti in range(ntile):
                    tile = (grp * 4 + ti) if not own else ((grp - 16) * 4 + ti)
                    k = it % 2; it += 1
                    src = x_seq if not own else x_own
                    fw.dma("sync", xts[k][:], src[tile * 128:(tile + 1) * 128, :], writes=[xbs[k]])
                    layernorm(xts[k], xbs[k], grep, brep, gbb, st8, sb8, mv, mvb, rstd)
                    if own:
                        fw.op("scalar", lambda h, k=k, tile=tile: h.activation(out=xts[k][:], in_=xts[k][:], func=AF.Identity,
                                                                                scale=mk[:, tile:tile + 1]),
                              reads=[xbs[k], mkb], writes=[xbs[k]])
                        fw.dma("gpsimd", ho_d[tile * 128:(tile + 1) * 128, :], xts[k][:], reads=[xbs[k]], writes=[dbuf("ho_d")])
                    fw.op("scalar", lambda h, k=k: h.copy(out=h16[k][:], in_=xts[k][:]), reads=[xbs[k]], writes=[h16b[k]])
                    transpose_to(h16[k], h16b[k], hTs[hb], hTb[hb], 16, col0=ti * 128)
                if not own:
                    fw.dma("gpsimd", hTv[:, :, 1 + grp * 512:1 + grp * 512 + 512], hTs[hb][:], reads=[hTb[hb]], writes=[dbuf("hT_d")])
                else:
                    g0 = (grp - 16) * 512
                    fw.dma("gpsimd", hTov[:, :, g0:g0 + ntile * 128], hTs[hb][:, :, 0:ntile * 128], reads=[hTb[hb]], writes=[dbuf("hTo_d")])
        fw.barrier()

        w_in_v = w_in.rearrange("(c p) n -> p c n", p=128)

        def proj_pass(st, src_v, srcname, ntok_tiles, blocks, epilogue, sh0=1, src_w=None):
            nb = len(blocks)
            wts = []
            wb = Buf()
            for bi, (col0, cidx, bias) in enumerate(blocks):
                nsh = 3 if cidx is not None else 1
                for sh in range(nsh):
                    wt = SB(st, "w_%d_%d" % (bi, sh), [128, 16, 512], BF16)
                    fw.dma("gpsimd", wt[:], w_in_v[:, :, col0:col0 + 512], writes=[wb])
                    if cidx is not None:
                        cw, cwb = rep_load(st, "cw_%d_%d" % (bi, sh), hy_conv_w[sh:sh + 1, cidx:cidx + 512], 512)
                        for c in range(16):
                            fw.op("vector", lambda h, c=c, wt=wt, cw=cw: h.tensor_tensor(out=wt[:, c, :], in0=wt[:, c, :], in1=cw[:], op=ALU.mult),
                                  reads=[wb, cwb], writes=[wb])
                    wts.append((bi, sh if cidx is not None else sh0, wt))
                if bias is not None:
                    b16 = SB(st, "b16_%d" % bi, [1, 512], BF16)
                    fw.dma("gpsimd", b16[:], bias, writes=[wb])
                    wts.append((bi, -1, b16))
            hw = [SB(st, "hw%d" % i, [128, 16, 514], BF16) for i in range(2)]; hwb = [Buf(), Buf()]
            ngrp = (ntok_tiles + 3) // 4
            for g in range(ngrp):
                k = g % 2
                nt = min(4, ntok_tiles - g * 4)
                wdt = nt * 128 + 2
                if src_w is not None:
                    wdt = min(wdt, src_w - g * 512)
                fw.dma("sync", hw[k][:, :, 0:wdt], src_v[:, :, g * 512:g * 512 + wdt], reads=[dbuf(srcname)], writes=[hwb[k]])
                for m in range(nt):
                    tile = g * 4 + m
                    pidx = [(tile * nb + bi) % 6 for bi in range(nb)]
                    for bi in range(nb):
                        mine = [(sh, wt) for (b2, sh, wt) in wts if b2 == bi]
                        nsteps = sum(16 if sh >= 0 else 1 for sh, _ in mine)
                        step = 0
                        for sh, wt in mine:
                            if sh < 0:
                                fw.op("tensor", lambda h, wt=wt, p=pidx[bi], s0=(step == 0), s1=(step == nsteps - 1):
                                      h.matmul(PS[p][:], ones16[0:1, :], wt[:], start=s0, stop=s1), reads=[wb, Bc], writes=[PSB[pidx[bi]]])
                                step += 1
                                continue
                            for c in range(16):
                                fw.op("tensor", lambda h, wt=wt, c=c, p=pidx[bi], off=m * 128 + sh, s0=(step == 0), s1=(step == nsteps - 1), k=k:
                                      h.matmul(PS[p][:], hw[k][:, c, off:off + 128], wt[:, c, :], start=s0, stop=s1),
                                      reads=[wb, hwb[k]], writes=[PSB[pidx[bi]]])
                                step += 1
                    epilogue(tile, pidx)

        def qk_epilogue_factory(st, gsrc, ropeC, ropeS, nheads_list, dstT, dstname, pref):
            grep_, gb_ = rep_load(st, pref + "g", gsrc, 128)
            ss = SB(st, pref + "ss", [128, 4], F32); ssb = Buf()
            junk = SB(st, pref + "junk", [128, 128], F32)
            xn = SB(st, pref + "xn", [128, 128], F32); xnb = Buf()
            t1 = SB(st, pref + "t1", [128, 128], F32); t2 = SB(st, pref + "t2", [128, 128], F32); tb_ = Buf()
            x16 = [SB(st, pref + "x16%d" % i, [128, 128], BF16) for i in range(2)]; x16b = [Buf(), Buf()]
            rc = [SB(st, pref + "rc%d" % i, [128, 128], F32) for i in range(2)]
            rs = [SB(st, pref + "rs%d" % i, [128, 128], F32) for i in range(2)]; rb = [Buf(), Buf()]
            stg = SB(st, pref + "stg", [128, 8, 128], BF16); stgb = Buf()
            cnt = [0]

            def fn(tile, p, heads):
                k = tile % 2
                fw.dma("sync", rc[k][:], ropeC[tile * 128:(tile + 1) * 128, :], writes=[rb[k]])
                fw.dma("sync", rs[k][:], ropeS[tile * 128:(tile + 1) * 128, :], writes=[rb[k]])
                for hi, (co, dh) in enumerate(heads):
                    fw.op("scalar", lambda h, hi=hi, co=co: h.activation(out=junk[:], in_=PS[p][:, co:co + 128], func=AF.Square,
                                                                         accum_out=ss[:, hi:hi + 1]), reads=[PSB[p]], writes=[ssb])
                nh = len(heads)
                fw.op("scalar", lambda h: h.activation(out=ss[:, 0:nh], in_=ss[:, 0:nh], func=AF.Sqrt, bias=QK_EPS, scale=1.0 / 128),
                      reads=[ssb], writes=[ssb])
                fw.op("vector", lambda h: h.reciprocal(out=ss[:, 0:nh], in_=ss[:, 0:nh]), reads=[ssb], writes=[ssb])
                for hi, (co, dh) in enumerate(heads):
                    j = cnt[0] % 2; cnt[0] += 1
                    fw.op("vector", lambda h, hi=hi, co=co: h.scalar_tensor_tensor(out=xn[:], in0=PS[p][:, co:co + 128], scalar=ss[:, hi:hi + 1],
                                                                                  in1=grep_[:], op0=ALU.mult, op1=ALU.mult),
                          reads=[PSB[p], ssb, gb_], writes=[xnb])
                    fw.op("vector", lambda h: h.tensor_tensor(out=t1[:], in0=xn[:], in1=rc[k][:], op=ALU.mult), reads=[xnb, rb[k]], writes=[tb_])
                    xv = xn[:].rearrange("p (a h d) -> p a h d", a=2, h=2)
                    sv = rs[k][:].rearrange("p (a h d) -> p a h d", a=2, h=2)
                    tv = t2[:].rearrange("p (a h d) -> p a h d", a=2, h=2)
                    fw.op("vector", lambda h: h.tensor_tensor(out=tv[:, :, 0, :], in0=xv[:, :, 1, :], in1=sv[:, :, 0, :], op=ALU.mult),
                          reads=[xnb, rb[k]], writes=[tb_])
                    fw.op("vector", lambda h: h.tensor_tensor(out=tv[:, :, 1, :], in0=xv[:, :, 0, :], in1=sv[:, :, 1, :], op=ALU.mult),
                          reads=[xnb, rb[k]], writes=[tb_])
                    fw.op("vector", lambda h, j=j: h.tensor_tensor(out=x16[j][:], in0=t1[:], in1=t2[:], op=ALU.add), reads=[tb_], writes=[x16b[j]])
                    fw.op("tensor", lambda h, j=j, hi=hi: h.transpose(PT[0][:, hi * 128:(hi + 1) * 128], x16[j][:], ident[:]),
                          reads=[x16b[j], Bc], writes=[PTB[0]])
                fw.op("scalar", lambda h: h.copy(out=stg[:, 0:nh, :], in_=PT[0][:, 0:nh * 128].rearrange("p (c t) -> p c t", t=128)),
                      reads=[PTB[0]], writes=[stgb])
                for hi, (co, dh) in enumerate(heads):
                    fw.dma("gpsimd", dstT[:, dh, tile * 128:(tile + 1) * 128], stg[:, hi, :], reads=[stgb], writes=[dbuf(dstname)])
            return fn

        hTv = hT_d.rearrange("(c p) s -> p c s", p=128)
        hTov = hTo_d.rearrange("(c p) s -> p c s", p=128)

        if stop == "S0":
            return nc
        with ExitStack() as st:
            kfn = qk_epilogue_factory(st, k_norm_g, ropeC_k, ropeS_k, 2, kT_d, "kT_d", "k")
            v16 = [SB(st, "v16_%d" % i, [128, 256], BF16) for i in range(2)]; v16b = [Buf(), Buf()]

            def kv_ep(tile, pidx):
                p = pidx[0]
                kfn(tile, p, [(0, 0), (128, 1)])
                k = tile % 2
                fw.op("scalar", lambda h: h.copy(out=v16[k][:], in_=PS[p][:, 256:512]), reads=[PSB[p]], writes=[v16b[k]])
                fw.dma("gpsimd", v_d[tile * 128:(tile + 1) * 128, :], v16[k][:], reads=[v16b[k]], writes=[dbuf("v_d")])
            proj_pass(st, hTv, "hT_d", 64, [(1024, None, None)], kv_ep)
        fw.barrier()

        if stop == "KV":
            return nc
        for qb in range(2):
            with ExitStack() as st:
                qfn = qk_epilogue_factory(st, q_norm_g, ropeC_q, ropeS_q, 4, qT_d, "qT_d", "q")

                def q_ep(tile, pidx, qb=qb):
                    qfn(tile, pidx[0], [(i * 128, qb * 4 + i) for i in range(4)])
                proj_pass(st, hTov, "hTo_d", 16, [(qb * 512, None, None)], q_ep)
            fw.barrier()

        if stop == "Q":
            return nc
        with ExitStack() as st:
            zr = SB(st, "zrow", [1, 2048], BF16); zrb = Buf()
            fw.op("vector", lambda h: h.memset(zr[:], 0.0), writes=[zrb])
            fw.dma("gpsimd", p_d[0:1, :], zr[:], reads=[zrb], writes=[Buf()])
            fw.dma("gpsimd", p_d[S + 1:S + 2, :], zr[:], reads=[zrb], writes=[Buf()])
        for cb in range(2):
            with ExitStack() as st:
                p16 = [SB(st, "p16_%d" % i, [128, 2, 512], BF16) for i in range(2)]; p16b = [Buf(), Buf()]

                def p_ep(tile, pidx, cb=cb):
                    k = tile % 2
                    fw.op("scalar", lambda h: h.copy(out=p16[k][:, 0, :], in_=PS[pidx[0]][:]), reads=[PSB[pidx[0]]], writes=[p16b[k]])
                    fw.op("vector", lambda h: h.tensor_copy(out=p16[k][:, 1, :], in_=PS[pidx[1]][:]), reads=[PSB[pidx[1]]], writes=[p16b[k]])
                    dst = p_d[1 + tile * 128:1 + (tile + 1) * 128, :].rearrange("p (a c) -> p a c", a=2)[:, :, cb * 512:(cb + 1) * 512]
                    fw.dma("gpsimd", dst, p16[k][:], reads=[p16b[k]], writes=[Buf()])
                c1 = 1024 + cb * 512; c2 = 2048 + cb * 512
                proj_pass(st, hTv, "hT_d", 64, [(1536 + c1, None, None), (1536 + c2, None, None)], p_ep)
            fw.barrier()
        with ExitStack() as st:
            p16 = [SB(st, "po16_%d" % i, [128, 2, 512], BF16) for i in range(2)]; p16b = [Buf(), Buf()]

            def po_ep(tile, pidx):
                k = tile % 2
                fw.op("scalar", lambda h: h.copy(out=p16[k][:, 0, :], in_=PS[pidx[0]][:]), reads=[PSB[pidx[0]]], writes=[p16b[k]])
                fw.op("vector", lambda h: h.tensor_copy(out=p16[k][:, 1, :], in_=PS[pidx[1]][:]), reads=[PSB[pidx[1]]], writes=[p16b[k]])
                fw.dma("gpsimd", po_d[tile * 128:(tile + 1) * 128, :].rearrange("p (a c) -> p a c", a=2), p16[k][:], reads=[p16b[k]], writes=[Buf()])
            proj_pass(st, hTov, "hTo_d", 17, [(1536, None, None), (1536 + 512, None, None)], po_ep, sh0=0, src_w=NOWN)
        fw.barrier()
        if stop == "Z":
            return nc

        def conv_stage(st, src, ntile, row0, W, ccol, emit):
            wr = []
            for i in range(3):
                wr.append(rep_load(st, "cvw%d" % i, hy_conv_w[i:i + 1, ccol:ccol + W], W))
            br_, brb_ = rep_load(st, "cvb", hy_conv_b[0:1, ccol:ccol + W], W)
            Pt = [[SB(st, "cvP%d_%d" % (k, i), [128, W], BF16) for i in range(3)] for k in range(2)]; Ptb = [[Buf() for i in range(3)] for k in range(2)]
            acc = SB(st, "cvacc", [128, W], F32); acc2 = SB(st, "cvacc2", [128, W], F32); ab = Buf(); ab2 = Buf()
            for t in range(ntile):
                k = t % 2
                for i in range(3):
                    r0 = row0 + t * 128 + i
                    fw.dma("sync", Pt[k][i][:], src[r0:r0 + 128, :], writes=[Ptb[k][i]])
                fw.op("vector", lambda h: h.tensor_tensor(out=acc[:], in0=Pt[k][0][:], in1=wr[0][0][:], op=ALU.mult), reads=[Ptb[k][0], wr[0][1], ab], writes=[ab])
                fw.op("vector", lambda h: h.tensor_tensor(out=acc2[:], in0=Pt[k][1][:], in1=wr[1][0][:], op=ALU.mult), reads=[Ptb[k][1], wr[1][1], ab2], writes=[ab2])
                fw.op("vector", lambda h: h.tensor_tensor(out=acc[:], in0=acc[:], in1=acc2[:], op=ALU.add), reads=[ab, ab2], writes=[ab])
                fw.op("vector", lambda h: h.tensor_tensor(out=acc2[:], in0=Pt[k][2][:], in1=wr[2][0][:], op=ALU.mult), reads=[Ptb[k][2], wr[2][1], ab2], writes=[ab2])
                fw.op("vector", lambda h: h.tensor_tensor(out=acc[:], in0=acc[:], in1=acc2[:], op=ALU.add), reads=[ab, ab2], writes=[ab])
                fw.op("vector", lambda h: h.tensor_tensor(out=acc[:], in0=acc[:], in1=br_[:], op=ALU.add), reads=[ab, brb_], writes=[ab])
                emit(t, acc, ab)

        with ExitStack() as st:
            z16 = [SB(st, "cvz%d" % i, [128, 1024], BF16) for i in range(2)]; z16b = [Buf(), Buf()]

            def z_emit(t, acc, ab):
                k = t % 2
                fw.op("vector", lambda h: h.tensor_tensor(out=z16[k][:], in0=acc[:, 0:1024], in1=acc[:, 1024:2048], op=ALU.mult), reads=[ab], writes=[z16b[k]])
                fw.dma("gpsimd", z_d[t * 128:(t + 1) * 128, :], z16[k][:], reads=[z16b[k]], writes=[Buf()])
            conv_stage(st, p_d, 64, 0, 2048, 1024, z_emit)
        fw.barrier()
        with ExitStack() as st:
            o16 = [SB(st, "cvo%d" % i, [128, 1024], BF16) for i in range(2)]; o16b = [Buf(), Buf()]

            def o_emit(t, acc, ab):
                k = t % 2
                fw.op("scalar", lambda h: h.copy(out=o16[k][:], in_=acc[:]), reads=[ab], writes=[o16b[k]])
                fw.dma("gpsimd", x0_d[t * 128:(t + 1) * 128, :], o16[k][:], reads=[o16b[k]], writes=[Buf()])
            conv_stage(st, po_d, 16, 0, 1024, 0, o_emit)
        fw.barrier()

        for cb in range(4):
            with ExitStack() as st:
                o16 = [SB(st, "g16_%d" % i, [128, 1024], BF16) for i in range(2)]; o16b = [Buf(), Buf()]

                def g_ep(tile, pidx, cb=cb):
                    k = tile % 2
                    for bi in range(2):
                        fw.op("scalar", lambda h, bi=bi: h.activation(out=o16[k][:, bi * 512:(bi + 1) * 512], in_=PS[pidx[bi]][:], func=AF.Sigmoid),
                              reads=[PSB[pidx[bi]]], writes=[o16b[k]])
                    fw.dma("gpsimd", g_d[tile * 128:(tile + 1) * 128, cb * 1024:(cb + 1) * 1024], o16[k][:], reads=[o16b[k]], writes=[dbuf("g_d")])
                c0 = cb * 1024
                proj_pass(st, hTov, "hTo_d", 16,
                          [(4608 + c0, None, b_gate[0:1, c0:c0 + 512]), (4608 + c0 + 512, None, b_gate[0:1, c0 + 512:c0 + 1024])], g_ep)
            fw.barrier()

        if stop == "S1":
            return nc
        with ExitStack() as st:
            kT = SB(st, "kT", [128, 2, S], BF16); kTb = Buf()
            vs = SB(st, "vs", [128, 64, 256], BF16); vsb = Buf()
            qT = SB(st, "qT", [128, 8, T], BF16); qTb = Buf()
            aT = SB(st, "aT", [128, 8, T], BF16); aTb = Buf()
            pT = [SB(st, "pT%d" % i, [128, 512], BF16) for i in range(3)]; pTb = [Buf() for _ in range(3)]
            rl = SB(st, "rl", [128, 512], F32); rlb = Buf()
            for hh in range(2):
                fw.dma("sync", kT[:, hh, :], kT_d[:, hh, :], reads=[dbuf("kT_d")], writes=[kTb])
            for g in range(4):
                fw.dma("sync", vs[:, g * 16:(g + 1) * 16, :], v_d[g * 2048:(g + 1) * 2048, :].rearrange("(t p) c -> p t c", p=128),
                       reads=[dbuf("v_d")], writes=[vsb])
            for g in range(4):
                fw.dma("sync", qT[:, g * 2:(g + 1) * 2, :], qT_d[:, g * 2:(g + 1) * 2, :], reads=[dbuf("qT_d")], writes=[qTb])
            sc = 1.0 / math.sqrt(128.0)
            pT4 = pT + [SB(st, "pT3", [128, 512], BF16), SB(st, "pT4x", [128, 512], BF16)]; pTb4 = pTb + [Buf(), Buf()]
            acc = [SB(st, "aacc%d" % i, [128, 512], F32) for i in range(2)]; accb = [Buf(), Buf()]
            iters = [(hd, qb, kc) for hd in range(8) for qb in range(4) for kc in range(64)]
            NIT = len(iters)

            def emit_qk(n):
                hd, qb, kc = iters[n]; kvh = hd // 4; si = n % 4; pj = n % 5
                fw.op("tensor", lambda h: h.matmul(PS[si][:], kT[:, kvh, kc * 128:(kc + 1) * 128], qT[:, hd, qb * 512:(qb + 1) * 512],
                                                   start=True, stop=True), reads=[kTb, qTb], writes=[PSB[si]])
                fw.op("scalar", lambda h: h.activation(out=pT4[pj][:], in_=PS[si][:], func=AF.Exp, scale=sc), reads=[PSB[si]], writes=[pTb4[pj]])

            def emit_pv(n):
                hd, qb, kc = iters[n]; kvh = hd // 4; pj = n % 5; g = n // 64; po = 4; a = g % 2
                fw.op("tensor", lambda h: h.matmul(PS[po][:], vs[:, kc, kvh * 128:(kvh + 1) * 128], pT4[pj][:], start=(kc == 0), stop=(kc == 63)),
                      reads=[vsb, pTb4[pj]], writes=[PSB[po]])
                if kc == 0:
                    fw.op("vector", lambda h: h.tensor_copy(out=acc[a][:], in_=pT4[pj][:]), reads=[pTb4[pj]], writes=[accb[a]])
                else:
                    fw.op("vector", lambda h: h.tensor_tensor(out=acc[a][:], in0=pT4[pj][:], in1=acc[a][:], op=ALU.add), reads=[pTb4[pj], accb[a]], writes=[accb[a]])
                if kc == 63:
                    fw.op("tensor", lambda h: h.matmul(PS[5][:], onesf[:], acc[a][:], start=True, stop=True), reads=[Bc, accb[a]], writes=[PSB[5]])
                    fw.op("vector", lambda h: h.reciprocal(out=rl[:], in_=PS[5][:]), reads=[PSB[5]], writes=[rlb])
                    fw.op("vector", lambda h: h.tensor_tensor(out=aT[:, hd, qb * 512:(qb + 1) * 512], in0=PS[po][:], in1=rl[:], op=ALU.mult),
                          reads=[PSB[po], rlb], writes=[aTb])

            emit_qk(0); emit_qk(1); emit_qk(2)
            for n in range(NIT):
                if n + 3 < NIT:
                    emit_qk(n + 3)
                emit_pv(n)
            for g in range(4):
                fw.dma("gpsimd", at_d[:, g * 2:(g + 1) * 2, :], aT[:, g * 2:(g + 1) * 2, :], reads=[aTb], writes=[dbuf("at_d")])
        fw.barrier()

        if stop == "S2":
            return nc
        persist = ExitStack()
        top.enter_context(persist)
        scl = SB(persist, "scl", [128, C], F32); sclb = Buf()
        drep, drepb = rep_load(persist, "drep", hy_bias_d, C)
        with ExitStack() as st:
            w1 = SB(st, "fw1", [33, 64], F32); w2 = SB(st, "fw2", [64, 64], F32); w3 = SB(st, "fw3", [64, 2048], F32)
            fb = SB(st, "fb", [64, 4], F32); fbb = Buf(); wl = Buf()
            fw.dma("sync", w1[:], filt_w1, writes=[wl]); fw.dma("sync", w2[:], filt_w2, writes=[wl]); fw.dma("sync", w3[:], filt_w3, writes=[wl])
            fw.dma("sync", fb[:, 0:1], filt_f1, writes=[fbb]); fw.dma("sync", fb[:, 1:2], filt_b1, writes=[fbb])
            fw.dma("sync", fb[:, 2:3], filt_f2, writes=[fbb]); fw.dma("sync", fb[:, 3:4], filt_b2, writes=[fbb])
            fbp = SB(st, "fbp", [64, 2], F32)
            fw.op("vector", lambda h: h.tensor_tensor(out=fbp[:, 0:1], in0=fb[:, 0:1], in1=fb[:, 1:2], op=ALU.mult), reads=[fbb], writes=[fbb])
            fw.op("vector", lambda h: h.tensor_tensor(out=fbp[:, 1:2], in0=fb[:, 2:3], in1=fb[:, 3:4], op=ALU.mult), reads=[fbb], writes=[fbb])
            fT = SB(st, "fT", [33, S], F32); fTb = Buf()
            fw.dma("sync", fT[:], featsT, writes=[fTb])
            h1T = SB(st, "fh1T", [64, 512], F32); h1b = Buf()
            h2T = SB(st, "fh2T", [64, S], F32); h2b = Buf()
            ar = SB(st, "far", [64, 512], F32); m1 = SB(st, "fm1", [64, 512], F32); m2 = SB(st, "fm2", [64, 512], F32); arb = Buf()

            def sin_layer(pidx, fcol, bcol, dst_ap, dstb):
                fw.op("scalar", lambda h: h.activation(out=ar[:], in_=PS[pidx][0:64, :], func=AF.Identity, scale=fb[:, fcol:fcol + 1], bias=fbp[:, bcol:bcol + 1]),
                      reads=[PSB[pidx], fbb], writes=[arb])
                fw.op("vector", lambda h: h.tensor_scalar(out=m1[:], in0=ar[:], scalar1=PI, scalar2=-2 * PI, op0=ALU.is_gt, op1=ALU.mult), reads=[arb], writes=[arb])
                fw.op("vector", lambda h: h.tensor_scalar(out=m2[:], in0=ar[:], scalar1=-PI, scalar2=2 * PI, op0=ALU.is_lt, op1=ALU.mult), reads=[arb], writes=[arb])
                fw.op("vector", lambda h: h.tensor_tensor(out=ar[:], in0=ar[:], in1=m1[:], op=ALU.add), reads=[arb], writes=[arb])
                fw.op("vector", lambda h: h.tensor_tensor(out=ar[:], in0=ar[:], in1=m2[:], op=ALU.add), reads=[arb], writes=[arb])
                fw.op("scalar", lambda h: h.activation(out=dst_ap, in_=ar[:], func=AF.Sin), reads=[arb], writes=[dstb])

            for pb in range(16):
                fw.op("tensor", lambda h, pb=pb: h.matmul(PS[0][0:64, :], w1[:], fT[:, pb * 512:(pb + 1) * 512], start=True, stop=True),
                      reads=[wl, fTb], writes=[PSB[0]])
                sin_layer(0, 0, 0, h1T[:], h1b)
                fw.op("tensor", lambda h: h.matmul(PS[1][0:64, :], w2[:], h1T[:], start=True, stop=True), reads=[wl, h1b], writes=[PSB[1]])
                sin_layer(1, 2, 1, h2T[:, pb * 512:(pb + 1) * 512], h2b)
            h2T16 = SB(st, "fh2T16", [64, S], BF16); h2b16 = Buf(); w316 = SB(st, "fw316", [64, 2048], BF16); wl16 = Buf()
            fw.op("vector", lambda h: h.tensor_copy(out=w316[:], in_=w3[:]), reads=[wl], writes=[wl16])
            for q_ in range(4):
                fw.op("scalar" if q_ % 2 else "vector", (lambda h, q_=q_: h.copy(out=h2T16[:, q_ * 2048:(q_ + 1) * 2048], in_=h2T[:, q_ * 2048:(q_ + 1) * 2048])) if q_ % 2 else
                      (lambda h, q_=q_: h.tensor_copy(out=h2T16[:, q_ * 2048:(q_ + 1) * 2048], in_=h2T[:, q_ * 2048:(q_ + 1) * 2048])), reads=[h2b], writes=[h2b16])
            adl, adlb = rep_load(st, "adl", absdelta, C)
            ngt = SB(st, "ngt", [128, 64], F32); m0s = SB(st, "m0s", [128, 1], F32); ngb = Buf()
            fw.dma("sync", ngt[:], negt, writes=[ngb]); fw.dma("sync", m0s[:], m0, writes=[ngb])
            dec = [SB(st, "dec%d" % i, [128, C], F32) for i in range(2)]; decb = [Buf(), Buf()]
            fo = [SB(st, "fo%d" % i, [128, 2048], F32) for i in range(2)]; fob = [Buf(), Buf()]
            fo16 = [SB(st, "fo16_%d" % i, [128, 2048], BF16) for i in range(2)]; fo16b = [Buf(), Buf()]
            sq = [SB(st, "fsq%d" % i, [128, 2048], BF16) for i in range(2)]; sqb = [Buf(), Buf()]
            for pt in range(64):
                k = pt % 2
                fw.op("scalar", lambda h: h.activation(out=dec[k][:], in_=adl[:], func=AF.Exp, scale=ngt[:, pt:pt + 1]), reads=[adlb, ngb], writes=[decb[k]])
                for cb in range(4):
                    fw.op("tensor", lambda h, cb=cb: h.matmul(PS[cb][:], h2T16[:, pt * 128:(pt + 1) * 128], w316[:, cb * 512:(cb + 1) * 512], start=True, stop=True),
                          reads=[wl16, h2b16], writes=[PSB[cb]])
                    fw.op("vector", lambda h, cb=cb: h.tensor_tensor(out=fo[k][:, cb * 512:(cb + 1) * 512], in0=PS[cb][:],
                                                                    in1=dec[k][:, (cb % 2) * 512:(cb % 2) * 512 + 512], op=ALU.mult),
                          reads=[PSB[cb], decb[k]], writes=[fob[k]])
                if pt == 0:
                    fw.op("vector", lambda h: h.tensor_scalar(out=fo[k][:, 1024:2048], in0=fo[k][:, 1024:2048], scalar1=m0s[:, 0:1], scalar2=None, op0=ALU.mult),
                          reads=[fob[k], ngb], writes=[fob[k]])
                fw.op("scalar", lambda h: h.copy(out=fo16[k][:], in_=fo[k][:]), reads=[fob[k]], writes=[fo16b[k]])
                fw.op("scalar", lambda h: h.activation(out=sq[k][:], in_=fo[k][:], func=AF.Square), reads=[fob[k]], writes=[sqb[k]])
                for cb in range(4):
                    fw.op("tensor", lambda h, cb=cb: h.matmul(PS[4 + cb % 2][:], ones16[:], sq[k][:, cb * 512:(cb + 1) * 512],
                                                              start=(pt == 0 and cb < 2), stop=(pt == 63 and cb >= 2)), reads=[Bc, sqb[k]], writes=[PSB[4 + cb % 2]])
                fw.dma("gpsimd", hf_d[pt * 128:(pt + 1) * 128, :], fo16[k][:, 0:1024], reads=[fo16b[k]], writes=[dbuf("hf_d")])
                fw.dma("gpsimd", hg_d[pt * 128:(pt + 1) * 128, :], fo16[k][:, 1024:2048], reads=[fo16b[k]], writes=[dbuf("hg_d")])
            for cb in range(2):
                fw.op("scalar", lambda h, cb=cb: h.activation(out=scl[:, cb * 512:(cb + 1) * 512], in_=PS[4 + cb][:], func=AF.Sqrt, bias=FILTER_EPS, scale=1.0),
                      reads=[PSB[4 + cb]], writes=[sclb])
            fw.op("vector", lambda h: h.reciprocal(out=scl[:], in_=scl[:]), reads=[sclb], writes=[sclb])
        fw.barrier()

        if stop == "S3a":
            return nc
        with ExitStack() as st:
            def cload(name, src, shape, dt=BF16):
                t = SB(st, name, shape, dt); b = Buf()
                fw.dma("gpsimd" if dt == BF16 else "sync", t[:], src, writes=[b])
                return t, b
            Fc, Fb1 = cload("Fc", cFc, [128, 128]); Fsn, Fb3 = cload("Fsn", cFsn, [128, 128])
            i2re, Fb8 = cload("i2re", ci2re, [128, 16]); i2im, Fb9 = cload("i2im", ci2im, [128, 16])
            FB = Buf()
            fw.op("vector", lambda h: h.tensor_copy(out=Fc[:, 0:1], in_=Fc[:, 0:1]), reads=[Fb1, Fb3, Fb8, Fb9], writes=[FB])

            st1 = ExitStack()
            zt_ = [SB(st1, "f1z%d" % i, [128, 16, 1024], BF16) for i in range(2)]; ztb_ = [Buf(), Buf()]
            for i_ in range(2):
                fw.op("vector", lambda h, i_=i_: h.memset(zt_[i_][64:128, :, :], 0.0), writes=[ztb_[i_]])
            ao = [SB(st1, "f1o%d" % i, [128, 2, 4, 1024], BF16) for i in range(2)]; aob = [Buf(), Buf()]

            def f1_pass(src, dst):
                sv = src.rearrange("(a b) c -> a b c", b=128)
                it = 0
                for nb in range(8):
                    k = nb % 2
                    fw.dma("sync", zt_[k][0:64, :, :], sv[:, nb * 16:(nb + 1) * 16, :], writes=[ztb_[k]])
                    for nn in range(16):
                        n1 = nb * 16 + nn
                        j = (n1 // 4) % 2; q = n1 % 4
                        for hf_ in range(2):
                            cs = slice(hf_ * 512, (hf_ + 1) * 512)
                            pr = (it % 3) * 2; pi_ = pr + 1; it += 1
                            fw.op("tensor", lambda h, nn=nn, pr=pr, cs=cs: h.matmul(PS[pr][:], Fc[:, :], zt_[k][:, nn, cs], start=True, stop=True),
                                  reads=[FB, ztb_[k]], writes=[PSB[pr]])
                            fw.op("tensor", lambda h, nn=nn, pi_=pi_, cs=cs: h.matmul(PS[pi_][:], Fsn[:, :], zt_[k][:, nn, cs], start=True, stop=True),
                                  reads=[FB, ztb_[k]], writes=[PSB[pi_]])
                            fw.op("scalar", lambda h, pr=pr, j=j, q=q, cs=cs: h.copy(out=ao[j][:, 0, q, cs], in_=PS[pr][:]), reads=[PSB[pr]], writes=[aob[j]])
                            fw.op("vector", lambda h, pi_=pi_, j=j, q=q, cs=cs: h.tensor_copy(out=ao[j][:, 1, q, cs], in_=PS[pi_][:]), reads=[PSB[pi_]], writes=[aob[j]])
                        if q == 3:
                            for r in range(2):
                                fw.dma("sync", dst[r][n1 - 3:n1 + 1, :, :].rearrange("n k c -> k n c"), ao[j][:, r, :, :], reads=[aob[j]], writes=[Buf()])

            f1_pass(hf_d, a_d[1])
            f1_pass(hg_d, a_d[2])
            f1_pass(z_d, a_d[0])
            fw.barrier()
            st1.close()
            if stop == "S3b1":
                return nc

            Et = [SB(st, "Et%d" % i, [128, 7, 128], BF16) for i in range(2)]; Etb = [Buf(), Buf()]
            ain = [[SB(st, "ain%d_%d" % (i, r), [128, 1024], BF16) for r in range(4)] for i in range(2)]; ainb = [[Buf() for r in range(4)] for i in range(2)]
            ho = [SB(st, "hho%d" % i, [128, 2, 1024], BF16) for i in range(2)]; hob = [Buf(), Buf()]
            tmps = [SB(st, "ftmp%d" % i, [128, 512], F32) for i in range(2)]; tmpbs = [Buf(), Buf()]

            def f2_loads(k2):
                e = k2 % 2
                fw.dma("sync", Et[e][:], cE[k2], writes=[Etb[e]])
                for r, sd in enumerate([a_d[1][0], a_d[1][1], a_d[2][0], a_d[2][1]]):
                    fw.dma("sync", ain[e][r][:], sd[:, k2, :], writes=[ainb[e][r]])
            f2_loads(0)
            for u in range(256):
                k2, cb = u // 2, u % 2
                k = u % 2; e = k2 % 2
                cs = slice(cb * 512, (cb + 1) * 512)
                pr = (u % 3) * 2; pi_ = pr + 1
                if cb == 0 and k2 + 1 < 128:
                    f2_loads(k2 + 1)
                for r, m in enumerate([0, 1, 0, 1]):
                    fw.op("tensor", lambda h, r=r, m=m: h.matmul(PS[pr][:], Et[e][:, m, :], ain[e][r][:, cs], start=(r == 0), stop=(r == 3)),
                          reads=[Etb[e], ainb[e][r]], writes=[PSB[pr]])
                for r, m in enumerate([2, 0, 1, 3]):
                    fw.op("tensor", lambda h, r=r, m=m: h.matmul(PS[pi_][:], Et[e][:, m, :], ain[e][r][:, cs], start=(r == 0), stop=(r == 3)),
                          reads=[Etb[e], ainb[e][r]], writes=[PSB[pi_]])
                fw.op("vector", lambda h: h.tensor_tensor(out=tmps[k][:], in0=PS[pr][:], in1=scl[:, cs], op=ALU.mult), reads=[PSB[pr], sclb], writes=[tmpbs[k]])
                fw.op("vector", lambda h: h.tensor_tensor(out=ho[e][:, 0, cs], in0=tmps[k][:], in1=drep[:, cs], op=ALU.add), reads=[tmpbs[k], drepb], writes=[hob[e]])
                fw.op("vector", lambda h: h.tensor_tensor(out=ho[e][:, 1, cs], in0=PS[pi_][:], in1=scl[:, cs], op=ALU.mult), reads=[PSB[pi_], sclb], writes=[hob[e]])
                if cb == 1:
                    fw.dma("sync", hh_d[0][k2, :, :], ho[e][:, 0, :], reads=[hob[e]], writes=[Buf()])
                    fw.dma("sync", hh_d[1][k2, :, :], ho[e][:, 1, :], reads=[hob[e]], writes=[Buf()])
            fw.barrier()
            if stop == "S3b2":
                return nc

            hin = [[SB(st, "hin%d_%d" % (i, r), [128, 1024], BF16) for r in range(2)] for i in range(2)]; hinb = [[Buf(), Buf()] for i in range(2)]
            xs = [SB(st, "fxs%d" % i, [128, 1024], BF16) for i in range(2)]; xsb = [Buf(), Buf()]
            ys = [SB(st, "fys%d" % i, [128, 1024], BF16) for i in range(2)]; ysb = [Buf(), Buf()]
            t4 = [SB(st, "ft4%d" % i, [128, 2048], BF16) for i in range(2)]; t4b = [[Buf() for _ in range(4)] for i in range(2)]
            bo = [SB(st, "fbo%d" % i, [128, 2, 1024], BF16) for i in range(2)]; bob = [Buf(), Buf()]

            def fu_loads(k2):
                e = k2 % 2
                fw.dma("sync", Et[e][:], cE[k2], writes=[Etb[e]])
                fw.dma("sync", ain[e][0][:], a_d[0][0][:, k2, :], writes=[ainb[e][0]])
                fw.dma("sync", ain[e][1][:], a_d[0][1][:, k2, :], writes=[ainb[e][1]])
                fw.dma("sync", hin[e][0][:], hh_d[0][k2, :, :], writes=[hinb[e][0]])
                fw.dma("sync", hin[e][1][:], hh_d[1][k2, :, :], writes=[hinb[e][1]])
            fu_loads(0)
            for u in range(256):
                k2, cb = u // 2, u % 2
                k = u % 2; e = k2 % 2
                cs = slice(cb * 512, (cb + 1) * 512)
                px = 0 if k == 0 else 4
                if cb == 0 and k2 + 1 < 128:
                    fu_loads(k2 + 1)
                fw.op("tensor", lambda h: h.matmul(PS[px][:], Et[e][:, 0, :], ain[e][0][:, cs], start=True, stop=False), reads=[Etb[e], ainb[e][0]], writes=[PSB[px]])
                fw.op("tensor", lambda h: h.matmul(PS[px][:], Et[e][:, 1, :], ain[e][1][:, cs], start=False, stop=True), reads=[Etb[e], ainb[e][1]], writes=[PSB[px]])
                fw.op("tensor", lambda h: h.matmul(PS[px + 1][:], Et[e][:, 2, :], ain[e][0][:, cs], start=True, stop=False), reads=[Etb[e], ainb[e][0]], writes=[PSB[px + 1]])
                fw.op("tensor", lambda h: h.matmul(PS[px + 1][:], Et[e][:, 0, :], ain[e][1][:, cs], start=False, stop=True), reads=[Etb[e], ainb[e][1]], writes=[PSB[px + 1]])
                fw.op("scalar", lambda h: h.copy(out=xs[k][:, 0:512], in_=PS[px][:]), reads=[PSB[px]], writes=[xsb[k]])
                fw.op("scalar", lambda h: h.copy(out=xs[k][:, 512:1024], in_=PS[px + 1][:]), reads=[PSB[px + 1]], writes=[xsb[k]])
                fw.op("vector", lambda h: h.tensor_tensor(out=t4[k][:, 0:512], in0=xs[k][:, 0:512], in1=hin[e][0][:, cs], op=ALU.mult), reads=[xsb[k], hinb[e][0]], writes=[t4b[k][0]])
                fw.op("vector", lambda h: h.tensor_tensor(out=t4[k][:, 512:1024], in0=xs[k][:, 512:1024], in1=hin[e][1][:, cs], op=ALU.mult), reads=[xsb[k], hinb[e][1]], writes=[t4b[k][1]])
                fw.op("vector", lambda h: h.tensor_tensor(out=t4[k][:, 1024:1536], in0=xs[k][:, 0:512], in1=hin[e][1][:, cs], op=ALU.mult), reads=[xsb[k], hinb[e][1]], writes=[t4b[k][2]])
                fw.op("vector", lambda h: h.tensor_tensor(out=t4[k][:, 1536:2048], in0=xs[k][:, 512:1024], in1=hin[e][0][:, cs], op=ALU.mult), reads=[xsb[k], hinb[e][0]], writes=[t4b[k][3]])
                fw.op("vector", lambda h: h.tensor_tensor(out=ys[k][:, 0:512], in0=t4[k][:, 0:512], in1=t4[k][:, 512:1024], op=ALU.subtract), reads=[t4b[k][0], t4b[k][1]], writes=[ysb[k]])
                fw.op("vector", lambda h: h.tensor_tensor(out=ys[k][:, 512:1024], in0=t4[k][:, 1024:1536], in1=t4[k][:, 1536:2048], op=ALU.add), reads=[t4b[k][2], t4b[k][3]], writes=[ysb[k]])
                fw.op("tensor", lambda h: h.matmul(PS[2][:], Et[e][:, 4, :], ys[k][:, 0:512], start=True, stop=False), reads=[Etb[e], ysb[k]], writes=[PSB[2]])
                fw.op("tensor", lambda h: h.matmul(PS[2][:], Et[e][:, 6, :], ys[k][:, 512:1024], start=False, stop=True), reads=[Etb[e], ysb[k]], writes=[PSB[2]])
                fw.op("tensor", lambda h: h.matmul(PS[3][:], Et[e][:, 5, :], ys[k][:, 0:512], start=True, stop=False), reads=[Etb[e], ysb[k]], writes=[PSB[3]])
                fw.op("tensor", lambda h: h.matmul(PS[3][:], Et[e][:, 4, :], ys[k][:, 512:1024], start=False, stop=True), reads=[Etb[e], ysb[k]], writes=[PSB[3]])
                fw.op("scalar", lambda h: h.copy(out=bo[e][:, 0, cs], in_=PS[2][:]), reads=[PSB[2]], writes=[bob[e]])
                fw.op("scalar", lambda h: h.copy(out=bo[e][:, 1, cs], in_=PS[3][:]), reads=[PSB[3]], writes=[bob[e]])
                if cb == 1:
                    fw.dma("sync", b_d[0][k2, :, :], bo[e][:, 0, :], reads=[bob[e]], writes=[Buf()])
                    fw.dma("sync", b_d[1][k2, :, :], bo[e][:, 1, :], reads=[bob[e]], writes=[Buf()])
            fw.barrier()
            if stop == "S3b3":
                return nc

            bin_ = [[SB(st, "bin%d_%d" % (i, r), [128, 16, 512], BF16) for r in range(2)] for i in range(2)]; binb = [Buf(), Buf()]
            yo = [SB(st, "fyo%d" % i, [16, 8, 512], F32) for i in range(2)]; yob = [Buf(), Buf()]
            yv = y_d.rearrange("(a b) c -> a b c", b=128)
            it = 0
            for cb in range(2):
                cs = slice(cb * 512, (cb + 1) * 512)
                for nb in range(8):
                    k = (cb * 8 + nb) % 2
                    for r in range(2):
                        fw.dma("sync", bin_[k][r][:], b_d[r][:, nb * 16:(nb + 1) * 16, cs], writes=[binb[k]])
                    for nn in range(16):
                        n1 = nb * 16 + nn
                        p = it % 6; j = (it // 8) % 2; q = it % 8; it += 1
                        fw.op("tensor", lambda h, nn=nn, p=p: h.matmul(PS[p][0:16, :], i2re[:], bin_[k][0][:, nn, :], start=True, stop=False), reads=[FB, binb[k]], writes=[PSB[p]])
                        fw.op("tensor", lambda h, nn=nn, p=p: h.matmul(PS[p][0:16, :], i2im[:], bin_[k][1][:, nn, :], start=False, stop=True), reads=[FB, binb[k]], writes=[PSB[p]])
                        if it % 2 == 0:
                            fw.op("scalar", lambda h, p=p, j=j, q=q: h.copy(out=yo[j][:, q, :], in_=PS[p][0:16, :]), reads=[PSB[p]], writes=[yob[j]])
                        else:
                            fw.op("vector", lambda h, p=p, j=j, q=q: h.tensor_copy(out=yo[j][:, q, :], in_=PS[p][0:16, :]), reads=[PSB[p]], writes=[yob[j]])
                        if q == 7:
                            fw.dma("sync", yv[:, n1 - 7:n1 + 1, cs], yo[j][:], reads=[yob[j]], writes=[Buf()])
        persist.close()
        fw.barrier()

        if stop == "S3b":
            return nc
        def wload(st, name, src, nchunk, ncol):
            t = SB(st, name, [128, nchunk, ncol], BF16); b = Buf()
            sv = src.rearrange("(c p) n -> p c n", p=128)
            for c0 in range(0, ncol, 512):
                fw.dma("gpsimd", t[:, :, c0:c0 + 512], sv[:, :, c0:c0 + 512], writes=[b])
            return t, b

        with ExitStack() as st:
            Wa, Wab = wload(st, "Wa", w_attn_o, 8, D)
            Wh, Whb = wload(st, "Wh", w_hy_o, 8, D)
            yt = [SB(st, "yt%d" % i, [128, C], F32) for i in range(2)]
            x0t = [SB(st, "x0t%d" % i, [128, C], BF16) for i in range(2)]
            gt = [SB(st, "gt%d" % i, [128, 4096], BF16) for i in range(2)]
            atT = [SB(st, "atT%d" % i, [128, 8, 128], BF16) for i in range(2)]; inb = [Buf(), Buf()]
            yh16 = SB(st, "yh16", [128, C], BF16); yhb = Buf()
            yhT = SB(st, "yhT", [128, 8, 128], BF16); yhTb = Buf()
            ya = SB(st, "ya", [128, 512], F32); yab = Buf()
            pre16 = SB(st, "pre16", [128, D], BF16); preb = Buf()
            preT = SB(st, "preTs", [128, 16, 128], BF16); preTb = Buf()
            preTv = preT_d.rearrange("(c p) t -> p c t", p=128)
            for t in range(16):
                k = t % 2
                rows = slice(t * 128, (t + 1) * 128)
                fw.dma("sync", yt[k][:], y_d[rows, :], reads=[dbuf("y_d")], writes=[inb[k]])
                fw.dma("sync", x0t[k][:], x0_d[rows, :], reads=[dbuf("x0_d")], writes=[inb[k]])
                fw.dma("sync", gt[k][:], g_d[rows, :], reads=[dbuf("g_d")], writes=[inb[k]])
                fw.dma("sync", atT[k][:], at_d[:, :, rows], reads=[dbuf("at_d")], writes=[inb[k]])
                fw.op("vector", lambda h: h.tensor_tensor(out=yh16[:], in0=yt[k][:], in1=x0t[k][:], op=ALU.mult), reads=[inb[k]], writes=[yhb])
                transpose_to(yh16, yhb, yhT, yhTb, 8)
                for cb in range(4):
                    cs = slice(cb * 512, (cb + 1) * 512)
                    pa = (cb % 3) * 2; ph = pa + 1
                    for c in range(8):
                        fw.op("tensor", lambda h, c=c, pa=pa: h.matmul(PS[pa][:], atT[k][:, c, :], Wa[:, c, cs], start=(c == 0), stop=(c == 7)),
                              reads=[inb[k], Wab], writes=[PSB[pa]])
                    for c in range(8):
                        fw.op("tensor", lambda h, c=c, ph=ph: h.matmul(PS[ph][:], yhT[:, c, :], Wh[:, c, cs], start=(c == 0), stop=(c == 7)),
                              reads=[yhTb, Whb], writes=[PSB[ph]])
                    fw.op("vector", lambda h, pa=pa: h.tensor_tensor(out=ya[:], in0=PS[pa][:], in1=gt[k][:, cs], op=ALU.mult), reads=[PSB[pa], inb[k]], writes=[yab])
                    fw.op("vector", lambda h, ph=ph: h.tensor_tensor(out=pre16[:, cs], in0=PS[ph][:], in1=gt[k][:, 2048 + cb * 512:2048 + (cb + 1) * 512], op=ALU.mult),
                          reads=[PSB[ph], inb[k]], writes=[preb])
                    fw.op("vector", lambda h: h.tensor_tensor(out=pre16[:, cs], in0=pre16[:, cs], in1=ya[:], op=ALU.add), reads=[preb, yab], writes=[preb])
                transpose_to(pre16, preb, preT, preTb, 16)
                fw.dma("gpsimd", preTv[:, :, rows], preT[:], reads=[preTb], writes=[dbuf("preT_d")])
        fw.barrier()

        if stop == "S4a":
            return nc
        with ExitStack() as st:
            Wo, Wob = wload(st, "Wo", w_out, 16, D)
            g1, g1b = rep_load(st, "g1", ln1_g, D); b1, b1b = rep_load(st, "b1", ln1_b, D)
            gbb = Buf()
            fw.op("vector", lambda h: h.tensor_copy(out=g1[:, 0:1], in_=g1[:, 0:1]), reads=[g1b, b1b], writes=[gbb])
            pT_ = [SB(st, "ppT%d" % i, [128, 16, 128], BF16) for i in range(2)]
            hres = [SB(st, "hres%d" % i, [128, D], F32) for i in range(2)]; inb = [Buf(), Buf()]
            h1t = [SB(st, "h1t%d" % i, [128, D], F32) for i in range(2)]; h1b_ = [Buf(), Buf()]
            h116 = SB(st, "h116", [128, D], BF16); h116b = Buf()
            h1Ts = SB(st, "h1Ts", [128, 16, 128], BF16); h1Tb = Buf()
            st8 = SB(st, "st8b", [128, 24], F32); sb8 = Buf(); mv = SB(st, "mvb", [128, 2], F32); mvb = Buf(); rstd = SB(st, "rstdb", [128, 1], F32)
            preTv = preT_d.rearrange("(c p) t -> p c t", p=128)
            h1Tv = h1T_d.rearrange("(c p) t -> p c t", p=128)
            for t in range(16):
                k = t % 2
                rows = slice(t * 128, (t + 1) * 128)
                fw.dma("sync", pT_[k][:], preTv[:, :, rows], reads=[dbuf("preT_d")], writes=[inb[k]])
                fw.dma("sync", hres[k][:], ho_d[128 * t + 1:128 * t + 129, :], reads=[dbuf("ho_d")], writes=[inb[k]])
                for cb in range(4):
                    cs = slice(cb * 512, (cb + 1) * 512)
                    p = (t * 4 + cb) % 6
                    for c in range(16):
                        fw.op("tensor", lambda h, c=c, p=p: h.matmul(PS[p][:], pT_[k][:, c, :], Wo[:, c, cs], start=(c == 0), stop=(c == 15)),
                              reads=[inb[k], Wob], writes=[PSB[p]])
                    fw.op("vector", lambda h, p=p: h.scalar_tensor_tensor(out=h1t[k][:, cs], in0=hres[k][:, cs], scalar=ALPHA, in1=PS[p][:], op0=ALU.mult, op1=ALU.add),
                          reads=[inb[k], PSB[p]], writes=[h1b_[k]])
                layernorm(h1t[k], h1b_[k], g1, b1, gbb, st8, sb8, mv, mvb, rstd)
                fw.dma("gpsimd", h1_d[rows, :], h1t[k][:], reads=[h1b_[k]], writes=[dbuf("h1_d")])
                fw.op("scalar", lambda h: h.copy(out=h116[:], in_=h1t[k][:]), reads=[h1b_[k]], writes=[h116b])
                transpose_to(h116, h116b, h1Ts, h1Tb, 16)
                fw.dma("gpsimd", h1Tv[:, :, rows], h1Ts[:], reads=[h1Tb], writes=[dbuf("h1T_d")])
        fw.barrier()

        if stop == "S4b":
            return nc
        persist2 = ExitStack()
        top.enter_context(persist2)
        I32 = mybir.dt.int32
        OH1 = SB(persist2, "OH1", [128, 16, 32], F32); OH2 = SB(persist2, "OH2", [128, 16, 32], F32)
        G12 = SB(persist2, "G12", [128, 16, 2], F32); Gb = Buf()
        SL1 = SB(persist2, "SL1", [128, 16], I32); SL2 = SB(persist2, "SL2", [128, 16], I32); SLb = Buf()
        IG = SB(persist2, "IG", [128, 64, 16], I32); ID = SB(persist2, "ID", [128, 64, 8], I32); IXb = Buf()
        zt16 = SB(persist2, "zt16", [128, D], BF16); ztb16 = Buf()
        fw.op("gpsimd", lambda h: h.memset(zt16[:], 0.0), writes=[ztb16])
        for i in range(64):
            fw.dma("sync", xs_d[i * 128:(i + 1) * 128, :], zt16[:], reads=[ztb16], writes=[Buf()])
        h1Tv = h1T_d.rearrange("(c p) t -> p c t", p=128)
        BIG = 1.0e30
        with ExitStack() as st:
            Wr = SB(st, "Wr", [128, 16, 36], BF16); Wrb = Buf()
            fw.dma("gpsimd", Wr[:], w_route.rearrange("(c p) n -> p c n", p=128), writes=[Wrb])
            br16 = SB(st, "br16", [1, 36], BF16)
            fw.dma("gpsimd", br16[:], b_route, writes=[Wrb])
            hT_ = [SB(st, "rhT%d" % i, [128, 16, 128], BF16) for i in range(2)]; inb = [Buf(), Buf()]
            lg = SB(st, "lg", [128, 36], F32); lgb = Buf()
            sm = SB(st, "rsm", [128, 16], F32); smb = Buf()
            oh = SB(st, "roh", [128, 96], F32); ohb = Buf()
            for t in range(16):
                k = t % 2
                fw.dma("sync", hT_[k][:], h1Tv[:, :, t * 128:(t + 1) * 128], reads=[dbuf("h1T_d")], writes=[inb[k]])
                p = t % 6
                for c in range(16):
                    fw.op("tensor", lambda h, c=c, p=p: h.matmul(PS[p][:, 0:36], hT_[k][:, c, :], Wr[:, c, :], start=(c == 0), stop=False), reads=[inb[k], Wrb], writes=[PSB[p]])
                fw.op("tensor", lambda h, p=p: h.matmul(PS[p][:, 0:36], ones16[0:1, :], br16[:], start=False, stop=True), reads=[Wrb, Bc], writes=[PSB[p]])
                fw.op("vector", lambda h, p=p: h.tensor_copy(out=lg[:], in_=PS[p][:, 0:36]), reads=[PSB[p]], writes=[lgb])
                V = lambda fn, r, w: fw.op("vector", fn, reads=r, writes=w)
                V(lambda h: h.tensor_reduce(out=sm[:, 0:1], in_=lg[:, 0:4], axis=mybir.AxisListType.X, op=ALU.max), [lgb], [smb])
                V(lambda h: h.tensor_scalar(out=oh[:, 0:4], in0=lg[:, 0:4], scalar1=sm[:, 0:1], scalar2=None, op0=ALU.is_equal), [lgb, smb], [ohb])
                V(lambda h: h.tensor_scalar(out=oh[:, 4:8], in0=lg[:, 0:4], scalar1=sm[:, 0:1], scalar2=None, op0=ALU.subtract), [lgb, smb], [ohb])
                fw.op("scalar", lambda h: h.activation(out=oh[:, 4:8], in_=oh[:, 4:8], func=AF.Exp, accum_out=sm[:, 1:2]), reads=[ohb, smb], writes=[ohb, smb])
                V(lambda h: h.reciprocal(out=sm[:, 2:3], in_=sm[:, 1:2]), [smb], [smb])
                V(lambda h: h.tensor_scalar(out=oh[:, 8:12], in0=oh[:, 0:4], scalar1=-1.0, scalar2=BIG, op0=ALU.add, op1=ALU.mult), [ohb], [ohb])
                for g_ in range(4):
                    V(lambda h, g_=g_: h.tensor_scalar(out=oh[:, 32 + 8 * g_:40 + 8 * g_], in0=lg[:, 4 + 8 * g_:12 + 8 * g_], scalar1=oh[:, 8 + g_:9 + g_], scalar2=None, op0=ALU.add),
                      [lgb, ohb], [ohb])
                V(lambda h: h.tensor_reduce(out=sm[:, 3:4], in_=oh[:, 32:64], axis=mybir.AxisListType.X, op=ALU.max), [ohb], [smb])
                V(lambda h: h.tensor_scalar(out=oh[:, 64:96], in0=oh[:, 32:64], scalar1=sm[:, 3:4], scalar2=None, op0=ALU.is_equal), [ohb, smb], [ohb])
                V(lambda h: h.scalar_tensor_tensor(out=oh[:, 32:64], in0=oh[:, 64:96], scalar=-BIG, in1=oh[:, 32:64], op0=ALU.mult, op1=ALU.add), [ohb], [ohb])
                V(lambda h: h.tensor_reduce(out=sm[:, 4:5], in_=oh[:, 32:64], axis=mybir.AxisListType.X, op=ALU.max), [ohb], [smb])
                V(lambda h: h.tensor_scalar(out=oh[:, 32:64], in0=oh[:, 32:64], scalar1=sm[:, 4:5], scalar2=None, op0=ALU.is_equal), [ohb, smb], [ohb])
                V(lambda h: h.tensor_tensor(out=sm[:, 5:6], in0=sm[:, 4:5], in1=sm[:, 3:4], op=ALU.subtract), [smb], [smb])
                fw.op("scalar", lambda h: h.activation(out=sm[:, 6:7], in_=sm[:, 5:6], func=AF.Exp), reads=[smb], writes=[smb])
                V(lambda h: h.tensor_scalar(out=sm[:, 7:8], in0=sm[:, 6:7], scalar1=1.0, scalar2=None, op0=ALU.add), [smb], [smb])
                V(lambda h: h.reciprocal(out=sm[:, 8:9], in_=sm[:, 7:8]), [smb], [smb])
                V(lambda h: h.tensor_tensor(out=sm[:, 9:10], in0=sm[:, 8:9], in1=sm[:, 2:3], op=ALU.mult), [smb], [smb])
                V(lambda h: h.tensor_tensor(out=sm[:, 10:11], in0=sm[:, 9:10], in1=sm[:, 6:7], op=ALU.mult), [smb], [smb])
                V(lambda h, t=t: h.tensor_copy(out=OH1[:, t, :], in_=oh[:, 64:96]), [ohb], [Gb])
                V(lambda h, t=t: h.tensor_copy(out=OH2[:, t, :], in_=oh[:, 32:64]), [ohb], [Gb])
                V(lambda h, t=t: h.tensor_copy(out=G12[:, t, :], in_=sm[:, 9:11]), [smb], [Gb])
            A = SB(st, "mA", [128, 16, 32], F32); Ab = Buf()
            R = SB(st, "mR", [128, 16, 32], F32); Rb = Buf()
            U = SB(st, "mU", [128, 128], F32); Ub = Buf()
            base = SB(st, "mbase", [128, 32], F32); baseb = Buf()
            ci = SB(st, "mci", [128, 32], I32); padf = SB(st, "mpadf", [128, 32], F32); pe = SB(st, "mpe", [128, 32], F32)
            pst = SB(st, "mpst", [128, 32], F32); one32 = SB(st, "mone32", [128, 32], F32); pb_ = Buf()
            sf = SB(st, "msf", [128, 32], F32); eb = SB(st, "meb", [128, 64], F32); junk32 = SB(st, "mj32", [128, 32], F32); ebb = Buf()
            igf = SB(st, "migf", [128, 64, 16], F32); bG = SB(st, "mbG", [128, 64], F32); bD = SB(st, "mbD", [128, 64], F32)
            pc = SB(st, "mpc", [128, 1], F32)
            fw.dma("sync", pc[:], pcol, writes=[ebb])
            V(lambda h: h.tensor_tensor(out=A[:], in0=OH1[:], in1=OH2[:], op=ALU.add), [Gb], [Ab])
            fw.op("gpsimd", lambda h: h.memset(U[:], 1.0), writes=[Ub])
            fw.op("gpsimd", lambda h: h.affine_select(out=U[:], in_=U[:], pattern=[[1, 128]], compare_op=ALU.is_gt, fill=0.0, base=0, channel_multiplier=-1),
                  reads=[Ub], writes=[Ub])
            V(lambda h: h.memset(base[:], 0.0), [], [baseb])
            V(lambda h: h.memset(one32[:], 1.0), [], [pb_])
            for i in range(16):
                pa = (2 * i) % 6; pb2 = (2 * i + 1) % 6
                fw.op("tensor", lambda h, i=i, pa=pa: h.matmul(PS[pa][:, 0:32], U[:], A[:, i, :], start=True, stop=True), reads=[Ub, Ab], writes=[PSB[pa]])
                fw.op("tensor", lambda h, i=i, pb2=pb2: h.matmul(PS[pb2][:, 0:32], onesf[:], A[:, i, :], start=True, stop=True), reads=[Bc, Ab], writes=[PSB[pb2]])
                V(lambda h, i=i, pa=pa: h.tensor_tensor(out=R[:, i, :], in0=PS[pa][:, 0:32], in1=base[:], op=ALU.add), [PSB[pa], baseb], [Rb])
                V(lambda h, pb2=pb2: h.tensor_tensor(out=base[:], in0=PS[pb2][:, 0:32], in1=base[:], op=ALU.add), [PSB[pb2], baseb], [baseb])
            V(lambda h: h.tensor_scalar(out=ci[:], in0=base[:], scalar1=127.0, scalar2=None, op0=ALU.add), [baseb], [pb_])
            V(lambda h: h.tensor_scalar(out=ci[:], in0=ci[:], scalar1=7, scalar2=7, op0=ALU.arith_shift_right, op1=ALU.logical_shift_left), [pb_], [pb_])
            V(lambda h: h.tensor_copy(out=padf[:], in_=ci[:]), [pb_], [pb_])
            V(lambda h: h.tensor_tensor_scan(out=pe[:], data0=one32[:], data1=padf[:], initial=0.0, op0=ALU.mult, op1=ALU.add), [pb_], [pb_])
            V(lambda h: h.tensor_tensor(out=pst[:], in0=pe[:], in1=padf[:], op=ALU.subtract), [pb_], [pb_])
            for i in range(16):
                V(lambda h, i=i: h.tensor_tensor(out=R[:, i, :], in0=R[:, i, :], in1=pst[:], op=ALU.add), [Rb, pb_], [Rb])
            for OH, SL, col in ((OH1, SL1, 0), (OH2, SL2, 1)):
                V(lambda h, OH=OH: h.tensor_tensor(out=A[:], in0=R[:], in1=OH[:], op=ALU.mult), [Rb, Gb, Ab], [Ab])
                V(lambda h: h.tensor_reduce(out=sf[:, 0:16], in_=A[:], axis=mybir.AxisListType.X, op=ALU.add), [Ab], [ebb])
                V(lambda h, SL=SL: h.tensor_copy(out=SL[:], in_=sf[:, 0:16]), [ebb], [SLb])
            for i in range(64):
                V(lambda h, i=i: h.tensor_scalar(out=junk32[:], in0=pe[:], scalar1=128.0 * i, scalar2=0.0, op0=ALU.is_le, op1=ALU.add, accum_out=eb[:, i:i + 1]),
                  [pb_], [ebb])
            V(lambda h: h.tensor_scalar(out=bG[:], in0=eb[:], scalar1=2048.0, scalar2=pc[:, 0:1], op0=ALU.mult, op1=ALU.add), [ebb], [ebb])
            V(lambda h: h.tensor_scalar(out=bD[:], in0=eb[:], scalar1=1024.0, scalar2=pc[:, 0:1], op0=ALU.mult, op1=ALU.add), [ebb], [ebb])
            for c in range(16):
                V(lambda h, c=c: h.tensor_scalar(out=igf[:, :, c], in0=bG[:], scalar1=128.0 * c, scalar2=None, op0=ALU.add), [ebb], [ebb])
            V(lambda h: h.tensor_copy(out=IG[:], in_=igf[:]), [ebb], [IXb])
            for c in range(8):
                V(lambda h, c=c: h.tensor_scalar(out=igf[:, :, c], in0=bD[:], scalar1=128.0 * c, scalar2=None, op0=ALU.add), [ebb, IXb], [ebb])
            V(lambda h: h.tensor_copy(out=ID[:], in_=igf[:, :, 0:8]), [ebb], [IXb])
        fw.barrier()

        if stop == "S5":
            return nc
        with ExitStack() as st:
            hl = [SB(st, "s_hl%d" % i, [128, D], F32) for i in range(2)]; hlb = [Buf(), Buf()]
            h16_ = [SB(st, "s_h16%d" % i, [128, D], BF16) for i in range(2)]; h16b_ = [Buf(), Buf()]
            for t in range(16):
                k = t % 2
                fw.dma("sync", hl[k][:], h1_d[t * 128:(t + 1) * 128, :], writes=[hlb[k]])
                fw.op("scalar", lambda h: h.copy(out=h16_[k][:], in_=hl[k][:]), reads=[hlb[k]], writes=[h16b_[k]])
                fw.idma(xs_d[:, :], h16_[k][:, :], SL1[:, t:t + 1], False, 8191, reads=[h16b_[k], SLb], writes=[Buf()])
                fw.idma(xs_d[:, :], h16_[k][:, :], SL2[:, t:t + 1], False, 8191, reads=[h16b_[k], SLb], writes=[Buf()])
        fw.barrier()

        if stop == "S6a":
            return nc
        weg = w_eg.rearrange("e k n -> (e k) n"); weu = w_eu.rearrange("e k n -> (e k) n"); wed = w_ed.rearrange("e k n -> (e k) n")
        with ExitStack() as st:
            NW = 8
            xb = [SB(st, "b_xb%d" % i, [128, D], BF16) for i in range(2)]; xbb = [Buf(), Buf()]
            xT = [SB(st, "b_xT%d" % i, [128, 16, 128], BF16) for i in range(2)]; xTb = [Buf(), Buf()]
            wgt = [SB(st, "b_wg%d" % i, [128, 1024], BF16) for i in range(NW)]; wgtb = [Buf() for _ in range(NW)]
            wut = [SB(st, "b_wu%d" % i, [128, 1024], BF16) for i in range(NW)]; wutb = [Buf() for _ in range(NW)]
            wdt = [[SB(st, "b_wd%d_%d" % (i, c), [128, D], BF16) for c in range(8)] for i in range(2)]
            wdtb = [[Buf() for c in range(8)] for i in range(2)]
            sg = [SB(st, "b_sg%d" % i, [128, 512], F32) for i in range(2)]; sgb = [Buf(), Buf()]
            Hs = [SB(st, "b_H%d" % i, [128, 1024], BF16) for i in range(2)]; Hsb = [Buf(), Buf()]
            HT = [SB(st, "b_HT%d" % i, [128, 8, 128], BF16) for i in range(2)]; HTb = [Buf(), Buf()]
            Ys = [SB(st, "b_Y%d" % i, [128, D], F32) for i in range(2)]; Ysb = [Buf(), Buf()]
            iw = 0
            for i in range(64):
                k = i % 2
                fw.dma("sync", xb[k][:], xs_d[i * 128:(i + 1) * 128, :], writes=[xbb[k]])
                transpose_to(xb[k], xbb[k], xT[k], xTb[k], 16)
                for c in range(16):
                    j = iw % NW; iw += 1
                    fw.idma(wgt[j][:, :], weg[:, :], IG[:, i, c:c + 1], True, 32 * 2048 - 1, reads=[IXb], writes=[wgtb[j]])
                    fw.idma(wut[j][:, :], weu[:, :], IG[:, i, c:c + 1], True, 32 * 2048 - 1, reads=[IXb], writes=[wutb[j]])
                    for hf_ in range(2):
                        fw.op("tensor", lambda h, c=c, j=j, hf_=hf_: h.matmul(PS[hf_][:], xT[k][:, c, :], wgt[j][:, hf_ * 512:(hf_ + 1) * 512], start=(c == 0), stop=(c == 15)),
                              reads=[xTb[k], wgtb[j]], writes=[PSB[hf_]])
                    for hf_ in range(2):
                        fw.op("tensor", lambda h, c=c, j=j, hf_=hf_: h.matmul(PS[2 + hf_][:], xT[k][:, c, :], wut[j][:, hf_ * 512:(hf_ + 1) * 512], start=(c == 0), stop=(c == 15)),
                              reads=[xTb[k], wutb[j]], writes=[PSB[2 + hf_]])
                for c in range(8):
                    fw.idma(wdt[k][c][:, :], wed[:, :], ID[:, i, c:c + 1], True, 32 * 1024 - 1, reads=[IXb], writes=[wdtb[k][c]])
                for hf_ in range(2):
                    fw.op("scalar", lambda h, hf_=hf_: h.activation(out=sg[hf_][:], in_=PS[hf_][:], func=AF.Silu), reads=[PSB[hf_]], writes=[sgb[hf_]])
                    fw.op("vector", lambda h, hf_=hf_: h.tensor_tensor(out=Hs[k][:, hf_ * 512:(hf_ + 1) * 512], in0=PS[2 + hf_][:], in1=sg[hf_][:], op=ALU.mult),
                          reads=[PSB[2 + hf_], sgb[hf_]], writes=[Hsb[k]])
                transpose_to(Hs[k], Hsb[k], HT[k], HTb[k], 8)
                for hf_ in range(2):
                    for c in range(8):
                        for q in range(2):
                            fw.op("tensor", lambda h, c=c, q=q, hf_=hf_: h.matmul(PS[4 + q][:], HT[k][:, c, :], wdt[k][c][:, hf_ * 1024 + q * 512:hf_ * 1024 + (q + 1) * 512],
                                                                                  start=(c == 0), stop=(c == 7)), reads=[HTb[k], wdtb[k][c]], writes=[PSB[4 + q]])
                    fw.op("scalar", lambda h, hf_=hf_: h.copy(out=Ys[k][:, hf_ * 1024:hf_ * 1024 + 512], in_=PS[4][:]), reads=[PSB[4]], writes=[Ysb[k]])
                    fw.op("vector", lambda h, hf_=hf_: h.tensor_copy(out=Ys[k][:, hf_ * 1024 + 512:(hf_ + 1) * 1024], in_=PS[5][:]), reads=[PSB[5]], writes=[Ysb[k]])
                fw.dma("sync", ys_d[i * 128:(i + 1) * 128, :], Ys[k][:], reads=[Ysb[k]], writes=[Buf()])
        fw.barrier()

        if stop == "S6b":
            return nc
        with ExitStack() as st:
            g2, g2b = rep_load(st, "g2", ln2_g, D); b2, b2b = rep_load(st, "b2", ln2_b, D)
            gbb = Buf()
            fw.op("vector", lambda h: h.tensor_copy(out=g2[:, 0:1], in_=g2[:, 0:1]), reads=[g2b, b2b], writes=[gbb])
            ht = [SB(st, "f_h%d" % i, [128, D], F32) for i in range(2)]; htb = [Buf(), Buf()]
            y1 = [SB(st, "f_y1%d" % i, [128, D], F32) for i in range(2)]; y1b = [Buf(), Buf()]
            y2 = [SB(st, "f_y2%d" % i, [128, D], F32) for i in range(2)]; y2b = [Buf(), Buf()]
            st8 = SB(st, "st8c", [128, 24], F32); sb8 = Buf(); mv = SB(st, "mvc", [128, 2], F32); mvb = Buf(); rstd = SB(st, "rstdc", [128, 1], F32)
            for t in range(16):
                k = t % 2
                rows = slice(t * 128, (t + 1) * 128)
                fw.dma("sync", ht[k][:], h1_d[rows, :], writes=[htb[k]])
                fw.idma(y1[k][:, :], ys_d[:, :], SL1[:, t:t + 1], True, 8191, reads=[SLb], writes=[y1b[k]])
                fw.idma(y2[k][:, :], ys_d[:, :], SL2[:, t:t + 1], True, 8191, reads=[SLb], writes=[y2b[k]])
                fw.op("vector", lambda h: h.tensor_scalar(out=ht[k][:], in0=ht[k][:], scalar1=ALPHA, scalar2=None, op0=ALU.mult), reads=[htb[k]], writes=[htb[k]])
                fw.op("vector", lambda h, t=t: h.scalar_tensor_tensor(out=ht[k][:], in0=y1[k][:], scalar=G12[:, t, 0:1], in1=ht[k][:], op0=ALU.mult, op1=ALU.add),
                      reads=[y1b[k], htb[k], Gb], writes=[htb[k]])
                fw.op("vector", lambda h, t=t: h.scalar_tensor_tensor(out=ht[k][:], in0=y2[k][:], scalar=G12[:, t, 1:2], in1=ht[k][:], op0=ALU.mult, op1=ALU.add),
                      reads=[y2b[k], htb[k], Gb], writes=[htb[k]])
                layernorm(ht[k], htb[k], g2, b2, gbb, st8, sb8, mv, mvb, rstd)
                fw.dma("sync", out[rows, :], ht[k][:], reads=[htb[k]], writes=[Buf()])
        persist2.close()
        fw.barrier()
    return nc


_CONST = {}


def _consts():
    if _CONST:
        return _CONST
    f32 = np.float32
    half = 64
    inv_freq = (10000.0 ** (-np.arange(0, half, 2, dtype=np.float64) / half)).astype(f32)
    row_idx = (np.arange(S) // 64).astype(f32)
    col_idx = (np.arange(S) % 64).astype(f32)
    ang_r = (row_idx[:, None] * inv_freq[None, :]).astype(f32)
    ang_c = (col_idx[:, None] * inv_freq[None, :]).astype(f32)
    cr, sr, cc, sc = np.cos(ang_r), np.sin(ang_r), np.cos(ang_c), np.sin(ang_c)
    _CONST["ropeC"] = np.concatenate([cr, cr, cc, cc], axis=1).astype(f32)
    _CONST["ropeS"] = np.concatenate([-sr, sr, -sc, sc], axis=1).astype(f32)
    n = np.arange(128, dtype=np.float64)
    a128 = 2 * np.pi * np.outer(n, n) / 128.0
    _CONST["Fc"] = np.cos(a128).astype(f32); _CONST["Fs"] = np.sin(a128).astype(f32)
    a16 = 2 * np.pi * np.outer(n, n) / float(NFFT)
    _CONST["C16"] = np.cos(a16).astype(f32); _CONST["S16"] = np.sin(a16).astype(f32)
    t = np.linspace(0.0, 1.0, S, dtype=f32)
    w = (2.0 * math.pi * np.arange(S, dtype=f32) / S).astype(f32)[:, None]
    bands = np.linspace(1e-4, 15, 16, dtype=f32)[None, :]
    feats = np.concatenate([t[:, None], np.cos(bands * w), -np.sin(bands * w)], axis=-1).astype(f32)
    _CONST["featsT"] = np.ascontiguousarray(feats.T)
    _CONST["negt"] = np.ascontiguousarray((-t).reshape(64, 128).T)
    min_decay = math.log(1e-2) / 1.5
    max_decay = math.log(1e-2) / 0.3
    _CONST["absdelta"] = np.abs(np.linspace(min_decay, max_decay, C, dtype=f32)).reshape(1, C).astype(f32)
    import ml_dtypes
    n1 = np.arange(128, dtype=np.float64)[None, :, None]
    k1 = np.arange(128, dtype=np.float64)[None, None, :]
    k2 = np.arange(128, dtype=np.float64)[:, None, None]
    th = 2 * np.pi * n1 * (128.0 * k1 + k2) / float(NFFT)
    ec, es = np.cos(th), np.sin(th)
    ect, est = ec.transpose(0, 2, 1), es.transpose(0, 2, 1)
    _CONST["cE"] = np.ascontiguousarray(np.stack([ec, es, -es, -ec, ect, est, -est], axis=2)).astype(ml_dtypes.bfloat16)
    m0 = np.ones((128, 1), f32); m0[0, 0] = 0.0
    _CONST["m0"] = m0
    return _CONST


def _in_maps(inp):
    cs = _consts()
    f32 = np.float32
    x = np.asarray(inp["x"], f32)
    common = {
        "ln_in_g": inp["ln_in_g"].reshape(1, D), "ln_in_b": inp["ln_in_b"].reshape(1, D),
        "w_in": inp["w_in"][0], "b_gate": inp["b_gate"][0].reshape(1, 4096),
        "q_norm_g": inp["q_norm_g"][0].reshape(1, 128), "k_norm_g": inp["k_norm_g"][0].reshape(1, 128),
        "hy_conv_w": inp["hy_conv_w"][0], "hy_conv_b": inp["hy_conv_b"][0].reshape(1, 3072),
        "filt_w1": inp["filt_w1"][0], "filt_b1": inp["filt_b1"][0].reshape(64, 1), "filt_f1": inp["filt_f1"][0].reshape(64, 1),
        "filt_w2": inp["filt_w2"][0], "filt_b2": inp["filt_b2"][0].reshape(64, 1), "filt_f2": inp["filt_f2"][0].reshape(64, 1),
        "filt_w3": inp["filt_w3"][0], "hy_bias_d": inp["hy_bias_d"][0].reshape(1, C),
        "w_attn_o": inp["w_attn_o"][0], "w_hy_o": inp["w_hy_o"][0], "w_out": inp["w_out"][0],
        "ln1_g": inp["ln1_g"][0].reshape(1, D), "ln1_b": inp["ln1_b"][0].reshape(1, D),
        "w_route": np.concatenate([inp["w_route_grp"][0], inp["w_route_exp"][0]], axis=1),
        "b_route": np.concatenate([inp["b_route_grp"][0], inp["b_route_exp"][0]], axis=0).reshape(1, 36),
        "w_eg": inp["w_exp_gate"][0], "w_eu": inp["w_exp_up"][0], "w_ed": inp["w_exp_down"][0],
        "ln2_g": inp["ln2_g"][0].reshape(1, D), "ln2_b": inp["ln2_b"][0].reshape(1, D),
        "ropeC_k": cs["ropeC"], "ropeS_k": cs["ropeS"],
        "cFc": cs["Fc"], "cFs": cs["Fs"], "cFsn": -cs["Fs"], "cFcn": -cs["Fc"],
        "cC16": cs["C16"], "cS16": cs["S16"], "cS16n": -cs["S16"],
        "pcol": np.arange(128, dtype=np.float32).reshape(128, 1), "featsT": cs["featsT"], "negt": cs["negt"], "absdelta": cs["absdelta"], "m0": cs["m0"],
    }
    common = {k: np.ascontiguousarray(np.asarray(v, f32)) for k, v in common.items()}
    common["cE"] = cs["cE"]
    maps = []
    for c in range(8):
        b, j = c // 4, c % 4
        q0 = j * T
        xo = np.zeros((NOWN, D), f32); mo = np.zeros((NOWN, 1), f32)
        lo = max(q0 - 1, 0); hi = min(q0 + T + 1, S)
        r0 = lo - (q0 - 1)
        xo[r0:r0 + (hi - lo)] = x[b, lo:hi]
        mo[r0:r0 + (hi - lo)] = 1.0
        m = dict(common)
        m["x_seq"] = np.ascontiguousarray(x[b]); m["x_own"] = xo; m["m_own"] = np.ascontiguousarray(mo.reshape(17, 128).T)
        m["ropeC_q"] = np.ascontiguousarray(cs["ropeC"][q0:q0 + T]); m["ropeS_q"] = np.ascontiguousarray(cs["ropeS"][q0:q0 + T])
        n2 = np.arange(16 * j, 16 * j + 16)
        m["ci2re"] = np.ascontiguousarray(cs["Fc"][:, n2] / float(NFFT)).astype(f32)
        m["ci2im"] = np.ascontiguousarray(-cs["Fs"][:, n2] / float(NFFT)).astype(f32)
        maps.append(m)
    return maps


def kernel(**inputs):
    inp = {k: np.asarray(v) for k, v in inputs.items()}
    nc = build_nc()
    maps = _in_maps(inp)
    res = run_bass_kernel_spmd(nc, maps, core_ids=list(range(8)))
    outp = np.zeros((2, S, D), np.float32)
    for c in range(8):
        b, j = c // 4, c % 4
        outp[b, j * T:(j + 1) * T] = res.results[c]["out"]
    return outp
```

```python
import math
from contextlib import ExitStack
import numpy as np
import concourse.bass as bass
import concourse.mybir as mybir
from concourse.bass_utils import run_bass_kernel_spmd

F32 = mybir.dt.float32
BF16 = mybir.dt.bfloat16
ALU = mybir.AluOpType
AF = mybir.ActivationFunctionType

D = 2048
S = 8192
T = 2048
NOWN = 17 * 128
C = 1024
INW = 8704
ALPHA = 2.0 ** 0.25
LN_EPS = 1e-5
QK_EPS = 1e-6
FILTER_EPS = 1e-6
NFFT = 16384
PI = math.pi


class Buf:
    __slots__ = ("w", "r")

    def __init__(self):
        self.w = None
        self.r = []


class FW:
    ENGS = ("tensor", "vector", "scalar", "gpsimd", "sync")

    def __init__(self, nc, stack, ndma=20):
        self.nc = nc
        self.cnt = {e: 0 for e in self.ENGS}
        self.seen = {e: {} for e in self.ENGS}
        self.sems = {}
        self.ndma = ndma
        self.dma_next = {e: 0 for e in self.ENGS}
        self.dma_tot = {}
        for e in self.ENGS:
            self.sems[e] = stack.enter_context(nc.semaphore("s_" + e))
        for q in ("sync", "gpsimd", "scalar"):
            for i in range(ndma):
                k = ("d", q, i)
                self.sems[k] = stack.enter_context(nc.semaphore("d_%s_%d" % (q, i)))
                self.dma_tot[k] = 0

    def _waits(self, eng, reads, writes):
        deps = {}

        def add(t):
            if t is not None and deps.get(t[0], 0) < t[1]:
                deps[t[0]] = t[1]
        for b in reads:
            add(b.w)
        for b in writes:
            add(b.w)
            for t in b.r:
                add(t)
        out = []
        seen = self.seen[eng]
        for key, val in deps.items():
            if key == eng and eng == "tensor":
                continue
            if seen.get(key, 0) >= val:
                continue
            seen[key] = val
            out.append((key, val))
        return out

    def _mark(self, tag, reads, writes):
        for b in writes:
            b.w = tag
            b.r = []
        for b in reads:
            b.r = [t for t in b.r if t[0] != tag[0]] + [tag]

    def op(self, eng, fn, reads=(), writes=()):
        waits = self._waits(eng, reads, writes)
        self.cnt[eng] += 1
        tag = (eng, self.cnt[eng])
        h = getattr(self.nc, eng)
        for key, val in waits:
            h.wait_ge(self.sems[key], val)
        fn(h).then_inc(self.sems[eng], 1)
        self._mark(tag, reads, writes)

    def idma(self, out, in_, idx, gather, bound, reads=(), writes=()):
        if not hasattr(self, "bregs"):
            self.bregs = {}
        if bound not in self.bregs:
            r = self.nc.gpsimd.alloc_register("bnd%d" % bound)
            self.nc.gpsimd.reg_mov(r, bound)
            self.bregs[bound] = r
        bound = self.bregs[bound]
        if gather:
            fn = lambda h: h.indirect_dma_start(out=out, out_offset=None, in_=in_, in_offset=bass.IndirectOffsetOnAxis(ap=idx, axis=0),
                                                bounds_check=bound, oob_is_err=False)
        else:
            fn = lambda h: h.indirect_dma_start(out=out, out_offset=bass.IndirectOffsetOnAxis(ap=idx, axis=0), in_=in_, in_offset=None,
                                                bounds_check=bound, oob_is_err=False)
        self.dma("gpsimd", None, None, reads=reads, writes=writes, fn=fn)

    def dma(self, q, out, in_, reads=(), writes=(), slow=False, fn=None):
        i = self.dma_next[q]
        self.dma_next[q] = (i + 1) % self.ndma
        key = ("d", q, i)
        waits = self._waits(q, reads, writes)
        prev = self.dma_tot[key]
        if prev > 0 and self.seen[q].get(key, 0) < prev:
            self.seen[q][key] = prev
            waits.append((key, prev))
        self.dma_tot[key] = prev + 16
        tag = (key, prev + 16)
        h = getattr(self.nc, q)
        for k2, val in waits:
            h.wait_ge(self.sems[k2], val)
        if fn is not None:
            inst = fn(h)
        elif slow:
            inst = h.dma_start(out=out, in_=in_, allow_slow_non_contiguous=True)
        else:
            inst = h.dma_start(out=out, in_=in_)
        inst.then_inc(self.sems[key], 16)
        self._mark(tag, reads, writes)

    def barrier(self):
        for e in self.ENGS:
            h = getattr(self.nc, e)
            seen = self.seen[e]
            for o in self.ENGS:
                if o == e or self.cnt[o] == 0:
                    continue
                if seen.get(o, 0) < self.cnt[o]:
                    seen[o] = self.cnt[o]
                    h.wait_ge(self.sems[o], self.cnt[o])
            for k, tot in self.dma_tot.items():
                if tot > 0 and seen.get(k, 0) < tot:
                    seen[k] = tot
                    h.wait_ge(self.sems[k], tot)


def build_nc(dbg=None, stop=None):
    nc = bass.Bass("TRN2", target_bir_lowering=False)
    ins = {}

    def IN(name, shape, dt=F32):
        ins[name] = nc.dram_tensor(name, list(shape), dt, kind="ExternalInput").ap()
        return ins[name]

    x_seq = IN("x_seq", [S, D]); x_own = IN("x_own", [NOWN, D]); m_own = IN("m_own", [128, 17])
    ln_in_g = IN("ln_in_g", [1, D]); ln_in_b = IN("ln_in_b", [1, D])
    w_in = IN("w_in", [D, INW]); b_gate = IN("b_gate", [1, 4096])
    q_norm_g = IN("q_norm_g", [1, 128]); k_norm_g = IN("k_norm_g", [1, 128])
    hy_conv_w = IN("hy_conv_w", [3, 3072]); hy_conv_b = IN("hy_conv_b", [1, 3072])
    filt_w1 = IN("filt_w1", [33, 64]); filt_b1 = IN("filt_b1", [64, 1]); filt_f1 = IN("filt_f1", [64, 1])
    filt_w2 = IN("filt_w2", [64, 64]); filt_b2 = IN("filt_b2", [64, 1]); filt_f2 = IN("filt_f2", [64, 1])
    filt_w3 = IN("filt_w3", [64, 2048]); hy_bias_d = IN("hy_bias_d", [1, C])
    w_attn_o = IN("w_attn_o", [1024, D]); w_hy_o = IN("w_hy_o", [C, D]); w_out = IN("w_out", [D, D])
    ln1_g = IN("ln1_g", [1, D]); ln1_b = IN("ln1_b", [1, D])
    w_route = IN("w_route", [D, 36]); b_route = IN("b_route", [1, 36])
    w_eg = IN("w_eg", [32, D, 1024]); w_eu = IN("w_eu", [32, D, 1024]); w_ed = IN("w_ed", [32, 1024, D])
    ln2_g = IN("ln2_g", [1, D]); ln2_b = IN("ln2_b", [1, D])
    ropeC_k = IN("ropeC_k", [S, 128]); ropeS_k = IN("ropeS_k", [S, 128])
    ropeC_q = IN("ropeC_q", [T, 128]); ropeS_q = IN("ropeS_q", [T, 128])
    cFc = IN("cFc", [128, 128]); cFs = IN("cFs", [128, 128]); cFsn = IN("cFsn", [128, 128]); cFcn = IN("cFcn", [128, 128])
    cC16 = IN("cC16", [128, 128]); cS16 = IN("cS16", [128, 128]); cS16n = IN("cS16n", [128, 128])
    cE = IN("cE", [128, 128, 7, 128], BF16); ci2re = IN("ci2re", [128, 16]); ci2im = IN("ci2im", [128, 16])
    pcol = IN("pcol", [128, 1]); featsT = IN("featsT", [33, S]); negt = IN("negt", [128, 64]); absdelta = IN("absdelta", [1, C]); m0 = IN("m0", [128, 1])

    out = nc.dram_tensor("out", [T, D], F32, kind="ExternalOutput").ap()

    def DR(name, shape, dt):
        kind = "ExternalOutput" if (dbg and name in dbg) else "Internal"
        return nc.dram_tensor(name, list(shape), dt, kind=kind).ap()

    hT_d = DR("hT_d", [D, S + 2], BF16); hTo_d = DR("hTo_d", [D, NOWN], BF16); ho_d = DR("ho_d", [NOWN, D], F32)
    kT_d = DR("kT_d", [128, 2, S], BF16); v_d = DR("v_d", [S, 256], BF16); qT_d = DR("qT_d", [128, 8, T], BF16)
    p_d = DR("p_d", [S + 2, 2048], BF16); po_d = DR("po_d", [NOWN, 1024], BF16); z_d = DR("z_d", [S, C], BF16); x0_d = DR("x0_d", [T, C], BF16); g_d = DR("g_d", [T, 4096], BF16)
    at_d = DR("at_d", [128, 8, T], BF16)
    hf_d = DR("hf_d", [S, C], BF16); hg_d = DR("hg_d", [S, C], BF16)
    a_d = [[DR("a_d%d%d" % (i, r), [128, 128, C], BF16) for r in range(2)] for i in range(3)]
    hh_d = [DR("hh_d%d" % r, [128, 128, C], BF16) for r in range(2)]
    b_d = [DR("b_d%d" % r, [128, 128, C], BF16) for r in range(2)]
    y_d = DR("y_d", [T, C], F32)
    xs_d = DR("xs_d", [8192, D], BF16); ys_d = DR("ys_d", [8192, D], F32); preT_d = DR("preT_d", [D, T], BF16); h1_d = DR("h1_d", [T, D], F32); h1T_d = DR("h1T_d", [D, T], BF16)

    with ExitStack() as top:
        fw = FW(nc, top)
        Dm = {}

        def dbuf(ap_name):
            return Buf()

        uid = [0]

        def SB(st, name, shape, dt):
            uid[0] += 1
            return st.enter_context(nc.sbuf_tensor("%s_u%d" % (name, uid[0]), list(shape), dt))

        PS = [top.enter_context(nc.psum_tensor("ps%d" % i, [128, 512], F32)) for i in range(6)]
        PSB = [Buf() for _ in range(6)]
        PT = [top.enter_context(nc.psum_tensor("pt%d" % i, [128, 1024], BF16)) for i in range(2)]
        PTB = [Buf() for _ in range(2)]

        ident = SB(top, "ident", [128, 128], BF16); identf = SB(top, "identf", [128, 128], F32)
        ones16 = SB(top, "ones16", [128, 128], BF16); onesf = SB(top, "onesf", [128, 128], F32)
        Bc = Buf()
        fw.op("gpsimd", lambda h: h.memset(identf[:], 1.0), writes=[Bc])
        fw.op("gpsimd", lambda h: h.affine_select(out=identf[:], in_=identf[:], pattern=[[-1, 128]],
                                                   compare_op=ALU.is_equal, fill=0.0, base=0, channel_multiplier=1),
              reads=[Bc], writes=[Bc])
        fw.op("vector", lambda h: h.tensor_copy(out=ident[:], in_=identf[:]), reads=[Bc], writes=[Bc])
        fw.op("vector", lambda h: h.memset(ones16[:], 1.0), writes=[Bc])
        fw.op("vector", lambda h: h.memset(onesf[:], 1.0), writes=[Bc])

        def rep_load(st, name, src_row, n, dt=F32, q="sync"):
            t = SB(st, name, [128, n], dt)
            b = Buf()
            fw.dma(q, t[:], src_row.to_broadcast([128, n]), writes=[b])
            return t, b

        def layernorm(xt, xb, grep, brep, gb, st8, sb8, mv, mvb, rstd, eps=LN_EPS):
            for i in range(4):
                fw.op("vector", lambda h, i=i: h.bn_stats(out=st8[:, i * 6:(i + 1) * 6], in_=xt[:, i * 512:(i + 1) * 512]),
                      reads=[xb], writes=[sb8])
            fw.op("vector", lambda h: h.bn_aggr(out=mv[:], in_=st8[:]), reads=[sb8], writes=[mvb])
            fw.op("scalar", lambda h: h.activation(out=rstd[:], in_=mv[:, 1:2], func=AF.Sqrt, bias=eps, scale=1.0),
                  reads=[mvb], writes=[sb8])
            fw.op("vector", lambda h: h.reciprocal(out=rstd[:], in_=rstd[:]), reads=[sb8], writes=[sb8])
            fw.op("vector", lambda h: h.tensor_scalar(out=xt[:], in0=xt[:], scalar1=mv[:, 0:1], scalar2=rstd[:, 0:1],
                                                       op0=ALU.subtract, op1=ALU.mult), reads=[xb, mvb, sb8], writes=[xb])
            fw.op("vector", lambda h: h.tensor_tensor(out=xt[:], in0=xt[:], in1=grep[:], op=ALU.mult), reads=[xb, gb], writes=[xb])
            fw.op("vector", lambda h: h.tensor_tensor(out=xt[:], in0=xt[:], in1=brep[:], op=ALU.add), reads=[xb, gb], writes=[xb])

        def transpose_to(src16, srcb, dst, dstb, nchunk, col0=0, ncols=128):
            for g4 in range(0, nchunk, 8):
                n = min(8, nchunk - g4)
                pi = (g4 // 8) % 2
                for c in range(n):
                    fw.op("tensor", lambda h, c=c: h.transpose(PT[pi][:, c * 128:c * 128 + ncols],
                                                               src16[0:ncols, (g4 + c) * 128:(g4 + c + 1) * 128], ident[0:ncols, 0:ncols]),
                          reads=[srcb, Bc], writes=[PTB[pi]])
                eng = "vector" if pi == 0 else "scalar"
                if eng == "vector":
                    fw.op("vector", lambda h: h.tensor_copy(
                        out=dst[:, g4:g4 + n, col0:col0 + ncols],
                        in_=PT[pi][:, 0:n * 128].rearrange("p (c t) -> p c t", t=128)[:, :, 0:ncols]),
                        reads=[PTB[pi]], writes=[dstb])
                else:
                    fw.op("scalar", lambda h: h.copy(
                        out=dst[:, g4:g4 + n, col0:col0 + ncols],
                        in_=PT[pi][:, 0:n * 128].rearrange("p (c t) -> p c t", t=128)[:, :, 0:ncols]),
                        reads=[PTB[pi]], writes=[dstb])

        with ExitStack() as st:
            grep, gb = rep_load(st, "lng", ln_in_g, D)
            brep, bb_ = rep_load(st, "lnb", ln_in_b, D)
            gbb = Buf()
            fw.op("vector", lambda h: h.tensor_copy(out=grep[:, 0:1], in_=grep[:, 0:1]), reads=[gb, bb_], writes=[gbb])
            xts = [SB(st, "xt%d" % i, [128, D], F32) for i in range(2)]; xbs = [Buf(), Buf()]
            h16 = [SB(st, "h16_%d" % i, [128, D], BF16) for i in range(2)]; h16b = [Buf(), Buf()]
            hTs = [SB(st, "hTs%d" % i, [128, 16, 512], BF16) for i in range(2)]; hTb = [Buf(), Buf()]
            st8 = SB(st, "st8", [128, 24], F32); sb8 = Buf(); mv = SB(st, "mv", [128, 2], F32); mvb = Buf()
            rstd = SB(st, "rstd", [128, 1], F32)
            mk = SB(st, "mk", [128, 17], F32); mkb = Buf()
            zt = SB(st, "zt", [128, 16, 1], BF16); ztb = Buf()
            fw.op("vector", lambda h: h.memset(zt[:], 0.0), writes=[ztb])
            hTv = hT_d.rearrange("(c p) s -> p c s", p=128)
            fw.dma("gpsimd", hTv[:, :, 0:1], zt[:], reads=[ztb], writes=[dbuf("hT_d")], slow=True)
            fw.dma("gpsimd", hTv[:, :, S + 1:S + 2], zt[:], reads=[ztb], writes=[dbuf("hT_d")], slow=True)
            fw.dma("sync", mk[:], m_own, writes=[mkb])
            hTov = hTo_d.rearrange("(c p) s -> p c s", p=128)
            it = 0
            for grp in range(16 + 5):
                own = grp >= 16
                ntile = 4 if not own else (4 if grp < 20 else 1)
                hb = grp % 2
                for ti in range(ntile):
                    tile = (grp * 4 + ti) if not own else ((grp - 16) * 4 + ti)
                    k = it % 2; it += 1
                    src = x_seq if not own else x_own
                    fw.dma("sync", xts[k][:], src[tile * 128:(tile + 1) * 128, :], writes=[xbs[k]])
                    layernorm(xts[k], xbs[k], grep, brep, gbb, st8, sb8, mv, mvb, rstd)
                    if own:
                        fw.op("scalar", lambda h, k=k, tile=tile: h.activation(out=xts[k][:], in_=xts[k][:], func=AF.Identity,
                                                                                scale=mk[:, tile:tile + 1]),
                              reads=[xbs[k], mkb], writes=[xbs[k]])
                        fw.dma("gpsimd", ho_d[tile * 128:(tile + 1) * 128, :], xts[k][:], reads=[xbs[k]], writes=[dbuf("ho_d")])
                    fw.op("scalar", lambda h, k=k: h.copy(out=h16[k][:], in_=xts[k][:]), reads=[xbs[k]], writes=[h16b[k]])
                    transpose_to(h16[k], h16b[k], hTs[hb], hTb[hb], 16, col0=ti * 128)
                if not own:
                    fw.dma("gpsimd", hTv[:, :, 1 + grp * 512:1 + grp * 512 + 512], hTs[hb][:], reads=[hTb[hb]], writes=[dbuf("hT_d")])
                else:
                    g0 = (grp - 16) * 512
                    fw.dma("gpsimd", hTov[:, :, g0:g0 + ntile * 128], hTs[hb][:, :, 0:ntile * 128], reads=[hTb[hb]], writes=[dbuf("hTo_d")])
        fw.barrier()

        w_in_v = w_in.rearrange("(c p) n -> p c n", p=128)

        def proj_pass(st, src_v, srcname, ntok_tiles, blocks, epilogue, sh0=1, src_w=None):
            nb = len(blocks)
            wts = []
            wb = Buf()
            for bi, (col0, cidx, bias) in enumerate(blocks):
                nsh = 3 if cidx is not None else 1
                for sh in range(nsh):
                    wt = SB(st, "w_%d_%d" % (bi, sh), [128, 16, 512], BF16)
                    fw.dma("gpsimd", wt[:], w_in_v[:, :, col0:col0 + 512], writes=[wb])
                    if cidx is not None:
                        cw, cwb = rep_load(st, "cw_%d_%d" % (bi, sh), hy_conv_w[sh:sh + 1, cidx:cidx + 512], 512)
                        for c in range(16):
                            fw.op("vector", lambda h, c=c, wt=wt, cw=cw: h.tensor_tensor(out=wt[:, c, :], in0=wt[:, c, :], in1=cw[:], op=ALU.mult),
                                  reads=[wb, cwb], writes=[wb])
                    wts.append((bi, sh if cidx is not None else sh0, wt))
                if bias is not None:
                    b16 = SB(st, "b16_%d" % bi, [1, 512], BF16)
                    fw.dma("gpsimd", b16[:], bias, writes=[wb])
                    wts.append((bi, -1, b16))
            hw = [SB(st, "hw%d" % i, [128, 16, 514], BF16) for i in range(2)]; hwb = [Buf(), Buf()]
            ngrp = (ntok_tiles + 3) // 4
            for g in range(ngrp):
                k = g % 2
                nt = min(4, ntok_tiles - g * 4)
                wdt = nt * 128 + 2
                if src_w is not None:
                    wdt = min(wdt, src_w - g * 512)
                fw.dma("sync", hw[k][:, :, 0:wdt], src_v[:, :, g * 512:g * 512 + wdt], reads=[dbuf(srcname)], writes=[hwb[k]])
                for m in range(nt):
                    tile = g * 4 + m
                    pidx = [(tile * nb + bi) % 6 for bi in range(nb)]
                    for bi in range(nb):
                        mine = [(sh, wt) for (b2, sh, wt) in wts if b2 == bi]
                        nsteps = sum(16 if sh >= 0 else 1 for sh, _ in mine)
                        step = 0
                        for sh, wt in mine:
                            if sh < 0:
                                fw.op("tensor", lambda h, wt=wt, p=pidx[bi], s0=(step == 0), s1=(step == nsteps - 1):
                                      h.matmul(PS[p][:], ones16[0:1, :], wt[:], start=s0, stop=s1), reads=[wb, Bc], writes=[PSB[pidx[bi]]])
                                step += 1
                                continue
                            for c in range(16):
                                fw.op("tensor", lambda h, wt=wt, c=c, p=pidx[bi], off=m * 128 + sh, s0=(step == 0), s1=(step == nsteps - 1), k=k:
                                      h.matmul(PS[p][:], hw[k][:, c, off:off + 128], wt[:, c, :], start=s0, stop=s1),
                                      reads=[wb, hwb[k]], writes=[PSB[pidx[bi]]])
                                step += 1
                    epilogue(tile, pidx)

        def qk_epilogue_factory(st, gsrc, ropeC, ropeS, nheads_list, dstT, dstname, pref):
            grep_, gb_ = rep_load(st, pref + "g", gsrc, 128)
            ss = SB(st, pref + "ss", [128, 4], F32); ssb = Buf()
            junk = SB(st, pref + "junk", [128, 128], F32)
            xn = SB(st, pref + "xn", [128, 128], F32); xnb = Buf()
            t1 = SB(st, pref + "t1", [128, 128], F32); t2 = SB(st, pref + "t2", [128, 128], F32); tb_ = Buf()
            x16 = [SB(st, pref + "x16%d" % i, [128, 128], BF16) for i in range(2)]; x16b = [Buf(), Buf()]
            rc = [SB(st, pref + "rc%d" % i, [128, 128], F32) for i in range(2)]
            rs = [SB(st, pref + "rs%d" % i, [128, 128], F32) for i in range(2)]; rb = [Buf(), Buf()]
            stg = SB(st, pref + "stg", [128, 8, 128], BF16); stgb = Buf()
            cnt = [0]

            def fn(tile, p, heads):
                k = tile % 2
                fw.dma("sync", rc[k][:], ropeC[tile * 128:(tile + 1) * 128, :], writes=[rb[k]])
                fw.dma("sync", rs[k][:], ropeS[tile * 128:(tile + 1) * 128, :], writes=[rb[k]])
                for hi, (co, dh) in enumerate(heads):
                    fw.op("scalar", lambda h, hi=hi, co=co: h.activation(out=junk[:], in_=PS[p][:, co:co + 128], func=AF.Square,
                                                                         accum_out=ss[:, hi:hi + 1]), reads=[PSB[p]], writes=[ssb])
                nh = len(heads)
                fw.op("scalar", lambda h: h.activation(out=ss[:, 0:nh], in_=ss[:, 0:nh], func=AF.Sqrt, bias=QK_EPS, scale=1.0 / 128),
                      reads=[ssb], writes=[ssb])
                fw.op("vector", lambda h: h.reciprocal(out=ss[:, 0:nh], in_=ss[:, 0:nh]), reads=[ssb], writes=[ssb])
                for hi, (co, dh) in enumerate(heads):
                    j = cnt[0] % 2; cnt[0] += 1
                    fw.op("vector", lambda h, hi=hi, co=co: h.scalar_tensor_tensor(out=xn[:], in0=PS[p][:, co:co + 128], scalar=ss[:, hi:hi + 1],
                                                                                  in1=grep_[:], op0=ALU.mult, op1=ALU.mult),
                          reads=[PSB[p], ssb, gb_], writes=[xnb])
                    fw.op("vector", lambda h: h.tensor_tensor(out=t1[:], in0=xn[:], in1=rc[k][:], op=ALU.mult), reads=[xnb, rb[k]], writes=[tb_])
                    xv = xn[:].rearrange("p (a h d) -> p a h d", a=2, h=2)
                    sv = rs[k][:].rearrange("p (a h d) -> p a h d", a=2, h=2)
                    tv = t2[:].rearrange("p (a h d) -> p a h d", a=2, h=2)
                    fw.op("vector", lambda h: h.tensor_tensor(out=tv[:, :, 0, :], in0=xv[:, :, 1, :], in1=sv[:, :, 0, :], op=ALU.mult),
                          reads=[xnb, rb[k]], writes=[tb_])
                    fw.op("vector", lambda h: h.tensor_tensor(out=tv[:, :, 1, :], in0=xv[:, :, 0, :], in1=sv[:, :, 1, :], op=ALU.mult),
                          reads=[xnb, rb[k]], writes=[tb_])
                    fw.op("vector", lambda h, j=j: h.tensor_tensor(out=x16[j][:], in0=t1[:], in1=t2[:], op=ALU.add), reads=[tb_], writes=[x16b[j]])
                    fw.op("tensor", lambda h, j=j, hi=hi: h.transpose(PT[0][:, hi * 128:(hi + 1) * 128], x16[j][:], ident[:]),
                          reads=[x16b[j], Bc], writes=[PTB[0]])
                fw.op("scalar", lambda h: h.copy(out=stg[:, 0:nh, :], in_=PT[0][:, 0:nh * 128].rearrange("p (c t) -> p c t", t=128)),
                      reads=[PTB[0]], writes=[stgb])
                for hi, (co, dh) in enumerate(heads):
                    fw.dma("gpsimd", dstT[:, dh, tile * 128:(tile + 1) * 128], stg[:, hi, :], reads=[stgb], writes=[dbuf(dstname)])
            return fn

        hTv = hT_d.rearrange("(c p) s -> p c s", p=128)
        hTov = hTo_d.rearrange("(c p) s -> p c s", p=128)

        if stop == "S0":
            return nc
        with ExitStack() as st:
            kfn = qk_epilogue_factory(st, k_norm_g, ropeC_k, ropeS_k, 2, kT_d, "kT_d", "k")
            v16 = [SB(st, "v16_%d" % i, [128, 256], BF16) for i in range(2)]; v16b = [Buf(), Buf()]

            def kv_ep(tile, pidx):
                p = pidx[0]
                kfn(tile, p, [(0, 0), (128, 1)])
                k = tile % 2
                fw.op("scalar", lambda h: h.copy(out=v16[k][:], in_=PS[p][:, 256:512]), reads=[PSB[p]], writes=[v16b[k]])
                fw.dma("gpsimd", v_d[tile * 128:(tile + 1) * 128, :], v16[k][:], reads=[v16b[k]], writes=[dbuf("v_d")])
            proj_pass(st, hTv, "hT_d", 64, [(1024, None, None)], kv_ep)
        fw.barrier()

        if stop == "KV":
            return nc
        for qb in range(2):
            with ExitStack() as st:
                qfn = qk_epilogue_factory(st, q_norm_g, ropeC_q, ropeS_q, 4, qT_d, "qT_d", "q")

                def q_ep(tile, pidx, qb=qb):
                    qfn(tile, pidx[0], [(i * 128, qb * 4 + i) for i in range(4)])
                proj_pass(st, hTov, "hTo_d", 16, [(qb * 512, None, None)], q_ep)
            fw.barrier()

        if stop == "Q":
            return nc
        with ExitStack() as st:
            zr = SB(st, "zrow", [1, 2048], BF16); zrb = Buf()
            fw.op("vector", lambda h: h.memset(zr[:], 0.0), writes=[zrb])
            fw.dma("gpsimd", p_d[0:1, :], zr[:], reads=[zrb], writes=[Buf()])
            fw.dma("gpsimd", p_d[S + 1:S + 2, :], zr[:], reads=[zrb], writes=[Buf()])
        for cb in range(2):
            with ExitStack() as st:
                p16 = [SB(st, "p16_%d" % i, [128, 2, 512], BF16) for i in range(2)]; p16b = [Buf(), Buf()]

                def p_ep(tile, pidx, cb=cb):
                    k = tile % 2
                    fw.op("scalar", lambda h: h.copy(out=p16[k][:, 0, :], in_=PS[pidx[0]][:]), reads=[PSB[pidx[0]]], writes=[p16b[k]])
                    fw.op("vector", lambda h: h.tensor_copy(out=p16[k][:, 1, :], in_=PS[pidx[1]][:]), reads=[PSB[pidx[1]]], writes=[p16b[k]])
                    dst = p_d[1 + tile * 128:1 + (tile + 1) * 128, :].rearrange("p (a c) -> p a c", a=2)[:, :, cb * 512:(cb + 1) * 512]
                    fw.dma("gpsimd", dst, p16[k][:], reads=[p16b[k]], writes=[Buf()])
                c1 = 1024 + cb * 512; c2 = 2048 + cb * 512
                proj_pass(st, hTv, "hT_d", 64, [(1536 + c1, None, None), (1536 + c2, None, None)], p_ep)
            fw.barrier()
        with ExitStack() as st:
            p16 = [SB(st, "po16_%d" % i, [128, 2, 512], BF16) for i in range(2)]; p16b = [Buf(), Buf()]

            def po_ep(tile, pidx):
                k = tile % 2
                fw.op("scalar", lambda h: h.copy(out=p16[k][:, 0, :], in_=PS[pidx[0]][:]), reads=[PSB[pidx[0]]], writes=[p16b[k]])
                fw.op("vector", lambda h: h.tensor_copy(out=p16[k][:, 1, :], in_=PS[pidx[1]][:]), reads=[PSB[pidx[1]]], writes=[p16b[k]])
                fw.dma("gpsimd", po_d[tile * 128:(tile + 1) * 128, :].rearrange("p (a c) -> p a c", a=2), p16[k][:], reads=[p16b[k]], writes=[Buf()])
            proj_pass(st, hTov, "hTo_d", 17, [(1536, None, None), (1536 + 512, None, None)], po_ep, sh0=0, src_w=NOWN)
        fw.barrier()
        if stop == "Z":
            return nc

        def conv_stage(st, src, ntile, row0, W, ccol, emit):
            wr = []
            for i in range(3):
                wr.append(rep_load(st, "cvw%d" % i, hy_conv_w[i:i + 1, ccol:ccol + W], W))
            br_, brb_ = rep_load(st, "cvb", hy_conv_b[0:1, ccol:ccol + W], W)
            Pt = [[SB(st, "cvP%d_%d" % (k, i), [128, W], BF16) for i in range(3)] for k in range(2)]; Ptb = [[Buf() for i in range(3)] for k in range(2)]
            acc = SB(st, "cvacc", [128, W], F32); acc2 = SB(st, "cvacc2", [128, W], F32); ab = Buf(); ab2 = Buf()
            return _conv_run(src, ntile, row0, wr, br_, brb_, Pt, Ptb, acc, acc2, ab, ab2, emit)

        def _conv_run(src, ntile, row0, wr, br_, brb_, Pt, Ptb, acc, acc2, ab, ab2, emit):
            for t in range(ntile):
                k = t % 2
                for i in range(3):
                    r0 = row0 + t * 128 + i
                    fw.dma("sync", Pt[k][i][:], src[r0:r0 + 128, :], writes=[Ptb[k][i]])
                fw.op("vector", lambda h: h.tensor_tensor(out=acc[:], in0=Pt[k][0][:], in1=wr[0][0][:], op=ALU.mult), reads=[Ptb[k][0], wr[0][1], ab], writes=[ab])
                fw.op("vector", lambda h: h.tensor_tensor(out=acc2[:], in0=Pt[k][1][:], in1=wr[1][0][:], op=ALU.mult), reads=[Ptb[k][1], wr[1][1], ab2], writes=[ab2])
                fw.op("vector", lambda h: h.tensor_tensor(out=acc[:], in0=acc[:], in1=acc2[:], op=ALU.add), reads=[ab, ab2], writes=[ab])
                fw.op("vector", lambda h: h.tensor_tensor(out=acc2[:], in0=Pt[k][2][:], in1=wr[2][0][:], op=ALU.mult), reads=[Ptb[k][2], wr[2][1], ab2], writes=[ab2])
                fw.op("vector", lambda h: h.tensor_tensor(out=acc[:], in0=acc[:], in1=acc2[:], op=ALU.add), reads=[ab, ab2], writes=[ab])
                fw.op("vector", lambda h: h.tensor_tensor(out=acc[:], in0=acc[:], in1=br_[:], op=ALU.add), reads=[ab, brb_], writes=[ab])
                emit(t, acc, ab)
                yield

        zst = ExitStack()
        if True:
            st = zst
            z16 = [SB(st, "cvz%d" % i, [128, 1024], BF16) for i in range(2)]; z16b = [Buf(), Buf()]

            def z_emit(t, acc, ab):
                k = t % 2
                fw.op("vector", lambda h: h.tensor_tensor(out=z16[k][:], in0=acc[:, 0:1024], in1=acc[:, 1024:2048], op=ALU.mult), reads=[ab], writes=[z16b[k]])
                fw.dma("gpsimd", z_d[t * 128:(t + 1) * 128, :], z16[k][:], reads=[z16b[k]], writes=[Buf()])
            zgen = conv_stage(st, p_d, 64, 0, 2048, 1024, z_emit)
        with ExitStack() as st:
            o16 = [SB(st, "cvo%d" % i, [128, 1024], BF16) for i in range(2)]; o16b = [Buf(), Buf()]

            def o_emit(t, acc, ab):
                k = t % 2
                fw.op("scalar", lambda h: h.copy(out=o16[k][:], in_=acc[:]), reads=[ab], writes=[o16b[k]])
                fw.dma("gpsimd", x0_d[t * 128:(t + 1) * 128, :], o16[k][:], reads=[o16b[k]], writes=[Buf()])
            for _ in conv_stage(st, po_d, 16, 0, 1024, 0, o_emit):
                pass
        fw.barrier()

        for cb in range(4):
            with ExitStack() as st:
                o16 = [SB(st, "g16_%d" % i, [128, 1024], BF16) for i in range(2)]; o16b = [Buf(), Buf()]

                def g_ep(tile, pidx, cb=cb):
                    k = tile % 2
                    for bi in range(2):
                        fw.op("scalar", lambda h, bi=bi: h.activation(out=o16[k][:, bi * 512:(bi + 1) * 512], in_=PS[pidx[bi]][:], func=AF.Sigmoid),
                              reads=[PSB[pidx[bi]]], writes=[o16b[k]])
                    fw.dma("gpsimd", g_d[tile * 128:(tile + 1) * 128, cb * 1024:(cb + 1) * 1024], o16[k][:], reads=[o16b[k]], writes=[dbuf("g_d")])
                    next(zgen, None)
                c0 = cb * 1024
                proj_pass(st, hTov, "hTo_d", 16,
                          [(4608 + c0, None, b_gate[0:1, c0:c0 + 512]), (4608 + c0 + 512, None, b_gate[0:1, c0 + 512:c0 + 1024])], g_ep)
            fw.barrier()
        for _ in zgen:
            pass
        fw.barrier()
        zst.close()

        if stop == "S1":
            return nc
        with ExitStack() as st:
            kT = SB(st, "kT", [128, 2, S], BF16); kTb = Buf()
            vs = SB(st, "vs", [128, 64, 256], BF16); vsb = Buf()
            qT = SB(st, "qT", [128, 8, T], BF16); qTb = Buf()
            aT = SB(st, "aT", [128, 8, T], BF16); aTb = Buf()
            pT = [SB(st, "pT%d" % i, [128, 512], BF16) for i in range(3)]; pTb = [Buf() for _ in range(3)]
            rl = SB(st, "rl", [128, 512], F32); rlb = Buf()
            for hh in range(2):
                fw.dma("sync", kT[:, hh, :], kT_d[:, hh, :], reads=[dbuf("kT_d")], writes=[kTb])
            for g in range(4):
                fw.dma("sync", vs[:, g * 16:(g + 1) * 16, :], v_d[g * 2048:(g + 1) * 2048, :].rearrange("(t p) c -> p t c", p=128),
                       reads=[dbuf("v_d")], writes=[vsb])
            for g in range(4):
                fw.dma("sync", qT[:, g * 2:(g + 1) * 2, :], qT_d[:, g * 2:(g + 1) * 2, :], reads=[dbuf("qT_d")], writes=[qTb])
            sc = 1.0 / math.sqrt(128.0)
            pT4 = pT + [SB(st, "pT3", [128, 512], BF16), SB(st, "pT4x", [128, 512], BF16)]; pTb4 = pTb + [Buf(), Buf()]
            acc = [SB(st, "aacc%d" % i, [128, 512], F32) for i in range(2)]; accb = [Buf(), Buf()]
            iters = [(hd, qb, kc) for hd in range(8) for qb in range(4) for kc in range(64)]
            NIT = len(iters)

            def emit_qk(n):
                hd, qb, kc = iters[n]; kvh = hd // 4; si = n % 4; pj = n % 5
                fw.op("tensor", lambda h: h.matmul(PS[si][:], kT[:, kvh, kc * 128:(kc + 1) * 128], qT[:, hd, qb * 512:(qb + 1) * 512],
                                                   start=True, stop=True), reads=[kTb, qTb], writes=[PSB[si]])
                fw.op("scalar", lambda h: h.activation(out=pT4[pj][:], in_=PS[si][:], func=AF.Exp, scale=sc), reads=[PSB[si]], writes=[pTb4[pj]])

            def emit_pv(n):
                hd, qb, kc = iters[n]; kvh = hd // 4; pj = n % 5; g = n // 64; po = 4; a = g % 2
                fw.op("tensor", lambda h: h.matmul(PS[po][:], vs[:, kc, kvh * 128:(kvh + 1) * 128], pT4[pj][:], start=(kc == 0), stop=(kc == 63)),
                      reads=[vsb, pTb4[pj]], writes=[PSB[po]])
                if kc == 0:
                    fw.op("vector", lambda h: h.tensor_copy(out=acc[a][:], in_=pT4[pj][:]), reads=[pTb4[pj]], writes=[accb[a]])
                else:
                    fw.op("vector", lambda h: h.tensor_tensor(out=acc[a][:], in0=pT4[pj][:], in1=acc[a][:], op=ALU.add), reads=[pTb4[pj], accb[a]], writes=[accb[a]])
                if kc == 63:
                    fw.op("tensor", lambda h: h.matmul(PS[5][:], onesf[:], acc[a][:], start=True, stop=True), reads=[Bc, accb[a]], writes=[PSB[5]])
                    fw.op("vector", lambda h: h.reciprocal(out=rl[:], in_=PS[5][:]), reads=[PSB[5]], writes=[rlb])
                    fw.op("vector", lambda h: h.tensor_tensor(out=aT[:, hd, qb * 512:(qb + 1) * 512], in0=PS[po][:], in1=rl[:], op=ALU.mult),
                          reads=[PSB[po], rlb], writes=[aTb])

            emit_qk(0); emit_qk(1); emit_qk(2)
            for n in range(NIT):
                if n + 3 < NIT:
                    emit_qk(n + 3)
                emit_pv(n)
            for g in range(4):
                fw.dma("gpsimd", at_d[:, g * 2:(g + 1) * 2, :], aT[:, g * 2:(g + 1) * 2, :], reads=[aTb], writes=[dbuf("at_d")])
        fw.barrier()

        if stop == "S2":
            return nc
        persist = ExitStack()
        top.enter_context(persist)
        scl = SB(persist, "scl", [128, C], F32); sclb = Buf()
        drep, drepb = rep_load(persist, "drep", hy_bias_d, C)
        with ExitStack() as st:
            w1 = SB(st, "fw1", [33, 64], F32); w2 = SB(st, "fw2", [64, 64], F32); w3 = SB(st, "fw3", [64, 2048], F32)
            fb = SB(st, "fb", [64, 4], F32); fbb = Buf(); wl = Buf()
            fw.dma("sync", w1[:], filt_w1, writes=[wl]); fw.dma("sync", w2[:], filt_w2, writes=[wl]); fw.dma("sync", w3[:], filt_w3, writes=[wl])
            fw.dma("sync", fb[:, 0:1], filt_f1, writes=[fbb]); fw.dma("sync", fb[:, 1:2], filt_b1, writes=[fbb])
            fw.dma("sync", fb[:, 2:3], filt_f2, writes=[fbb]); fw.dma("sync", fb[:, 3:4], filt_b2, writes=[fbb])
            fbp = SB(st, "fbp", [64, 2], F32)
            fw.op("vector", lambda h: h.tensor_tensor(out=fbp[:, 0:1], in0=fb[:, 0:1], in1=fb[:, 1:2], op=ALU.mult), reads=[fbb], writes=[fbb])
            fw.op("vector", lambda h: h.tensor_tensor(out=fbp[:, 1:2], in0=fb[:, 2:3], in1=fb[:, 3:4], op=ALU.mult), reads=[fbb], writes=[fbb])
            fT = SB(st, "fT", [33, S], F32); fTb = Buf()
            fw.dma("sync", fT[:], featsT, writes=[fTb])
            h1T = SB(st, "fh1T", [64, 512], F32); h1b = Buf()
            h2T = SB(st, "fh2T", [64, S], F32); h2b = Buf()
            ar = SB(st, "far", [64, 512], F32); m1 = SB(st, "fm1", [64, 512], F32); m2 = SB(st, "fm2", [64, 512], F32); arb = Buf()

            def sin_layer(pidx, fcol, bcol, dst_ap, dstb):
                fw.op("scalar", lambda h: h.activation(out=ar[:], in_=PS[pidx][0:64, :], func=AF.Identity, scale=fb[:, fcol:fcol + 1], bias=fbp[:, bcol:bcol + 1]),
                      reads=[PSB[pidx], fbb], writes=[arb])
                fw.op("vector", lambda h: h.tensor_scalar(out=m1[:], in0=ar[:], scalar1=PI, scalar2=-2 * PI, op0=ALU.is_gt, op1=ALU.mult), reads=[arb], writes=[arb])
                fw.op("vector", lambda h: h.tensor_scalar(out=m2[:], in0=ar[:], scalar1=-PI, scalar2=2 * PI, op0=ALU.is_lt, op1=ALU.mult), reads=[arb], writes=[arb])
                fw.op("vector", lambda h: h.tensor_tensor(out=ar[:], in0=ar[:], in1=m1[:], op=ALU.add), reads=[arb], writes=[arb])
                fw.op("vector", lambda h: h.tensor_tensor(out=ar[:], in0=ar[:], in1=m2[:], op=ALU.add), reads=[arb], writes=[arb])
                fw.op("scalar", lambda h: h.activation(out=dst_ap, in_=ar[:], func=AF.Sin), reads=[arb], writes=[dstb])

            for pb in range(16):
                fw.op("tensor", lambda h, pb=pb: h.matmul(PS[0][0:64, :], w1[:], fT[:, pb * 512:(pb + 1) * 512], start=True, stop=True),
                      reads=[wl, fTb], writes=[PSB[0]])
                sin_layer(0, 0, 0, h1T[:], h1b)
                fw.op("tensor", lambda h: h.matmul(PS[1][0:64, :], w2[:], h1T[:], start=True, stop=True), reads=[wl, h1b], writes=[PSB[1]])
                sin_layer(1, 2, 1, h2T[:, pb * 512:(pb + 1) * 512], h2b)
            h2T16 = SB(st, "fh2T16", [64, S], BF16); h2b16 = Buf(); w316 = SB(st, "fw316", [64, 2048], BF16); wl16 = Buf()
            fw.op("vector", lambda h: h.tensor_copy(out=w316[:], in_=w3[:]), reads=[wl], writes=[wl16])
            for q_ in range(4):
                fw.op("scalar" if q_ % 2 else "vector", (lambda h, q_=q_: h.copy(out=h2T16[:, q_ * 2048:(q_ + 1) * 2048], in_=h2T[:, q_ * 2048:(q_ + 1) * 2048])) if q_ % 2 else
                      (lambda h, q_=q_: h.tensor_copy(out=h2T16[:, q_ * 2048:(q_ + 1) * 2048], in_=h2T[:, q_ * 2048:(q_ + 1) * 2048])), reads=[h2b], writes=[h2b16])
            adl, adlb = rep_load(st, "adl", absdelta, C)
            ngt = SB(st, "ngt", [128, 64], F32); m0s = SB(st, "m0s", [128, 1], F32); ngb = Buf()
            fw.dma("sync", ngt[:], negt, writes=[ngb]); fw.dma("sync", m0s[:], m0, writes=[ngb])
            dec = [SB(st, "dec%d" % i, [128, C], F32) for i in range(2)]; decb = [Buf(), Buf()]
            fo = [SB(st, "fo%d" % i, [128, 2048], F32) for i in range(2)]; fob = [Buf(), Buf()]
            fo16 = [SB(st, "fo16_%d" % i, [128, 2048], BF16) for i in range(2)]; fo16b = [Buf(), Buf()]
            sq = [SB(st, "fsq%d" % i, [128, 2048], BF16) for i in range(2)]; sqb = [Buf(), Buf()]
            for pt in range(64):
                k = pt % 2
                fw.op("scalar", lambda h: h.activation(out=dec[k][:], in_=adl[:], func=AF.Exp, scale=ngt[:, pt:pt + 1]), reads=[adlb, ngb], writes=[decb[k]])
                for cb in range(4):
                    fw.op("tensor", lambda h, cb=cb: h.matmul(PS[cb][:], h2T16[:, pt * 128:(pt + 1) * 128], w316[:, cb * 512:(cb + 1) * 512], start=True, stop=True),
                          reads=[wl16, h2b16], writes=[PSB[cb]])
                    fw.op("vector", lambda h, cb=cb: h.tensor_tensor(out=fo[k][:, cb * 512:(cb + 1) * 512], in0=PS[cb][:],
                                                                    in1=dec[k][:, (cb % 2) * 512:(cb % 2) * 512 + 512], op=ALU.mult),
                          reads=[PSB[cb], decb[k]], writes=[fob[k]])
                if pt == 0:
                    fw.op("vector", lambda h: h.tensor_scalar(out=fo[k][:, 1024:2048], in0=fo[k][:, 1024:2048], scalar1=m0s[:, 0:1], scalar2=None, op0=ALU.mult),
                          reads=[fob[k], ngb], writes=[fob[k]])
                fw.op("scalar", lambda h: h.copy(out=fo16[k][:], in_=fo[k][:]), reads=[fob[k]], writes=[fo16b[k]])
                fw.op("scalar", lambda h: h.activation(out=sq[k][:], in_=fo[k][:], func=AF.Square), reads=[fob[k]], writes=[sqb[k]])
                for cb in range(4):
                    fw.op("tensor", lambda h, cb=cb: h.matmul(PS[4 + cb % 2][:], ones16[:], sq[k][:, cb * 512:(cb + 1) * 512],
                                                              start=(pt == 0 and cb < 2), stop=(pt == 63 and cb >= 2)), reads=[Bc, sqb[k]], writes=[PSB[4 + cb % 2]])
                fw.dma("gpsimd", hf_d[pt * 128:(pt + 1) * 128, :], fo16[k][:, 0:1024], reads=[fo16b[k]], writes=[dbuf("hf_d")])
                fw.dma("gpsimd", hg_d[pt * 128:(pt + 1) * 128, :], fo16[k][:, 1024:2048], reads=[fo16b[k]], writes=[dbuf("hg_d")])
            for cb in range(2):
                fw.op("scalar", lambda h, cb=cb: h.activation(out=scl[:, cb * 512:(cb + 1) * 512], in_=PS[4 + cb][:], func=AF.Sqrt, bias=FILTER_EPS, scale=1.0),
                      reads=[PSB[4 + cb]], writes=[sclb])
            fw.op("vector", lambda h: h.reciprocal(out=scl[:], in_=scl[:]), reads=[sclb], writes=[sclb])
        fw.barrier()

        if stop == "S3a":
            return nc
        with ExitStack() as st:
            def cload(name, src, shape, dt=BF16):
                t = SB(st, name, shape, dt); b = Buf()
                fw.dma("gpsimd" if dt == BF16 else "sync", t[:], src, writes=[b])
                return t, b
            Fc, Fb1 = cload("Fc", cFc, [128, 128]); Fsn, Fb3 = cload("Fsn", cFsn, [128, 128])
            i2re, Fb8 = cload("i2re", ci2re, [128, 16]); i2im, Fb9 = cload("i2im", ci2im, [128, 16])
            FB = Buf()
            fw.op("vector", lambda h: h.tensor_copy(out=Fc[:, 0:1], in_=Fc[:, 0:1]), reads=[Fb1, Fb3, Fb8, Fb9], writes=[FB])

            st1 = ExitStack()
            zt_ = [SB(st1, "f1z%d" % i, [128, 16, 1024], BF16) for i in range(2)]; ztb_ = [Buf(), Buf()]
            for i_ in range(2):
                fw.op("vector", lambda h, i_=i_: h.memset(zt_[i_][64:128, :, :], 0.0), writes=[ztb_[i_]])
            ao = [SB(st1, "f1o%d" % i, [128, 2, 4, 1024], BF16) for i in range(2)]; aob = [Buf(), Buf()]

            def f1_pass(src, dst):
                sv = src.rearrange("(a b) c -> a b c", b=128)
                it = 0
                for nb in range(8):
                    k = nb % 2
                    fw.dma("sync", zt_[k][0:64, :, :], sv[:, nb * 16:(nb + 1) * 16, :], writes=[ztb_[k]])
                    for nn in range(16):
                        n1 = nb * 16 + nn
                        j = (n1 // 4) % 2; q = n1 % 4
                        for hf_ in range(2):
                            cs = slice(hf_ * 512, (hf_ + 1) * 512)
                            pr = (it % 3) * 2; pi_ = pr + 1; it += 1
                            fw.op("tensor", lambda h, nn=nn, pr=pr, cs=cs: h.matmul(PS[pr][:], Fc[:, :], zt_[k][:, nn, cs], start=True, stop=True),
                                  reads=[FB, ztb_[k]], writes=[PSB[pr]])
                            fw.op("tensor", lambda h, nn=nn, pi_=pi_, cs=cs: h.matmul(PS[pi_][:], Fsn[:, :], zt_[k][:, nn, cs], start=True, stop=True),
                                  reads=[FB, ztb_[k]], writes=[PSB[pi_]])
                            fw.op("scalar", lambda h, pr=pr, j=j, q=q, cs=cs: h.copy(out=ao[j][:, 0, q, cs], in_=PS[pr][:]), reads=[PSB[pr]], writes=[aob[j]])
                            fw.op("vector", lambda h, pi_=pi_, j=j, q=q, cs=cs: h.tensor_copy(out=ao[j][:, 1, q, cs], in_=PS[pi_][:]), reads=[PSB[pi_]], writes=[aob[j]])
                        if q == 3:
                            for r in range(2):
                                fw.dma("sync", dst[r][n1 - 3:n1 + 1, :, :].rearrange("n k c -> k n c"), ao[j][:, r, :, :], reads=[aob[j]], writes=[Buf()])

            f1_pass(hf_d, a_d[1])
            f1_pass(hg_d, a_d[2])
            f1_pass(z_d, a_d[0])
            fw.barrier()
            st1.close()
            if stop == "S3b1":
                return nc

            Et = [SB(st, "Et%d" % i, [128, 7, 128], BF16) for i in range(2)]; Etb = [Buf(), Buf()]
            ain = [[SB(st, "ain%d_%d" % (i, r), [128, 1024], BF16) for r in range(4)] for i in range(2)]; ainb = [[Buf() for r in range(4)] for i in range(2)]
            ho = [SB(st, "hho%d" % i, [128, 2, 1024], BF16) for i in range(2)]; hob = [Buf(), Buf()]
            tmps = [SB(st, "ftmp%d" % i, [128, 512], F32) for i in range(2)]; tmpbs = [Buf(), Buf()]

            def f2_loads(k2):
                e = k2 % 2
                fw.dma("sync", Et[e][:], cE[k2], writes=[Etb[e]])
                for r, sd in enumerate([a_d[1][0], a_d[1][1], a_d[2][0], a_d[2][1]]):
                    fw.dma("sync", ain[e][r][:], sd[:, k2, :], writes=[ainb[e][r]])
            f2_loads(0)
            for u in range(256):
                k2, cb = u // 2, u % 2
                k = u % 2; e = k2 % 2
                cs = slice(cb * 512, (cb + 1) * 512)
                pr = (u % 3) * 2; pi_ = pr + 1
                if cb == 0 and k2 + 1 < 128:
                    f2_loads(k2 + 1)
                for r, m in enumerate([0, 1, 0, 1]):
                    fw.op("tensor", lambda h, r=r, m=m: h.matmul(PS[pr][:], Et[e][:, m, :], ain[e][r][:, cs], start=(r == 0), stop=(r == 3)),
                          reads=[Etb[e], ainb[e][r]], writes=[PSB[pr]])
                for r, m in enumerate([2, 0, 1, 3]):
                    fw.op("tensor", lambda h, r=r, m=m: h.matmul(PS[pi_][:], Et[e][:, m, :], ain[e][r][:, cs], start=(r == 0), stop=(r == 3)),
                          reads=[Etb[e], ainb[e][r]], writes=[PSB[pi_]])
                fw.op("vector", lambda h: h.tensor_tensor(out=tmps[k][:], in0=PS[pr][:], in1=scl[:, cs], op=ALU.mult), reads=[PSB[pr], sclb], writes=[tmpbs[k]])
                fw.op("vector", lambda h: h.tensor_tensor(out=ho[e][:, 0, cs], in0=tmps[k][:], in1=drep[:, cs], op=ALU.add), reads=[tmpbs[k], drepb], writes=[hob[e]])
                fw.op("vector", lambda h: h.tensor_tensor(out=ho[e][:, 1, cs], in0=PS[pi_][:], in1=scl[:, cs], op=ALU.mult), reads=[PSB[pi_], sclb], writes=[hob[e]])
                if cb == 1:
                    fw.dma("sync", hh_d[0][k2, :, :], ho[e][:, 0, :], reads=[hob[e]], writes=[Buf()])
                    fw.dma("sync", hh_d[1][k2, :, :], ho[e][:, 1, :], reads=[hob[e]], writes=[Buf()])
            fw.barrier()
            if stop == "S3b2":
                return nc

            hin = [[SB(st, "hin%d_%d" % (i, r), [128, 1024], BF16) for r in range(2)] for i in range(2)]; hinb = [[Buf(), Buf()] for i in range(2)]
            xs = [SB(st, "fxs%d" % i, [128, 1024], BF16) for i in range(2)]; xsb = [Buf(), Buf()]
            ys = [SB(st, "fys%d" % i, [128, 1024], BF16) for i in range(2)]; ysb = [Buf(), Buf()]
            t4 = [SB(st, "ft4%d" % i, [128, 2048], BF16) for i in range(2)]; t4b = [[Buf() for _ in range(4)] for i in range(2)]
            bo = [SB(st, "fbo%d" % i, [128, 2, 1024], BF16) for i in range(2)]; bob = [Buf(), Buf()]

            def fu_loads(k2):
                e = k2 % 2
                fw.dma("sync", Et[e][:], cE[k2], writes=[Etb[e]])
                fw.dma("sync", ain[e][0][:], a_d[0][0][:, k2, :], writes=[ainb[e][0]])
                fw.dma("sync", ain[e][1][:], a_d[0][1][:, k2, :], writes=[ainb[e][1]])
                fw.dma("sync", hin[e][0][:], hh_d[0][k2, :, :], writes=[hinb[e][0]])
                fw.dma("sync", hin[e][1][:], hh_d[1][k2, :, :], writes=[hinb[e][1]])
            fu_loads(0)
            for u in range(256):
                k2, cb = u // 2, u % 2
                k = u % 2; e = k2 % 2
                cs = slice(cb * 512, (cb + 1) * 512)
                px = 0 if k == 0 else 4
                if cb == 0 and k2 + 1 < 128:
                    fu_loads(k2 + 1)
                fw.op("tensor", lambda h: h.matmul(PS[px][:], Et[e][:, 0, :], ain[e][0][:, cs], start=True, stop=False), reads=[Etb[e], ainb[e][0]], writes=[PSB[px]])
                fw.op("tensor", lambda h: h.matmul(PS[px][:], Et[e][:, 1, :], ain[e][1][:, cs], start=False, stop=True), reads=[Etb[e], ainb[e][1]], writes=[PSB[px]])
                fw.op("tensor", lambda h: h.matmul(PS[px + 1][:], Et[e][:, 2, :], ain[e][0][:, cs], start=True, stop=False), reads=[Etb[e], ainb[e][0]], writes=[PSB[px + 1]])
                fw.op("tensor", lambda h: h.matmul(PS[px + 1][:], Et[e][:, 0, :], ain[e][1][:, cs], start=False, stop=True), reads=[Etb[e], ainb[e][1]], writes=[PSB[px + 1]])
                fw.op("scalar", lambda h: h.copy(out=xs[k][:, 0:512], in_=PS[px][:]), reads=[PSB[px]], writes=[xsb[k]])
                fw.op("scalar", lambda h: h.copy(out=xs[k][:, 512:1024], in_=PS[px + 1][:]), reads=[PSB[px + 1]], writes=[xsb[k]])
                fw.op("vector", lambda h: h.tensor_tensor(out=t4[k][:, 0:512], in0=xs[k][:, 0:512], in1=hin[e][0][:, cs], op=ALU.mult), reads=[xsb[k], hinb[e][0]], writes=[t4b[k][0]])
                fw.op("vector", lambda h: h.tensor_tensor(out=t4[k][:, 512:1024], in0=xs[k][:, 512:1024], in1=hin[e][1][:, cs], op=ALU.mult), reads=[xsb[k], hinb[e][1]], writes=[t4b[k][1]])
                fw.op("vector", lambda h: h.tensor_tensor(out=t4[k][:, 1024:1536], in0=xs[k][:, 0:512], in1=hin[e][1][:, cs], op=ALU.mult), reads=[xsb[k], hinb[e][1]], writes=[t4b[k][2]])
                fw.op("vector", lambda h: h.tensor_tensor(out=t4[k][:, 1536:2048], in0=xs[k][:, 512:1024], in1=hin[e][0][:, cs], op=ALU.mult), reads=[xsb[k], hinb[e][0]], writes=[t4b[k][3]])
                fw.op("vector", lambda h: h.tensor_tensor(out=ys[k][:, 0:512], in0=t4[k][:, 0:512], in1=t4[k][:, 512:1024], op=ALU.subtract), reads=[t4b[k][0], t4b[k][1]], writes=[ysb[k]])
                fw.op("vector", lambda h: h.tensor_tensor(out=ys[k][:, 512:1024], in0=t4[k][:, 1024:1536], in1=t4[k][:, 1536:2048], op=ALU.add), reads=[t4b[k][2], t4b[k][3]], writes=[ysb[k]])
                fw.op("tensor", lambda h: h.matmul(PS[2][:], Et[e][:, 4, :], ys[k][:, 0:512], start=True, stop=False), reads=[Etb[e], ysb[k]], writes=[PSB[2]])
                fw.op("tensor", lambda h: h.matmul(PS[2][:], Et[e][:, 6, :], ys[k][:, 512:1024], start=False, stop=True), reads=[Etb[e], ysb[k]], writes=[PSB[2]])
                fw.op("tensor", lambda h: h.matmul(PS[3][:], Et[e][:, 5, :], ys[k][:, 0:512], start=True, stop=False), reads=[Etb[e], ysb[k]], writes=[PSB[3]])
                fw.op("tensor", lambda h: h.matmul(PS[3][:], Et[e][:, 4, :], ys[k][:, 512:1024], start=False, stop=True), reads=[Etb[e], ysb[k]], writes=[PSB[3]])
                fw.op("scalar", lambda h: h.copy(out=bo[e][:, 0, cs], in_=PS[2][:]), reads=[PSB[2]], writes=[bob[e]])
                fw.op("scalar", lambda h: h.copy(out=bo[e][:, 1, cs], in_=PS[3][:]), reads=[PSB[3]], writes=[bob[e]])
                if cb == 1:
                    fw.dma("sync", b_d[0][k2, :, :], bo[e][:, 0, :], reads=[bob[e]], writes=[Buf()])
                    fw.dma("sync", b_d[1][k2, :, :], bo[e][:, 1, :], reads=[bob[e]], writes=[Buf()])
            fw.barrier()
            if stop == "S3b3":
                return nc

            bin_ = [[SB(st, "bin%d_%d" % (i, r), [128, 16, 512], BF16) for r in range(2)] for i in range(2)]; binb = [Buf(), Buf()]
            yo = [SB(st, "fyo%d" % i, [16, 8, 512], F32) for i in range(2)]; yob = [Buf(), Buf()]
            yv = y_d.rearrange("(a b) c -> a b c", b=128)
            it = 0
            for cb in range(2):
                cs = slice(cb * 512, (cb + 1) * 512)
                for nb in range(8):
                    k = (cb * 8 + nb) % 2
                    for r in range(2):
                        fw.dma("sync", bin_[k][r][:], b_d[r][:, nb * 16:(nb + 1) * 16, cs], writes=[binb[k]])
                    for nn in range(16):
                        n1 = nb * 16 + nn
                        p = it % 6; j = (it // 8) % 2; q = it % 8; it += 1
                        fw.op("tensor", lambda h, nn=nn, p=p: h.matmul(PS[p][0:16, :], i2re[:], bin_[k][0][:, nn, :], start=True, stop=False), reads=[FB, binb[k]], writes=[PSB[p]])
                        fw.op("tensor", lambda h, nn=nn, p=p: h.matmul(PS[p][0:16, :], i2im[:], bin_[k][1][:, nn, :], start=False, stop=True), reads=[FB, binb[k]], writes=[PSB[p]])
                        if it % 2 == 0:
                            fw.op("scalar", lambda h, p=p, j=j, q=q: h.copy(out=yo[j][:, q, :], in_=PS[p][0:16, :]), reads=[PSB[p]], writes=[yob[j]])
                        else:
                            fw.op("vector", lambda h, p=p, j=j, q=q: h.tensor_copy(out=yo[j][:, q, :], in_=PS[p][0:16, :]), reads=[PSB[p]], writes=[yob[j]])
                        if q == 7:
                            fw.dma("sync", yv[:, n1 - 7:n1 + 1, cs], yo[j][:], reads=[yob[j]], writes=[Buf()])
        persist.close()
        fw.barrier()

        if stop == "S3b":
            return nc
        def wload(st, name, src, nchunk, ncol):
            t = SB(st, name, [128, nchunk, ncol], BF16); b = Buf()
            sv = src.rearrange("(c p) n -> p c n", p=128)
            for c0 in range(0, ncol, 512):
                fw.dma("gpsimd", t[:, :, c0:c0 + 512], sv[:, :, c0:c0 + 512], writes=[b])
            return t, b

        with ExitStack() as st:
            Wa, Wab = wload(st, "Wa", w_attn_o, 8, D)
            Wh, Whb = wload(st, "Wh", w_hy_o, 8, D)
            yt = [SB(st, "yt%d" % i, [128, C], F32) for i in range(2)]
            x0t = [SB(st, "x0t%d" % i, [128, C], BF16) for i in range(2)]
            gt = [SB(st, "gt%d" % i, [128, 4096], BF16) for i in range(2)]
            atT = [SB(st, "atT%d" % i, [128, 8, 128], BF16) for i in range(2)]; inb = [Buf(), Buf()]
            yh16 = SB(st, "yh16", [128, C], BF16); yhb = Buf()
            yhT = SB(st, "yhT", [128, 8, 128], BF16); yhTb = Buf()
            ya = SB(st, "ya", [128, 512], F32); yab = Buf()
            pre16 = SB(st, "pre16", [128, D], BF16); preb = Buf()
            preT = SB(st, "preTs", [128, 16, 128], BF16); preTb = Buf()
            preTv = preT_d.rearrange("(c p) t -> p c t", p=128)
            for t in range(16):
                k = t % 2
                rows = slice(t * 128, (t + 1) * 128)
                fw.dma("sync", yt[k][:], y_d[rows, :], reads=[dbuf("y_d")], writes=[inb[k]])
                fw.dma("sync", x0t[k][:], x0_d[rows, :], reads=[dbuf("x0_d")], writes=[inb[k]])
                fw.dma("sync", gt[k][:], g_d[rows, :], reads=[dbuf("g_d")], writes=[inb[k]])
                fw.dma("sync", atT[k][:], at_d[:, :, rows], reads=[dbuf("at_d")], writes=[inb[k]])
                fw.op("vector", lambda h: h.tensor_tensor(out=yh16[:], in0=yt[k][:], in1=x0t[k][:], op=ALU.mult), reads=[inb[k]], writes=[yhb])
                transpose_to(yh16, yhb, yhT, yhTb, 8)
                for cb in range(4):
                    cs = slice(cb * 512, (cb + 1) * 512)
                    pa = (cb % 3) * 2; ph = pa + 1
                    for c in range(8):
                        fw.op("tensor", lambda h, c=c, pa=pa: h.matmul(PS[pa][:], atT[k][:, c, :], Wa[:, c, cs], start=(c == 0), stop=(c == 7)),
                              reads=[inb[k], Wab], writes=[PSB[pa]])
                    for c in range(8):
                        fw.op("tensor", lambda h, c=c, ph=ph: h.matmul(PS[ph][:], yhT[:, c, :], Wh[:, c, cs], start=(c == 0), stop=(c == 7)),
                              reads=[yhTb, Whb], writes=[PSB[ph]])
                    fw.op("vector", lambda h, pa=pa: h.tensor_tensor(out=ya[:], in0=PS[pa][:], in1=gt[k][:, cs], op=ALU.mult), reads=[PSB[pa], inb[k]], writes=[yab])
                    fw.op("vector", lambda h, ph=ph: h.tensor_tensor(out=pre16[:, cs], in0=PS[ph][:], in1=gt[k][:, 2048 + cb * 512:2048 + (cb + 1) * 512], op=ALU.mult),
                          reads=[PSB[ph], inb[k]], writes=[preb])
                    fw.op("vector", lambda h: h.tensor_tensor(out=pre16[:, cs], in0=pre16[:, cs], in1=ya[:], op=ALU.add), reads=[preb, yab], writes=[preb])
                transpose_to(pre16, preb, preT, preTb, 16)
                fw.dma("gpsimd", preTv[:, :, rows], preT[:], reads=[preTb], writes=[dbuf("preT_d")])
        fw.barrier()

        if stop == "S4a":
            return nc
        with ExitStack() as st:
            Wo, Wob = wload(st, "Wo", w_out, 16, D)
            g1, g1b = rep_load(st, "g1", ln1_g, D); b1, b1b = rep_load(st, "b1", ln1_b, D)
            gbb = Buf()
            fw.op("vector", lambda h: h.tensor_copy(out=g1[:, 0:1], in_=g1[:, 0:1]), reads=[g1b, b1b], writes=[gbb])
            pT_ = [SB(st, "ppT%d" % i, [128, 16, 128], BF16) for i in range(2)]
            hres = [SB(st, "hres%d" % i, [128, D], F32) for i in range(2)]; inb = [Buf(), Buf()]
            h1t = [SB(st, "h1t%d" % i, [128, D], F32) for i in range(2)]; h1b_ = [Buf(), Buf()]
            h116 = SB(st, "h116", [128, D], BF16); h116b = Buf()
            h1Ts = SB(st, "h1Ts", [128, 16, 128], BF16); h1Tb = Buf()
            st8 = SB(st, "st8b", [128, 24], F32); sb8 = Buf(); mv = SB(st, "mvb", [128, 2], F32); mvb = Buf(); rstd = SB(st, "rstdb", [128, 1], F32)
            preTv = preT_d.rearrange("(c p) t -> p c t", p=128)
            h1Tv = h1T_d.rearrange("(c p) t -> p c t", p=128)
            for t in range(16):
                k = t % 2
                rows = slice(t * 128, (t + 1) * 128)
                fw.dma("sync", pT_[k][:], preTv[:, :, rows], reads=[dbuf("preT_d")], writes=[inb[k]])
                fw.dma("sync", hres[k][:], ho_d[128 * t + 1:128 * t + 129, :], reads=[dbuf("ho_d")], writes=[inb[k]])
                for cb in range(4):
                    cs = slice(cb * 512, (cb + 1) * 512)
                    p = (t * 4 + cb) % 6
                    for c in range(16):
                        fw.op("tensor", lambda h, c=c, p=p: h.matmul(PS[p][:], pT_[k][:, c, :], Wo[:, c, cs], start=(c == 0), stop=(c == 15)),
                              reads=[inb[k], Wob], writes=[PSB[p]])
                    fw.op("vector", lambda h, p=p: h.scalar_tensor_tensor(out=h1t[k][:, cs], in0=hres[k][:, cs], scalar=ALPHA, in1=PS[p][:], op0=ALU.mult, op1=ALU.add),
                          reads=[inb[k], PSB[p]], writes=[h1b_[k]])
                layernorm(h1t[k], h1b_[k], g1, b1, gbb, st8, sb8, mv, mvb, rstd)
                fw.dma("gpsimd", h1_d[rows, :], h1t[k][:], reads=[h1b_[k]], writes=[dbuf("h1_d")])
                fw.op("scalar", lambda h: h.copy(out=h116[:], in_=h1t[k][:]), reads=[h1b_[k]], writes=[h116b])
                transpose_to(h116, h116b, h1Ts, h1Tb, 16)
                fw.dma("gpsimd", h1Tv[:, :, rows], h1Ts[:], reads=[h1Tb], writes=[dbuf("h1T_d")])
        fw.barrier()

        if stop == "S4b":
            return nc
        persist2 = ExitStack()
        top.enter_context(persist2)
        I32 = mybir.dt.int32
        OH1 = SB(persist2, "OH1", [128, 16, 32], F32); OH2 = SB(persist2, "OH2", [128, 16, 32], F32)
        G12 = SB(persist2, "G12", [128, 16, 2], F32); Gb = Buf()
        SL1 = SB(persist2, "SL1", [128, 16], I32); SL2 = SB(persist2, "SL2", [128, 16], I32); SLb = Buf()
        IG = SB(persist2, "IG", [128, 64, 16], I32); ID = SB(persist2, "ID", [128, 64, 8], I32); IXb = Buf()
        zt16 = SB(persist2, "zt16", [128, D], BF16); ztb16 = Buf()
        fw.op("gpsimd", lambda h: h.memset(zt16[:], 0.0), writes=[ztb16])
        for i in range(64):
            fw.dma("sync", xs_d[i * 128:(i + 1) * 128, :], zt16[:], reads=[ztb16], writes=[Buf()])
        h1Tv = h1T_d.rearrange("(c p) t -> p c t", p=128)
        BIG = 1.0e30
        with ExitStack() as st:
            Wr = SB(st, "Wr", [128, 16, 36], BF16); Wrb = Buf()
            fw.dma("gpsimd", Wr[:], w_route.rearrange("(c p) n -> p c n", p=128), writes=[Wrb])
            br16 = SB(st, "br16", [1, 36], BF16)
            fw.dma("gpsimd", br16[:], b_route, writes=[Wrb])
            hT_ = [SB(st, "rhT%d" % i, [128, 16, 128], BF16) for i in range(2)]; inb = [Buf(), Buf()]
            lg = SB(st, "lg", [128, 36], F32); lgb = Buf()
            sm = SB(st, "rsm", [128, 16], F32); smb = Buf()
            oh = SB(st, "roh", [128, 96], F32); ohb = Buf()
            for t in range(16):
                k = t % 2
                fw.dma("sync", hT_[k][:], h1Tv[:, :, t * 128:(t + 1) * 128], reads=[dbuf("h1T_d")], writes=[inb[k]])
                p = t % 6
                for c in range(16):
                    fw.op("tensor", lambda h, c=c, p=p: h.matmul(PS[p][:, 0:36], hT_[k][:, c, :], Wr[:, c, :], start=(c == 0), stop=False), reads=[inb[k], Wrb], writes=[PSB[p]])
                fw.op("tensor", lambda h, p=p: h.matmul(PS[p][:, 0:36], ones16[0:1, :], br16[:], start=False, stop=True), reads=[Wrb, Bc], writes=[PSB[p]])
                fw.op("vector", lambda h, p=p: h.tensor_copy(out=lg[:], in_=PS[p][:, 0:36]), reads=[PSB[p]], writes=[lgb])
                V = lambda fn, r, w: fw.op("vector", fn, reads=r, writes=w)
                V(lambda h: h.tensor_reduce(out=sm[:, 0:1], in_=lg[:, 0:4], axis=mybir.AxisListType.X, op=ALU.max), [lgb], [smb])
                V(lambda h: h.tensor_scalar(out=oh[:, 0:4], in0=lg[:, 0:4], scalar1=sm[:, 0:1], scalar2=None, op0=ALU.is_equal), [lgb, smb], [ohb])
                V(lambda h: h.tensor_scalar(out=oh[:, 4:8], in0=lg[:, 0:4], scalar1=sm[:, 0:1], scalar2=None, op0=ALU.subtract), [lgb, smb], [ohb])
                fw.op("scalar", lambda h: h.activation(out=oh[:, 4:8], in_=oh[:, 4:8], func=AF.Exp, accum_out=sm[:, 1:2]), reads=[ohb, smb], writes=[ohb, smb])
                V(lambda h: h.reciprocal(out=sm[:, 2:3], in_=sm[:, 1:2]), [smb], [smb])
                V(lambda h: h.tensor_scalar(out=oh[:, 8:12], in0=oh[:, 0:4], scalar1=-1.0, scalar2=BIG, op0=ALU.add, op1=ALU.mult), [ohb], [ohb])
                for g_ in range(4):
                    V(lambda h, g_=g_: h.tensor_scalar(out=oh[:, 32 + 8 * g_:40 + 8 * g_], in0=lg[:, 4 + 8 * g_:12 + 8 * g_], scalar1=oh[:, 8 + g_:9 + g_], scalar2=None, op0=ALU.add),
                      [lgb, ohb], [ohb])
                V(lambda h: h.tensor_reduce(out=sm[:, 3:4], in_=oh[:, 32:64], axis=mybir.AxisListType.X, op=ALU.max), [ohb], [smb])
                V(lambda h: h.tensor_scalar(out=oh[:, 64:96], in0=oh[:, 32:64], scalar1=sm[:, 3:4], scalar2=None, op0=ALU.is_equal), [ohb, smb], [ohb])
                V(lambda h: h.scalar_tensor_tensor(out=oh[:, 32:64], in0=oh[:, 64:96], scalar=-BIG, in1=oh[:, 32:64], op0=ALU.mult, op1=ALU.add), [ohb], [ohb])
                V(lambda h: h.tensor_reduce(out=sm[:, 4:5], in_=oh[:, 32:64], axis=mybir.AxisListType.X, op=ALU.max), [ohb], [smb])
                V(lambda h: h.tensor_scalar(out=oh[:, 32:64], in0=oh[:, 32:64], scalar1=sm[:, 4:5], scalar2=None, op0=ALU.is_equal), [ohb, smb], [ohb])
                V(lambda h: h.tensor_tensor(out=sm[:, 5:6], in0=sm[:, 4:5], in1=sm[:, 3:4], op=ALU.subtract), [smb], [smb])
                fw.op("scalar", lambda h: h.activation(out=sm[:, 6:7], in_=sm[:, 5:6], func=AF.Exp), reads=[smb], writes=[smb])
                V(lambda h: h.tensor_scalar(out=sm[:, 7:8], in0=sm[:, 6:7], scalar1=1.0, scalar2=None, op0=ALU.add), [smb], [smb])
                V(lambda h: h.reciprocal(out=sm[:, 8:9], in_=sm[:, 7:8]), [smb], [smb])
                V(lambda h: h.tensor_tensor(out=sm[:, 9:10], in0=sm[:, 8:9], in1=sm[:, 2:3], op=ALU.mult), [smb], [smb])
                V(lambda h: h.tensor_tensor(out=sm[:, 10:11], in0=sm[:, 9:10], in1=sm[:, 6:7], op=ALU.mult), [smb], [smb])
                V(lambda h, t=t: h.tensor_copy(out=OH1[:, t, :], in_=oh[:, 64:96]), [ohb], [Gb])
                V(lambda h, t=t: h.tensor_copy(out=OH2[:, t, :], in_=oh[:, 32:64]), [ohb], [Gb])
                V(lambda h, t=t: h.tensor_copy(out=G12[:, t, :], in_=sm[:, 9:11]), [smb], [Gb])
            A = SB(st, "mA", [128, 16, 32], F32); Ab = Buf()
            R = SB(st, "mR", [128, 16, 32], F32); Rb = Buf()
            U = SB(st, "mU", [128, 128], F32); Ub = Buf()
            base = SB(st, "mbase", [128, 32], F32); baseb = Buf()
            ci = SB(st, "mci", [128, 32], I32); padf = SB(st, "mpadf", [128, 32], F32); pe = SB(st, "mpe", [128, 32], F32)
            pst = SB(st, "mpst", [128, 32], F32); one32 = SB(st, "mone32", [128, 32], F32); pb_ = Buf()
            sf = SB(st, "msf", [128, 32], F32); eb = SB(st, "meb", [128, 64], F32); junk32 = SB(st, "mj32", [128, 32], F32); ebb = Buf()
            igf = SB(st, "migf", [128, 64, 16], F32); bG = SB(st, "mbG", [128, 64], F32); bD = SB(st, "mbD", [128, 64], F32)
            pc = SB(st, "mpc", [128, 1], F32)
            fw.dma("sync", pc[:], pcol, writes=[ebb])
            V(lambda h: h.tensor_tensor(out=A[:], in0=OH1[:], in1=OH2[:], op=ALU.add), [Gb], [Ab])
            fw.op("gpsimd", lambda h: h.memset(U[:], 1.0), writes=[Ub])
            fw.op("gpsimd", lambda h: h.affine_select(out=U[:], in_=U[:], pattern=[[1, 128]], compare_op=ALU.is_gt, fill=0.0, base=0, channel_multiplier=-1),
                  reads=[Ub], writes=[Ub])
            V(lambda h: h.memset(base[:], 0.0), [], [baseb])
            V(lambda h: h.memset(one32[:], 1.0), [], [pb_])
            for i in range(16):
                pa = (2 * i) % 6; pb2 = (2 * i + 1) % 6
                fw.op("tensor", lambda h, i=i, pa=pa: h.matmul(PS[pa][:, 0:32], U[:], A[:, i, :], start=True, stop=True), reads=[Ub, Ab], writes=[PSB[pa]])
                fw.op("tensor", lambda h, i=i, pb2=pb2: h.matmul(PS[pb2][:, 0:32], onesf[:], A[:, i, :], start=True, stop=True), reads=[Bc, Ab], writes=[PSB[pb2]])
                V(lambda h, i=i, pa=pa: h.tensor_tensor(out=R[:, i, :], in0=PS[pa][:, 0:32], in1=base[:], op=ALU.add), [PSB[pa], baseb], [Rb])
                V(lambda h, pb2=pb2: h.tensor_tensor(out=base[:], in0=PS[pb2][:, 0:32], in1=base[:], op=ALU.add), [PSB[pb2], baseb], [baseb])
            V(lambda h: h.tensor_scalar(out=ci[:], in0=base[:], scalar1=127.0, scalar2=None, op0=ALU.add), [baseb], [pb_])
            V(lambda h: h.tensor_scalar(out=ci[:], in0=ci[:], scalar1=7, scalar2=7, op0=ALU.arith_shift_right, op1=ALU.logical_shift_left), [pb_], [pb_])
            V(lambda h: h.tensor_copy(out=padf[:], in_=ci[:]), [pb_], [pb_])
            V(lambda h: h.tensor_tensor_scan(out=pe[:], data0=one32[:], data1=padf[:], initial=0.0, op0=ALU.mult, op1=ALU.add), [pb_], [pb_])
            V(lambda h: h.tensor_tensor(out=pst[:], in0=pe[:], in1=padf[:], op=ALU.subtract), [pb_], [pb_])
            for i in range(16):
                V(lambda h, i=i: h.tensor_tensor(out=R[:, i, :], in0=R[:, i, :], in1=pst[:], op=ALU.add), [Rb, pb_], [Rb])
            for OH, SL, col in ((OH1, SL1, 0), (OH2, SL2, 1)):
                V(lambda h, OH=OH: h.tensor_tensor(out=A[:], in0=R[:], in1=OH[:], op=ALU.mult), [Rb, Gb, Ab], [Ab])
                V(lambda h: h.tensor_reduce(out=sf[:, 0:16], in_=A[:], axis=mybir.AxisListType.X, op=ALU.add), [Ab], [ebb])
                V(lambda h, SL=SL: h.tensor_copy(out=SL[:], in_=sf[:, 0:16]), [ebb], [SLb])
            for i in range(64):
                V(lambda h, i=i: h.tensor_scalar(out=junk32[:], in0=pe[:], scalar1=128.0 * i, scalar2=0.0, op0=ALU.is_le, op1=ALU.add, accum_out=eb[:, i:i + 1]),
                  [pb_], [ebb])
            V(lambda h: h.tensor_scalar(out=bG[:], in0=eb[:], scalar1=2048.0, scalar2=pc[:, 0:1], op0=ALU.mult, op1=ALU.add), [ebb], [ebb])
            V(lambda h: h.tensor_scalar(out=bD[:], in0=eb[:], scalar1=1024.0, scalar2=pc[:, 0:1], op0=ALU.mult, op1=ALU.add), [ebb], [ebb])
            for c in range(16):
                V(lambda h, c=c: h.tensor_scalar(out=igf[:, :, c], in0=bG[:], scalar1=128.0 * c, scalar2=None, op0=ALU.add), [ebb], [ebb])
            V(lambda h: h.tensor_copy(out=IG[:], in_=igf[:]), [ebb], [IXb])
            for c in range(8):
                V(lambda h, c=c: h.tensor_scalar(out=igf[:, :, c], in0=bD[:], scalar1=128.0 * c, scalar2=None, op0=ALU.add), [ebb, IXb], [ebb])
            V(lambda h: h.tensor_copy(out=ID[:], in_=igf[:, :, 0:8]), [ebb], [IXb])
        fw.barrier()

        if stop == "S5":
            return nc
        with ExitStack() as st:
            hl = [SB(st, "s_hl%d" % i, [128, D], F32) for i in range(2)]; hlb = [Buf(), Buf()]
            h16_ = [SB(st, "s_h16%d" % i, [128, D], BF16) for i in range(2)]; h16b_ = [Buf(), Buf()]
            for t in range(16):
                k = t % 2
                fw.dma("sync", hl[k][:], h1_d[t * 128:(t + 1) * 128, :], writes=[hlb[k]])
                fw.op("scalar", lambda h: h.copy(out=h16_[k][:], in_=hl[k][:]), reads=[hlb[k]], writes=[h16b_[k]])
                fw.idma(xs_d[:, :], h16_[k][:, :], SL1[:, t:t + 1], False, 8191, reads=[h16b_[k], SLb], writes=[Buf()])
                fw.idma(xs_d[:, :], h16_[k][:, :], SL2[:, t:t + 1], False, 8191, reads=[h16b_[k], SLb], writes=[Buf()])
        fw.barrier()

        if stop == "S6a":
            return nc
        weg = w_eg.rearrange("e k n -> (e k) n"); weu = w_eu.rearrange("e k n -> (e k) n"); wed = w_ed.rearrange("e k n -> (e k) n")
        with ExitStack() as st:
            NW = 8
            xb = [SB(st, "b_xb%d" % i, [128, D], BF16) for i in range(2)]; xbb = [Buf(), Buf()]
            xT = [SB(st, "b_xT%d" % i, [128, 16, 128], BF16) for i in range(2)]; xTb = [Buf(), Buf()]
            wgt = [SB(st, "b_wg%d" % i, [128, 1024], BF16) for i in range(NW)]; wgtb = [Buf() for _ in range(NW)]
            wut = [SB(st, "b_wu%d" % i, [128, 1024], BF16) for i in range(NW)]; wutb = [Buf() for _ in range(NW)]
            wdt = [[SB(st, "b_wd%d_%d" % (i, c), [128, D], BF16) for c in range(8)] for i in range(2)]
            wdtb = [[Buf() for c in range(8)] for i in range(2)]
            sg = [SB(st, "b_sg%d" % i, [128, 512], F32) for i in range(2)]; sgb = [Buf(), Buf()]
            Hs = [SB(st, "b_H%d" % i, [128, 1024], BF16) for i in range(2)]; Hsb = [Buf(), Buf()]
            HT = [SB(st, "b_HT%d" % i, [128, 8, 128], BF16) for i in range(2)]; HTb = [Buf(), Buf()]
            Ys = [SB(st, "b_Y%d" % i, [128, D], F32) for i in range(2)]; Ysb = [Buf(), Buf()]
            iw = 0
            for i in range(64):
                k = i % 2
                fw.dma("sync", xb[k][:], xs_d[i * 128:(i + 1) * 128, :], writes=[xbb[k]])
                transpose_to(xb[k], xbb[k], xT[k], xTb[k], 16)
                for c in range(16):
                    j = iw % NW; iw += 1
                    fw.idma(wgt[j][:, :], weg[:, :], IG[:, i, c:c + 1], True, 32 * 2048 - 1, reads=[IXb], writes=[wgtb[j]])
                    fw.idma(wut[j][:, :], weu[:, :], IG[:, i, c:c + 1], True, 32 * 2048 - 1, reads=[IXb], writes=[wutb[j]])
                    for hf_ in range(2):
                        fw.op("tensor", lambda h, c=c, j=j, hf_=hf_: h.matmul(PS[hf_][:], xT[k][:, c, :], wgt[j][:, hf_ * 512:(hf_ + 1) * 512], start=(c == 0), stop=(c == 15)),
                              reads=[xTb[k], wgtb[j]], writes=[PSB[hf_]])
                    for hf_ in range(2):
                        fw.op("tensor", lambda h, c=c, j=j, hf_=hf_: h.matmul(PS[2 + hf_][:], xT[k][:, c, :], wut[j][:, hf_ * 512:(hf_ + 1) * 512], start=(c == 0), stop=(c == 15)),
                              reads=[xTb[k], wutb[j]], writes=[PSB[2 + hf_]])
                for c in range(8):
                    fw.idma(wdt[k][c][:, :], wed[:, :], ID[:, i, c:c + 1], True, 32 * 1024 - 1, reads=[IXb], writes=[wdtb[k][c]])
                for hf_ in range(2):
                    fw.op("scalar", lambda h, hf_=hf_: h.activation(out=sg[hf_][:], in_=PS[hf_][:], func=AF.Silu), reads=[PSB[hf_]], writes=[sgb[hf_]])
                    fw.op("vector", lambda h, hf_=hf_: h.tensor_tensor(out=Hs[k][:, hf_ * 512:(hf_ + 1) * 512], in0=PS[2 + hf_][:], in1=sg[hf_][:], op=ALU.mult),
                          reads=[PSB[2 + hf_], sgb[hf_]], writes=[Hsb[k]])
                transpose_to(Hs[k], Hsb[k], HT[k], HTb[k], 8)
                for hf_ in range(2):
                    for c in range(8):
                        for q in range(2):
                            fw.op("tensor", lambda h, c=c, q=q, hf_=hf_: h.matmul(PS[4 + q][:], HT[k][:, c, :], wdt[k][c][:, hf_ * 1024 + q * 512:hf_ * 1024 + (q + 1) * 512],
                                                                                  start=(c == 0), stop=(c == 7)), reads=[HTb[k], wdtb[k][c]], writes=[PSB[4 + q]])
                    fw.op("scalar", lambda h, hf_=hf_: h.copy(out=Ys[k][:, hf_ * 1024:hf_ * 1024 + 512], in_=PS[4][:]), reads=[PSB[4]], writes=[Ysb[k]])
                    fw.op("vector", lambda h, hf_=hf_: h.tensor_copy(out=Ys[k][:, hf_ * 1024 + 512:(hf_ + 1) * 1024], in_=PS[5][:]), reads=[PSB[5]], writes=[Ysb[k]])
                fw.dma("sync", ys_d[i * 128:(i + 1) * 128, :], Ys[k][:], reads=[Ysb[k]], writes=[Buf()])
        fw.barrier()

        if stop == "S6b":
            return nc
        with ExitStack() as st:
            g2, g2b = rep_load(st, "g2", ln2_g, D); b2, b2b = rep_load(st, "b2", ln2_b, D)
            gbb = Buf()
            fw.op("vector", lambda h: h.tensor_copy(out=g2[:, 0:1], in_=g2[:, 0:1]), reads=[g2b, b2b], writes=[gbb])
            ht = [SB(st, "f_h%d" % i, [128, D], F32) for i in range(2)]; htb = [Buf(), Buf()]
            y1 = [SB(st, "f_y1%d" % i, [128, D], F32) for i in range(2)]; y1b = [Buf(), Buf()]
            y2 = [SB(st, "f_y2%d" % i, [128, D], F32) for i in range(2)]; y2b = [Buf(), Buf()]
            st8 = SB(st, "st8c", [128, 24], F32); sb8 = Buf(); mv = SB(st, "mvc", [128, 2], F32); mvb = Buf(); rstd = SB(st, "rstdc", [128, 1], F32)
            for t in range(16):
                k = t % 2
                rows = slice(t * 128, (t + 1) * 128)
                fw.dma("sync", ht[k][:], h1_d[rows, :], writes=[htb[k]])
                fw.idma(y1[k][:, :], ys_d[:, :], SL1[:, t:t + 1], True, 8191, reads=[SLb], writes=[y1b[k]])
                fw.idma(y2[k][:, :], ys_d[:, :], SL2[:, t:t + 1], True, 8191, reads=[SLb], writes=[y2b[k]])
                fw.op("vector", lambda h: h.tensor_scalar(out=ht[k][:], in0=ht[k][:], scalar1=ALPHA, scalar2=None, op0=ALU.mult), reads=[htb[k]], writes=[htb[k]])
                fw.op("vector", lambda h, t=t: h.scalar_tensor_tensor(out=ht[k][:], in0=y1[k][:], scalar=G12[:, t, 0:1], in1=ht[k][:], op0=ALU.mult, op1=ALU.add),
                      reads=[y1b[k], htb[k], Gb], writes=[htb[k]])
                fw.op("vector", lambda h, t=t: h.scalar_tensor_tensor(out=ht[k][:], in0=y2[k][:], scalar=G12[:, t, 1:2], in1=ht[k][:], op0=ALU.mult, op1=ALU.add),
                      reads=[y2b[k], htb[k], Gb], writes=[htb[k]])
                layernorm(ht[k], htb[k], g2, b2, gbb, st8, sb8, mv, mvb, rstd)
                fw.dma("sync", out[rows, :], ht[k][:], reads=[htb[k]], writes=[Buf()])
        persist2.close()
        fw.barrier()
    return nc


_CONST = {}


def _consts():
    if _CONST:
        return _CONST
    f32 = np.float32
    half = 64
    inv_freq = (10000.0 ** (-np.arange(0, half, 2, dtype=np.float64) / half)).astype(f32)
    row_idx = (np.arange(S) // 64).astype(f32)
    col_idx = (np.arange(S) % 64).astype(f32)
    ang_r = (row_idx[:, None] * inv_freq[None, :]).astype(f32)
    ang_c = (col_idx[:, None] * inv_freq[None, :]).astype(f32)
    cr, sr, cc, sc = np.cos(ang_r), np.sin(ang_r), np.cos(ang_c), np.sin(ang_c)
    _CONST["ropeC"] = np.concatenate([cr, cr, cc, cc], axis=1).astype(f32)
    _CONST["ropeS"] = np.concatenate([-sr, sr, -sc, sc], axis=1).astype(f32)
    n = np.arange(128, dtype=np.float64)
    a128 = 2 * np.pi * np.outer(n, n) / 128.0
    _CONST["Fc"] = np.cos(a128).astype(f32); _CONST["Fs"] = np.sin(a128).astype(f32)
    a16 = 2 * np.pi * np.outer(n, n) / float(NFFT)
    _CONST["C16"] = np.cos(a16).astype(f32); _CONST["S16"] = np.sin(a16).astype(f32)
    t = np.linspace(0.0, 1.0, S, dtype=f32)
    w = (2.0 * math.pi * np.arange(S, dtype=f32) / S).astype(f32)[:, None]
    bands = np.linspace(1e-4, 15, 16, dtype=f32)[None, :]
    feats = np.concatenate([t[:, None], np.cos(bands * w), -np.sin(bands * w)], axis=-1).astype(f32)
    _CONST["featsT"] = np.ascontiguousarray(feats.T)
    _CONST["negt"] = np.ascontiguousarray((-t).reshape(64, 128).T)
    min_decay = math.log(1e-2) / 1.5
    max_decay = math.log(1e-2) / 0.3
    _CONST["absdelta"] = np.abs(np.linspace(min_decay, max_decay, C, dtype=f32)).reshape(1, C).astype(f32)
    import ml_dtypes
    n1 = np.arange(128, dtype=np.float64)[None, :, None]
    k1 = np.arange(128, dtype=np.float64)[None, None, :]
    k2 = np.arange(128, dtype=np.float64)[:, None, None]
    th = 2 * np.pi * n1 * (128.0 * k1 + k2) / float(NFFT)
    ec, es = np.cos(th), np.sin(th)
    ect, est = ec.transpose(0, 2, 1), es.transpose(0, 2, 1)
    _CONST["cE"] = np.ascontiguousarray(np.stack([ec, es, -es, -ec, ect, est, -est], axis=2)).astype(ml_dtypes.bfloat16)
    m0 = np.ones((128, 1), f32); m0[0, 0] = 0.0
    _CONST["m0"] = m0
    return _CONST


def _in_maps(inp):
    cs = _consts()
    f32 = np.float32
    x = np.asarray(inp["x"], f32)
    common = {
        "ln_in_g": inp["ln_in_g"].reshape(1, D), "ln_in_b": inp["ln_in_b"].reshape(1, D),
        "w_in": inp["w_in"][0], "b_gate": inp["b_gate"][0].reshape(1, 4096),
        "q_norm_g": inp["q_norm_g"][0].reshape(1, 128), "k_norm_g": inp["k_norm_g"][0].reshape(1, 128),
        "hy_conv_w": inp["hy_conv_w"][0], "hy_conv_b": inp["hy_conv_b"][0].reshape(1, 3072),
        "filt_w1": inp["filt_w1"][0], "filt_b1": inp["filt_b1"][0].reshape(64, 1), "filt_f1": inp["filt_f1"][0].reshape(64, 1),
        "filt_w2": inp["filt_w2"][0], "filt_b2": inp["filt_b2"][0].reshape(64, 1), "filt_f2": inp["filt_f2"][0].reshape(64, 1),
        "filt_w3": inp["filt_w3"][0], "hy_bias_d": inp["hy_bias_d"][0].reshape(1, C),
        "w_attn_o": inp["w_attn_o"][0], "w_hy_o": inp["w_hy_o"][0], "w_out": inp["w_out"][0],
        "ln1_g": inp["ln1_g"][0].reshape(1, D), "ln1_b": inp["ln1_b"][0].reshape(1, D),
        "w_route": np.concatenate([inp["w_route_grp"][0], inp["w_route_exp"][0]], axis=1),
        "b_route": np.concatenate([inp["b_route_grp"][0], inp["b_route_exp"][0]], axis=0).reshape(1, 36),
        "w_eg": inp["w_exp_gate"][0], "w_eu": inp["w_exp_up"][0], "w_ed": inp["w_exp_down"][0],
        "ln2_g": inp["ln2_g"][0].reshape(1, D), "ln2_b": inp["ln2_b"][0].reshape(1, D),
        "ropeC_k": cs["ropeC"], "ropeS_k": cs["ropeS"],
        "cFc": cs["Fc"], "cFs": cs["Fs"], "cFsn": -cs["Fs"], "cFcn": -cs["Fc"],
        "cC16": cs["C16"], "cS16": cs["S16"], "cS16n": -cs["S16"],
        "pcol": np.arange(128, dtype=np.float32).reshape(128, 1), "featsT": cs["featsT"], "negt": cs["negt"], "absdelta": cs["absdelta"], "m0": cs["m0"],
    }
    common = {k: np.ascontiguousarray(np.asarray(v, f32)) for k, v in common.items()}
    common["cE"] = cs["cE"]
    maps = []
    for c in range(8):
        b, j = c // 4, c % 4
        q0 = j * T
        xo = np.zeros((NOWN, D), f32); mo = np.zeros((NOWN, 1), f32)
        lo = max(q0 - 1, 0); hi = min(q0 + T + 1, S)
        r0 = lo - (q0 - 1)
        xo[r0:r0 + (hi - lo)] = x[b, lo:hi]
        mo[r0:r0 + (hi - lo)] = 1.0
        m = dict(common)
        m["x_seq"] = np.ascontiguousarray(x[b]); m["x_own"] = xo; m["m_own"] = np.ascontiguousarray(mo.reshape(17, 128).T)
        m["ropeC_q"] = np.ascontiguousarray(cs["ropeC"][q0:q0 + T]); m["ropeS_q"] = np.ascontiguousarray(cs["ropeS"][q0:q0 + T])
        n2 = np.arange(16 * j, 16 * j + 16)
        m["ci2re"] = np.ascontiguousarray(cs["Fc"][:, n2] / float(NFFT)).astype(f32)
        m["ci2im"] = np.ascontiguousarray(-cs["Fs"][:, n2] / float(NFFT)).astype(f32)
        maps.append(m)
    return maps


def kernel(**inputs):
    inp = {k: np.asarray(v) for k, v in inputs.items()}
    nc = build_nc()
    maps = _in_maps(inp)
    res = run_bass_kernel_spmd(nc, maps, core_ids=list(range(8)))
    outp = np.zeros((2, S, D), np.float32)
    for c in range(8):
        b, j = c // 4, c % 4
        outp[b, j * T:(j + 1) * T] = res.results[c]["out"]
    return outp
```

```python
import math
from contextlib import ExitStack
import numpy as np
import concourse.bass as bass
import concourse.mybir as mybir
from concourse.bass_utils import run_bass_kernel_spmd

F32 = mybir.dt.float32
BF16 = mybir.dt.bfloat16
ALU = mybir.AluOpType
AF = mybir.ActivationFunctionType

D = 2048
S = 8192
T = 2048
NOWN = 17 * 128
C = 1024
INW = 8704
ALPHA = 2.0 ** 0.25
LN_EPS = 1e-5
QK_EPS = 1e-6
FILTER_EPS = 1e-6
NFFT = 16384
PI = math.pi


class Buf:
    __slots__ = ("w", "r")

    def __init__(self):
        self.w = None
        self.r = []


class FW:
    ENGS = ("tensor", "vector", "scalar", "gpsimd", "sync")

    def __init__(self, nc, stack, ndma=20):
        self.nc = nc
        self.cnt = {e: 0 for e in self.ENGS}
        self.seen = {e: {} for e in self.ENGS}
        self.sems = {}
        self.ndma = ndma
        self.dma_next = {e: 0 for e in self.ENGS}
        self.dma_tot = {}
        for e in self.ENGS:
            self.sems[e] = stack.enter_context(nc.semaphore("s_" + e))
        for q in ("sync", "gpsimd", "scalar"):
            for i in range(ndma):
                k = ("d", q, i)
                self.sems[k] = stack.enter_context(nc.semaphore("d_%s_%d" % (q, i)))
                self.dma_tot[k] = 0

    def _waits(self, eng, reads, writes):
        deps = {}

        def add(t):
            if t is not None and deps.get(t[0], 0) < t[1]:
                deps[t[0]] = t[1]
        for b in reads:
            add(b.w)
        for b in writes:
            add(b.w)
            for t in b.r:
                add(t)
        out = []
        seen = self.seen[eng]
        for key, val in deps.items():
            if key == eng and eng == "tensor":
                continue
            if seen.get(key, 0) >= val:
                continue
            seen[key] = val
            out.append((key, val))
        return out

    def _mark(self, tag, reads, writes):
        for b in writes:
            b.w = tag
            b.r = []
        for b in reads:
            b.r = [t for t in b.r if t[0] != tag[0]] + [tag]

    def op(self, eng, fn, reads=(), writes=()):
        waits = self._waits(eng, reads, writes)
        self.cnt[eng] += 1
        tag = (eng, self.cnt[eng])
        h = getattr(self.nc, eng)
        for key, val in waits:
            h.wait_ge(self.sems[key], val)
        fn(h).then_inc(self.sems[eng], 1)
        self._mark(tag, reads, writes)

    def idma(self, out, in_, idx, gather, bound, reads=(), writes=()):
        if not hasattr(self, "bregs"):
            self.bregs = {}
        if bound not in self.bregs:
            r = self.nc.gpsimd.alloc_register("bnd%d" % bound)
            self.nc.gpsimd.reg_mov(r, bound)
            self.bregs[bound] = r
        bound = self.bregs[bound]
        if gather:
            fn = lambda h: h.indirect_dma_start(out=out, out_offset=None, in_=in_, in_offset=bass.IndirectOffsetOnAxis(ap=idx, axis=0),
                                                bounds_check=bound, oob_is_err=False)
        else:
            fn = lambda h: h.indirect_dma_start(out=out, out_offset=bass.IndirectOffsetOnAxis(ap=idx, axis=0), in_=in_, in_offset=None,
                                                bounds_check=bound, oob_is_err=False)
        self.dma("gpsimd", None, None, reads=reads, writes=writes, fn=fn)

    def dma(self, q, out, in_, reads=(), writes=(), slow=False, fn=None):
        i = self.dma_next[q]
        self.dma_next[q] = (i + 1) % self.ndma
        key = ("d", q, i)
        waits = self._waits(q, reads, writes)
        prev = self.dma_tot[key]
        if prev > 0 and self.seen[q].get(key, 0) < prev:
            self.seen[q][key] = prev
            waits.append((key, prev))
        self.dma_tot[key] = prev + 16
        tag = (key, prev + 16)
        h = getattr(self.nc, q)
        for k2, val in waits:
            h.wait_ge(self.sems[k2], val)
        if fn is not None:
            inst = fn(h)
        elif slow:
            inst = h.dma_start(out=out, in_=in_, allow_slow_non_contiguous=True)
        else:
            inst = h.dma_start(out=out, in_=in_)
        inst.then_inc(self.sems[key], 16)
        self._mark(tag, reads, writes)

    def barrier(self):
        for e in self.ENGS:
            h = getattr(self.nc, e)
            seen = self.seen[e]
            for o in self.ENGS:
                if o == e or self.cnt[o] == 0:
                    continue
                if seen.get(o, 0) < self.cnt[o]:
                    seen[o] = self.cnt[o]
                    h.wait_ge(self.sems[o], self.cnt[o])
            for k, tot in self.dma_tot.items():
                if tot > 0 and seen.get(k, 0) < tot:
                    seen[k] = tot
                    h.wait_ge(self.sems[k], tot)


def build_nc(dbg=None, stop=None):
    nc = bass.Bass("TRN2", target_bir_lowering=False)
    ins = {}

    def IN(name, shape, dt=F32):
        ins[name] = nc.dram_tensor(name, list(shape), dt, kind="ExternalInput").ap()
        return ins[name]

    x_seq = IN("x_seq", [S, D]); x_own = IN("x_own", [NOWN, D]); m_own = IN("m_own", [128, 17])
    ln_in_g = IN("ln_in_g", [1, D]); ln_in_b = IN("ln_in_b", [1, D])
    w_in = IN("w_in", [D, INW]); b_gate = IN("b_gate", [1, 4096])
    q_norm_g = IN("q_norm_g", [1, 128]); k_norm_g = IN("k_norm_g", [1, 128])
    hy_conv_w = IN("hy_conv_w", [3, 3072]); hy_conv_b = IN("hy_conv_b", [1, 3072])
    filt_w1 = IN("filt_w1", [33, 64]); filt_b1 = IN("filt_b1", [64, 1]); filt_f1 = IN("filt_f1", [64, 1])
    filt_w2 = IN("filt_w2", [64, 64]); filt_b2 = IN("filt_b2", [64, 1]); filt_f2 = IN("filt_f2", [64, 1])
    filt_w3 = IN("filt_w3", [64, 2048]); hy_bias_d = IN("hy_bias_d", [1, C])
    w_attn_o = IN("w_attn_o", [1024, D]); w_hy_o = IN("w_hy_o", [C, D]); w_out = IN("w_out", [D, D])
    ln1_g = IN("ln1_g", [1, D]); ln1_b = IN("ln1_b", [1, D])
    w_route = IN("w_route", [D, 36]); b_route = IN("b_route", [1, 36])
    w_eg = IN("w_eg", [32, D, 1024]); w_eu = IN("w_eu", [32, D, 1024]); w_ed = IN("w_ed", [32, 1024, D])
    ln2_g = IN("ln2_g", [1, D]); ln2_b = IN("ln2_b", [1, D])
    ropeC_k = IN("ropeC_k", [S, 128]); ropeS_k = IN("ropeS_k", [S, 128])
    ropeC_q = IN("ropeC_q", [T, 128]); ropeS_q = IN("ropeS_q", [T, 128])
    cFc = IN("cFc", [128, 128]); cFs = IN("cFs", [128, 128]); cFsn = IN("cFsn", [128, 128]); cFcn = IN("cFcn", [128, 128])
    cC16 = IN("cC16", [128, 128]); cS16 = IN("cS16", [128, 128]); cS16n = IN("cS16n", [128, 128])
    cE = IN("cE", [128, 128, 7, 128], BF16); ci2re = IN("ci2re", [128, 16]); ci2im = IN("ci2im", [128, 16])
    pcol = IN("pcol", [128, 1]); featsT = IN("featsT", [33, S]); negt = IN("negt", [128, 64]); absdelta = IN("absdelta", [1, C]); m0 = IN("m0", [128, 1])

    out = nc.dram_tensor("out", [T, D], F32, kind="ExternalOutput").ap()

    def DR(name, shape, dt):
        kind = "ExternalOutput" if (dbg and name in dbg) else "Internal"
        return nc.dram_tensor(name, list(shape), dt, kind=kind).ap()

    hT_d = DR("hT_d", [D, S + 2], BF16); hTo_d = DR("hTo_d", [D, NOWN], BF16); ho_d = DR("ho_d", [NOWN, D], F32)
    kT_d = DR("kT_d", [128, 2, S], BF16); v_d = DR("v_d", [S, 256], BF16); qT_d = DR("qT_d", [128, 8, T], BF16)
    p_d = DR("p_d", [S + 2, 2048], BF16); po_d = DR("po_d", [NOWN, 1024], BF16); z_d = DR("z_d", [S, C], BF16); x0_d = DR("x0_d", [T, C], BF16); g_d = DR("g_d", [T, 4096], BF16)
    at_d = DR("at_d", [128, 8, T], BF16)
    hf_d = DR("hf_d", [S, C], BF16); hg_d = DR("hg_d", [S, C], BF16)
    a_d = [[DR("a_d%d%d" % (i, r), [128, 128, C], BF16) for r in range(2)] for i in range(3)]
    hh_d = [DR("hh_d%d" % r, [128, 128, C], BF16) for r in range(2)]
    b_d = [DR("b_d%d" % r, [128, 128, C], BF16) for r in range(2)]
    y_d = DR("y_d", [T, C], F32)
    xs_d = DR("xs_d", [8192, D], BF16); ys_d = DR("ys_d", [8192, D], F32); preT_d = DR("preT_d", [D, T], BF16); h1_d = DR("h1_d", [T, D], F32); h1T_d = DR("h1T_d", [D, T], BF16)

    with ExitStack() as top:
        fw = FW(nc, top)
        Dm = {}

        def dbuf(ap_name):
            return Buf()

        uid = [0]

        def SB(st, name, shape, dt):
            uid[0] += 1
            return st.enter_context(nc.sbuf_tensor("%s_u%d" % (name, uid[0]), list(shape), dt))

        PS = [top.enter_context(nc.psum_tensor("ps%d" % i, [128, 512], F32)) for i in range(6)]
        PSB = [Buf() for _ in range(6)]
        PT = [top.enter_context(nc.psum_tensor("pt%d" % i, [128, 1024], BF16)) for i in range(2)]
        PTB = [Buf() for _ in range(2)]

        ident = SB(top, "ident", [128, 128], BF16); identf = SB(top, "identf", [128, 128], F32)
        ones16 = SB(top, "ones16", [128, 128], BF16); onesf = SB(top, "onesf", [128, 128], F32)
        Bc = Buf()
        fw.op("gpsimd", lambda h: h.memset(identf[:], 1.0), writes=[Bc])
        fw.op("gpsimd", lambda h: h.affine_select(out=identf[:], in_=identf[:], pattern=[[-1, 128]],
                                                   compare_op=ALU.is_equal, fill=0.0, base=0, channel_multiplier=1),
              reads=[Bc], writes=[Bc])
        fw.op("vector", lambda h: h.tensor_copy(out=ident[:], in_=identf[:]), reads=[Bc], writes=[Bc])
        fw.op("vector", lambda h: h.memset(ones16[:], 1.0), writes=[Bc])
        fw.op("vector", lambda h: h.memset(onesf[:], 1.0), writes=[Bc])

        def rep_load(st, name, src_row, n, dt=F32, q="sync"):
            t = SB(st, name, [128, n], dt)
            b = Buf()
            fw.dma(q, t[:], src_row.to_broadcast([128, n]), writes=[b])
            return t, b

        def layernorm(xt, xb, grep, brep, gb, st8, sb8, mv, mvb, rstd, eps=LN_EPS):
            for i in range(4):
                fw.op("vector", lambda h, i=i: h.bn_stats(out=st8[:, i * 6:(i + 1) * 6], in_=xt[:, i * 512:(i + 1) * 512]),
                      reads=[xb], writes=[sb8])
            fw.op("vector", lambda h: h.bn_aggr(out=mv[:], in_=st8[:]), reads=[sb8], writes=[mvb])
            fw.op("scalar", lambda h: h.activation(out=rstd[:], in_=mv[:, 1:2], func=AF.Sqrt, bias=eps, scale=1.0),
                  reads=[mvb], writes=[sb8])
            fw.op("vector", lambda h: h.reciprocal(out=rstd[:], in_=rstd[:]), reads=[sb8], writes=[sb8])
            fw.op("vector", lambda h: h.tensor_scalar(out=xt[:], in0=xt[:], scalar1=mv[:, 0:1], scalar2=rstd[:, 0:1],
                                                       op0=ALU.subtract, op1=ALU.mult), reads=[xb, mvb, sb8], writes=[xb])
            fw.op("vector", lambda h: h.tensor_tensor(out=xt[:], in0=xt[:], in1=grep[:], op=ALU.mult), reads=[xb, gb], writes=[xb])
            fw.op("vector", lambda h: h.tensor_tensor(out=xt[:], in0=xt[:], in1=brep[:], op=ALU.add), reads=[xb, gb], writes=[xb])

        def transpose_to(src16, srcb, dst, dstb, nchunk, col0=0, ncols=128):
            for g4 in range(0, nchunk, 8):
                n = min(8, nchunk - g4)
                pi = (g4 // 8) % 2
                for c in range(n):
                    fw.op("tensor", lambda h, c=c: h.transpose(PT[pi][:, c * 128:c * 128 + ncols],
                                                               src16[0:ncols, (g4 + c) * 128:(g4 + c + 1) * 128], ident[0:ncols, 0:ncols]),
                          reads=[srcb, Bc], writes=[PTB[pi]])
                eng = "vector" if pi == 0 else "scalar"
                if eng == "vector":
                    fw.op("vector", lambda h: h.tensor_copy(
                        out=dst[:, g4:g4 + n, col0:col0 + ncols],
                        in_=PT[pi][:, 0:n * 128].rearrange("p (c t) -> p c t", t=128)[:, :, 0:ncols]),
                        reads=[PTB[pi]], writes=[dstb])
                else:
                    fw.op("scalar", lambda h: h.copy(
                        out=dst[:, g4:g4 + n, col0:col0 + ncols],
                        in_=PT[pi][:, 0:n * 128].rearrange("p (c t) -> p c t", t=128)[:, :, 0:ncols]),
                        reads=[PTB[pi]], writes=[dstb])

        with ExitStack() as st:
            grep, gb = rep_load(st, "lng", ln_in_g, D)
            brep, bb_ = rep_load(st, "lnb", ln_in_b, D)
            gbb = Buf()
            fw.op("vector", lambda h: h.tensor_copy(out=grep[:, 0:1], in_=grep[:, 0:1]), reads=[gb, bb_], writes=[gbb])
            xts = [SB(st, "xt%d" % i, [128, D], F32) for i in range(2)]; xbs = [Buf(), Buf()]
            h16 = [SB(st, "h16_%d" % i, [128, D], BF16) for i in range(2)]; h16b = [Buf(), Buf()]
            hTs = [SB(st, "hTs%d" % i, [128, 16, 512], BF16) for i in range(2)]; hTb = [Buf(), Buf()]
            st8 = SB(st, "st8", [128, 24], F32); sb8 = Buf(); mv = SB(st, "mv", [128, 2], F32); mvb = Buf()
            rstd = SB(st, "rstd", [128, 1], F32)
            mk = SB(st, "mk", [128, 17], F32); mkb = Buf()
            zt = SB(st, "zt", [128, 16, 1], BF16); ztb = Buf()
            fw.op("vector", lambda h: h.memset(zt[:], 0.0), writes=[ztb])
            hTv = hT_d.rearrange("(c p) s -> p c s", p=128)
            fw.dma("gpsimd", hTv[:, :, 0:1], zt[:], reads=[ztb], writes=[dbuf("hT_d")], slow=True)
            fw.dma("gpsimd", hTv[:, :, S + 1:S + 2], zt[:], reads=[ztb], writes=[dbuf("hT_d")], slow=True)
            fw.dma("sync", mk[:], m_own, writes=[mkb])
            hTov = hTo_d.rearrange("(c p) s -> p c s", p=128)
            it = 0
            for grp in range(16 + 5):
                own = grp >= 16
                ntile = 4 if not own else (4 if grp < 20 else 1)
                hb = grp % 2
                for ti in range(ntile):
                    tile = (grp * 4 + ti) if not own else ((grp - 16) * 4 + ti)
                    k = it % 2; it += 1
                    src = x_seq if not own else x_own
                    fw.dma("sync", xts[k][:], src[tile * 128:(tile + 1) * 128, :], writes=[xbs[k]])
                    layernorm(xts[k], xbs[k], grep, brep, gbb, st8, sb8, mv, mvb, rstd)
                    if own:
                        fw.op("scalar", lambda h, k=k, tile=tile: h.activation(out=xts[k][:], in_=xts[k][:], func=AF.Identity,
                                                                                scale=mk[:, tile:tile + 1]),
                              reads=[xbs[k], mkb], writes=[xbs[k]])
                        fw.dma("gpsimd", ho_d[tile * 128:(tile + 1) * 128, :], xts[k][:], reads=[xbs[k]], writes=[dbuf("ho_d")])
                    fw.op("scalar", lambda h, k=k: h.copy(out=h16[k][:], in_=xts[k][:]), reads=[xbs[k]], writes=[h16b[k]])
                    transpose_to(h16[k], h16b[k], hTs[hb], hTb[hb], 16, col0=ti * 128)
                if not own:
                    fw.dma("gpsimd", hTv[:, :, 1 + grp * 512:1 + grp * 512 + 512], hTs[hb][:], reads=[hTb[hb]], writes=[dbuf("hT_d")])
                else:
                    g0 = (grp - 16) * 512
                    fw.dma("gpsimd", hTov[:, :, g0:g0 + ntile * 128], hTs[hb][:, :, 0:ntile * 128], reads=[hTb[hb]], writes=[dbuf("hTo_d")])
        fw.barrier()

        w_in_v = w_in.rearrange("(c p) n -> p c n", p=128)

        def proj_pass(st, src_v, srcname, ntok_tiles, blocks, epilogue, sh0=1, src_w=None):
            nb = len(blocks)
            wts = []
            wb = Buf()
            for bi, (col0, cidx, bias) in enumerate(blocks):
                nsh = 3 if cidx is not None else 1
                for sh in range(nsh):
                    wt = SB(st, "w_%d_%d" % (bi, sh), [128, 16, 512], BF16)
                    fw.dma("gpsimd", wt[:], w_in_v[:, :, col0:col0 + 512], writes=[wb])
                    if cidx is not None:
                        cw, cwb = rep_load(st, "cw_%d_%d" % (bi, sh), hy_conv_w[sh:sh + 1, cidx:cidx + 512], 512)
                        for c in range(16):
                            fw.op("vector", lambda h, c=c, wt=wt, cw=cw: h.tensor_tensor(out=wt[:, c, :], in0=wt[:, c, :], in1=cw[:], op=ALU.mult),
                                  reads=[wb, cwb], writes=[wb])
                    wts.append((bi, sh if cidx is not None else sh0, wt))
                if bias is not None:
                    b16 = SB(st, "b16_%d" % bi, [1, 512], BF16)
                    fw.dma("gpsimd", b16[:], bias, writes=[wb])
                    wts.append((bi, -1, b16))
            hw = [SB(st, "hw%d" % i, [128, 16, 514], BF16) for i in range(2)]; hwb = [Buf(), Buf()]
            ngrp = (ntok_tiles + 3) // 4
            for g in range(ngrp):
                k = g % 2
                nt = min(4, ntok_tiles - g * 4)
                wdt = nt * 128 + 2
                if src_w is not None:
                    wdt = min(wdt, src_w - g * 512)
                fw.dma("sync", hw[k][:, :, 0:wdt], src_v[:, :, g * 512:g * 512 + wdt], reads=[dbuf(srcname)], writes=[hwb[k]])
                for m in range(nt):
                    tile = g * 4 + m
                    pidx = [(tile * nb + bi) % 6 for bi in range(nb)]
                    for bi in range(nb):
                        mine = [(sh, wt) for (b2, sh, wt) in wts if b2 == bi]
                        nsteps = sum(16 if sh >= 0 else 1 for sh, _ in mine)
                        step = 0
                        for sh, wt in mine:
                            if sh < 0:
                                fw.op("tensor", lambda h, wt=wt, p=pidx[bi], s0=(step == 0), s1=(step == nsteps - 1):
                                      h.matmul(PS[p][:], ones16[0:1, :], wt[:], start=s0, stop=s1), reads=[wb, Bc], writes=[PSB[pidx[bi]]])
                                step += 1
                                continue
                            for c in range(16):
                                fw.op("tensor", lambda h, wt=wt, c=c, p=pidx[bi], off=m * 128 + sh, s0=(step == 0), s1=(step == nsteps - 1), k=k:
                                      h.matmul(PS[p][:], hw[k][:, c, off:off + 128], wt[:, c, :], start=s0, stop=s1),
                                      reads=[wb, hwb[k]], writes=[PSB[pidx[bi]]])
                                step += 1
                    epilogue(tile, pidx)

        def qk_epilogue_factory(st, gsrc, ropeC, ropeS, nheads_list, dstT, dstname, pref):
            grep_, gb_ = rep_load(st, pref + "g", gsrc, 128)
            ss = SB(st, pref + "ss", [128, 4], F32); ssb = Buf()
            junk = SB(st, pref + "junk", [128, 128], F32)
            xn = SB(st, pref + "xn", [128, 128], F32); xnb = Buf()
            t1 = SB(st, pref + "t1", [128, 128], F32); t2 = SB(st, pref + "t2", [128, 128], F32); tb_ = Buf()
            x16 = [SB(st, pref + "x16%d" % i, [128, 128], BF16) for i in range(2)]; x16b = [Buf(), Buf()]
            rc = [SB(st, pref + "rc%d" % i, [128, 128], F32) for i in range(2)]
            rs = [SB(st, pref + "rs%d" % i, [128, 128], F32) for i in range(2)]; rb = [Buf(), Buf()]
            stg = SB(st, pref + "stg", [128, 8, 128], BF16); stgb = Buf()
            cnt = [0]

            def fn(tile, p, heads):
                k = tile % 2
                fw.dma("sync", rc[k][:], ropeC[tile * 128:(tile + 1) * 128, :], writes=[rb[k]])
                fw.dma("sync", rs[k][:], ropeS[tile * 128:(tile + 1) * 128, :], writes=[rb[k]])
                for hi, (co, dh) in enumerate(heads):
                    fw.op("scalar", lambda h, hi=hi, co=co: h.activation(out=junk[:], in_=PS[p][:, co:co + 128], func=AF.Square,
                                                                         accum_out=ss[:, hi:hi + 1]), reads=[PSB[p]], writes=[ssb])
                nh = len(heads)
                fw.op("scalar", lambda h: h.activation(out=ss[:, 0:nh], in_=ss[:, 0:nh], func=AF.Sqrt, bias=QK_EPS, scale=1.0 / 128),
                      reads=[ssb], writes=[ssb])
                fw.op("vector", lambda h: h.reciprocal(out=ss[:, 0:nh], in_=ss[:, 0:nh]), reads=[ssb], writes=[ssb])
                for hi, (co, dh) in enumerate(heads):
                    j = cnt[0] % 2; cnt[0] += 1
                    fw.op("vector", lambda h, hi=hi, co=co: h.scalar_tensor_tensor(out=xn[:], in0=PS[p][:, co:co + 128], scalar=ss[:, hi:hi + 1],
                                                                                  in1=grep_[:], op0=ALU.mult, op1=ALU.mult),
                          reads=[PSB[p], ssb, gb_], writes=[xnb])
                    fw.op("vector", lambda h: h.tensor_tensor(out=t1[:], in0=xn[:], in1=rc[k][:], op=ALU.mult), reads=[xnb, rb[k]], writes=[tb_])
                    xv = xn[:].rearrange("p (a h d) -> p a h d", a=2, h=2)
                    sv = rs[k][:].rearrange("p (a h d) -> p a h d", a=2, h=2)
                    tv = t2[:].rearrange("p (a h d) -> p a h d", a=2, h=2)
                    fw.op("vector", lambda h: h.tensor_tensor(out=tv[:, :, 0, :], in0=xv[:, :, 1, :], in1=sv[:, :, 0, :], op=ALU.mult),
                          reads=[xnb, rb[k]], writes=[tb_])
                    fw.op("vector", lambda h: h.tensor_tensor(out=tv[:, :, 1, :], in0=xv[:, :, 0, :], in1=sv[:, :, 1, :], op=ALU.mult),
                          reads=[xnb, rb[k]], writes=[tb_])
                    fw.op("vector", lambda h, j=j: h.tensor_tensor(out=x16[j][:], in0=t1[:], in1=t2[:], op=ALU.add), reads=[tb_], writes=[x16b[j]])
                    fw.op("tensor", lambda h, j=j, hi=hi: h.transpose(PT[0][:, hi * 128:(hi + 1) * 128], x16[j][:], ident[:]),
                          reads=[x16b[j], Bc], writes=[PTB[0]])
                fw.op("scalar", lambda h: h.copy(out=stg[:, 0:nh, :], in_=PT[0][:, 0:nh * 128].rearrange("p (c t) -> p c t", t=128)),
                      reads=[PTB[0]], writes=[stgb])
                for hi, (co, dh) in enumerate(heads):
                    fw.dma("gpsimd", dstT[:, dh, tile * 128:(tile + 1) * 128], stg[:, hi, :], reads=[stgb], writes=[dbuf(dstname)])
            return fn

        hTv = hT_d.rearrange("(c p) s -> p c s", p=128)
        hTov = hTo_d.rearrange("(c p) s -> p c s", p=128)

        if stop == "S0":
            return nc
        with ExitStack() as st:
            kfn = qk_epilogue_factory(st, k_norm_g, ropeC_k, ropeS_k, 2, kT_d, "kT_d", "k")
            v16 = [SB(st, "v16_%d" % i, [128, 256], BF16) for i in range(2)]; v16b = [Buf(), Buf()]

            def kv_ep(tile, pidx):
                p = pidx[0]
                kfn(tile, p, [(0, 0), (128, 1)])
                k = tile % 2
                fw.op("scalar", lambda h: h.copy(out=v16[k][:], in_=PS[p][:, 256:512]), reads=[PSB[p]], writes=[v16b[k]])
                fw.dma("gpsimd", v_d[tile * 128:(tile + 1) * 128, :], v16[k][:], reads=[v16b[k]], writes=[dbuf("v_d")])
            proj_pass(st, hTv, "hT_d", 64, [(1024, None, None)], kv_ep)
        fw.barrier()

        if stop == "KV":
            return nc
        for qb in range(2):
            with ExitStack() as st:
                qfn = qk_epilogue_factory(st, q_norm_g, ropeC_q, ropeS_q, 4, qT_d, "qT_d", "q")

                def q_ep(tile, pidx, qb=qb):
                    qfn(tile, pidx[0], [(i * 128, qb * 4 + i) for i in range(4)])
                proj_pass(st, hTov, "hTo_d", 16, [(qb * 512, None, None)], q_ep)
            fw.barrier()

        if stop == "Q":
            return nc
        with ExitStack() as st:
            zr = SB(st, "zrow", [1, 2048], BF16); zrb = Buf()
            fw.op("vector", lambda h: h.memset(zr[:], 0.0), writes=[zrb])
            fw.dma("gpsimd", p_d[0:1, :], zr[:], reads=[zrb], writes=[Buf()])
            fw.dma("gpsimd", p_d[S + 1:S + 2, :], zr[:], reads=[zrb], writes=[Buf()])
            fw.barrier()
        for cb in range(2):
            with ExitStack() as st:
                p16 = [SB(st, "p16_%d" % i, [128, 2, 512], BF16) for i in range(2)]; p16b = [Buf(), Buf()]

                def p_ep(tile, pidx, cb=cb):
                    k = tile % 2
                    fw.op("scalar", lambda h: h.copy(out=p16[k][:, 0, :], in_=PS[pidx[0]][:]), reads=[PSB[pidx[0]]], writes=[p16b[k]])
                    fw.op("vector", lambda h: h.tensor_copy(out=p16[k][:, 1, :], in_=PS[pidx[1]][:]), reads=[PSB[pidx[1]]], writes=[p16b[k]])
                    dst = p_d[1 + tile * 128:1 + (tile + 1) * 128, :].rearrange("p (a c) -> p a c", a=2)[:, :, cb * 512:(cb + 1) * 512]
                    fw.dma("gpsimd", dst, p16[k][:], reads=[p16b[k]], writes=[Buf()])
                c1 = 1024 + cb * 512; c2 = 2048 + cb * 512
                proj_pass(st, hTv, "hT_d", 64, [(1536 + c1, None, None), (1536 + c2, None, None)], p_ep)
            fw.barrier()
        with ExitStack() as st:
            p16 = [SB(st, "po16_%d" % i, [128, 2, 512], BF16) for i in range(2)]; p16b = [Buf(), Buf()]

            def po_ep(tile, pidx):
                k = tile % 2
                fw.op("scalar", lambda h: h.copy(out=p16[k][:, 0, :], in_=PS[pidx[0]][:]), reads=[PSB[pidx[0]]], writes=[p16b[k]])
                fw.op("vector", lambda h: h.tensor_copy(out=p16[k][:, 1, :], in_=PS[pidx[1]][:]), reads=[PSB[pidx[1]]], writes=[p16b[k]])
                fw.dma("gpsimd", po_d[tile * 128:(tile + 1) * 128, :].rearrange("p (a c) -> p a c", a=2), p16[k][:], reads=[p16b[k]], writes=[Buf()])
            proj_pass(st, hTov, "hTo_d", 17, [(1536, None, None), (1536 + 512, None, None)], po_ep, sh0=0, src_w=NOWN)
        fw.barrier()
        if stop == "Z":
            return nc

        def conv_stage(st, src, ntile, row0, W, ccol, emit):
            wr = []
            for i in range(3):
                wr.append(rep_load(st, "cvw%d" % i, hy_conv_w[i:i + 1, ccol:ccol + W], W))
            br_, brb_ = rep_load(st, "cvb", hy_conv_b[0:1, ccol:ccol + W], W)
            Pt = [[SB(st, "cvP%d_%d" % (k, i), [128, W], BF16) for i in range(3)] for k in range(2)]; Ptb = [[Buf() for i in range(3)] for k in range(2)]
            acc = SB(st, "cvacc", [128, W], F32); acc2 = SB(st, "cvacc2", [128, W], F32); ab = Buf(); ab2 = Buf()
            return _conv_run(src, ntile, row0, wr, br_, brb_, Pt, Ptb, acc, acc2, ab, ab2, emit)

        def _conv_run(src, ntile, row0, wr, br_, brb_, Pt, Ptb, acc, acc2, ab, ab2, emit):
            for t in range(ntile):
                k = t % 2
                for i in range(3):
                    r0 = row0 + t * 128 + i
                    fw.dma("sync", Pt[k][i][:], src[r0:r0 + 128, :], writes=[Ptb[k][i]])
                fw.op("vector", lambda h: h.tensor_tensor(out=acc[:], in0=Pt[k][0][:], in1=wr[0][0][:], op=ALU.mult), reads=[Ptb[k][0], wr[0][1], ab], writes=[ab])
                fw.op("vector", lambda h: h.tensor_tensor(out=acc2[:], in0=Pt[k][1][:], in1=wr[1][0][:], op=ALU.mult), reads=[Ptb[k][1], wr[1][1], ab2], writes=[ab2])
                fw.op("vector", lambda h: h.tensor_tensor(out=acc[:], in0=acc[:], in1=acc2[:], op=ALU.add), reads=[ab, ab2], writes=[ab])
                fw.op("vector", lambda h: h.tensor_tensor(out=acc2[:], in0=Pt[k][2][:], in1=wr[2][0][:], op=ALU.mult), reads=[Ptb[k][2], wr[2][1], ab2], writes=[ab2])
                fw.op("vector", lambda h: h.tensor_tensor(out=acc[:], in0=acc[:], in1=acc2[:], op=ALU.add), reads=[ab, ab2], writes=[ab])
                fw.op("vector", lambda h: h.tensor_tensor(out=acc[:], in0=acc[:], in1=br_[:], op=ALU.add), reads=[ab, brb_], writes=[ab])
                emit(t, acc, ab)
                yield

        zst = ExitStack()
        if True:
            st = zst
            z16 = [SB(st, "cvz%d" % i, [128, 1024], BF16) for i in range(2)]; z16b = [Buf(), Buf()]

            def z_emit(t, acc, ab):
                k = t % 2
                fw.op("vector", lambda h: h.tensor_tensor(out=z16[k][:], in0=acc[:, 0:1024], in1=acc[:, 1024:2048], op=ALU.mult), reads=[ab], writes=[z16b[k]])
                fw.dma("gpsimd", z_d[t * 128:(t + 1) * 128, :], z16[k][:], reads=[z16b[k]], writes=[Buf()])
            zgen = conv_stage(st, p_d, 64, 0, 2048, 1024, z_emit)
        with ExitStack() as st:
            o16 = [SB(st, "cvo%d" % i, [128, 1024], BF16) for i in range(2)]; o16b = [Buf(), Buf()]

            def o_emit(t, acc, ab):
                k = t % 2
                fw.op("scalar", lambda h: h.copy(out=o16[k][:], in_=acc[:]), reads=[ab], writes=[o16b[k]])
                fw.dma("gpsimd", x0_d[t * 128:(t + 1) * 128, :], o16[k][:], reads=[o16b[k]], writes=[Buf()])
            for _ in conv_stage(st, po_d, 16, 0, 1024, 0, o_emit):
                pass
        fw.barrier()

        for cb in range(4):
            with ExitStack() as st:
                o16 = [SB(st, "g16_%d" % i, [128, 1024], BF16) for i in range(2)]; o16b = [Buf(), Buf()]

                def g_ep(tile, pidx, cb=cb):
                    k = tile % 2
                    for bi in range(2):
                        fw.op("scalar", lambda h, bi=bi: h.activation(out=o16[k][:, bi * 512:(bi + 1) * 512], in_=PS[pidx[bi]][:], func=AF.Sigmoid),
                              reads=[PSB[pidx[bi]]], writes=[o16b[k]])
                    fw.dma("gpsimd", g_d[tile * 128:(tile + 1) * 128, cb * 1024:(cb + 1) * 1024], o16[k][:], reads=[o16b[k]], writes=[dbuf("g_d")])
                    next(zgen, None)
                c0 = cb * 1024
                proj_pass(st, hTov, "hTo_d", 16,
                          [(4608 + c0, None, b_gate[0:1, c0:c0 + 512]), (4608 + c0 + 512, None, b_gate[0:1, c0 + 512:c0 + 1024])], g_ep)
            fw.barrier()
        for _ in zgen:
            pass
        fw.barrier()
        zst.close()

        if stop == "S1":
            return nc
        with ExitStack() as st:
            kT = SB(st, "kT", [128, 2, S], BF16); kTb = Buf()
            vs = SB(st, "vs", [128, 64, 256], BF16); vsb = Buf()
            qT = SB(st, "qT", [128, 8, T], BF16); qTb = Buf()
            aT = SB(st, "aT", [128, 8, T], BF16); aTb = Buf()
            pT = [SB(st, "pT%d" % i, [128, 512], BF16) for i in range(3)]; pTb = [Buf() for _ in range(3)]
            rl = SB(st, "rl", [128, 512], F32); rlb = Buf()
            for hh in range(2):
                fw.dma("sync", kT[:, hh, :], kT_d[:, hh, :], reads=[dbuf("kT_d")], writes=[kTb])
            for g in range(4):
                fw.dma("sync", vs[:, g * 16:(g + 1) * 16, :], v_d[g * 2048:(g + 1) * 2048, :].rearrange("(t p) c -> p t c", p=128),
                       reads=[dbuf("v_d")], writes=[vsb])
            for g in range(4):
                fw.dma("sync", qT[:, g * 2:(g + 1) * 2, :], qT_d[:, g * 2:(g + 1) * 2, :], reads=[dbuf("qT_d")], writes=[qTb])
            sc = 1.0 / math.sqrt(128.0)
            pT4 = pT + [SB(st, "pT3", [128, 512], BF16), SB(st, "pT4x", [128, 512], BF16)]; pTb4 = pTb + [Buf(), Buf()]
            acc = [SB(st, "aacc%d" % i, [128, 512], F32) for i in range(2)]; accb = [Buf(), Buf()]
            iters = [(hd, qb, kc) for hd in range(8) for qb in range(4) for kc in range(64)]
            NIT = len(iters)

            def emit_qk(n):
                hd, qb, kc = iters[n]; kvh = hd // 4; si = n % 4; pj = n % 5
                fw.op("tensor", lambda h: h.matmul(PS[si][:], kT[:, kvh, kc * 128:(kc + 1) * 128], qT[:, hd, qb * 512:(qb + 1) * 512],
                                                   start=True, stop=True), reads=[kTb, qTb], writes=[PSB[si]])
                fw.op("scalar", lambda h: h.activation(out=pT4[pj][:], in_=PS[si][:], func=AF.Exp, scale=sc), reads=[PSB[si]], writes=[pTb4[pj]])

            def emit_pv(n):
                hd, qb, kc = iters[n]; kvh = hd // 4; pj = n % 5; g = n // 64; po = 4; a = g % 2
                fw.op("tensor", lambda h: h.matmul(PS[po][:], vs[:, kc, kvh * 128:(kvh + 1) * 128], pT4[pj][:], start=(kc == 0), stop=(kc == 63)),
                      reads=[vsb, pTb4[pj]], writes=[PSB[po]])
                if kc == 0:
                    fw.op("vector", lambda h: h.tensor_copy(out=acc[a][:], in_=pT4[pj][:]), reads=[pTb4[pj]], writes=[accb[a]])
                else:
                    fw.op("vector", lambda h: h.tensor_tensor(out=acc[a][:], in0=pT4[pj][:], in1=acc[a][:], op=ALU.add), reads=[pTb4[pj], accb[a]], writes=[accb[a]])
                if kc == 63:
                    fw.op("tensor", lambda h: h.matmul(PS[5][:], onesf[:], acc[a][:], start=True, stop=True), reads=[Bc, accb[a]], writes=[PSB[5]])
                    fw.op("vector", lambda h: h.reciprocal(out=rl[:], in_=PS[5][:]), reads=[PSB[5]], writes=[rlb])
                    fw.op("vector", lambda h: h.tensor_tensor(out=aT[:, hd, qb * 512:(qb + 1) * 512], in0=PS[po][:], in1=rl[:], op=ALU.mult),
                          reads=[PSB[po], rlb], writes=[aTb])

            emit_qk(0); emit_qk(1); emit_qk(2)
            for n in range(NIT):
                if n + 3 < NIT:
                    emit_qk(n + 3)
                emit_pv(n)
            for g in range(4):
                fw.dma("gpsimd", at_d[:, g * 2:(g + 1) * 2, :], aT[:, g * 2:(g + 1) * 2, :], reads=[aTb], writes=[dbuf("at_d")])
        fw.barrier()

        if stop == "S2":
            return nc
        persist = ExitStack()
        top.enter_context(persist)
        scl = SB(persist, "scl", [128, C], F32); sclb = Buf()
        drep, drepb = rep_load(persist, "drep", hy_bias_d, C)
        with ExitStack() as st:
            w1 = SB(st, "fw1", [33, 64], F32); w2 = SB(st, "fw2", [64, 64], F32); w3 = SB(st, "fw3", [64, 2048], F32)
            fb = SB(st, "fb", [64, 4], F32); fbb = Buf(); wl = Buf()
            fw.dma("sync", w1[:], filt_w1, writes=[wl]); fw.dma("sync", w2[:], filt_w2, writes=[wl]); fw.dma("sync", w3[:], filt_w3, writes=[wl])
            fw.dma("sync", fb[:, 0:1], filt_f1, writes=[fbb]); fw.dma("sync", fb[:, 1:2], filt_b1, writes=[fbb])
            fw.dma("sync", fb[:, 2:3], filt_f2, writes=[fbb]); fw.dma("sync", fb[:, 3:4], filt_b2, writes=[fbb])
            fbp = SB(st, "fbp", [64, 2], F32)
            fw.op("vector", lambda h: h.tensor_tensor(out=fbp[:, 0:1], in0=fb[:, 0:1], in1=fb[:, 1:2], op=ALU.mult), reads=[fbb], writes=[fbb])
            fw.op("vector", lambda h: h.tensor_tensor(out=fbp[:, 1:2], in0=fb[:, 2:3], in1=fb[:, 3:4], op=ALU.mult), reads=[fbb], writes=[fbb])
            fT = SB(st, "fT", [33, S], F32); fTb = Buf()
            fw.dma("sync", fT[:], featsT, writes=[fTb])
            h1T = SB(st, "fh1T", [64, 512], F32); h1b = Buf()
            h2T = SB(st, "fh2T", [64, S], F32); h2b = Buf()
            ar = SB(st, "far", [64, 512], F32); m1 = SB(st, "fm1", [64, 512], F32); m2 = SB(st, "fm2", [64, 512], F32); arb = Buf()

            def sin_layer(pidx, fcol, bcol, dst_ap, dstb):
                fw.op("scalar", lambda h: h.activation(out=ar[:], in_=PS[pidx][0:64, :], func=AF.Identity, scale=fb[:, fcol:fcol + 1], bias=fbp[:, bcol:bcol + 1]),
                      reads=[PSB[pidx], fbb], writes=[arb])
                fw.op("vector", lambda h: h.tensor_scalar(out=m1[:], in0=ar[:], scalar1=PI, scalar2=-2 * PI, op0=ALU.is_gt, op1=ALU.mult), reads=[arb], writes=[arb])
                fw.op("vector", lambda h: h.tensor_scalar(out=m2[:], in0=ar[:], scalar1=-PI, scalar2=2 * PI, op0=ALU.is_lt, op1=ALU.mult), reads=[arb], writes=[arb])
                fw.op("vector", lambda h: h.tensor_tensor(out=ar[:], in0=ar[:], in1=m1[:], op=ALU.add), reads=[arb], writes=[arb])
                fw.op("vector", lambda h: h.tensor_tensor(out=ar[:], in0=ar[:], in1=m2[:], op=ALU.add), reads=[arb], writes=[arb])
                fw.op("scalar", lambda h: h.activation(out=dst_ap, in_=ar[:], func=AF.Sin), reads=[arb], writes=[dstb])

            for pb in range(16):
                fw.op("tensor", lambda h, pb=pb: h.matmul(PS[0][0:64, :], w1[:], fT[:, pb * 512:(pb + 1) * 512], start=True, stop=True),
                      reads=[wl, fTb], writes=[PSB[0]])
                sin_layer(0, 0, 0, h1T[:], h1b)
                fw.op("tensor", lambda h: h.matmul(PS[1][0:64, :], w2[:], h1T[:], start=True, stop=True), reads=[wl, h1b], writes=[PSB[1]])
                sin_layer(1, 2, 1, h2T[:, pb * 512:(pb + 1) * 512], h2b)
            h2T16 = SB(st, "fh2T16", [64, S], BF16); h2b16 = Buf(); w316 = SB(st, "fw316", [64, 2048], BF16); wl16 = Buf()
            fw.op("vector", lambda h: h.tensor_copy(out=w316[:], in_=w3[:]), reads=[wl], writes=[wl16])
            for q_ in range(4):
                fw.op("scalar" if q_ % 2 else "vector", (lambda h, q_=q_: h.copy(out=h2T16[:, q_ * 2048:(q_ + 1) * 2048], in_=h2T[:, q_ * 2048:(q_ + 1) * 2048])) if q_ % 2 else
                      (lambda h, q_=q_: h.tensor_copy(out=h2T16[:, q_ * 2048:(q_ + 1) * 2048], in_=h2T[:, q_ * 2048:(q_ + 1) * 2048])), reads=[h2b], writes=[h2b16])
            adl, adlb = rep_load(st, "adl", absdelta, C)
            ngt = SB(st, "ngt", [128, 64], F32); m0s = SB(st, "m0s", [128, 1], F32); ngb = Buf()
            fw.dma("sync", ngt[:], negt, writes=[ngb]); fw.dma("sync", m0s[:], m0, writes=[ngb])
            dec = [SB(st, "dec%d" % i, [128, C], F32) for i in range(2)]; decb = [Buf(), Buf()]
            fo = [SB(st, "fo%d" % i, [128, 2048], F32) for i in range(2)]; fob = [Buf(), Buf()]
            fo16 = [SB(st, "fo16_%d" % i, [128, 2048], BF16) for i in range(2)]; fo16b = [Buf(), Buf()]
            sq = [SB(st, "fsq%d" % i, [128, 2048], BF16) for i in range(2)]; sqb = [Buf(), Buf()]
            for pt in range(64):
                k = pt % 2
                fw.op("scalar", lambda h: h.activation(out=dec[k][:], in_=adl[:], func=AF.Exp, scale=ngt[:, pt:pt + 1]), reads=[adlb, ngb], writes=[decb[k]])
                for cb in range(4):
                    fw.op("tensor", lambda h, cb=cb: h.matmul(PS[cb][:], h2T16[:, pt * 128:(pt + 1) * 128], w316[:, cb * 512:(cb + 1) * 512], start=True, stop=True),
                          reads=[wl16, h2b16], writes=[PSB[cb]])
                    fw.op("vector", lambda h, cb=cb: h.tensor_tensor(out=fo[k][:, cb * 512:(cb + 1) * 512], in0=PS[cb][:],
                                                                    in1=dec[k][:, (cb % 2) * 512:(cb % 2) * 512 + 512], op=ALU.mult),
                          reads=[PSB[cb], decb[k]], writes=[fob[k]])
                if pt == 0:
                    fw.op("vector", lambda h: h.tensor_scalar(out=fo[k][:, 1024:2048], in0=fo[k][:, 1024:2048], scalar1=m0s[:, 0:1], scalar2=None, op0=ALU.mult),
                          reads=[fob[k], ngb], writes=[fob[k]])
                fw.op("scalar", lambda h: h.copy(out=fo16[k][:], in_=fo[k][:]), reads=[fob[k]], writes=[fo16b[k]])
                fw.op("scalar", lambda h: h.activation(out=sq[k][:], in_=fo[k][:], func=AF.Square), reads=[fob[k]], writes=[sqb[k]])
                for cb in range(4):
                    fw.op("tensor", lambda h, cb=cb: h.matmul(PS[4 + cb % 2][:], ones16[:], sq[k][:, cb * 512:(cb + 1) * 512],
                                                              start=(pt == 0 and cb < 2), stop=(pt == 63 and cb >= 2)), reads=[Bc, sqb[k]], writes=[PSB[4 + cb % 2]])
                fw.dma("gpsimd", hf_d[pt * 128:(pt + 1) * 128, :], fo16[k][:, 0:1024], reads=[fo16b[k]], writes=[dbuf("hf_d")])
                fw.dma("gpsimd", hg_d[pt * 128:(pt + 1) * 128, :], fo16[k][:, 1024:2048], reads=[fo16b[k]], writes=[dbuf("hg_d")])
            for cb in range(2):
                fw.op("scalar", lambda h, cb=cb: h.activation(out=scl[:, cb * 512:(cb + 1) * 512], in_=PS[4 + cb][:], func=AF.Sqrt, bias=FILTER_EPS, scale=1.0),
                      reads=[PSB[4 + cb]], writes=[sclb])
            fw.op("vector", lambda h: h.reciprocal(out=scl[:], in_=scl[:]), reads=[sclb], writes=[sclb])
        fw.barrier()

        if stop == "S3a":
            return nc
        with ExitStack() as st:
            def cload(name, src, shape, dt=BF16):
                t = SB(st, name, shape, dt); b = Buf()
                fw.dma("gpsimd" if dt == BF16 else "sync", t[:], src, writes=[b])
                return t, b
            Fc, Fb1 = cload("Fc", cFc, [128, 128]); Fsn, Fb3 = cload("Fsn", cFsn, [128, 128])
            i2re, Fb8 = cload("i2re", ci2re, [128, 16]); i2im, Fb9 = cload("i2im", ci2im, [128, 16])
            FB = Buf()
            fw.op("vector", lambda h: h.tensor_copy(out=Fc[:, 0:1], in_=Fc[:, 0:1]), reads=[Fb1, Fb3, Fb8, Fb9], writes=[FB])

            st1 = ExitStack()
            zt_ = [SB(st1, "f1z%d" % i, [128, 16, 1024], BF16) for i in range(2)]; ztb_ = [Buf(), Buf()]
            for i_ in range(2):
                fw.op("vector", lambda h, i_=i_: h.memset(zt_[i_][64:128, :, :], 0.0), writes=[ztb_[i_]])
            ao = [SB(st1, "f1o%d" % i, [128, 2, 4, 1024], BF16) for i in range(2)]; aob = [Buf(), Buf()]

            def f1_pass(src, dst):
                sv = src.rearrange("(a b) c -> a b c", b=128)
                it = 0
                for nb in range(8):
                    k = nb % 2
                    fw.dma("sync", zt_[k][0:64, :, :], sv[:, nb * 16:(nb + 1) * 16, :], writes=[ztb_[k]])
                    for nn in range(16):
                        n1 = nb * 16 + nn
                        j = (n1 // 4) % 2; q = n1 % 4
                        for hf_ in range(2):
                            cs = slice(hf_ * 512, (hf_ + 1) * 512)
                            pr = (it % 3) * 2; pi_ = pr + 1; it += 1
                            fw.op("tensor", lambda h, nn=nn, pr=pr, cs=cs: h.matmul(PS[pr][:], Fc[:, :], zt_[k][:, nn, cs], start=True, stop=True),
                                  reads=[FB, ztb_[k]], writes=[PSB[pr]])
                            fw.op("tensor", lambda h, nn=nn, pi_=pi_, cs=cs: h.matmul(PS[pi_][:], Fsn[:, :], zt_[k][:, nn, cs], start=True, stop=True),
                                  reads=[FB, ztb_[k]], writes=[PSB[pi_]])
                            fw.op("scalar", lambda h, pr=pr, j=j, q=q, cs=cs: h.copy(out=ao[j][:, 0, q, cs], in_=PS[pr][:]), reads=[PSB[pr]], writes=[aob[j]])
                            fw.op("vector", lambda h, pi_=pi_, j=j, q=q, cs=cs: h.tensor_copy(out=ao[j][:, 1, q, cs], in_=PS[pi_][:]), reads=[PSB[pi_]], writes=[aob[j]])
                        if q == 3:
                            for r in range(2):
                                fw.dma("sync", dst[r][n1 - 3:n1 + 1, :, :].rearrange("n k c -> k n c"), ao[j][:, r, :, :], reads=[aob[j]], writes=[Buf()])

            f1_pass(hf_d, a_d[1])
            f1_pass(hg_d, a_d[2])
            f1_pass(z_d, a_d[0])
            fw.barrier()
            st1.close()
            if stop == "S3b1":
                return nc

            Et = [SB(st, "Et%d" % i, [128, 7, 128], BF16) for i in range(2)]; Etb = [Buf(), Buf()]
            ain = [[SB(st, "ain%d_%d" % (i, r), [128, 1024], BF16) for r in range(4)] for i in range(2)]; ainb = [[Buf() for r in range(4)] for i in range(2)]
            ho = [SB(st, "hho%d" % i, [128, 2, 1024], BF16) for i in range(2)]; hob = [Buf(), Buf()]
            tmps = [SB(st, "ftmp%d" % i, [128, 512], F32) for i in range(2)]; tmpbs = [Buf(), Buf()]

            def f2_loads(k2):
                e = k2 % 2
                fw.dma("sync", Et[e][:], cE[k2], writes=[Etb[e]])
                for r, sd in enumerate([a_d[1][0], a_d[1][1], a_d[2][0], a_d[2][1]]):
                    fw.dma("sync", ain[e][r][:], sd[:, k2, :], writes=[ainb[e][r]])
            f2_loads(0)
            for u in range(256):
                k2, cb = u // 2, u % 2
                k = u % 2; e = k2 % 2
                cs = slice(cb * 512, (cb + 1) * 512)
                pr = (u % 3) * 2; pi_ = pr + 1
                if cb == 0 and k2 + 1 < 128:
                    f2_loads(k2 + 1)
                for r, m in enumerate([0, 1, 0, 1]):
                    fw.op("tensor", lambda h, r=r, m=m: h.matmul(PS[pr][:], Et[e][:, m, :], ain[e][r][:, cs], start=(r == 0), stop=(r == 3)),
                          reads=[Etb[e], ainb[e][r]], writes=[PSB[pr]])
                for r, m in enumerate([2, 0, 1, 3]):
                    fw.op("tensor", lambda h, r=r, m=m: h.matmul(PS[pi_][:], Et[e][:, m, :], ain[e][r][:, cs], start=(r == 0), stop=(r == 3)),
                          reads=[Etb[e], ainb[e][r]], writes=[PSB[pi_]])
                fw.op("vector", lambda h: h.tensor_tensor(out=tmps[k][:], in0=PS[pr][:], in1=scl[:, cs], op=ALU.mult), reads=[PSB[pr], sclb], writes=[tmpbs[k]])
                fw.op("vector", lambda h: h.tensor_tensor(out=ho[e][:, 0, cs], in0=tmps[k][:], in1=drep[:, cs], op=ALU.add), reads=[tmpbs[k], drepb], writes=[hob[e]])
                fw.op("vector", lambda h: h.tensor_tensor(out=ho[e][:, 1, cs], in0=PS[pi_][:], in1=scl[:, cs], op=ALU.mult), reads=[PSB[pi_], sclb], writes=[hob[e]])
                if cb == 1:
                    fw.dma("sync", hh_d[0][k2, :, :], ho[e][:, 0, :], reads=[hob[e]], writes=[Buf()])
                    fw.dma("sync", hh_d[1][k2, :, :], ho[e][:, 1, :], reads=[hob[e]], writes=[Buf()])
            fw.barrier()
            if stop == "S3b2":
                return nc

            hin = [[SB(st, "hin%d_%d" % (i, r), [128, 1024], BF16) for r in range(2)] for i in range(2)]; hinb = [[Buf(), Buf()] for i in range(2)]
            xs = [SB(st, "fxs%d" % i, [128, 1024], BF16) for i in range(2)]; xsb = [Buf(), Buf()]
            ys = [SB(st, "fys%d" % i, [128, 1024], BF16) for i in range(2)]; ysb = [Buf(), Buf()]
            t4 = [SB(st, "ft4%d" % i, [128, 2048], BF16) for i in range(2)]; t4b = [[Buf() for _ in range(4)] for i in range(2)]
            bo = [SB(st, "fbo%d" % i, [128, 2, 1024], BF16) for i in range(2)]; bob = [Buf(), Buf()]

            def fu_loads(k2):
                e = k2 % 2
                fw.dma("sync", Et[e][:], cE[k2], writes=[Etb[e]])
                fw.dma("sync", ain[e][0][:], a_d[0][0][:, k2, :], writes=[ainb[e][0]])
                fw.dma("sync", ain[e][1][:], a_d[0][1][:, k2, :], writes=[ainb[e][1]])
                fw.dma("sync", hin[e][0][:], hh_d[0][k2, :, :], writes=[hinb[e][0]])
                fw.dma("sync", hin[e][1][:], hh_d[1][k2, :, :], writes=[hinb[e][1]])
            fu_loads(0)
            for u in range(256):
                k2, cb = u // 2, u % 2
                k = u % 2; e = k2 % 2
                cs = slice(cb * 512, (cb + 1) * 512)
                px = 0 if k == 0 else 4
                if cb == 0 and k2 + 1 < 128:
                    fu_loads(k2 + 1)
                fw.op("tensor", lambda h: h.matmul(PS[px][:], Et[e][:, 0, :], ain[e][0][:, cs], start=True, stop=False), reads=[Etb[e], ainb[e][0]], writes=[PSB[px]])
                fw.op("tensor", lambda h: h.matmul(PS[px][:], Et[e][:, 1, :], ain[e][1][:, cs], start=False, stop=True), reads=[Etb[e], ainb[e][1]], writes=[PSB[px]])
                fw.op("tensor", lambda h: h.matmul(PS[px + 1][:], Et[e][:, 2, :], ain[e][0][:, cs], start=True, stop=False), reads=[Etb[e], ainb[e][0]], writes=[PSB[px + 1]])
                fw.op("tensor", lambda h: h.matmul(PS[px + 1][:], Et[e][:, 0, :], ain[e][1][:, cs], start=False, stop=True), reads=[Etb[e], ainb[e][1]], writes=[PSB[px + 1]])
                fw.op("scalar", lambda h: h.copy(out=xs[k][:, 0:512], in_=PS[px][:]), reads=[PSB[px]], writes=[xsb[k]])
                fw.op("scalar", lambda h: h.copy(out=xs[k][:, 512:1024], in_=PS[px + 1][:]), reads=[PSB[px + 1]], writes=[xsb[k]])
                fw.op("vector", lambda h: h.tensor_tensor(out=t4[k][:, 0:512], in0=xs[k][:, 0:512], in1=hin[e][0][:, cs], op=ALU.mult), reads=[xsb[k], hinb[e][0]], writes=[t4b[k][0]])
                fw.op("vector", lambda h: h.tensor_tensor(out=t4[k][:, 512:1024], in0=xs[k][:, 512:1024], in1=hin[e][1][:, cs], op=ALU.mult), reads=[xsb[k], hinb[e][1]], writes=[t4b[k][1]])
                fw.op("vector", lambda h: h.tensor_tensor(out=t4[k][:, 1024:1536], in0=xs[k][:, 0:512], in1=hin[e][1][:, cs], op=ALU.mult), reads=[xsb[k], hinb[e][1]], writes=[t4b[k][2]])
                fw.op("vector", lambda h: h.tensor_tensor(out=t4[k][:, 1536:2048], in0=xs[k][:, 512:1024], in1=hin[e][0][:, cs], op=ALU.mult), reads=[xsb[k], hinb[e][0]], writes=[t4b[k][3]])
                fw.op("vector", lambda h: h.tensor_tensor(out=ys[k][:, 0:512], in0=t4[k][:, 0:512], in1=t4[k][:, 512:1024], op=ALU.subtract), reads=[t4b[k][0], t4b[k][1]], writes=[ysb[k]])
                fw.op("vector", lambda h: h.tensor_tensor(out=ys[k][:, 512:1024], in0=t4[k][:, 1024:1536], in1=t4[k][:, 1536:2048], op=ALU.add), reads=[t4b[k][2], t4b[k][3]], writes=[ysb[k]])
                fw.op("tensor", lambda h: h.matmul(PS[2][:], Et[e][:, 4, :], ys[k][:, 0:512], start=True, stop=False), reads=[Etb[e], ysb[k]], writes=[PSB[2]])
                fw.op("tensor", lambda h: h.matmul(PS[2][:], Et[e][:, 6, :], ys[k][:, 512:1024], start=False, stop=True), reads=[Etb[e], ysb[k]], writes=[PSB[2]])
                fw.op("tensor", lambda h: h.matmul(PS[3][:], Et[e][:, 5, :], ys[k][:, 0:512], start=True, stop=False), reads=[Etb[e], ysb[k]], writes=[PSB[3]])
                fw.op("tensor", lambda h: h.matmul(PS[3][:], Et[e][:, 4, :], ys[k][:, 512:1024], start=False, stop=True), reads=[Etb[e], ysb[k]], writes=[PSB[3]])
                fw.op("scalar", lambda h: h.copy(out=bo[e][:, 0, cs], in_=PS[2][:]), reads=[PSB[2]], writes=[bob[e]])
                fw.op("scalar", lambda h: h.copy(out=bo[e][:, 1, cs], in_=PS[3][:]), reads=[PSB[3]], writes=[bob[e]])
                if cb == 1:
                    fw.dma("sync", b_d[0][k2, :, :], bo[e][:, 0, :], reads=[bob[e]], writes=[Buf()])
                    fw.dma("sync", b_d[1][k2, :, :], bo[e][:, 1, :], reads=[bob[e]], writes=[Buf()])
            fw.barrier()
            if stop == "S3b3":
                return nc

            bin_ = [[SB(st, "bin%d_%d" % (i, r), [128, 16, 512], BF16) for r in range(2)] for i in range(2)]; binb = [Buf(), Buf()]
            yo = [SB(st, "fyo%d" % i, [16, 8, 512], F32) for i in range(2)]; yob = [Buf(), Buf()]
            yv = y_d.rearrange("(a b) c -> a b c", b=128)
            it = 0
            for cb in range(2):
                cs = slice(cb * 512, (cb + 1) * 512)
                for nb in range(8):
                    k = (cb * 8 + nb) % 2
                    for r in range(2):
                        fw.dma("sync", bin_[k][r][:], b_d[r][:, nb * 16:(nb + 1) * 16, cs], writes=[binb[k]])
                    for nn in range(16):
                        n1 = nb * 16 + nn
                        p = it % 6; j = (it // 8) % 2; q = it % 8; it += 1
                        fw.op("tensor", lambda h, nn=nn, p=p: h.matmul(PS[p][0:16, :], i2re[:], bin_[k][0][:, nn, :], start=True, stop=False), reads=[FB, binb[k]], writes=[PSB[p]])
                        fw.op("tensor", lambda h, nn=nn, p=p: h.matmul(PS[p][0:16, :], i2im[:], bin_[k][1][:, nn, :], start=False, stop=True), reads=[FB, binb[k]], writes=[PSB[p]])
                        if it % 2 == 0:
                            fw.op("scalar", lambda h, p=p, j=j, q=q: h.copy(out=yo[j][:, q, :], in_=PS[p][0:16, :]), reads=[PSB[p]], writes=[yob[j]])
                        else:
                            fw.op("vector", lambda h, p=p, j=j, q=q: h.tensor_copy(out=yo[j][:, q, :], in_=PS[p][0:16, :]), reads=[PSB[p]], writes=[yob[j]])
                        if q == 7:
                            fw.dma("sync", yv[:, n1 - 7:n1 + 1, cs], yo[j][:], reads=[yob[j]], writes=[Buf()])
        persist.close()
        fw.barrier()

        if stop == "S3b":
            return nc
        def wload(st, name, src, nchunk, ncol):
            t = SB(st, name, [128, nchunk, ncol], BF16); b = Buf()
            sv = src.rearrange("(c p) n -> p c n", p=128)
            for c0 in range(0, ncol, 512):
                fw.dma("gpsimd", t[:, :, c0:c0 + 512], sv[:, :, c0:c0 + 512], writes=[b])
            return t, b

        with ExitStack() as st:
            Wa, Wab = wload(st, "Wa", w_attn_o, 8, D)
            Wh, Whb = wload(st, "Wh", w_hy_o, 8, D)
            yt = [SB(st, "yt%d" % i, [128, C], F32) for i in range(2)]
            x0t = [SB(st, "x0t%d" % i, [128, C], BF16) for i in range(2)]
            gt = [SB(st, "gt%d" % i, [128, 4096], BF16) for i in range(2)]
            atT = [SB(st, "atT%d" % i, [128, 8, 128], BF16) for i in range(2)]; inb = [Buf(), Buf()]
            yh16 = SB(st, "yh16", [128, C], BF16); yhb = Buf()
            yhT = SB(st, "yhT", [128, 8, 128], BF16); yhTb = Buf()
            ya = SB(st, "ya", [128, 512], F32); yab = Buf()
            pre16 = SB(st, "pre16", [128, D], BF16); preb = Buf()
            preT = SB(st, "preTs", [128, 16, 128], BF16); preTb = Buf()
            preTv = preT_d.rearrange("(c p) t -> p c t", p=128)
            for t in range(16):
                k = t % 2
                rows = slice(t * 128, (t + 1) * 128)
                fw.dma("sync", yt[k][:], y_d[rows, :], reads=[dbuf("y_d")], writes=[inb[k]])
                fw.dma("sync", x0t[k][:], x0_d[rows, :], reads=[dbuf("x0_d")], writes=[inb[k]])
                fw.dma("sync", gt[k][:], g_d[rows, :], reads=[dbuf("g_d")], writes=[inb[k]])
                fw.dma("sync", atT[k][:], at_d[:, :, rows], reads=[dbuf("at_d")], writes=[inb[k]])
                fw.op("vector", lambda h: h.tensor_tensor(out=yh16[:], in0=yt[k][:], in1=x0t[k][:], op=ALU.mult), reads=[inb[k]], writes=[yhb])
                transpose_to(yh16, yhb, yhT, yhTb, 8)
                for cb in range(4):
                    cs = slice(cb * 512, (cb + 1) * 512)
                    pa = (cb % 3) * 2; ph = pa + 1
                    for c in range(8):
                        fw.op("tensor", lambda h, c=c, pa=pa: h.matmul(PS[pa][:], atT[k][:, c, :], Wa[:, c, cs], start=(c == 0), stop=(c == 7)),
                              reads=[inb[k], Wab], writes=[PSB[pa]])
                    for c in range(8):
                        fw.op("tensor", lambda h, c=c, ph=ph: h.matmul(PS[ph][:], yhT[:, c, :], Wh[:, c, cs], start=(c == 0), stop=(c == 7)),
                              reads=[yhTb, Whb], writes=[PSB[ph]])
                    fw.op("vector", lambda h, pa=pa: h.tensor_tensor(out=ya[:], in0=PS[pa][:], in1=gt[k][:, cs], op=ALU.mult), reads=[PSB[pa], inb[k]], writes=[yab])
                    fw.op("vector", lambda h, ph=ph: h.tensor_tensor(out=pre16[:, cs], in0=PS[ph][:], in1=gt[k][:, 2048 + cb * 512:2048 + (cb + 1) * 512], op=ALU.mult),
                          reads=[PSB[ph], inb[k]], writes=[preb])
                    fw.op("vector", lambda h: h.tensor_tensor(out=pre16[:, cs], in0=pre16[:, cs], in1=ya[:], op=ALU.add), reads=[preb, yab], writes=[preb])
                transpose_to(pre16, preb, preT, preTb, 16)
                fw.dma("gpsimd", preTv[:, :, rows], preT[:], reads=[preTb], writes=[dbuf("preT_d")])
        fw.barrier()

        if stop == "S4a":
            return nc
        with ExitStack() as st:
            Wo, Wob = wload(st, "Wo", w_out, 16, D)
            g1, g1b = rep_load(st, "g1", ln1_g, D); b1, b1b = rep_load(st, "b1", ln1_b, D)
            gbb = Buf()
            fw.op("vector", lambda h: h.tensor_copy(out=g1[:, 0:1], in_=g1[:, 0:1]), reads=[g1b, b1b], writes=[gbb])
            pT_ = [SB(st, "ppT%d" % i, [128, 16, 128], BF16) for i in range(2)]
            hres = [SB(st, "hres%d" % i, [128, D], F32) for i in range(2)]; inb = [Buf(), Buf()]
            h1t = [SB(st, "h1t%d" % i, [128, D], F32) for i in range(2)]; h1b_ = [Buf(), Buf()]
            h116 = SB(st, "h116", [128, D], BF16); h116b = Buf()
            h1Ts = SB(st, "h1Ts", [128, 16, 128], BF16); h1Tb = Buf()
            st8 = SB(st, "st8b", [128, 24], F32); sb8 = Buf(); mv = SB(st, "mvb", [128, 2], F32); mvb = Buf(); rstd = SB(st, "rstdb", [128, 1], F32)
            preTv = preT_d.rearrange("(c p) t -> p c t", p=128)
            h1Tv = h1T_d.rearrange("(c p) t -> p c t", p=128)
            for t in range(16):
                k = t % 2
                rows = slice(t * 128, (t + 1) * 128)
                fw.dma("sync", pT_[k][:], preTv[:, :, rows], reads=[dbuf("preT_d")], writes=[inb[k]])
                fw.dma("sync", hres[k][:], ho_d[128 * t + 1:128 * t + 129, :], reads=[dbuf("ho_d")], writes=[inb[k]])
                for cb in range(4):
                    cs = slice(cb * 512, (cb + 1) * 512)
                    p = (t * 4 + cb) % 6
                    for c in range(16):
                        fw.op("tensor", lambda h, c=c, p=p: h.matmul(PS[p][:], pT_[k][:, c, :], Wo[:, c, cs], start=(c == 0), stop=(c == 15)),
                              reads=[inb[k], Wob], writes=[PSB[p]])
                    fw.op("vector", lambda h, p=p: h.scalar_tensor_tensor(out=h1t[k][:, cs], in0=hres[k][:, cs], scalar=ALPHA, in1=PS[p][:], op0=ALU.mult, op1=ALU.add),
                          reads=[inb[k], PSB[p]], writes=[h1b_[k]])
                layernorm(h1t[k], h1b_[k], g1, b1, gbb, st8, sb8, mv, mvb, rstd)
                fw.dma("gpsimd", h1_d[rows, :], h1t[k][:], reads=[h1b_[k]], writes=[dbuf("h1_d")])
                fw.op("scalar", lambda h: h.copy(out=h116[:], in_=h1t[k][:]), reads=[h1b_[k]], writes=[h116b])
                transpose_to(h116, h116b, h1Ts, h1Tb, 16)
                fw.dma("gpsimd", h1Tv[:, :, rows], h1Ts[:], reads=[h1Tb], writes=[dbuf("h1T_d")])
        fw.barrier()

        if stop == "S4b":
            return nc
        persist2 = ExitStack()
        top.enter_context(persist2)
        I32 = mybir.dt.int32
        OH1 = SB(persist2, "OH1", [128, 16, 32], F32); OH2 = SB(persist2, "OH2", [128, 16, 32], F32)
        G12 = SB(persist2, "G12", [128, 16, 2], F32); Gb = Buf()
        SL1 = SB(persist2, "SL1", [128, 16], I32); SL2 = SB(persist2, "SL2", [128, 16], I32); SLb = Buf()
        IG = SB(persist2, "IG", [128, 64, 16], I32); ID = SB(persist2, "ID", [128, 64, 8], I32); IXb = Buf()
        zt16 = SB(persist2, "zt16", [128, D], BF16); ztb16 = Buf()
        fw.op("gpsimd", lambda h: h.memset(zt16[:], 0.0), writes=[ztb16])
        for i in range(64):
            fw.dma("sync", xs_d[i * 128:(i + 1) * 128, :], zt16[:], reads=[ztb16], writes=[Buf()])
        h1Tv = h1T_d.rearrange("(c p) t -> p c t", p=128)
        BIG = 1.0e30
        with ExitStack() as st:
            Wr = SB(st, "Wr", [128, 16, 36], BF16); Wrb = Buf()
            fw.dma("gpsimd", Wr[:], w_route.rearrange("(c p) n -> p c n", p=128), writes=[Wrb])
            br16 = SB(st, "br16", [1, 36], BF16)
            fw.dma("gpsimd", br16[:], b_route, writes=[Wrb])
            hT_ = [SB(st, "rhT%d" % i, [128, 16, 128], BF16) for i in range(2)]; inb = [Buf(), Buf()]
            lg = SB(st, "lg", [128, 36], F32); lgb = Buf()
            sm = SB(st, "rsm", [128, 16], F32); smb = Buf()
            oh = SB(st, "roh", [128, 96], F32); ohb = Buf()
            for t in range(16):
                k = t % 2
                fw.dma("sync", hT_[k][:], h1Tv[:, :, t * 128:(t + 1) * 128], reads=[dbuf("h1T_d")], writes=[inb[k]])
                p = t % 6
                for c in range(16):
                    fw.op("tensor", lambda h, c=c, p=p: h.matmul(PS[p][:, 0:36], hT_[k][:, c, :], Wr[:, c, :], start=(c == 0), stop=False), reads=[inb[k], Wrb], writes=[PSB[p]])
                fw.op("tensor", lambda h, p=p: h.matmul(PS[p][:, 0:36], ones16[0:1, :], br16[:], start=False, stop=True), reads=[Wrb, Bc], writes=[PSB[p]])
                fw.op("vector", lambda h, p=p: h.tensor_copy(out=lg[:], in_=PS[p][:, 0:36]), reads=[PSB[p]], writes=[lgb])
                V = lambda fn, r, w: fw.op("vector", fn, reads=r, writes=w)
                V(lambda h: h.tensor_reduce(out=sm[:, 0:1], in_=lg[:, 0:4], axis=mybir.AxisListType.X, op=ALU.max), [lgb], [smb])
                V(lambda h: h.tensor_scalar(out=oh[:, 0:4], in0=lg[:, 0:4], scalar1=sm[:, 0:1], scalar2=None, op0=ALU.is_equal), [lgb, smb], [ohb])
                V(lambda h: h.tensor_scalar(out=oh[:, 4:8], in0=lg[:, 0:4], scalar1=sm[:, 0:1], scalar2=None, op0=ALU.subtract), [lgb, smb], [ohb])
                fw.op("scalar", lambda h: h.activation(out=oh[:, 4:8], in_=oh[:, 4:8], func=AF.Exp, accum_out=sm[:, 1:2]), reads=[ohb, smb], writes=[ohb, smb])
                V(lambda h: h.reciprocal(out=sm[:, 2:3], in_=sm[:, 1:2]), [smb], [smb])
                V(lambda h: h.tensor_scalar(out=oh[:, 8:12], in0=oh[:, 0:4], scalar1=-1.0, scalar2=BIG, op0=ALU.add, op1=ALU.mult), [ohb], [ohb])
                for g_ in range(4):
                    V(lambda h, g_=g_: h.tensor_scalar(out=oh[:, 32 + 8 * g_:40 + 8 * g_], in0=lg[:, 4 + 8 * g_:12 + 8 * g_], scalar1=oh[:, 8 + g_:9 + g_], scalar2=None, op0=ALU.add),
                      [lgb, ohb], [ohb])
                V(lambda h: h.tensor_reduce(out=sm[:, 3:4], in_=oh[:, 32:64], axis=mybir.AxisListType.X, op=ALU.max), [ohb], [smb])
                V(lambda h: h.tensor_scalar(out=oh[:, 64:96], in0=oh[:, 32:64], scalar1=sm[:, 3:4], scalar2=None, op0=ALU.is_equal), [ohb, smb], [ohb])
                V(lambda h: h.scalar_tensor_tensor(out=oh[:, 32:64], in0=oh[:, 64:96], scalar=-BIG, in1=oh[:, 32:64], op0=ALU.mult, op1=ALU.add), [ohb], [ohb])
                V(lambda h: h.tensor_reduce(out=sm[:, 4:5], in_=oh[:, 32:64], axis=mybir.AxisListType.X, op=ALU.max), [ohb], [smb])
                V(lambda h: h.tensor_scalar(out=oh[:, 32:64], in0=oh[:, 32:64], scalar1=sm[:, 4:5], scalar2=None, op0=ALU.is_equal), [ohb, smb], [ohb])
                V(lambda h: h.tensor_tensor(out=sm[:, 5:6], in0=sm[:, 4:5], in1=sm[:, 3:4], op=ALU.subtract), [smb], [smb])
                fw.op("scalar", lambda h: h.activation(out=sm[:, 6:7], in_=sm[:, 5:6], func=AF.Exp), reads=[smb], writes=[smb])
                V(lambda h: h.tensor_scalar(out=sm[:, 7:8], in0=sm[:, 6:7], scalar1=1.0, scalar2=None, op0=ALU.add), [smb], [smb])
                V(lambda h: h.reciprocal(out=sm[:, 8:9], in_=sm[:, 7:8]), [smb], [smb])
                V(lambda h: h.tensor_tensor(out=sm[:, 9:10], in0=sm[:, 8:9], in1=sm[:, 2:3], op=ALU.mult), [smb], [smb])
                V(lambda h: h.tensor_tensor(out=sm[:, 10:11], in0=sm[:, 9:10], in1=sm[:, 6:7], op=ALU.mult), [smb], [smb])
                V(lambda h, t=t: h.tensor_copy(out=OH1[:, t, :], in_=oh[:, 64:96]), [ohb], [Gb])
                V(lambda h, t=t: h.tensor_copy(out=OH2[:, t, :], in_=oh[:, 32:64]), [ohb], [Gb])
                V(lambda h, t=t: h.tensor_copy(out=G12[:, t, :], in_=sm[:, 9:11]), [smb], [Gb])
            A = SB(st, "mA", [128, 16, 32], F32); Ab = Buf()
            R = SB(st, "mR", [128, 16, 32], F32); Rb = Buf()
            U = SB(st, "mU", [128, 128], F32); Ub = Buf()
            base = SB(st, "mbase", [128, 32], F32); baseb = Buf()
            ci = SB(st, "mci", [128, 32], I32); padf = SB(st, "mpadf", [128, 32], F32); pe = SB(st, "mpe", [128, 32], F32)
            pst = SB(st, "mpst", [128, 32], F32); one32 = SB(st, "mone32", [128, 32], F32); pb_ = Buf()
            sf = SB(st, "msf", [128, 32], F32); eb = SB(st, "meb", [128, 64], F32); junk32 = SB(st, "mj32", [128, 32], F32); ebb = Buf()
            igf = SB(st, "migf", [128, 64, 16], F32); bG = SB(st, "mbG", [128, 64], F32); bD = SB(st, "mbD", [128, 64], F32)
            pc = SB(st, "mpc", [128, 1], F32)
            fw.dma("sync", pc[:], pcol, writes=[ebb])
            V(lambda h: h.tensor_tensor(out=A[:], in0=OH1[:], in1=OH2[:], op=ALU.add), [Gb], [Ab])
            fw.op("gpsimd", lambda h: h.memset(U[:], 1.0), writes=[Ub])
            fw.op("gpsimd", lambda h: h.affine_select(out=U[:], in_=U[:], pattern=[[1, 128]], compare_op=ALU.is_gt, fill=0.0, base=0, channel_multiplier=-1),
                  reads=[Ub], writes=[Ub])
            V(lambda h: h.memset(base[:], 0.0), [], [baseb])
            V(lambda h: h.memset(one32[:], 1.0), [], [pb_])
            for i in range(16):
                pa = (2 * i) % 6; pb2 = (2 * i + 1) % 6
                fw.op("tensor", lambda h, i=i, pa=pa: h.matmul(PS[pa][:, 0:32], U[:], A[:, i, :], start=True, stop=True), reads=[Ub, Ab], writes=[PSB[pa]])
                fw.op("tensor", lambda h, i=i, pb2=pb2: h.matmul(PS[pb2][:, 0:32], onesf[:], A[:, i, :], start=True, stop=True), reads=[Bc, Ab], writes=[PSB[pb2]])
                V(lambda h, i=i, pa=pa: h.tensor_tensor(out=R[:, i, :], in0=PS[pa][:, 0:32], in1=base[:], op=ALU.add), [PSB[pa], baseb], [Rb])
                V(lambda h, pb2=pb2: h.tensor_tensor(out=base[:], in0=PS[pb2][:, 0:32], in1=base[:], op=ALU.add), [PSB[pb2], baseb], [baseb])
            V(lambda h: h.tensor_scalar(out=ci[:], in0=base[:], scalar1=127.0, scalar2=None, op0=ALU.add), [baseb], [pb_])
            V(lambda h: h.tensor_scalar(out=ci[:], in0=ci[:], scalar1=7, scalar2=7, op0=ALU.arith_shift_right, op1=ALU.logical_shift_left), [pb_], [pb_])
            V(lambda h: h.tensor_copy(out=padf[:], in_=ci[:]), [pb_], [pb_])
            V(lambda h: h.tensor_tensor_scan(out=pe[:], data0=one32[:], data1=padf[:], initial=0.0, op0=ALU.mult, op1=ALU.add), [pb_], [pb_])
            V(lambda h: h.tensor_tensor(out=pst[:], in0=pe[:], in1=padf[:], op=ALU.subtract), [pb_], [pb_])
            for i in range(16):
                V(lambda h, i=i: h.tensor_tensor(out=R[:, i, :], in0=R[:, i, :], in1=pst[:], op=ALU.add), [Rb, pb_], [Rb])
            for OH, SL, col in ((OH1, SL1, 0), (OH2, SL2, 1)):
                V(lambda h, OH=OH: h.tensor_tensor(out=A[:], in0=R[:], in1=OH[:], op=ALU.mult), [Rb, Gb, Ab], [Ab])
                V(lambda h: h.tensor_reduce(out=sf[:, 0:16], in_=A[:], axis=mybir.AxisListType.X, op=ALU.add), [Ab], [ebb])
                V(lambda h, SL=SL: h.tensor_copy(out=SL[:], in_=sf[:, 0:16]), [ebb], [SLb])
            for i in range(64):
                V(lambda h, i=i: h.tensor_scalar(out=junk32[:], in0=pe[:], scalar1=128.0 * i, scalar2=0.0, op0=ALU.is_le, op1=ALU.add, accum_out=eb[:, i:i + 1]),
                  [pb_], [ebb])
            V(lambda h: h.tensor_scalar(out=bG[:], in0=eb[:], scalar1=2048.0, scalar2=pc[:, 0:1], op0=ALU.mult, op1=ALU.add), [ebb], [ebb])
            V(lambda h: h.tensor_scalar(out=bD[:], in0=eb[:], scalar1=1024.0, scalar2=pc[:, 0:1], op0=ALU.mult, op1=ALU.add), [ebb], [ebb])
            for c in range(16):
                V(lambda h, c=c: h.tensor_scalar(out=igf[:, :, c], in0=bG[:], scalar1=128.0 * c, scalar2=None, op0=ALU.add), [ebb], [ebb])
            V(lambda h: h.tensor_copy(out=IG[:], in_=igf[:]), [ebb], [IXb])
            for c in range(8):
                V(lambda h, c=c: h.tensor_scalar(out=igf[:, :, c], in0=bD[:], scalar1=128.0 * c, scalar2=None, op0=ALU.add), [ebb, IXb], [ebb])
            V(lambda h: h.tensor_copy(out=ID[:], in_=igf[:, :, 0:8]), [ebb], [IXb])
        fw.barrier()

        if stop == "S5":
            return nc
        with ExitStack() as st:
            hl = [SB(st, "s_hl%d" % i, [128, D], F32) for i in range(2)]; hlb = [Buf(), Buf()]
            h16_ = [SB(st, "s_h16%d" % i, [128, D], BF16) for i in range(2)]; h16b_ = [Buf(), Buf()]
            for t in range(16):
                k = t % 2
                fw.dma("sync", hl[k][:], h1_d[t * 128:(t + 1) * 128, :], writes=[hlb[k]])
                fw.op("scalar", lambda h: h.copy(out=h16_[k][:], in_=hl[k][:]), reads=[hlb[k]], writes=[h16b_[k]])
                fw.idma(xs_d[:, :], h16_[k][:, :], SL1[:, t:t + 1], False, 8191, reads=[h16b_[k], SLb], writes=[Buf()])
                fw.idma(xs_d[:, :], h16_[k][:, :], SL2[:, t:t + 1], False, 8191, reads=[h16b_[k], SLb], writes=[Buf()])
        fw.barrier()

        if stop == "S6a":
            return nc
        weg = w_eg.rearrange("e k n -> (e k) n"); weu = w_eu.rearrange("e k n -> (e k) n"); wed = w_ed.rearrange("e k n -> (e k) n")
        with ExitStack() as st:
            NW = 8
            xb = [SB(st, "b_xb%d" % i, [128, D], BF16) for i in range(2)]; xbb = [Buf(), Buf()]
            xT = [SB(st, "b_xT%d" % i, [128, 16, 128], BF16) for i in range(2)]; xTb = [Buf(), Buf()]
            wgt = [SB(st, "b_wg%d" % i, [128, 1024], BF16) for i in range(NW)]; wgtb = [Buf() for _ in range(NW)]
            wut = [SB(st, "b_wu%d" % i, [128, 1024], BF16) for i in range(NW)]; wutb = [Buf() for _ in range(NW)]
            wdt = [[SB(st, "b_wd%d_%d" % (i, c), [128, D], BF16) for c in range(8)] for i in range(2)]
            wdtb = [[Buf() for c in range(8)] for i in range(2)]
            sg = [SB(st, "b_sg%d" % i, [128, 512], F32) for i in range(2)]; sgb = [Buf(), Buf()]
            Hs = [SB(st, "b_H%d" % i, [128, 1024], BF16) for i in range(2)]; Hsb = [Buf(), Buf()]
            HT = [SB(st, "b_HT%d" % i, [128, 8, 128], BF16) for i in range(2)]; HTb = [Buf(), Buf()]
            Ys = [SB(st, "b_Y%d" % i, [128, D], F32) for i in range(2)]; Ysb = [Buf(), Buf()]
            iw = 0
            for i in range(64):
                k = i % 2
                fw.dma("sync", xb[k][:], xs_d[i * 128:(i + 1) * 128, :], writes=[xbb[k]])
                transpose_to(xb[k], xbb[k], xT[k], xTb[k], 16)
                for c in range(16):
                    j = iw % NW; iw += 1
                    fw.idma(wgt[j][:, :], weg[:, :], IG[:, i, c:c + 1], True, 32 * 2048 - 1, reads=[IXb], writes=[wgtb[j]])
                    fw.idma(wut[j][:, :], weu[:, :], IG[:, i, c:c + 1], True, 32 * 2048 - 1, reads=[IXb], writes=[wutb[j]])
                    for hf_ in range(2):
                        fw.op("tensor", lambda h, c=c, j=j, hf_=hf_: h.matmul(PS[hf_][:], xT[k][:, c, :], wgt[j][:, hf_ * 512:(hf_ + 1) * 512], start=(c == 0), stop=(c == 15)),
                              reads=[xTb[k], wgtb[j]], writes=[PSB[hf_]])
                    for hf_ in range(2):
                        fw.op("tensor", lambda h, c=c, j=j, hf_=hf_: h.matmul(PS[2 + hf_][:], xT[k][:, c, :], wut[j][:, hf_ * 512:(hf_ + 1) * 512], start=(c == 0), stop=(c == 15)),
                              reads=[xTb[k], wutb[j]], writes=[PSB[2 + hf_]])
                for c in range(8):
                    fw.idma(wdt[k][c][:, :], wed[:, :], ID[:, i, c:c + 1], True, 32 * 1024 - 1, reads=[IXb], writes=[wdtb[k][c]])
                for hf_ in range(2):
                    fw.op("scalar", lambda h, hf_=hf_: h.activation(out=sg[hf_][:], in_=PS[hf_][:], func=AF.Silu), reads=[PSB[hf_]], writes=[sgb[hf_]])
                    fw.op("vector", lambda h, hf_=hf_: h.tensor_tensor(out=Hs[k][:, hf_ * 512:(hf_ + 1) * 512], in0=PS[2 + hf_][:], in1=sg[hf_][:], op=ALU.mult),
                          reads=[PSB[2 + hf_], sgb[hf_]], writes=[Hsb[k]])
                transpose_to(Hs[k], Hsb[k], HT[k], HTb[k], 8)
                for hf_ in range(2):
                    for c in range(8):
                        for q in range(2):
                            fw.op("tensor", lambda h, c=c, q=q, hf_=hf_: h.matmul(PS[4 + q][:], HT[k][:, c, :], wdt[k][c][:, hf_ * 1024 + q * 512:hf_ * 1024 + (q + 1) * 512],
                                                                                  start=(c == 0), stop=(c == 7)), reads=[HTb[k], wdtb[k][c]], writes=[PSB[4 + q]])
                    fw.op("scalar", lambda h, hf_=hf_: h.copy(out=Ys[k][:, hf_ * 1024:hf_ * 1024 + 512], in_=PS[4][:]), reads=[PSB[4]], writes=[Ysb[k]])
                    fw.op("vector", lambda h, hf_=hf_: h.tensor_copy(out=Ys[k][:, hf_ * 1024 + 512:(hf_ + 1) * 1024], in_=PS[5][:]), reads=[PSB[5]], writes=[Ysb[k]])
                fw.dma("sync", ys_d[i * 128:(i + 1) * 128, :], Ys[k][:], reads=[Ysb[k]], writes=[Buf()])
        fw.barrier()

        if stop == "S6b":
            return nc
        with ExitStack() as st:
            g2, g2b = rep_load(st, "g2", ln2_g, D); b2, b2b = rep_load(st, "b2", ln2_b, D)
            gbb = Buf()
            fw.op("vector", lambda h: h.tensor_copy(out=g2[:, 0:1], in_=g2[:, 0:1]), reads=[g2b, b2b], writes=[gbb])
            ht = [SB(st, "f_h%d" % i, [128, D], F32) for i in range(2)]; htb = [Buf(), Buf()]
            y1 = [SB(st, "f_y1%d" % i, [128, D], F32) for i in range(2)]; y1b = [Buf(), Buf()]
            y2 = [SB(st, "f_y2%d" % i, [128, D], F32) for i in range(2)]; y2b = [Buf(), Buf()]
            st8 = SB(st, "st8c", [128, 24], F32); sb8 = Buf(); mv = SB(st, "mvc", [128, 2], F32); mvb = Buf(); rstd = SB(st, "rstdc", [128, 1], F32)
            for t in range(16):
                k = t % 2
                rows = slice(t * 128, (t + 1) * 128)
                fw.dma("sync", ht[k][:], h1_d[rows, :], writes=[htb[k]])
                fw.idma(y1[k][:, :], ys_d[:, :], SL1[:, t:t + 1], True, 8191, reads=[SLb], writes=[y1b[k]])
                fw.idma(y2[k][:, :], ys_d[:, :], SL2[:, t:t + 1], True, 8191, reads=[SLb], writes=[y2b[k]])
                fw.op("vector", lambda h: h.tensor_scalar(out=ht[k][:], in0=ht[k][:], scalar1=ALPHA, scalar2=None, op0=ALU.mult), reads=[htb[k]], writes=[htb[k]])
                fw.op("vector", lambda h, t=t: h.scalar_tensor_tensor(out=ht[k][:], in0=y1[k][:], scalar=G12[:, t, 0:1], in1=ht[k][:], op0=ALU.mult, op1=ALU.add),
                      reads=[y1b[k], htb[k], Gb], writes=[htb[k]])
                fw.op("vector", lambda h, t=t: h.scalar_tensor_tensor(out=ht[k][:], in0=y2[k][:], scalar=G12[:, t, 1:2], in1=ht[k][:], op0=ALU.mult, op1=ALU.add),
                      reads=[y2b[k], htb[k], Gb], writes=[htb[k]])
                layernorm(ht[k], htb[k], g2, b2, gbb, st8, sb8, mv, mvb, rstd)
                fw.dma("sync", out[rows, :], ht[k][:], reads=[htb[k]], writes=[Buf()])
        persist2.close()
        fw.barrier()
    return nc


_CONST = {}


def _consts():
    if _CONST:
        return _CONST
    f32 = np.float32
    half = 64
    inv_freq = (10000.0 ** (-np.arange(0, half, 2, dtype=np.float64) / half)).astype(f32)
    row_idx = (np.arange(S) // 64).astype(f32)
    col_idx = (np.arange(S) % 64).astype(f32)
    ang_r = (row_idx[:, None] * inv_freq[None, :]).astype(f32)
    ang_c = (col_idx[:, None] * inv_freq[None, :]).astype(f32)
    cr, sr, cc, sc = np.cos(ang_r), np.sin(ang_r), np.cos(ang_c), np.sin(ang_c)
    _CONST["ropeC"] = np.concatenate([cr, cr, cc, cc], axis=1).astype(f32)
    _CONST["ropeS"] = np.concatenate([-sr, sr, -sc, sc], axis=1).astype(f32)
    n = np.arange(128, dtype=np.float64)
    a128 = 2 * np.pi * np.outer(n, n) / 128.0
    _CONST["Fc"] = np.cos(a128).astype(f32); _CONST["Fs"] = np.sin(a128).astype(f32)
    a16 = 2 * np.pi * np.outer(n, n) / float(NFFT)
    _CONST["C16"] = np.cos(a16).astype(f32); _CONST["S16"] = np.sin(a16).astype(f32)
    t = np.linspace(0.0, 1.0, S, dtype=f32)
    w = (2.0 * math.pi * np.arange(S, dtype=f32) / S).astype(f32)[:, None]
    bands = np.linspace(1e-4, 15, 16, dtype=f32)[None, :]
    feats = np.concatenate([t[:, None], np.cos(bands * w), -np.sin(bands * w)], axis=-1).astype(f32)
    _CONST["featsT"] = np.ascontiguousarray(feats.T)
    _CONST["negt"] = np.ascontiguousarray((-t).reshape(64, 128).T)
    min_decay = math.log(1e-2) / 1.5
    max_decay = math.log(1e-2) / 0.3
    _CONST["absdelta"] = np.abs(np.linspace(min_decay, max_decay, C, dtype=f32)).reshape(1, C).astype(f32)
    import ml_dtypes
    n1 = np.arange(128, dtype=np.float64)[None, :, None]
    k1 = np.arange(128, dtype=np.float64)[None, None, :]
    k2 = np.arange(128, dtype=np.float64)[:, None, None]
    th = 2 * np.pi * n1 * (128.0 * k1 + k2) / float(NFFT)
    ec, es = np.cos(th), np.sin(th)
    ect, est = ec.transpose(0, 2, 1), es.transpose(0, 2, 1)
    _CONST["cE"] = np.ascontiguousarray(np.stack([ec, es, -es, -ec, ect, est, -est], axis=2)).astype(ml_dtypes.bfloat16)
    m0 = np.ones((128, 1), f32); m0[0, 0] = 0.0
    _CONST["m0"] = m0
    return _CONST


def _in_maps(inp):
    cs = _consts()
    f32 = np.float32
    x = np.asarray(inp["x"], f32)
    common = {
        "ln_in_g": inp["ln_in_g"].reshape(1, D), "ln_in_b": inp["ln_in_b"].reshape(1, D),
        "w_in": inp["w_in"][0], "b_gate": inp["b_gate"][0].reshape(1, 4096),
        "q_norm_g": inp["q_norm_g"][0].reshape(1, 128), "k_norm_g": inp["k_norm_g"][0].reshape(1, 128),
        "hy_conv_w": inp["hy_conv_w"][0], "hy_conv_b": inp["hy_conv_b"][0].reshape(1, 3072),
        "filt_w1": inp["filt_w1"][0], "filt_b1": inp["filt_b1"][0].reshape(64, 1), "filt_f1": inp["filt_f1"][0].reshape(64, 1),
        "filt_w2": inp["filt_w2"][0], "filt_b2": inp["filt_b2"][0].reshape(64, 1), "filt_f2": inp["filt_f2"][0].reshape(64, 1),
        "filt_w3": inp["filt_w3"][0], "hy_bias_d": inp["hy_bias_d"][0].reshape(1, C),
        "w_attn_o": inp["w_attn_o"][0], "w_hy_o": inp["w_hy_o"][0], "w_out": inp["w_out"][0],
        "ln1_g": inp["ln1_g"][0].reshape(1, D), "ln1_b": inp["ln1_b"][0].reshape(1, D),
        "w_route": np.concatenate([inp["w_route_grp"][0], inp["w_route_exp"][0]], axis=1),
        "b_route": np.concatenate([inp["b_route_grp"][0], inp["b_route_exp"][0]], axis=0).reshape(1, 36),
        "w_eg": inp["w_exp_gate"][0], "w_eu": inp["w_exp_up"][0], "w_ed": inp["w_exp_down"][0],
        "ln2_g": inp["ln2_g"][0].reshape(1, D), "ln2_b": inp["ln2_b"][0].reshape(1, D),
        "ropeC_k": cs["ropeC"], "ropeS_k": cs["ropeS"],
        "cFc": cs["Fc"], "cFs": cs["Fs"], "cFsn": -cs["Fs"], "cFcn": -cs["Fc"],
        "cC16": cs["C16"], "cS16": cs["S16"], "cS16n": -cs["S16"],
        "pcol": np.arange(128, dtype=np.float32).reshape(128, 1), "featsT": cs["featsT"], "negt": cs["negt"], "absdelta": cs["absdelta"], "m0": cs["m0"],
    }
    common = {k: np.ascontiguousarray(np.asarray(v, f32)) for k, v in common.items()}
    common["cE"] = cs["cE"]
    maps = []
    for c in range(8):
        b, j = c // 4, c % 4
        q0 = j * T
        xo = np.zeros((NOWN, D), f32); mo = np.zeros((NOWN, 1), f32)
        lo = max(q0 - 1, 0); hi = min(q0 + T + 1, S)
        r0 = lo - (q0 - 1)
        xo[r0:r0 + (hi - lo)] = x[b, lo:hi]
        mo[r0:r0 + (hi - lo)] = 1.0
        m = dict(common)
        m["x_seq"] = np.ascontiguousarray(x[b]); m["x_own"] = xo; m["m_own"] = np.ascontiguousarray(mo.reshape(17, 128).T)
        m["ropeC_q"] = np.ascontiguousarray(cs["ropeC"][q0:q0 + T]); m["ropeS_q"] = np.ascontiguousarray(cs["ropeS"][q0:q0 + T])
        n2 = np.arange(16 * j, 16 * j + 16)
        m["ci2re"] = np.ascontiguousarray(cs["Fc"][:, n2] / float(NFFT)).astype(f32)
        m["ci2im"] = np.ascontiguousarray(-cs["Fs"][:, n2] / float(NFFT)).astype(f32)
        maps.append(m)
    return maps


def kernel(**inputs):
    inp = {k: np.asarray(v) for k, v in inputs.items()}
    nc = build_nc()
    maps = _in_maps(inp)
    res = run_bass_kernel_spmd(nc, maps, core_ids=list(range(8)))
    outp = np.zeros((2, S, D), np.float32)
    for c in range(8):
        b, j = c // 4, c % 4
        outp[b, j * T:(j + 1) * T] = res.results[c]["out"]
    return outp
```
